# Optimizing a Trainium2 kernel written in Bass

```python
import math
import jax, jax.numpy as jnp
from jax import lax
import numpy as np

D_MODEL = 1024
BATCH = 8
SEQ = 4096
DEPTH = 2

MEM_LEN = 256
N_BRANCH = 5
BRANCH_W = D_MODEL // 2
CONV_K = 4
EPS = 1e-6
GDN_HEADS = 4
GDN_DK = BRANCH_W // GDN_HEADS
GDN_DV = BRANCH_W // GDN_HEADS
GDN_CHUNK = 64
GDN_QKV = 2 * GDN_HEADS * GDN_DK + GDN_HEADS * GDN_DV
SG_CHUNK = 128
SG_GROUPS = 4
SG_GW = BRANCH_W // SG_GROUPS
DSA_HEADS = 8
DSA_HD = BRANCH_W // DSA_HEADS
IDX_HEADS = 8
IDX_HD = 64
DSA_TOPK_MAX = 256
Q_BLOCK = 128
SSD_HEADS = 8
SSD_HD = BRANCH_W // SSD_HEADS
SSD_GROUPS = 2
SSD_STATE = 128
SSD_CHUNK = 128
SSD_XBC = SSD_HEADS * SSD_HD + 2 * SSD_GROUPS * SSD_STATE
MEM_HEADS = 4
MEM_HD = BRANCH_W // MEM_HEADS

IN_SPLITS = (
    GDN_QKV, GDN_HEADS, GDN_HEADS, BRANCH_W,
    BRANCH_W, BRANCH_W, BRANCH_W,
    DSA_HEADS * DSA_HD, DSA_HD, DSA_HD, IDX_HEADS * IDX_HD, IDX_HD, IDX_HEADS, BRANCH_W,
    BRANCH_W, SSD_XBC, SSD_HEADS,
    MEM_HEADS * MEM_HD, BRANCH_W,
)
IN_COLS = sum(IN_SPLITS)

kernel_name = 'hybrid_gated_parallel_mixer'


def rms_norm(x, g):
    xf = x.astype(jnp.float32)
    y = xf * lax.rsqrt(jnp.mean(xf * xf, axis=-1, keepdims=True) + EPS)
    return (y * g.astype(jnp.float32)).astype(x.dtype)


def l2_norm(x):
    xf = x.astype(jnp.float32)
    return (xf * lax.rsqrt(jnp.sum(xf * xf, axis=-1, keepdims=True) + EPS)).astype(x.dtype)


def layer_norm(x, g, b):
    xf = x.astype(jnp.float32)
    mu = jnp.mean(xf, axis=-1, keepdims=True)
    var = jnp.mean(jnp.square(xf - mu), axis=-1, keepdims=True)
    return ((xf - mu) * lax.rsqrt(var + 1e-5) * g.astype(jnp.float32) + b.astype(jnp.float32)).astype(x.dtype)


def causal_depthwise_conv(x, w, b=None):
    K = w.shape[0]
    L = x.shape[1]
    xp = jnp.pad(x, ((0, 0), (K - 1, 0), (0, 0)))
    y = xp[:, 0:L] * w[0]
    for j in range(1, K):
        y = y + xp[:, j:j + L] * w[j]
    return y if b is None else y + b


def alibi_slopes(n):
    return 2.0 ** (-8.0 * jnp.arange(1, n + 1, dtype=jnp.float32) / n)


def gated_delta_rule(q, k, v, log_decay, beta):
    f32 = jnp.float32
    Bsz, L, H, DK = q.shape
    DV = v.shape[-1]
    C = GDN_CHUNK
    N = L // C

    def chunks(t):
        return jnp.swapaxes(t.astype(f32).reshape(Bsz, N, C, H, *t.shape[3:]), 2, 3)

    q, k, v, gl, beta = map(chunks, (q, k, v, log_decay, beta))
    gc = jnp.cumsum(gl, axis=-1)
    incl = jnp.tril(jnp.ones((C, C), bool))
    strict = jnp.tril(jnp.ones((C, C), bool), -1)
    diff = gc[..., :, None] - gc[..., None, :]
    decay = jnp.where(incl, jnp.exp(jnp.where(incl, diff, 0.0)), 0.0)
    kk = jnp.einsum('bnhcd,bnhsd->bnhcs', k, k)
    a_mat = jnp.eye(C, dtype=f32) + jnp.where(strict, beta[..., :, None] * kk * decay, 0.0)
    rhs = jnp.concatenate([(beta * jnp.exp(gc))[..., None] * k, beta[..., None] * v], axis=-1)
    sol = lax.linalg.triangular_solve(a_mat, rhs, left_side=True, lower=True, unit_diagonal=True)
    w_k, u0 = sol[..., :DK], sol[..., DK:]
    qk = jnp.einsum('bnhcd,bnhsd->bnhcs', q, k) * decay
    q_dec = q * jnp.exp(gc)[..., None]
    k_dec = k * jnp.exp(gc[..., -1:] - gc)[..., None]
    chunk_decay = jnp.exp(gc[..., -1])

    def step(S, inp):
        qd, kd, qkc, wk, u0c, cd = inp
        u = u0c - jnp.einsum('bhcd,bhvd->bhcv', wk, S)
        o = jnp.einsum('bhcd,bhvd->bhcv', qd, S) + jnp.einsum('bhcs,bhsv->bhcv', qkc, u)
        S = cd[..., None, None] * S + jnp.einsum('bhsv,bhsd->bhvd', u, kd)
        return S, o

    xs = tuple(jnp.moveaxis(t, 1, 0) for t in (q_dec, k_dec, qk, w_k, u0, chunk_decay))
    _, o = lax.scan(step, jnp.zeros((Bsz, H, DV, DK), f32), xs)
    return jnp.transpose(o, (1, 0, 3, 2, 4)).reshape(Bsz, L, H, DV)


def gdn_branch(qkv, a_in, b_in, gate, conv_w, a_log, dt_bias, norm_g):
    Bsz, L, _ = qkv.shape
    qkv = jax.nn.silu(causal_depthwise_conv(qkv, conv_w))
    q, k, v = jnp.split(qkv, [GDN_HEADS * GDN_DK, 2 * GDN_HEADS * GDN_DK], axis=-1)
    q = l2_norm(q.reshape(Bsz, L, GDN_HEADS, GDN_DK)) * GDN_DK ** -0.5
    k = l2_norm(k.reshape(Bsz, L, GDN_HEADS, GDN_DK))
    v = v.reshape(Bsz, L, GDN_HEADS, GDN_DV)
    log_decay = -jnp.exp(a_log.astype(jnp.float32)) * jax.nn.softplus(a_in.astype(jnp.float32) + dt_bias)
    beta = jax.nn.sigmoid(b_in.astype(jnp.float32))
    o = gated_delta_rule(q, k, v, log_decay, beta).astype(qkv.dtype)
    o = rms_norm(o, norm_g).reshape(Bsz, L, BRANCH_W)
    return o * jax.nn.silu(gate)


def spatial_gating_branch(u, v, gate, ln_g, ln_b, w_s, b_s):
    Bsz, L, _ = u.shape
    N = L // SG_CHUNK
    u = jax.nn.gelu(u)
    v = layer_norm(jax.nn.gelu(v), ln_g, ln_b).reshape(Bsz, N, SG_CHUNK, SG_GROUPS, SG_GW)
    w_causal = jnp.where(jnp.tril(jnp.ones((SG_CHUNK, SG_CHUNK), bool)), w_s, 0.0)
    mixed = jnp.einsum('gts,bnsgc->bntgc', w_causal, v) + jnp.swapaxes(b_s, 0, 1)[None, None, :, :, None]
    return u * mixed.reshape(Bsz, L, BRANCH_W) * jax.nn.silu(gate)


def dsa_branch(q, k, v, iq, ik, iw, gate, q_norm_g, k_norm_g):
    f32 = jnp.float32
    Bsz, L, _ = q.shape
    top_k = min(DSA_TOPK_MAX, L // 4)
    n_blocks = L // Q_BLOCK
    q = rms_norm(q.reshape(Bsz, L, DSA_HEADS, DSA_HD), q_norm_g)
    k = rms_norm(k, k_norm_g)
    iq = iq.reshape(Bsz, L, IDX_HEADS, IDX_HD)
    iw = iw * (IDX_HEADS ** -0.5 * IDX_HD ** -0.5)
    slopes = alibi_slopes(DSA_HEADS)
    batch_ix = jnp.arange(Bsz)[:, None, None]
    pos_k = jnp.arange(L)

    def block(t0):
        qb = lax.dynamic_slice_in_dim(q, t0, Q_BLOCK, axis=1)
        iqb = lax.dynamic_slice_in_dim(iq, t0, Q_BLOCK, axis=1)
        iwb = lax.dynamic_slice_in_dim(iw, t0, Q_BLOCK, axis=1)
        pos_q = t0 + jnp.arange(Q_BLOCK)
        s_idx = jnp.einsum('bths,bth->bts', jax.nn.relu(jnp.einsum('bthd,bsd->bths', iqb, ik)), iwb).astype(f32)
        s_idx = jnp.where(pos_k[None, :] <= pos_q[:, None], s_idx, -jnp.inf)
        _, sel = lax.top_k(s_idx, top_k)
        valid = sel <= pos_q[None, :, None]
        k_sel = k[batch_ix, sel]
        v_sel = v[batch_ix, sel]
        logits = jnp.einsum('bthd,btkd->bhtk', qb, k_sel).astype(f32) * DSA_HD ** -0.5
        dist = (pos_q[:, None] - sel).astype(f32)
        logits = logits - slopes[None, :, None, None] * dist[:, None]
        logits = jnp.where(valid[:, None], logits, -jnp.inf)
        p = jax.nn.softmax(logits, axis=-1).astype(v.dtype)
        return jnp.einsum('bhtk,btkd->bthd', p, v_sel)

    out = lax.map(block, jnp.arange(n_blocks) * Q_BLOCK)
    out = jnp.swapaxes(out, 0, 1).reshape(Bsz, L, BRANCH_W)
    return out * jax.nn.silu(gate)


def ssd_branch(z, xbc, dt_in, conv_w, conv_b, a_log, dt_bias, d_skip, norm_g):
    f32 = jnp.float32
    Bsz, L, _ = z.shape
    C = SSD_CHUNK
    N = L // C
    G, R, P, DS = SSD_GROUPS, SSD_HEADS // SSD_GROUPS, SSD_HD, SSD_STATE
    xbc = jax.nn.silu(causal_depthwise_conv(xbc, conv_w, conv_b)).astype(f32)
    xs, bm, cm = jnp.split(xbc, [SSD_HEADS * P, SSD_HEADS * P + G * DS], axis=-1)
    xs = xs.reshape(Bsz, N, C, G, R, P)
    bm = bm.reshape(Bsz, N, C, G, DS)
    cm = cm.reshape(Bsz, N, C, G, DS)
    dt = jax.nn.softplus(dt_in.astype(f32) + dt_bias)
    a = -jnp.exp(a_log.astype(f32))
    log_a = jnp.transpose((dt * a).reshape(Bsz, N, C, G, R), (0, 1, 3, 4, 2))
    cs = jnp.cumsum(log_a, axis=-1)
    xdt = xs * dt.reshape(Bsz, N, C, G, R)[..., None]
    incl = jnp.tril(jnp.ones((C, C), bool))
    seg = cs[..., :, None] - cs[..., None, :]
    lmat = jnp.where(incl, jnp.exp(jnp.where(incl, seg, 0.0)), 0.0)
    cb = jnp.einsum('bncgd,bnsgd->bngcs', cm, bm)
    y_diag = jnp.einsum('bngcs,bngrcs,bnsgrp->bncgrp', cb, lmat, xdt)
    decay_states = jnp.exp(cs[..., -1:] - cs)
    states = jnp.einsum('bnsgd,bngrs,bnsgrp->bngrpd', bm, decay_states, xdt)
    chunk_decay = jnp.exp(cs[..., -1])

    def step(S, inp):
        st, cd = inp
        return cd[..., None, None] * S + st, S

    _, s_prev = lax.scan(step, jnp.zeros((Bsz, G, R, P, DS), f32),
                         (jnp.moveaxis(states, 1, 0), jnp.moveaxis(chunk_decay, 1, 0)))
    s_prev = jnp.moveaxis(s_prev, 0, 1)
    y_off = jnp.einsum('bncgd,bngrpd,bngrc->bncgrp', cm, s_prev, jnp.exp(cs))
    y = y_diag + y_off + xs * d_skip.reshape(G, R)[:, :, None]
    y = y.reshape(Bsz, L, BRANCH_W).astype(z.dtype)
    return rms_norm(y * jax.nn.silu(z), norm_g)


def memory_branch(q, gate, mem, mem_norm_g, w_mem_kv, q_norm_g, k_norm_g):
    Bsz, L, _ = q.shape
    M = mem.shape[1]
    q = rms_norm(q.reshape(Bsz, L, MEM_HEADS, MEM_HD), q_norm_g)
    k, v = jnp.split(rms_norm(mem, mem_norm_g) @ w_mem_kv, 2, axis=-1)
    k = rms_norm(k.reshape(Bsz, M, MEM_HEADS, MEM_HD), k_norm_g)
    v = v.reshape(Bsz, M, MEM_HEADS, MEM_HD)
    logits = jnp.einsum('blhd,bmhd->bhlm', q, k).astype(jnp.float32) * MEM_HD ** -0.5
    p = jax.nn.softmax(logits, axis=-1).astype(v.dtype)
    o = jnp.einsum('bhlm,bmhd->blhd', p, v).reshape(Bsz, L, BRANCH_W)
    return o * jax.nn.silu(gate)


def _normal(key, shape, scale):
    return scale * jax.random.normal(key, shape, jnp.float32)


def _gain(key, shape, scale=0.02):
    return 1.0 + scale * jax.random.normal(key, shape, jnp.float32)


def _dt_bias(key, shape):
    dt = jnp.exp(jax.random.uniform(key, shape, jnp.float32, math.log(1e-3), math.log(1e-1)))
    return dt + jnp.log(-jnp.expm1(-dt))


def _a_log(key, shape):
    return jnp.log(jax.random.uniform(key, shape, jnp.float32, 1.0, 16.0))


def setup_inputs(seed: int = 0) -> dict:
    key = jax.random.key(seed)
    ks = jax.random.split(key, 27)
    return {
        'x': _normal(ks[0], (BATCH, SEQ, D_MODEL), 1.0),
        'mem': _normal(ks[1], (BATCH, MEM_LEN, D_MODEL), 1.0),
        'norm_g': _gain(ks[2], (DEPTH, D_MODEL)),
        'w_in': _normal(ks[3], (DEPTH, D_MODEL, IN_COLS), D_MODEL ** -0.5),
        'gdn_conv_w': _normal(ks[4], (DEPTH, CONV_K, GDN_QKV), CONV_K ** -0.5),
        'gdn_a_log': _a_log(ks[5], (DEPTH, GDN_HEADS)),
        'gdn_dt_bias': _dt_bias(ks[6], (DEPTH, GDN_HEADS)),
        'gdn_norm_g': _gain(ks[7], (DEPTH, GDN_DV)),
        'sg_ln_g': _gain(ks[8], (DEPTH, BRANCH_W)),
        'sg_ln_b': _normal(ks[9], (DEPTH, BRANCH_W), 0.02),
        'sg_w': _normal(ks[10], (DEPTH, SG_GROUPS, SG_CHUNK, SG_CHUNK), SG_CHUNK ** -0.5),
        'sg_b': _gain(ks[11], (DEPTH, SG_GROUPS, SG_CHUNK)),
        'dsa_q_norm_g': _gain(ks[12], (DEPTH, DSA_HD)),
        'dsa_k_norm_g': _gain(ks[13], (DEPTH, DSA_HD)),
        'ssd_conv_w': _normal(ks[14], (DEPTH, CONV_K, SSD_XBC), CONV_K ** -0.5),
        'ssd_conv_b': _normal(ks[15], (DEPTH, SSD_XBC), 0.02),
        'ssd_a_log': _a_log(ks[16], (DEPTH, SSD_HEADS)),
        'ssd_dt_bias': _dt_bias(ks[17], (DEPTH, SSD_HEADS)),
        'ssd_d': _gain(ks[18], (DEPTH, SSD_HEADS), 0.1),
        'ssd_norm_g': _gain(ks[19], (DEPTH, BRANCH_W)),
        'mem_norm_g': _gain(ks[20], (DEPTH, D_MODEL)),
        'w_mem_kv': _normal(ks[21], (DEPTH, D_MODEL, 2 * BRANCH_W), D_MODEL ** -0.5),
        'mem_q_norm_g': _gain(ks[22], (DEPTH, MEM_HD)),
        'mem_k_norm_g': _gain(ks[23], (DEPTH, MEM_HD)),
        'w_gate': _normal(ks[24], (DEPTH, N_BRANCH, D_MODEL, D_MODEL), D_MODEL ** -0.5),
        'w_branch': _normal(ks[25], (DEPTH, N_BRANCH, BRANCH_W, D_MODEL), BRANCH_W ** -0.5),
        'w_out': _normal(ks[26], (DEPTH, D_MODEL, D_MODEL), 0.5 * D_MODEL ** -0.5),
    }


def reference(x, mem, norm_g, w_in, gdn_conv_w, gdn_a_log, gdn_dt_bias, gdn_norm_g,
              sg_ln_g, sg_ln_b, sg_w, sg_b, dsa_q_norm_g, dsa_k_norm_g,
              ssd_conv_w, ssd_conv_b, ssd_a_log, ssd_dt_bias, ssd_d, ssd_norm_g,
              mem_norm_g, w_mem_kv, mem_q_norm_g, mem_k_norm_g, w_gate, w_branch, w_out):
    split_at = np.cumsum(IN_SPLITS)[:-1].tolist()
    for i in range(DEPTH):
        h = rms_norm(x, norm_g[i])
        (a_qkv, a_a, a_b, a_gate,
         b_u, b_v, b_gate,
         c_q, c_k, c_v, c_iq, c_ik, c_iw, c_gate,
         d_z, d_xbc, d_dt,
         m_q, m_gate) = jnp.split(h @ w_in[i], split_at, axis=-1)
        ys = (
            gdn_branch(a_qkv, a_a, a_b, a_gate, gdn_conv_w[i], gdn_a_log[i], gdn_dt_bias[i], gdn_norm_g[i]),
            spatial_gating_branch(b_u, b_v, b_gate, sg_ln_g[i], sg_ln_b[i], sg_w[i], sg_b[i]),
            dsa_branch(c_q, c_k, c_v, c_iq, c_ik, c_iw, c_gate, dsa_q_norm_g[i], dsa_k_norm_g[i]),
            ssd_branch(d_z, d_xbc, d_dt, ssd_conv_w[i], ssd_conv_b[i], ssd_a_log[i], ssd_dt_bias[i],
                       ssd_d[i], ssd_norm_g[i]),
            memory_branch(m_q, m_gate, mem, mem_norm_g[i], w_mem_kv[i], mem_q_norm_g[i], mem_k_norm_g[i]),
        )
        merged = jnp.zeros_like(x)
        for p in range(N_BRANCH):
            merged = merged + jax.nn.sigmoid(h @ w_gate[i, p]) * (ys[p] @ w_branch[i, p])
        x = x + merged @ w_out[i]
    return x
```

```python
from contextlib import ExitStack
import numpy as np
import concourse.bass as bass
import concourse.mybir as mybir
from concourse.bass_utils import run_bass_kernel_spmd

F32 = mybir.dt.float32
BF16 = mybir.dt.bfloat16
AF = mybir.ActivationFunctionType
ALU = mybir.AluOpType
AX = mybir.AxisListType


class Buf:
    __slots__ = ("name", "ap", "w", "r", "excl")

    def __init__(self, name, ap=None):
        self.name = name
        self.ap = ap
        self.excl = False
        self.w = None
        self.r = {}

    def __getitem__(self, idx):
        return self.ap[idx]


class View:
    def __init__(self, parent, ap):
        self.parent = parent
        self.ap = ap
        self.name = parent.name
        self.excl = parent.excl

    def __getitem__(self, idx):
        return self.ap[idx]

    @property
    def w(self):
        return self.parent.w

    @w.setter
    def w(self, v):
        self.parent.w = v

    @property
    def r(self):
        return self.parent.r

    @r.setter
    def r(self, v):
        self.parent.r = v


class Prog:
    ENG = ("pe", "dve", "act", "pool", "sp")
    SEM_LIMIT = 30000
    NDMA = 6

    def __init__(self, nc, same_engine_sync=True):
        self.nc = nc
        self.same = same_engine_sync
        self.stack = ExitStack()
        self.ops = {e: [] for e in self.ENG}
        self.cnt = {e: 0 for e in self.ENG}
        self.owner = {}
        self.nsem = 0
        self.cur = {e: self._newsem(e) for e in self.ENG}
        self.seen = {e: {} for e in self.ENG}
        self.dsem = {}
        self.drr = {}
        self.allsems = []
        self.nbuf = 0

    def _newsem(self, owner):
        s = getattr(self, "semstack", self.stack).enter_context(self.nc.semaphore("s%d_%s" % (self.nsem, owner)))
        self.nsem += 1
        self.owner[id(s)] = owner
        return s

    def sbuf(self, name, shape, dtype):
        self.nbuf += 1
        t = self.stack.enter_context(self.nc.sbuf_tensor("%s_%d" % (name, self.nbuf), list(shape), dtype))
        return Buf(name, t)

    def psum(self, name, shape, dtype):
        self.nbuf += 1
        t = self.stack.enter_context(self.nc.psum_tensor("%s_%d" % (name, self.nbuf), list(shape), dtype))
        b = Buf(name, t)
        b.excl = True
        return b

    def buf(self, name, ap=None):
        return Buf(name, ap)

    def _deps(self, eng, reads, writes):
        deps = {}

        def add(ev):
            if ev is None:
                return
            s, v = ev
            k = id(s)
            if k not in deps or deps[k][1] < v:
                deps[k] = (s, v)

        for b in reads:
            add(b.w)
        for b in writes:
            add(b.w)
            for ev in b.r.values():
                add(ev)
        waits = []
        seen = self.seen[eng]
        for k, (s, v) in deps.items():
            if self.owner.get(k) == eng:
                if eng == "pe" or eng == "sp" or not self.same:
                    continue
            if seen.get(k, 0) >= v:
                continue
            seen[k] = v
            waits.append((s, v))
        return waits

    def _commit(self, ev, reads, writes):
        for b in reads:
            k = id(ev[0])
            b.r[k] = ev
        for b in writes:
            b.w = ev
            b.r = {}

    def op(self, eng, fn, reads=(), writes=()):
        ex = [b for b in reads if getattr(b, "excl", False)]
        if ex:
            writes = list(writes) + ex
        waits = self._deps(eng, reads, writes)
        if self.cnt[eng] >= self.SEM_LIMIT:
            self.cur[eng] = self._newsem(eng)
            self.cnt[eng] = 0
        self.cnt[eng] += 1
        ev = (self.cur[eng], self.cnt[eng])
        self.ops[eng].append((waits, fn, ("c", self.cur[eng], self.cnt[eng])))
        self._commit(ev, reads, writes)
        return ev

    def dma(self, q, out, in_, reads=(), writes=(), **kw):
        waits = self._deps(q, reads, writes)
        if q not in self.dsem:
            self.dsem[q] = [[self._newsem("dma_" + q), 0] for _ in range(self.NDMA)]
            self.drr[q] = 0
        slot = self.dsem[q][self.drr[q] % self.NDMA]
        self.drr[q] += 1
        s, c = slot
        if c > 0:
            seen = self.seen[q]
            if seen.get(id(s), 0) < 16 * c:
                seen[id(s)] = 16 * c
                waits.append((s, 16 * c))
        if 16 * (c + 1) > self.SEM_LIMIT:
            s = self._newsem("dma_" + q)
            slot[0] = s
            c = 0
        slot[1] = c + 1
        ev = (s, 16 * (c + 1))
        self.ops[q].append((waits, lambda e, out=out, in_=in_, kw=kw: e.dma_start(out=out, in_=in_, **kw), ("d", s, 16)))
        self._commit(ev, reads, writes)
        return ev

    def make_identity(self, b, n=128):
        self.op("pool", lambda e: e.memset(b.ap[:], 1.0), writes=[b])
        self.op("pool", lambda e: e.affine_select(out=b.ap[:], in_=b.ap[:], pattern=[[-1, n]], compare_op=ALU.is_ge,
                                                  fill=0.0, base=0, channel_multiplier=1), reads=[b], writes=[b])
        self.op("pool", lambda e: e.affine_select(out=b.ap[:], in_=b.ap[:], pattern=[[1, n]], compare_op=ALU.is_ge,
                                                  fill=0.0, base=0, channel_multiplier=-1), reads=[b], writes=[b])


    def matmul(self, out, lhsT, rhs, start, stop, reads, writes):
        return self.op("pe", lambda e: e.matmul(out, lhsT, rhs, start=start, stop=stop), reads, writes)

    def transpose(self, out, in_, ident, reads, writes):
        return self.op("pe", lambda e: e.transpose(out, in_, ident), reads, writes)

    def act(self, out, in_, func, reads, writes, **kw):
        return self.op("act", lambda e: e.activation(out, in_, func, **kw), reads, writes)

    def tt(self, eng, out, a, b, op, reads, writes):
        return self.op(eng, lambda e: e.tensor_tensor(out, a, b, op), reads, writes)

    def ts(self, eng, out, a, s1, s2, op0, op1, reads, writes, **kw):
        if op1 is None:
            return self.op(eng, lambda e: e.tensor_scalar(out, a, s1, None, op0, **kw), reads, writes)
        return self.op(eng, lambda e: e.tensor_scalar(out, a, s1, s2, op0, op1, **kw), reads, writes)

    def stt(self, eng, out, in0, scalar, in1, op0, op1, reads, writes):
        return self.op(eng, lambda e: e.scalar_tensor_tensor(out, in0, scalar, in1, op0, op1), reads, writes)

    def copy(self, eng, out, in_, reads, writes):
        if eng == "act":
            return self.op(eng, lambda e: e.copy(out, in_), reads, writes)
        return self.op(eng, lambda e: e.tensor_copy(out, in_), reads, writes)

    def memset(self, eng, ap, val, writes):
        return self.op(eng, lambda e: e.memset(ap, val), (), writes)

    def recip(self, out, in_, reads, writes):
        return self.op("dve", lambda e: e.reciprocal(out, in_), reads, writes)

    def barrier(self):
        finals = []
        for e in self.ENG:
            if self.cnt[e] > 0:
                finals.append((self.cur[e], self.cnt[e]))
        for q, slots in self.dsem.items():
            for s, c in slots:
                if c > 0:
                    finals.append((s, 16 * c))
        for e in self.ENG:
            waits = []
            for s, v in finals:
                if self.seen[e].get(id(s), 0) < v:
                    self.seen[e][id(s)] = v
                    waits.append((s, v))
            if waits:
                self.ops[e].append((waits, None, None))

    def scope(self):
        return _Scope(self)

    def flush(self, final=False):
        nc = self.nc
        finals = []
        if final:
            for e in self.ENG:
                if self.cnt[e] > 0:
                    finals.append((self.cur[e], self.cnt[e]))
            for q, slots in self.dsem.items():
                for s, c in slots:
                    if c > 0:
                        finals.append((s, 16 * c))
        if not hasattr(self, "actual"):
            self.actual = {}
            self.amap = {}
        ref = set()
        for e in self.ENG:
            for waits, fn, inc in self.ops[e]:
                for s, v in waits:
                    if self.owner.get(id(s)) in self.ENG:
                        ref.add((id(s), v))
        for s, v in finals:
            if self.owner.get(id(s)) in self.ENG:
                ref.add((id(s), v))
        for e in self.ENG:
            for waits, fn, inc in self.ops[e]:
                if inc is not None and inc[0] == "c":
                    key = (id(inc[1]), inc[2])
                    if key in ref:
                        self.actual[key[0]] = self.actual.get(key[0], 0) + 1
                        self.amap[key] = self.actual[key[0]]

        def tr(s, v):
            if self.owner.get(id(s)) in self.ENG:
                return self.amap[(id(s), v)]
            return v

        engs = {"pe": "tensor", "dve": "vector", "act": "scalar", "pool": "gpsimd", "sp": "sync"}
        with nc.Block() as block:
            for e in self.ENG:
                ops = self.ops[e]
                if not ops and not (final and e == "sp"):
                    continue

                def body(engine, ops=ops, e=e):
                    for waits, fn, inc in ops:
                        for s, v in waits:
                            engine.wait_ge(s, tr(s, v))
                        if fn is not None:
                            ins = fn(engine)
                            if inc[0] == "d":
                                ins.then_inc(inc[1], 16)
                            elif (id(inc[1]), inc[2]) in self.amap:
                                ins.then_inc(inc[1], 1)
                    if final and e == "sp":
                        for s, v in finals:
                            engine.wait_ge(s, tr(s, v))

                getattr(block, engs[e])(body)
        self.nops = getattr(self, "nops", 0) + sum(len(v) for v in self.ops.values())
        self.ops = {e: [] for e in self.ENG}

    def finish(self):
        self.flush(final=True)
        self.stack.close()


class _Scope:
    def __init__(self, P):
        self.P = P

    def __enter__(self):
        self.saved = self.P.stack
        self.P.semstack = getattr(self.P, "semstack", self.saved)
        self.P.stack = ExitStack()
        return self

    def __exit__(self, *a):
        self.P.barrier()
        self.P.flush()
        self.P.stack.close()
        self.P.stack = self.saved
        return False


T = 4096
NT = 32
NTR = [32]
D = 1024
INC = 7896
EPS = 1e-6

O_AQ, O_AK, O_AV = 0, 512, 1024
O_AA, O_AB, O_AG = 1536, 1540, 1544
O_BU, O_BV, O_BG = 2056, 2568, 3080
O_CQ, O_CK, O_CV, O_CIQ, O_CIK, O_CIW, O_CG = 3592, 4104, 4168, 4232, 4744, 4808, 4816
O_DZ, O_DX, O_DDT = 5328, 5840, 6864
O_MQ, O_MG = 6872, 7384

PARAMS = [("norm_g", [2, 1024]), ("w_in", [2, 1024, INC]), ("gdn_conv_w", [2, 4, 1536]), ("gdn_a_log", [2, 4]),
          ("gdn_dt_bias", [2, 4]), ("gdn_norm_g", [2, 128]), ("sg_ln_g", [2, 512]), ("sg_ln_b", [2, 512]),
          ("sg_w", [2, 4, 128, 128]), ("sg_b", [2, 4, 128]), ("dsa_q_norm_g", [2, 64]), ("dsa_k_norm_g", [2, 64]),
          ("ssd_conv_w", [2, 4, 1024]), ("ssd_conv_b", [2, 1024]), ("ssd_a_log", [2, 8]), ("ssd_dt_bias", [2, 8]),
          ("ssd_d", [2, 8]), ("ssd_norm_g", [2, 512]), ("mem_norm_g", [2, 1024]), ("w_mem_kv", [2, 1024, 1024]),
          ("mem_q_norm_g", [2, 128]), ("mem_k_norm_g", [2, 128]), ("w_gate", [2, 5, 1024, 1024]),
          ("w_branch", [2, 5, 512, 1024]), ("w_out", [2, 1024, 1024])]


class Ctx:
    pass


STOP = [99]


def declare(nc, dbg=False, skip=()):
    C = Ctx()
    C.nc = nc
    if "x" not in skip:
        C.x = nc.dram_tensor("x", [T, D], F32, kind="ExternalInput").ap()
    C.mem = nc.dram_tensor("mem", [256, D], F32, kind="ExternalInput").ap()
    C.prm = {}
    for name, shp in PARAMS:
        if name in skip:
            continue
        C.prm[name] = nc.dram_tensor(name, shp, F32, kind="ExternalInput").ap()
    C.out = nc.dram_tensor("out", [T, D], F32, kind="ExternalOutput").ap()
    kind = "ExternalOutput" if dbg else "Internal"
    C.scr = {}

    def scr(name, shape, dt):
        C.scr[name] = nc.dram_tensor("scr_" + name, shape, dt, kind=kind).ap()

    for n in ("gq", "gk", "gv", "cq", "ciq", "mq"):
        scr(n, [512, T], BF16)
    scr("xbc", [1024, T], BF16)
    scr("ck", [64, T], BF16)
    scr("cik", [64, T], BF16)
    for n in ("ag", "bu", "bv", "bg", "cg", "dz", "mg"):
        scr(n, [T, 512], BF16)
    scr("misc", [T, 88], F32)
    scr("ysT", [5, 512, T], BF16)
    scr("x1", [T, D], F32)
    return C


def setup_consts(P, C):
    K = Ctx()
    K.ident = P.sbuf("ident", [128, 128], BF16)
    P.make_identity(K.ident)
    K.ones = P.sbuf("ones", [128, 128], BF16)
    P.memset("pool", K.ones[:], 1.0, [K.ones])
    K.blk2 = P.sbuf("blk2", [128, 128], BF16)
    P.memset("pool", K.blk2[:], 0.0, [K.blk2])
    P.memset("pool", K.blk2[0:64, 0:64], 1.0, [K.blk2])
    P.memset("pool", K.blk2[64:128, 64:128], 1.0, [K.blk2])
    K.eps = P.sbuf("epsc", [128, 1], F32)
    P.memset("pool", K.eps[:], EPS, [K.eps])
    P.barrier()
    return K


def alloc_hT(P, K):
    K.hT = P.sbuf("hT", [128, 8, T], BF16)
    K.hTb = [Buf("hT%d" % t, K.hT.ap) for t in range(NT)]


def alloc_merged(P, K):
    K.mT = P.sbuf("mT", [128, 8, T], BF16)
    K.mTb = [Buf("mT%d" % t, K.mT.ap) for t in range(8)]


def phase_norm(P, C, K, l, x_src):
    with P.scope():
        gb = P.sbuf("gb", [128, D], F32)
        P.dma("sp", gb[:], C.prm["norm_g"][l:l + 1, :].to_broadcast([128, D]), writes=[gb])
        xin = [P.sbuf("xin%d" % i, [128, D], F32) for i in range(2)]
        junk = P.sbuf("junk", [128, D], BF16)
        ss = [P.sbuf("ss%d" % i, [128, 1], F32) for i in range(2)]
        rt = [P.sbuf("rt%d" % i, [128, 1], F32) for i in range(2)]
        hb = [P.sbuf("hb%d" % i, [128, D], BF16) for i in range(2)]
        pt = [P.psum("pt%d" % i, [128, 4, 128], BF16) for i in range(2)]
        for t in range(NT):
            xi, s_, r_, h_ = xin[t % 2], ss[t % 2], rt[t % 2], hb[t % 2]
            P.dma("sp", xi[:], x_src[t * 128:(t + 1) * 128, :], writes=[xi])
            P.act(junk[:], xi[:], AF.Square, [xi], [junk, s_], accum_out=s_[:])
            P.act(r_[:], s_[:], AF.Sqrt, [s_, K.eps], [r_], scale=1.0 / D, bias=K.eps[:])
            P.recip(r_[:], r_[:], [r_], [r_])
            P.stt("dve", h_[:], xi[:], r_[:], gb[:], ALU.mult, ALU.mult, [xi, r_, gb], [h_])
            for half in range(2):
                p_ = pt[half]
                for j in range(4):
                    kc = half * 4 + j
                    P.transpose(p_[:, j, :], h_[:, kc * 128:(kc + 1) * 128], K.ident[:], [h_, K.ident], [p_])
                if half == 0:
                    P.copy("dve", K.hT[:, 0:4, t * 128:(t + 1) * 128], p_[:], [p_], [K.hTb[t]])
                else:
                    P.copy("act", K.hT[:, 4:8, t * 128:(t + 1) * 128], p_[:], [p_], [K.hTb[t]])


def phase_inproj(P, C, K, l):
    w_in = C.prm["w_in"][l]
    S = C.scr
    with P.scope():
        cwg = P.sbuf("cwg", [128, 4, 12], F32)
        cws = P.sbuf("cws", [128, 4, 8], F32)
        for k in range(4):
            P.dma("sp", cwg[:, k, :], C.prm["gdn_conv_w"][l][k].rearrange("(c p) -> p c", p=128), writes=[cwg],
                  allow_slow_non_contiguous=True)
            P.dma("sp", cws[:, k, :], C.prm["ssd_conv_w"][l][k].rearrange("(c p) -> p c", p=128), writes=[cws],
                  allow_slow_non_contiguous=True)
        cbs = P.sbuf("cbs", [128, 8], F32)
        P.dma("sp", cbs[:], C.prm["ssd_conv_b"][l].rearrange("(c p) -> p c", p=128), writes=[cbs],
              allow_slow_non_contiguous=True)
        gq2 = P.sbuf("gq2", [128, 1], F32)
        for i in range(2):
            P.dma("sp", gq2[i * 64:(i + 1) * 64, :], C.prm["dsa_q_norm_g"][l].rearrange("(p o) -> p o", o=1), writes=[gq2])
        gk1 = P.sbuf("gk1", [64, 1], F32)
        P.dma("sp", gk1[:], C.prm["dsa_k_norm_g"][l].rearrange("(p o) -> p o", o=1), writes=[gk1])
        gmq = P.sbuf("gmq", [128, 1], F32)
        P.dma("sp", gmq[:], C.prm["mem_q_norm_g"][l].rearrange("(p o) -> p o", o=1), writes=[gmq])
        P.ts("dve", gq2[:], gq2[:], 0.125, None, ALU.mult, None, [gq2], [gq2])
        P.ts("dve", gmq[:], gmq[:], 128 ** -0.5, None, ALU.mult, None, [gmq], [gmq])

        wb = [P.sbuf("wb%d" % i, [128, 8, 512], BF16) for i in range(2)]
        acc = [P.psum("acc%d" % i, [128, 512], F32) for i in range(3)]
        ssp = [P.psum("ssp%d" % i, [128, 512], F32) for i in range(2)]
        xpad = [P.sbuf("xpad%d" % i, [128, 515], F32) for i in range(2)]
        yb = [P.sbuf("yb%d" % i, [128, 512], F32) for i in range(2)]
        sb = [P.sbuf("sb%d" % i, [128, 512], F32) for i in range(2)]
        sq = [P.sbuf("sq%d" % i, [128, 512], BF16) for i in range(2)]
        rtb = [P.sbuf("rtb%d" % i, [128, 512], F32) for i in range(2)]
        ob = [P.sbuf("ob%d" % i, [128, 512], BF16) for i in range(3)]
        mo = [P.sbuf("mo%d" % i, [128, 88], F32) for i in range(2)]
        st = Ctx()
        st.g = 0
        st.a = 0
        st.e = 0
        st.o = 0

        def load_w(col0, ncols):
            w = wb[st.g % 2]
            st.g += 1
            P.dma("pool", w[:, :, 0:ncols], w_in[:, col0:col0 + ncols].rearrange("(kc k) c -> k kc c", k=128),
                  writes=[w])
            return w

        def fm_group(col0, ncols, kind, dst, cw=None, cb=None, gain=None, scale=1.0):
            w = load_w(col0, ncols)
            nch = (ncols + 127) // 128
            for j in range(nch):
                m = min(128, ncols - j * 128)
                for tg in range(8):
                    a = acc[st.a % 3]
                    st.a += 1
                    for kc in range(8):
                        P.matmul(a[0:m, :], w[:, kc, j * 128:j * 128 + m], K.hT[:, kc, tg * 512:(tg + 1) * 512],
                                 kc == 0, kc == 7, [w] + K.hTb[tg * 4:tg * 4 + 4], [a])
                    e = st.e
                    st.e += 1
                    o = ob[st.o % 3]
                    st.o += 1
                    dsl = dst[j * 128:j * 128 + m, tg * 512:(tg + 1) * 512]
                    if kind == "raw":
                        P.copy("act", o[0:m, :], a[0:m, :], [a], [o])
                        P.dma("sp", dsl, o[0:m, :], reads=[o])
                        continue
                    if kind in ("conv", "conv_l2"):
                        xp, xn = xpad[tg % 2], xpad[(tg + 1) % 2]
                        y = yb[e % 2]
                        if tg == 0:
                            P.memset("pool", xp[:, 0:3], 0.0, [xp])
                        P.copy("act", xp[:, 3:515], a[:], [a], [xp])
                        cwb, cwo = cw
                        cj = cwo + j
                        P.ts("dve", y[:], xp[:, 0:512], cwb[:, 0, cj:cj + 1], None, ALU.mult, None, [xp, cwb], [y])
                        for k in range(1, 4):
                            P.stt("dve", y[:], xp[:, k:k + 512], cwb[:, k, cj:cj + 1], y[:], ALU.mult, ALU.add, [xp, cwb, y], [y])
                        if tg < 7:
                            P.copy("pool", xn[:, 0:3], xp[:, 512:515], [xp], [xn])
                        if kind == "conv":
                            if cb is not None:
                                P.act(o[:], y[:], AF.Silu, [y, cb[0]], [o], bias=cb[0][:, cb[1] + j:cb[1] + j + 1])
                            else:
                                P.act(o[:], y[:], AF.Silu, [y], [o])
                            P.dma("sp", dsl, o[:], reads=[o])
                            continue
                        s_ = sb[e % 2]
                        P.act(s_[:], y[:], AF.Silu, [y], [s_])
                        src_ = s_
                        ones = K.ones
                        nrm_scale = 1.0
                    else:
                        s_ = sb[e % 2]
                        P.copy("act", s_[0:m, :], a[0:m, :], [a], [s_])
                        ones = K.blk2 if kind == "rms64" else K.ones
                        nrm_scale = (1.0 / 64) if kind == "rms64" else (1.0 / 128)
                    q_ = sq[e % 2]
                    P.act(q_[0:m, :], s_[0:m, :], AF.Square, [s_], [q_])
                    sp_ = ssp[e % 2]
                    P.matmul(sp_[0:m, :], ones[0:m, 0:m], q_[0:m, :], True, True, [ones, q_], [sp_])
                    r_ = rtb[e % 2]
                    P.act(r_[0:m, :], sp_[0:m, :], AF.Sqrt, [sp_, K.eps], [r_], scale=nrm_scale, bias=K.eps[0:m, :])
                    P.recip(r_[0:m, :], r_[0:m, :], [r_], [r_])
                    if gain is not None:
                        P.stt("dve", o[0:m, :], s_[0:m, :], gain[0:m, :], r_[0:m, :], ALU.mult, ALU.mult, [s_, gain, r_], [o])
                    else:
                        P.stt("dve", o[0:m, :], s_[0:m, :], scale, r_[0:m, :], ALU.mult, ALU.mult, [s_, r_], [o])
                    P.dma("sp", dsl, o[0:m, :], reads=[o])

        def tm_group(col0, func, dst):
            w = load_w(col0, 512)
            for t in range(NT):
                a = acc[st.a % 3]
                st.a += 1
                for kc in range(8):
                    P.matmul(a[:], K.hT[:, kc, t * 128:(t + 1) * 128], w[:, kc, :], kc == 0, kc == 7,
                             [w, K.hTb[t]], [a])
                o = ob[st.o % 3]
                st.o += 1
                if func is None:
                    P.copy("act", o[:], a[:], [a], [o])
                else:
                    P.act(o[:], a[:], func, [a], [o])
                P.dma("sp", dst[t * 128:(t + 1) * 128, :], o[:], reads=[o])

        def misc_group():
            w = wb[st.g % 2]
            st.g += 1
            for (c0, n, d0) in ((O_AA, 8, 0), (O_CIW, 8, 8), (O_DDT, 8, 16), (O_CV, 64, 24)):
                P.dma("pool", w[:, :, d0:d0 + n], w_in[:, c0:c0 + n].rearrange("(kc k) c -> k kc c", k=128), writes=[w])
            for t in range(NT):
                a = acc[st.a % 3]
                st.a += 1
                for kc in range(8):
                    P.matmul(a[:, 0:88], K.hT[:, kc, t * 128:(t + 1) * 128], w[:, kc, 0:88], kc == 0, kc == 7,
                             [w, K.hTb[t]], [a])
                o = mo[t % 2]
                P.copy("act", o[:], a[:, 0:88], [a], [o])
                P.dma("sp", S["misc"][t * 128:(t + 1) * 128, :], o[:], reads=[o])

        misc_group()
        fm_group(O_CIQ, 512, "raw", S["ciq"])
        fm_group(O_CIK, 64, "raw", S["cik"])
        fm_group(O_CQ, 512, "rms64", S["cq"], gain=gq2)
        fm_group(O_CK, 64, "rms64", S["ck"], gain=gk1)
        fm_group(O_MQ, 512, "rms128", S["mq"], gain=gmq)
        fm_group(O_AQ, 512, "conv_l2", S["gq"], cw=(cwg, 0), scale=128 ** -0.5)
        fm_group(O_AK, 512, "conv_l2", S["gk"], cw=(cwg, 4), scale=1.0)
        fm_group(O_AV, 512, "conv", S["gv"], cw=(cwg, 8))
        fm_group(O_DX, 512, "conv", S["xbc"][0:512], cw=(cws, 0), cb=(cbs, 0))
        fm_group(O_DX + 512, 512, "conv", S["xbc"][512:1024], cw=(cws, 4), cb=(cbs, 4))
        tm_group(O_AG, AF.Silu, S["ag"])
        tm_group(O_BG, AF.Silu, S["bg"])
        tm_group(O_CG, AF.Silu, S["cg"])
        tm_group(O_DZ, AF.Silu, S["dz"])
        tm_group(O_MG, AF.Silu, S["mg"])
        tm_group(O_BU, AF.Gelu, S["bu"])
        tm_group(O_BV, AF.Gelu, S["bv"])


def phase_merge(P, C, K, l):
    wgd = C.prm["w_gate"][l]
    wbd = C.prm["w_branch"][l]
    ysT = C.scr["ysT"]
    with P.scope():
        wbuf = [P.sbuf("mw%d" % i, [128, 7680], BF16) for i in range(2)]
        yT = [P.sbuf("yT%d" % i, [128, 4, 512], BF16) for i in range(3)]
        gps = [P.psum("gps%d" % i, [128, 512], F32) for i in range(2)]
        zps = [P.psum("zps%d" % i, [128, 512], F32) for i in range(2)]
        sg = [P.sbuf("sg%d" % i, [128, 512], F32) for i in range(2)]
        tmp = [P.sbuf("tmp%d" % i, [128, 512], F32) for i in range(2)]
        mac = [P.sbuf("mac%d" % i, [128, 512], F32) for i in range(2)]
        cnt = 0

        def views(w):
            return (w[:, 0:5120].rearrange("k (p kc n) -> k p kc n", p=5, kc=8),
                    w[:, 5120:7680].rearrange("k (p cc n) -> k p cc n", p=5, cc=4))

        def load(nch):
            w = wbuf[nch % 2]
            wg, wbr = views(w)
            for p in range(5):
                P.dma("pool", wg[:, p], wgd[p][:, nch * 128:(nch + 1) * 128].rearrange("(kc k) n -> k kc n", k=128), writes=[w])
                P.dma("pool", wbr[:, p], wbd[p][:, nch * 128:(nch + 1) * 128].rearrange("(cc k) n -> k cc n", k=128), writes=[w])

        load(0)
        for nch in range(8):
            w = wbuf[nch % 2]
            wg, wbr = views(w)
            if nch + 1 < 8:
                load(nch + 1)
            for tg in range(8):
                m_ = mac[tg % 2]
                for p in range(5):
                    y = yT[cnt % 3]
                    g_, z_ = gps[cnt % 2], zps[cnt % 2]
                    s_, t_ = sg[cnt % 2], tmp[cnt % 2]
                    cnt += 1
                    P.dma("sp", y[:], ysT[p][:, tg * 512:(tg + 1) * 512].rearrange("(cc c) t -> c cc t", c=128), writes=[y])
                    for kc in range(8):
                        P.matmul(g_[:], wg[:, p, kc, :], K.hT[:, kc, tg * 512:(tg + 1) * 512], kc == 0, kc == 7,
                                 [w] + K.hTb[tg * 4:tg * 4 + 4], [g_])
                    for cc in range(4):
                        P.matmul(z_[:], wbr[:, p, cc, :], y[:, cc, :], cc == 0, cc == 3, [w, y], [z_])
                    P.act(s_[:], g_[:], AF.Sigmoid, [g_], [s_])
                    if p == 0:
                        P.tt("dve", m_[:], z_[:], s_[:], ALU.mult, [z_, s_], [m_])
                    elif p < 4:
                        P.tt("dve", t_[:], z_[:], s_[:], ALU.mult, [z_, s_], [t_])
                        P.tt("pool", m_[:], m_[:], t_[:], ALU.add, [m_, t_], [m_])
                    else:
                        P.tt("dve", t_[:], z_[:], s_[:], ALU.mult, [z_, s_], [t_])
                        P.tt("pool", K.mT[:, nch, tg * 512:(tg + 1) * 512], m_[:], t_[:], ALU.add, [m_, t_], [K.mTb[tg]])


def phase_outproj(P, C, K, l, x_src, x_dst):
    with P.scope():
        wo = P.sbuf("wo", [128, 8, D], BF16)
        P.dma("pool", wo[:], C.prm["w_out"][l].rearrange("(kc k) n -> k kc n", k=128), writes=[wo])
        xin = [P.sbuf("oxin%d" % i, [128, 512], F32) for i in range(2)]
        xo = [P.sbuf("oxo%d" % i, [128, 512], F32) for i in range(2)]
        ops_ = [P.psum("ops%d" % i, [128, 512], F32) for i in range(2)]
        c = 0
        for t in range(NT):
            for hf in range(2):
                xi, o, ps = xin[c % 2], xo[c % 2], ops_[c % 2]
                c += 1
                P.dma("sp", xi[:], x_src[t * 128:(t + 1) * 128, hf * 512:(hf + 1) * 512], writes=[xi])
                for kc in range(8):
                    P.matmul(ps[:], K.mT[:, kc, t * 128:(t + 1) * 128], wo[:, kc, hf * 512:(hf + 1) * 512], kc == 0, kc == 7,
                             [wo, K.mTb[t // 4]], [ps])
                P.tt("dve", o[:], ps[:], xi[:], ALU.add, [ps, xi], [o])
                P.dma("sp", x_dst[t * 128:(t + 1) * 128, hf * 512:(hf + 1) * 512], o[:], reads=[o])


class YOut:
    def __init__(self, P, C, K, p, tag, nps=1):
        self.P, self.C, self.K, self.p = P, C, K, p
        self.ps = [P.psum("yo_ps%s%d" % (tag, i), [128, 4, 128], BF16) for i in range(nps)]
        self.sb = [P.sbuf("yo_sb%s%d" % (tag, i), [128, 4, 128], BF16) for i in range(2)]
        self.n = 0

    def emit(self, y, t, eng="act"):
        P, K = self.P, self.K
        ps, sb = self.ps[self.n % len(self.ps)], self.sb[self.n % 2]
        self.n += 1
        for cc in range(4):
            P.transpose(ps[:, cc, :], y[:, cc * 128:(cc + 1) * 128], K.ident[:], [y, K.ident], [ps])
        P.copy(eng, sb[:], ps[:], [ps], [sb])
        P.dma("sp", self.C.scr["ysT"][self.p][:, t * 128:(t + 1) * 128].rearrange("(cc c) t -> c cc t", c=128), sb[:], reads=[sb])


def phase_mem(P, C, K, l):
    S = C.scr
    with P.scope():
        gb = P.sbuf("m_gb", [128, D], F32)
        P.dma("sp", gb[:], C.prm["mem_norm_g"][l:l + 1, :].to_broadcast([128, D]), writes=[gb])
        gk = P.sbuf("m_gk", [128, 1], F32)
        P.dma("sp", gk[:], C.prm["mem_k_norm_g"][l].rearrange("(p o) -> p o", o=1), writes=[gk])
        wkv = P.sbuf("m_wkv", [128, 8, D], BF16)
        P.dma("pool", wkv[:], C.prm["w_mem_kv"][l].rearrange("(kc k) n -> k kc n", k=128), writes=[wkv])
        memT = P.sbuf("memT", [128, 8, 256], BF16)
        kT = P.sbuf("m_kT", [128, 4, 256], BF16)
        vaug = [P.sbuf("m_va%d" % i, [128, 4, 129], BF16) for i in range(2)]
        xin = P.sbuf("m_x", [128, D], F32)
        junk = P.sbuf("m_junk", [128, D], BF16)
        ss = P.sbuf("m_ss", [128, 1], F32)
        hb = P.sbuf("m_hb", [128, D], BF16)
        pt = P.psum("m_pt", [128, 4, 128], BF16)
        pa = P.psum("m_pa", [128, 512], F32)
        pb = P.psum("m_pb", [128, 512], F32)
        for mt in range(2):
            P.dma("sp", xin[:], C.mem[mt * 128:(mt + 1) * 128, :], writes=[xin])
            P.act(junk[:], xin[:], AF.Square, [xin], [junk, ss], accum_out=ss[:])
            P.act(ss[:], ss[:], AF.Sqrt, [ss, K.eps], [ss], scale=1.0 / D, bias=K.eps[:])
            P.recip(ss[:], ss[:], [ss], [ss])
            P.stt("dve", hb[:], xin[:], ss[:], gb[:], ALU.mult, ALU.mult, [xin, ss, gb], [hb])
            for half in range(2):
                for j in range(4):
                    kc = half * 4 + j
                    P.transpose(pt[:, j, :], hb[:, kc * 128:(kc + 1) * 128], K.ident[:], [hb, K.ident], [pt])
                P.copy("dve", memT[:, half * 4:half * 4 + 4, mt * 128:(mt + 1) * 128], pt[:], [pt], [memT])
        sq = P.sbuf("m_sq", [128, 256], BF16)
        kf = P.sbuf("m_kf", [128, 256], F32)
        rr = P.sbuf("m_rr", [128, 256], F32)
        for h in range(4):
            for kc in range(8):
                P.matmul(pa[:, 0:256], wkv[:, kc, h * 128:(h + 1) * 128], memT[:, kc, :], kc == 0, kc == 7, [wkv, memT], [pa])
            P.copy("act", kf[:], pa[:, 0:256], [pa], [kf])
            P.act(sq[:], kf[:], AF.Square, [kf], [sq])
            P.matmul(pb[:, 0:256], K.ones[:], sq[:], True, True, [K.ones, sq], [pb])
            P.act(rr[:], pb[:, 0:256], AF.Sqrt, [pb, K.eps], [rr], scale=1.0 / 128, bias=K.eps[:])
            P.recip(rr[:], rr[:], [rr], [rr])
            P.stt("dve", kT[:, h, :], kf[:], gk[:], rr[:], ALU.mult, ALU.mult, [kf, gk, rr], [kT])
        for mt in range(2):
            for kc in range(8):
                P.matmul(pa[:], memT[:, kc, mt * 128:(mt + 1) * 128], wkv[:, kc, 512:1024], kc == 0, kc == 7, [wkv, memT], [pa])
            P.memset("pool", vaug[mt][:, :, 128:129], 1.0, [vaug[mt]])
            P.copy("act", vaug[mt][:, :, 0:128], pa[:].rearrange("m (h d) -> m h d", h=4), [pa], [vaug[mt]])
        qT = [P.sbuf("m_qT%d" % i, [128, 4, 128], BF16) for i in range(2)]
        gt = [P.sbuf("m_gt%d" % i, [128, 512], BF16) for i in range(2)]
        lg = [P.psum("m_lg%d" % i, [128, 4, 128], F32) for i in range(2)]
        pT = [P.sbuf("m_pT%d" % i, [128, 4, 128], BF16) for i in range(2)]
        po = P.psum("m_po", [128, 4, 256], F32)
        rd = [P.sbuf("m_rd%d" % i, [128, 4], F32) for i in range(2)]
        yb = [P.sbuf("m_y%d" % i, [128, 512], BF16) for i in range(2)]
        yo = YOut(P, C, K, 4, "m")
        for t in range(NTR[0]):
            q, g = qT[t % 2], gt[t % 2]
            P.dma("sp", q[:], S["mq"][:, t * 128:(t + 1) * 128].rearrange("(h d) t -> d h t", d=128), writes=[q])
            P.dma("sp", g[:], S["mg"][t * 128:(t + 1) * 128, :], writes=[g])
            for mt in range(2):
                for h in range(4):
                    P.matmul(lg[mt][:, h, :], kT[:, h, mt * 128:(mt + 1) * 128], q[:, h, :], True, True, [kT, q], [lg[mt]])
                P.act(pT[mt][:], lg[mt][:], AF.Exp, [lg[mt]], [pT[mt]])
            for h in range(4):
                for mt in range(2):
                    P.matmul(po[:, h, 0:129], pT[mt][:, h, :], vaug[mt][:, h, :], mt == 0, mt == 1, [pT[mt], vaug[mt]], [po])
            r_ = rd[t % 2]
            y = yb[t % 2]
            P.recip(r_[:], po[:, :, 128], [po], [r_])
            for h in range(4):
                P.stt("dve", y[:, h * 128:(h + 1) * 128], po[:, h, 0:128], r_[:, h:h + 1], g[:, h * 128:(h + 1) * 128],
                      ALU.mult, ALU.mult, [po, r_, g], [y])
            yo.emit(y, t)


def phase_sg(P, C, K, l):
    S = C.scr
    with P.scope():
        lng = P.sbuf("b_lng", [128, 512], F32)
        lnb = P.sbuf("b_lnb", [128, 512], F32)
        P.dma("sp", lng[:], C.prm["sg_ln_g"][l:l + 1, :].to_broadcast([128, 512]), writes=[lng])
        P.dma("sp", lnb[:], C.prm["sg_ln_b"][l:l + 1, :].to_broadcast([128, 512]), writes=[lnb])
        bsT = P.sbuf("b_bsT", [128, 4], F32)
        P.dma("sp", bsT[:], C.prm["sg_b"][l].rearrange("g t -> t g"), writes=[bsT], allow_slow_non_contiguous=True)
        eps5 = P.sbuf("b_eps5", [128, 1], F32)
        P.memset("pool", eps5[:], 1e-5, [eps5])
        wf = P.sbuf("b_wf", [128, 4, 128], F32)
        wbf = P.sbuf("b_wbf", [128, 4, 128], BF16)
        WcT = P.sbuf("b_WcT", [128, 4, 128], BF16)
        pt = P.psum("b_pt", [128, 4, 128], BF16)
        P.dma("sp", wf[:], C.prm["sg_w"][l].rearrange("g t s -> t g s"), writes=[wf])
        for g in range(4):
            P.op("pool", lambda e, g=g: e.affine_select(out=wf[:, g, :], in_=wf[:, g, :], pattern=[[-1, 128]], compare_op=ALU.is_ge,
                                                        fill=0.0, base=0, channel_multiplier=1), [wf], [wf])
        P.barrier()
        P.copy("dve", wbf[:], wf[:], [wf], [wbf])
        for g in range(4):
            P.transpose(pt[:, g, :], wbf[:, g, :], K.ident[:], [wbf, K.ident], [pt])
        P.copy("dve", WcT[:], pt[:], [pt], [WcT])

        vin = [P.sbuf("b_v%d" % i, [128, 512], BF16) for i in range(2)]
        uin = [P.sbuf("b_u%d" % i, [128, 512], BF16) for i in range(2)]
        gin = [P.sbuf("b_g%d" % i, [128, 512], BF16) for i in range(2)]
        st6 = [P.sbuf("b_st%d" % i, [128, 6], F32) for i in range(2)]
        mv = [P.sbuf("b_mv%d" % i, [128, 2], F32) for i in range(2)]
        rs = [P.sbuf("b_rs%d" % i, [128, 1], F32) for i in range(2)]
        vn = [P.sbuf("b_vn%d" % i, [128, 512], F32) for i in range(2)]
        vnb = [P.sbuf("b_vnb%d" % i, [128, 512], BF16) for i in range(2)]
        mx = [P.psum("b_mx%d" % i, [128, 512], F32) for i in range(2)]
        tm = [P.sbuf("b_tm%d" % i, [128, 512], F32) for i in range(2)]
        yb = [P.sbuf("b_y%d" % i, [128, 512], BF16) for i in range(2)]
        yo = YOut(P, C, K, 1, "b")
        for t in range(NTR[0]):
            i = t % 2
            v, u, g = vin[i], uin[i], gin[i]
            sl = slice(t * 128, (t + 1) * 128)
            P.dma("sp", v[:], S["bv"][sl, :], writes=[v])
            P.dma("sp", u[:], S["bu"][sl, :], writes=[u])
            P.dma("sp", g[:], S["bg"][sl, :], writes=[g])
            P.op("dve", lambda e, i=i: e.bn_stats(st6[i][:], vin[i][:]), [v], [st6[i]])
            P.op("dve", lambda e, i=i: e.bn_aggr(mv[i][:], st6[i][:]), [st6[i]], [mv[i]])
            P.act(rs[i][:], mv[i][:, 1:2], AF.Sqrt, [mv[i], eps5], [rs[i]], bias=eps5[:])
            P.recip(rs[i][:], rs[i][:], [rs[i]], [rs[i]])
            P.ts("dve", vn[i][:], v[:], mv[i][:, 0:1], rs[i][:], ALU.subtract, ALU.mult, [v, mv[i], rs[i]], [vn[i]])
            P.tt("pool", vn[i][:], vn[i][:], lng[:], ALU.mult, [vn[i], lng], [vn[i]])
            P.tt("pool", vnb[i][:], vn[i][:], lnb[:], ALU.add, [vn[i], lnb], [vnb[i]])
            for gg in range(4):
                P.matmul(mx[i][:, gg * 128:(gg + 1) * 128], WcT[:, gg, :], vnb[i][:, gg * 128:(gg + 1) * 128], True, True,
                         [WcT, vnb[i]], [mx[i]])
            for gg in range(4):
                c = slice(gg * 128, (gg + 1) * 128)
                P.stt("dve", tm[i][:, c], mx[i][:, c], bsT[:, gg:gg + 1], u[:, c], ALU.add, ALU.mult, [mx[i], bsT, u], [tm[i]])
            P.tt("pool", yb[i][:], tm[i][:], g[:], ALU.mult, [tm[i], g], [yb[i]])
            yo.emit(yb[i], t)


def bc(ap, shape):
    return ap.to_broadcast(list(shape))


def phase_ssd(P, C, K, l):
    S = C.scr
    with P.scope():
        uinc = P.sbuf("d_uinc", [128, 128], F32)
        P.memset("pool", uinc[:], 1.0, [uinc])
        P.op("pool", lambda e: e.affine_select(out=uinc[:], in_=uinc[:], pattern=[[1, 128]], compare_op=ALU.is_ge,
                                               fill=0.0, base=0, channel_multiplier=-1), [uinc], [uinc])
        onesf = P.sbuf("d_onesf", [128, 128], F32)
        P.memset("pool", onesf[:], 1.0, [onesf])
        P.barrier()
        aB = P.sbuf("d_aB", [128, 8], F32)
        P.dma("sp", aB[:], C.prm["ssd_a_log"][l:l + 1, :].to_broadcast([128, 8]), writes=[aB])
        P.act(aB[:], aB[:], AF.Exp, [aB], [aB])
        P.ts("dve", aB[:], aB[:], -1.0, None, ALU.mult, None, [aB], [aB])
        dtb = P.sbuf("d_dtb", [128, 8], F32)
        P.dma("sp", dtb[:], C.prm["ssd_dt_bias"][l:l + 1, :].to_broadcast([128, 8]), writes=[dtb])
        dsk = P.sbuf("d_dsk", [128, 8], F32)
        P.dma("sp", dsk[:], C.prm["ssd_d"][l:l + 1, :].to_broadcast([128, 8]), writes=[dsk])
        ngB = P.sbuf("d_ngB", [128, 512], F32)
        P.dma("sp", ngB[:], C.prm["ssd_norm_g"][l:l + 1, :].to_broadcast([128, 512]), writes=[ngB])
        S32 = P.sbuf("d_S32", [128, 512], F32)
        Sbf = P.sbuf("d_Sbf", [128, 512], BF16)
        P.memset("pool", S32[:], 0.0, [S32])
        P.memset("pool", Sbf[:], 0.0, [Sbf])

        xf = [P.sbuf("d_xf%d" % i, [128, 8, 128], BF16) for i in range(2)]
        dtin = [P.sbuf("d_dtin%d" % i, [128, 8], F32) for i in range(2)]
        zg = [P.sbuf("d_zg%d" % i, [128, 512], BF16) for i in range(2)]
        dt = P.sbuf("d_dt", [128, 8], F32)
        la = P.sbuf("d_la", [128, 8], F32)
        lab = P.sbuf("d_lab", [128, 8, 128], F32)
        cs = P.sbuf("d_cs", [128, 16], F32)
        pre = P.sbuf("d_pre", [128, 24], F32)
        E = P.sbuf("d_E", [128, 24], F32)
        seg = P.sbuf("d_seg", [128, 8, 128], F32)
        Lm = P.sbuf("d_L", [128, 8, 128], F32)
        MT = P.sbuf("d_MT", [128, 8, 128], BF16)
        cbm = P.sbuf("d_cbm", [128, 2, 128], F32)
        xs = P.sbuf("d_xs", [128, 512], BF16)
        btm = P.sbuf("d_btm", [128, 2, 128], BF16)
        xdt = P.sbuf("d_xdt", [128, 512], BF16)
        xdtd = P.sbuf("d_xdtd", [128, 512], BF16)
        t1 = P.sbuf("d_t1", [128, 512], F32)
        y1 = P.sbuf("d_y1", [128, 512], F32)
        y2 = P.sbuf("d_y2", [128, 512], F32)
        junk = P.sbuf("d_junk", [128, 512], BF16)
        ssq = P.sbuf("d_ssq", [128, 1], F32)
        yb = [P.sbuf("d_yb%d" % i, [128, 512], BF16) for i in range(2)]

        small = P.psum("d_small", [128, 512], F32)
        csps = View(small, small[:, 0:16])
        cbps = View(small, small[:, 128:384])
        csrow = P.psum("d_csrow", [128, 8, 128], F32)
        tp = P.psum("d_tp", [128, 768], BF16)
        yps = P.psum("d_yps", [128, 512], F32)
        yoff = P.psum("d_yoff", [128, 512], F32)
        stp = P.psum("d_stp", [128, 512], F32)
        yo = YOut(P, C, K, 3, "d")

        for t in range(NTR[0]):
            i = t % 2
            sl = slice(t * 128, (t + 1) * 128)
            x_ = xf[i]
            P.dma("sp", x_[:], S["xbc"][:, sl].rearrange("(c p) t -> p c t", p=128), writes=[x_])
            P.dma("sp", dtin[i][:], S["misc"][sl, 16:24], writes=[dtin[i]])
            P.dma("sp", zg[i][:], S["dz"][sl, :], writes=[zg[i]])
            P.tt("dve", dt[:], dtin[i][:], dtb[:], ALU.add, [dtin[i], dtb], [dt])
            P.act(dt[:], dt[:], AF.Exp, [dt], [dt])
            P.act(dt[:], dt[:], AF.Ln, [dt], [dt], bias=1.0)
            P.tt("dve", la[:], dt[:], aB[:], ALU.mult, [dt, aB], [la])
            if STOP[0] <= 1:
                continue
            P.matmul(csps[:, 0:8], uinc[:], la[:], True, True, [uinc, la], [csps])
            P.matmul(csps[:, 8:16], onesf[:], la[:], True, True, [onesf, la], [csps])
            P.copy("dve", cs[:], csps[:], [csps], [cs])
            if STOP[0] <= 2:
                continue
            P.copy("dve", lab[:], bc(la[:].unsqueeze(2), [128, 8, 128]), [la], [lab])
            for h in range(8):
                P.matmul(csrow[:, h, :], lab[:, h, :], uinc[:], True, True, [lab, uinc], [csrow])
            if STOP[0] <= 3:
                continue
            P.tt("dve", pre[:, 0:8], cs[:, 8:16], cs[:, 0:8], ALU.subtract, [cs], [pre])
            P.copy("dve", pre[:, 8:16], cs[:, 8:16], [cs], [pre])
            P.copy("dve", pre[:, 16:24], cs[:, 0:8], [cs], [pre])
            P.act(E[:], pre[:], AF.Exp, [pre], [E])
            if STOP[0] <= 4:
                continue
            P.tt("dve", seg[:], csrow[:], bc(cs[:, 0:8].unsqueeze(2), [128, 8, 128]), ALU.subtract, [csrow, cs], [seg])
            P.ts("dve", seg[:], seg[:], 0.0, None, ALU.min, None, [seg], [seg])
            P.act(Lm[:], seg[:], AF.Exp, [seg], [Lm])
            if STOP[0] <= 5:
                continue
            for g in range(2):
                P.matmul(cbps[:, g * 128:(g + 1) * 128], x_[:, 4 + g, :], x_[:, 6 + g, :], True, True, [x_], [cbps])
            P.tt("dve", cbm[:], cbps[:].rearrange("s (g c) -> s g c", g=2), bc(uinc[:].unsqueeze(1), [128, 2, 128]), ALU.mult,
                 [cbps, uinc], [cbm])
            for g in range(2):
                P.tt("dve", MT[:, g * 4:(g + 1) * 4, :], Lm[:, g * 4:(g + 1) * 4, :], bc(cbm[:, g:g + 1, :], [128, 4, 128]), ALU.mult,
                     [Lm, cbm], [MT])
            if STOP[0] <= 6:
                continue
            for c in range(4):
                P.transpose(tp[:, c * 128:(c + 1) * 128], x_[:, c, :], K.ident[:], [x_, K.ident], [tp])
            for g in range(2):
                P.transpose(tp[:, 512 + g * 128:512 + (g + 1) * 128], x_[:, 4 + g, :], K.ident[:], [x_, K.ident], [tp])
            P.copy("act", xs[:], tp[:, 0:512], [tp], [xs])
            P.copy("act", btm[:], tp[:, 512:768].rearrange("s (g d) -> s g d", g=2), [tp], [btm])
            xs3 = xs[:].rearrange("s (h p) -> s h p", h=8)
            P.tt("dve", xdt[:].rearrange("s (h p) -> s h p", h=8), xs3, bc(dt[:].unsqueeze(2), [128, 8, 64]), ALU.mult, [xs, dt], [xdt])
            P.tt("dve", xdtd[:].rearrange("s (h p) -> s h p", h=8), xdt[:].rearrange("s (h p) -> s h p", h=8),
                 bc(E[:, 0:8].unsqueeze(2), [128, 8, 64]), ALU.mult, [xdt, E], [xdtd])
            if STOP[0] <= 7:
                continue
            for h in range(8):
                P.matmul(yps[:, h * 64:(h + 1) * 64], MT[:, h, :], xdt[:, h * 64:(h + 1) * 64], True, True, [MT, xdt], [yps])
            for g in range(2):
                P.matmul(yoff[:, g * 256:(g + 1) * 256], x_[:, 6 + g, :], Sbf[:, g * 256:(g + 1) * 256], True, True, [x_, Sbf], [yoff])
            P.tt("dve", t1[:].rearrange("s (h p) -> s h p", h=8), yoff[:].rearrange("s (h p) -> s h p", h=8),
                 bc(E[:, 16:24].unsqueeze(2), [128, 8, 64]), ALU.mult, [yoff, E], [t1])
            P.tt("dve", y1[:], yps[:], t1[:], ALU.add, [yps, t1], [y1])
            P.tt("pool", t1[:].rearrange("s (h p) -> s h p", h=8), xs3, bc(dsk[:].unsqueeze(2), [128, 8, 64]), ALU.mult, [xs, dsk], [t1])
            P.tt("pool", y1[:], y1[:], t1[:], ALU.add, [y1, t1], [y1])
            P.tt("pool", y2[:], y1[:], zg[i][:], ALU.mult, [y1, zg[i]], [y2])
            P.act(junk[:], y2[:], AF.Square, [y2], [junk, ssq], accum_out=ssq[:])
            P.act(ssq[:], ssq[:], AF.Sqrt, [ssq, K.eps], [ssq], scale=1.0 / 512, bias=K.eps[:])
            P.recip(ssq[:], ssq[:], [ssq], [ssq])
            P.stt("dve", yb[i][:], y2[:], ssq[:], ngB[:], ALU.mult, ALU.mult, [y2, ssq, ngB], [yb[i]])
            yo.emit(yb[i], t)
            if STOP[0] <= 8:
                continue
            for g in range(2):
                P.matmul(stp[:, g * 256:(g + 1) * 256], btm[:, g, :], xdtd[:, g * 256:(g + 1) * 256], True, True, [btm, xdtd], [stp])
            P.tt("dve", S32[:].rearrange("d (h p) -> d h p", h=8), S32[:].rearrange("d (h p) -> d h p", h=8),
                 bc(E[:, 8:16].unsqueeze(2), [128, 8, 64]), ALU.mult, [S32, E], [S32])
            P.tt("dve", S32[:], S32[:], stp[:], ALU.add, [S32, stp], [S32])
            P.copy("act", Sbf[:], S32[:], [S32], [Sbf])


NBIS = 18


def phase_dsa(P, C, K, l):
    S = C.scr
    I32 = mybir.dt.int32
    with P.scope():
        rb = [P.sbuf("c_rb%d" % i, [1, T], BF16) for i in range(3)]
        P.op("pool", lambda e: e.iota(rb[0][:], pattern=[[0, NT], [1, 128]], base=0, channel_multiplier=0,
                                      allow_small_or_imprecise_dtypes=True), (), [rb[0]])
        P.op("pool", lambda e: e.iota(rb[1][:], pattern=[[1, NT], [0, 128]], base=0, channel_multiplier=0,
                                      allow_small_or_imprecise_dtypes=True), (), [rb[1]])
        P.memset("pool", rb[2][:], 1.0, [rb[2]])
        posrow = P.sbuf("c_posrow", [128, T], F32)
        P.op("pool", lambda e: e.iota(posrow[:], pattern=[[1, T]], base=0, channel_multiplier=0,
                                      allow_small_or_imprecise_dtypes=True), (), [posrow])
        sl1 = P.sbuf("c_sl1", [1, 8, 128], BF16)
        sl128 = P.sbuf("c_sl128", [1, 8, 128], BF16)
        nsl64 = P.sbuf("c_nsl64", [1, 8, 128], F32)
        nsl1 = P.sbuf("c_nsl1", [1, 8, 128], F32)
        for h in range(8):
            sl = 2.0 ** -(h + 1)
            P.memset("pool", sl1[:, h, :], sl, [sl1])
            P.memset("pool", sl128[:, h, :], 128 * sl, [sl128])
            P.memset("pool", nsl64[:, h, :], -64 * sl, [nsl64])
            P.memset("pool", nsl1[:, h, :], -sl, [nsl1])
        idf = P.sbuf("c_idf", [128, 128], F32)
        P.make_identity(idf)
        clow = P.sbuf("c_clow", [128, 128], BF16)
        P.memset("pool", clow[:], 1.0, [clow])
        P.op("pool", lambda e: e.affine_select(out=clow[:], in_=clow[:], pattern=[[-1, 128]], compare_op=ALU.is_ge,
                                               fill=0.0, base=0, channel_multiplier=1), [clow], [clow])
        cneg = P.sbuf("c_cneg", [128, 128], F32)
        P.memset("pool", cneg[:], 0.0, [cneg])
        P.op("pool", lambda e: e.affine_select(out=cneg[:], in_=cneg[:], pattern=[[-1, 128]], compare_op=ALU.is_ge,
                                               fill=-1e30, base=0, channel_multiplier=1), [cneg], [cneg])
        pw = P.sbuf("c_pw", [128, NBIS], F32)
        for k in range(NBIS):
            P.memset("pool", pw[:, k:k + 1], 2.0 ** -(k + 1), [pw])
        vaug = P.sbuf("c_vaug", [128, NT, 65], BF16)
        P.memset("pool", vaug[:, :, 64:65], 1.0, [vaug])
        P.barrier()
        kTa = P.sbuf("c_kTa", [68, T], BF16)
        ikT = P.sbuf("c_ikT", [64, T], BF16)
        P.dma("sp", kTa[0:64, :], S["ck"], writes=[kTa])
        P.dma("sp", ikT[:], S["cik"], writes=[ikT])
        P.dma("sp", kTa[64:65, :], rb[0][:], reads=[rb[0]], writes=[kTa])
        P.dma("sp", kTa[65:66, :], rb[1][:], reads=[rb[1]], writes=[kTa])
        P.dma("sp", kTa[66:67, :], rb[2][:], reads=[rb[2]], writes=[kTa])
        P.dma("sp", kTa[67:68, :], rb[2][:], reads=[rb[2]], writes=[kTa])
        for s4 in range(0, NT, 4):
            P.dma("pool", vaug[:, s4:s4 + 4, 0:64], S["misc"][s4 * 128:(s4 + 4) * 128, 24:88].rearrange("(st s) d -> s st d", s=128),
                  writes=[vaug])

        iqT = [P.sbuf("c_iqT%d" % i, [64, 8, 128], BF16) for i in range(2)]
        qTa = [P.sbuf("c_qTa%d" % i, [68, 8, 128], BF16) for i in range(2)]
        for i in range(2):
            P.dma("sp", qTa[i][64:65], sl1[:], reads=[sl1], writes=[qTa[i]])
            P.dma("sp", qTa[i][65:66], sl128[:], reads=[sl128], writes=[qTa[i]])
        iw = [P.sbuf("c_iw%d" % i, [128, 8], F32) for i in range(2)]
        gt = [P.sbuf("c_gt%d" % i, [128, 512], BF16) for i in range(2)]
        iwa = P.sbuf("c_iwa", [128, 8], F32)
        iws = P.sbuf("c_iws", [128, 8], F32)
        sidx = P.sbuf("c_sidx", [128, T], F32)
        junk = P.sbuf("c_junk", [128, T], F32)
        M01 = P.sbuf("c_M01", [128, T], BF16)
        M01T = P.sbuf("c_M01T", [128, NT, 128], BF16)
        rl = [P.sbuf("c_rl%d" % i, [128, 512], F32) for i in range(2)]
        ex = [P.sbuf("c_ex%d" % i, [128, 8, 128], BF16) for i in range(2)]
        pT = [P.sbuf("c_pT%d" % i, [128, 8, 128], BF16) for i in range(2)]
        mn = P.sbuf("c_mn", [128, 1], F32)
        mxv = P.sbuf("c_mx", [128, 1], F32)
        W = P.sbuf("c_W", [128, NBIS], F32)
        lo = P.sbuf("c_lo", [128, 1], F32)
        mid = P.sbuf("c_mid", [128, 1], F32)
        cnt = P.sbuf("c_cnt", [128, 1], F32)
        dd = P.sbuf("c_dd", [128, 1], F32)
        smax = P.sbuf("c_smax", [128, 1], F32)
        smaxb = P.sbuf("c_smaxb", [128, 32], F32)
        srow_i = P.sbuf("c_srow_i", [1, 128], I32)
        nhi_i = P.sbuf("c_nhi_i", [1, 128], I32)
        nlo_i = P.sbuf("c_nlo_i", [1, 128], I32)
        nhi_f = P.sbuf("c_nhi_f", [1, 128], F32)
        nlo_f = P.sbuf("c_nlo_f", [1, 128], F32)
        r2 = [P.sbuf("c_r2%d" % i, [1, 8, 128], BF16) for i in range(2)]
        r3 = [P.sbuf("c_r3%d" % i, [1, 8, 128], BF16) for i in range(2)]
        rden = P.sbuf("c_rden", [128, 8], F32)
        ot = P.sbuf("c_ot", [128, 512], F32)
        yb = [P.sbuf("c_yb%d" % i, [128, 512], BF16) for i in range(2)]

        sps = [P.psum("c_sps%d" % i, [128, 512], F32) for i in range(2)]
        lg = P.psum("c_lg", [128, 8, 128], F32)
        ops_ = P.psum("c_ops", [128, 8, 128], F32)
        mtp = P.psum("c_mtp", [128, 4, 128], BF16)
        yo = YOut(P, C, K, 2, "c")
        IWS = (8 ** -0.5) * (64 ** -0.5)
        nsp = 0
        for t in range(NTR[0]):
            i = t % 2
            sl = slice(t * 128, (t + 1) * 128)
            nk = t + 1
            Lk = nk * 128
            P.dma("sp", iqT[i][:], S["ciq"][:, sl].rearrange("(h d) t -> d h t", d=64), writes=[iqT[i]])
            P.dma("sp", qTa[i][0:64], S["cq"][:, sl].rearrange("(h d) t -> d h t", d=64), writes=[qTa[i]])
            P.dma("sp", iw[i][:], S["misc"][sl, 8:16], writes=[iw[i]])
            P.dma("sp", gt[i][:], S["cg"][sl, :], writes=[gt[i]])
            if STOP[0] <= 1:
                continue
            if t >= 2:
                P.act(iwa[:], iw[i][:], AF.Abs, [iw[i]], [iwa], scale=IWS)
                P.act(iws[:], iw[i][:], AF.Sign, [iw[i]], [iws])
                for c0 in range(0, Lk, 512):
                    w_ = min(512, Lk - c0)
                    for h in range(8):
                        sp_ = sps[nsp % 2]
                        r_ = rl[nsp % 2]
                        nsp += 1
                        P.matmul(sp_[:, 0:w_], iqT[i][:, h, :], ikT[:, c0:c0 + w_], True, True, [iqT[i], ikT], [sp_])
                        P.act(r_[:, 0:w_], sp_[:, 0:w_], AF.Relu, [sp_, iwa], [r_], scale=iwa[:, h:h + 1])
                        if h == 0:
                            P.ts("dve", sidx[:, c0:c0 + w_], r_[:, 0:w_], iws[:, 0:1], None, ALU.mult, None, [r_, iws], [sidx])
                        else:
                            P.stt("dve", sidx[:, c0:c0 + w_], r_[:, 0:w_], iws[:, h:h + 1], sidx[:, c0:c0 + w_], ALU.mult, ALU.add,
                                  [r_, iws, sidx], [sidx])
                P.op("dve", lambda e, Lk=Lk: e.tensor_reduce(mn[:], sidx[:, 0:Lk], AX.X, ALU.min), [sidx], [mn])
                P.op("dve", lambda e, Lk=Lk: e.tensor_reduce(mxv[:], sidx[:, 0:Lk], AX.X, ALU.max), [sidx], [mxv])
                P.tt("dve", sidx[:, t * 128:Lk], sidx[:, t * 128:Lk], cneg[:], ALU.add, [sidx, cneg], [sidx])
                P.tt("dve", mxv[:], mxv[:], mn[:], ALU.subtract, [mxv, mn], [mxv])
                P.ts("dve", W[:], pw[:], mxv[:], None, ALU.mult, None, [pw, mxv], [W])
                P.copy("dve", lo[:], mn[:], [mn], [lo])
                for k in range(NBIS):
                    P.tt("dve", mid[:], lo[:], W[:, k:k + 1], ALU.add, [lo, W], [mid])
                    P.ts("dve", junk[:, 0:Lk], sidx[:, 0:Lk], mid[:], None, ALU.is_ge, ALU.add, [sidx, mid], [junk, cnt],
                         accum_out=cnt[:])
                    P.stt("dve", dd[:], cnt[:], 255.5, W[:, k:k + 1], ALU.is_ge, ALU.mult, [cnt, W], [dd])
                    P.tt("dve", lo[:], lo[:], dd[:], ALU.add, [lo, dd], [lo])
                P.ts("dve", M01[:, 0:Lk], sidx[:, 0:Lk], lo[:], None, ALU.is_ge, None, [sidx, lo], [M01])
            else:
                if t > 0:
                    P.memset("pool", M01[:, 0:t * 128], 1.0, [M01])
                P.copy("pool", M01[:, t * 128:Lk], clow[:], [clow], [M01])
            if STOP[0] <= 2:
                continue
            P.tt("dve", junk[:, 0:Lk], M01[:, 0:Lk], posrow[:, 0:Lk], ALU.mult, [M01, posrow], [junk])
            P.op("dve", lambda e, Lk=Lk: e.tensor_reduce(smax[:], junk[:, 0:Lk], AX.X, ALU.max), [junk], [smax])
            srp = sps[nsp % 2]
            nsp += 1
            P.copy("dve", smaxb[:], bc(smax[:], [128, 32]), [smax], [smaxb])
            P.matmul(srp[0:32, 0:128], smaxb[:], idf[:], True, True, [smaxb, idf], [srp])
            if STOP[0] <= 2.5:
                continue
            P.copy("dve", srow_i[:], srp[0:1, 0:128], [srp], [srow_i])
            P.ts("dve", nhi_i[:], srow_i[:], 6, None, ALU.arith_shift_right, None, [srow_i], [nhi_i])
            P.ts("dve", nlo_i[:], srow_i[:], 63, None, ALU.bitwise_and, None, [srow_i], [nlo_i])
            P.copy("dve", nhi_f[:], nhi_i[:], [nhi_i], [nhi_f])
            P.copy("dve", nlo_f[:], nlo_i[:], [nlo_i], [nlo_f])
            P.tt("dve", r2[i][:], nsl64[:], bc(nhi_f[:].unsqueeze(1), [1, 8, 128]), ALU.mult, [nsl64, nhi_f], [r2[i]])
            P.tt("dve", r3[i][:], nsl1[:], bc(nlo_f[:].unsqueeze(1), [1, 8, 128]), ALU.mult, [nsl1, nlo_f], [r3[i]])
            if STOP[0] <= 2.7:
                continue
            P.dma("sp", qTa[i][66:67], r2[i][:], reads=[r2[i]], writes=[qTa[i]])
            P.dma("sp", qTa[i][67:68], r3[i][:], reads=[r3[i]], writes=[qTa[i]])
            if STOP[0] <= 3:
                continue
            for k0 in range(0, nk, 4):
                n4 = min(4, nk - k0)
                for j in range(n4):
                    P.transpose(mtp[:, j, :], M01[:, (k0 + j) * 128:(k0 + j + 1) * 128], K.ident[:], [M01, K.ident], [mtp])
                P.copy("act", M01T[:, k0:k0 + n4, :], mtp[:, 0:n4, :], [mtp], [M01T])
            if STOP[0] <= 4:
                continue
            for kt in range(nk):
                e_ = ex[kt % 2]
                p_ = pT[kt % 2]
                for half in range(2):
                    P.matmul(lg[:, half * 4:(half + 1) * 4, :], kTa[:, kt * 128:(kt + 1) * 128],
                             qTa[i][:, half * 4:(half + 1) * 4, :], True, True, [kTa, qTa[i]], [lg])
                P.act(e_[:], lg[:], AF.Exp, [lg], [e_])
                P.stt("dve", p_[:], e_[:], 3.0e38, bc(M01T[:, kt:kt + 1, :], [128, 8, 128]), ALU.min, ALU.mult, [e_, M01T], [p_])
                for h in range(8):
                    P.matmul(ops_[:, h, 0:65], p_[:, h, :], vaug[:, kt, :], kt == 0 and h % 4 == 0, kt == nk - 1 and h % 4 == 3,
                             [p_, vaug], [ops_])
            if STOP[0] <= 5:
                continue
            P.recip(rden[:], ops_[:, :, 64], [ops_], [rden])
            P.tt("dve", ot[:].rearrange("t (h d) -> t h d", h=8), ops_[:, :, 0:64], bc(rden[:].unsqueeze(2), [128, 8, 64]), ALU.mult,
                 [ops_, rden], [ot])
            P.tt("pool", yb[i][:], ot[:], gt[i][:], ALU.mult, [ot, gt[i]], [yb[i]])
            yo.emit(yb[i], t)


def phase_gdn(P, C, K, l):
    S = C.scr
    NCH = NTR[0] * 2
    with P.scope():
        uinc = P.sbuf("a_uinc", [128, 128], F32)
        P.memset("pool", uinc[:], 1.0, [uinc])
        P.op("pool", lambda e: e.affine_select(out=uinc[:], in_=uinc[:], pattern=[[1, 128]], compare_op=ALU.is_ge,
                                               fill=0.0, base=0, channel_multiplier=-1), [uinc], [uinc])
        lstr = P.sbuf("a_lstr", [128, 128], F32)
        P.memset("pool", lstr[:], 1.0, [lstr])
        P.op("pool", lambda e: e.affine_select(out=lstr[:], in_=lstr[:], pattern=[[-1, 128]], compare_op=ALU.is_gt,
                                               fill=0.0, base=0, channel_multiplier=1), [lstr], [lstr])
        idf = P.sbuf("a_idf", [128, 128], F32)
        P.make_identity(idf)
        onesf = P.sbuf("a_onesf", [128, 128], F32)
        P.memset("pool", onesf[:], 1.0, [onesf])
        P.barrier()
        aB = P.sbuf("a_aB", [64, 4], F32)
        P.dma("sp", aB[:], C.prm["gdn_a_log"][l:l + 1, :].to_broadcast([64, 4]), writes=[aB])
        P.act(aB[:], aB[:], AF.Exp, [aB], [aB])
        P.ts("dve", aB[:], aB[:], -1.0, None, ALU.mult, None, [aB], [aB])
        dtb = P.sbuf("a_dtb", [64, 4], F32)
        P.dma("sp", dtb[:], C.prm["gdn_dt_bias"][l:l + 1, :].to_broadcast([64, 4]), writes=[dtb])
        ng = P.sbuf("a_ng", [64, 128], F32)
        P.dma("sp", ng[:], C.prm["gdn_norm_g"][l:l + 1, :].to_broadcast([64, 128]), writes=[ng])
        ST32 = P.sbuf("a_ST32", [128, 4, 128], F32)
        STb = P.sbuf("a_STb", [128, 4, 128], BF16)
        P.memset("pool", ST32[:], 0.0, [ST32])
        P.memset("pool", STb[:], 0.0, [STb])

        def dbl(name, shape, dt):
            return [P.sbuf("%s%d" % (name, i), shape, dt) for i in range(2)]

        qT, kT, vT = dbl("a_qT", [128, 4, 64], BF16), dbl("a_kT", [128, 4, 64], BF16), dbl("a_vT", [128, 4, 64], BF16)
        ab, gate = dbl("a_ab", [64, 8], F32), dbl("a_gate", [64, 512], BF16)
        sp_, gl, be = dbl("a_sp", [64, 4], F32), dbl("a_gl", [64, 4], F32), dbl("a_be", [64, 4], F32)
        gc, gt128, cd128 = dbl("a_gc", [64, 4], F32), dbl("a_gt", [128, 4], F32), dbl("a_cd", [128, 4], F32)
        lab = dbl("a_lab", [64, 4, 128], F32)
        pre, E, bE = dbl("a_pre", [64, 8], F32), dbl("a_E", [64, 8], F32), dbl("a_bE", [64, 4], F32)
        seg, nseg = dbl("a_seg", [64, 4, 64], F32), dbl("a_nseg", [64, 4, 64], F32)
        dI, dL = dbl("a_dI", [64, 4, 64], F32), dbl("a_dL", [64, 4, 64], F32)
        egr = dbl("a_egr", [128, 4, 64], F32)
        qdT = dbl("a_qdT", [128, 4, 64], BF16)
        qkT = dbl("a_qkT", [64, 4, 64], BF16)
        Nm = [dbl("a_N", [64, 4, 64], F32), dbl("a_N2", [64, 4, 64], F32)]
        Mm = [dbl("a_M", [64, 4, 64], F32), dbl("a_M2", [64, 4, 64], F32)]
        X, Xb = dbl("a_X", [64, 4, 64], F32), dbl("a_Xb", [64, 4, 64], BF16)
        kb, kdec, vb = dbl("a_kb", [64, 4, 128], BF16), dbl("a_kdec", [64, 4, 128], BF16), dbl("a_vb", [64, 4, 128], BF16)
        wkT, u0 = dbl("a_wkT", [128, 4, 64], BF16), dbl("a_u0", [64, 4, 128], F32)
        u = dbl("a_u", [64, 4, 128], BF16)
        o, sq = dbl("a_o", [64, 4, 128], F32), dbl("a_sq", [64, 4, 128], F32)
        ssq = dbl("a_ssq", [64, 4], F32)
        y = dbl("a_y", [64, 512], F32)
        ysb = dbl("a_ysb", [128, 4, 64], BF16)

        b0 = P.psum("a_b0", [128, 512], F32)
        gcrow, wk_ps = View(b0, b0[:, 0:256].rearrange("p (h c) -> p h c", h=4)), View(b0, b0[:, 256:512].rearrange("p (h c) -> p h c", h=4))
        b1 = P.psum("a_b1", [128, 512], F32)
        sm_ps, ytp = View(b1, b1[:, 0:16]), View(b1, b1[:, 256:512].rearrange("p (h c) -> p h c", h=4))
        b2 = P.psum("a_b2", [64, 512], F32)
        kk_ps, qk_ps = View(b2, b2[:, 0:256].rearrange("p (h c) -> p h c", h=4)), View(b2, b2[:, 256:512].rearrange("p (h c) -> p h c", h=4))
        b3 = P.psum("a_b3", [64, 512], F32)
        P_ps, Q_ps = View(b3, b3[:, 0:256].rearrange("p (h c) -> p h c", h=4)), View(b3, b3[:, 256:512].rearrange("p (h c) -> p h c", h=4))
        b4 = P.psum("a_b4", [64, 512], F32)
        XP_ps, M_ps = View(b4, b4[:, 0:256].rearrange("p (h c) -> p h c", h=4)), View(b4, b4[:, 256:512].rearrange("p (h c) -> p h c", h=4))
        kv_ps = P.psum("a_kvps", [64, 2, 512], BF16)
        ktm, vtm = View(kv_ps, kv_ps[:, 0, :].rearrange("p (h d) -> p h d", h=4)), View(kv_ps, kv_ps[:, 1, :].rearrange("p (h d) -> p h d", h=4))
        uwo = P.psum("a_uwo", [64, 4, 128], F32)
        Sn_ps = P.psum("a_Sn", [128, 4, 128], F32)
        u64, i64 = uinc[0:64, 0:64], idf[0:64, 0:64]

        def b4c(ap, n):
            return bc(ap.unsqueeze(2), [ap.shape[0], 4, n])

        for ch in range(NCH):
            i = ch % 2
            cs = slice(ch * 64, (ch + 1) * 64)
            for dst, nm in ((qT[i], "gq"), (kT[i], "gk"), (vT[i], "gv")):
                P.dma("sp", dst[:], S[nm][:, cs].rearrange("(h d) t -> d h t", d=128), writes=[dst])
            P.dma("sp", ab[i][:], S["misc"][cs, 0:8], writes=[ab[i]])
            P.dma("sp", gate[i][:], S["ag"][cs, :], writes=[gate[i]])
            P.tt("dve", sp_[i][:], ab[i][:, 0:4], dtb[:], ALU.add, [ab[i], dtb], [sp_[i]])
            P.act(sp_[i][:], sp_[i][:], AF.Exp, [sp_[i]], [sp_[i]])
            P.act(sp_[i][:], sp_[i][:], AF.Ln, [sp_[i]], [sp_[i]], bias=1.0)
            P.tt("dve", gl[i][:], sp_[i][:], aB[:], ALU.mult, [sp_[i], aB], [gl[i]])
            P.act(be[i][:], ab[i][:, 4:8], AF.Exp, [ab[i]], [be[i]], scale=-1.0)
            P.ts("dve", be[i][:], be[i][:], 1.0, None, ALU.add, None, [be[i]], [be[i]])
            P.recip(be[i][:], be[i][:], [be[i]], [be[i]])
            P.matmul(sm_ps[0:64, 0:4], u64, gl[i][:], True, True, [uinc, gl[i]], [sm_ps])
            P.matmul(sm_ps[:, 4:8], onesf[0:64, :], gl[i][:], True, True, [onesf, gl[i]], [sm_ps])
            P.copy("dve", gc[i][:], sm_ps[0:64, 0:4], [sm_ps], [gc[i]])
            P.copy("dve", gt128[i][:], sm_ps[:, 4:8], [sm_ps], [gt128[i]])
            P.copy("dve", lab[i][:], b4c(gl[i][:], 128), [gl[i]], [lab[i]])
            for h in range(4):
                P.matmul(gcrow[:, h, :], lab[i][:, h, :], u64, True, True, [lab[i], uinc], [gcrow])
            P.copy("dve", pre[i][:, 0:4], gc[i][:], [gc[i]], [pre[i]])
            P.tt("dve", pre[i][:, 4:8], gt128[i][0:64, :], gc[i][:], ALU.subtract, [gt128[i], gc[i]], [pre[i]])
            P.act(E[i][:], pre[i][:], AF.Exp, [pre[i]], [E[i]])
            P.act(cd128[i][:], gt128[i][:], AF.Exp, [gt128[i]], [cd128[i]])
            P.tt("dve", bE[i][:], be[i][:], E[i][:, 0:4], ALU.mult, [be[i], E[i]], [bE[i]])
            if STOP[0] <= 1:
                continue
            P.tt("dve", seg[i][:], gcrow[0:64], b4c(gc[i][:], 64), ALU.subtract, [gcrow, gc[i]], [seg[i]])
            P.ts("dve", nseg[i][:], seg[i][:], -1.0, 0.0, ALU.mult, ALU.min, [seg[i]], [nseg[i]])
            P.ts("dve", seg[i][:], seg[i][:], 0.0, None, ALU.min, None, [seg[i]], [seg[i]])
            P.act(dI[i][:], seg[i][:], AF.Exp, [seg[i]], [dI[i]])
            P.act(dL[i][:], nseg[i][:], AF.Exp, [nseg[i]], [dL[i]])
            P.tt("dve", dI[i][:], dI[i][:], bc(u64.unsqueeze(1), [64, 4, 64]), ALU.mult, [dI[i], uinc], [dI[i]])
            P.tt("dve", dL[i][:], dL[i][:], bc(lstr[0:64, 0:64].unsqueeze(1), [64, 4, 64]), ALU.mult, [dL[i], lstr], [dL[i]])
            P.act(egr[i][:], gcrow[:], AF.Exp, [gcrow], [egr[i]])
            P.tt("dve", qdT[i][:], qT[i][:], egr[i][:], ALU.mult, [qT[i], egr[i]], [qdT[i]])
            if STOP[0] <= 2:
                continue
            for h in range(4):
                P.matmul(kk_ps[:, h, :], kT[i][:, h, :], kT[i][:, h, :], True, True, [kT[i]], [kk_ps])
            for h in range(4):
                P.matmul(qk_ps[:, h, :], kT[i][:, h, :], qT[i][:, h, :], True, True, [kT[i], qT[i]], [qk_ps])
            N0, M0 = Nm[0][i], Mm[0][i]
            P.tt("dve", N0[:], kk_ps[:], dL[i][:], ALU.mult, [kk_ps, dL[i]], [N0])
            P.tt("dve", N0[:], N0[:], b4c(be[i][:], 64), ALU.mult, [N0, be[i]], [N0])
            P.tt("dve", qkT[i][:], qk_ps[:], dI[i][:], ALU.mult, [qk_ps, dI[i]], [qkT[i]])
            if STOP[0] <= 3:
                continue
            for h in range(4):
                P.transpose(M_ps[:, h, :], N0[:, h, :], i64, [N0, idf], [M_ps])
            P.copy("act", M0[:], M_ps[:], [M_ps], [M0])
            if STOP[0] <= 4:
                continue
            P.tt("dve", X[i][:], bc(i64.unsqueeze(1), [64, 4, 64]), M0[:], ALU.subtract, [idf, M0], [X[i]])
            Pc, Qc = N0, M0
            if STOP[0] <= 4.1:
                continue
            for st in range(1, 6):
                if STOP[0] <= 4.2 and st > 1:
                    break
                Pn, Qn = Nm[st % 2][i], Mm[st % 2][i]
                for h in range(4):
                    P.matmul(P_ps[:, h, :], Qc[:, h, :], Pc[:, h, :], True, True, [Qc, Pc], [P_ps])
                if st < 5:
                    for h in range(4):
                        P.matmul(Q_ps[:, h, :], Pc[:, h, :], Qc[:, h, :], True, True, [Qc, Pc], [Q_ps])
                if STOP[0] <= 4.15:
                    break
                P.copy("act", Pn[:], P_ps[:], [P_ps], [Pn])
                if st < 5:
                    P.copy("dve", Qn[:], Q_ps[:], [Q_ps], [Qn])
                if STOP[0] <= 4.17:
                    break
                for h in range(4):
                    P.matmul(XP_ps[:, h, :], Pn[:, h, :], X[i][:, h, :], True, True, [Pn, X[i]], [XP_ps])
                P.tt("dve", X[i][:], X[i][:], XP_ps[:], ALU.add, [X[i], XP_ps], [X[i]])
                Pc, Qc = Pn, Qn
            if STOP[0] <= 4.5:
                continue
            P.copy("act", Xb[i][:], X[i][:], [X[i]], [Xb[i]])
            if STOP[0] <= 5:
                continue
            for h in range(4):
                P.transpose(ktm[:, h, :], kT[i][:, h, :], K.ident[:], [kT[i], K.ident], [ktm])
            for h in range(4):
                P.transpose(vtm[:, h, :], vT[i][:, h, :], K.ident[:], [vT[i], K.ident], [vtm])
            P.tt("dve", kb[i][:], ktm[:], b4c(bE[i][:], 128), ALU.mult, [ktm, bE[i]], [kb[i]])
            P.tt("dve", kdec[i][:], ktm[:], b4c(E[i][:, 4:8], 128), ALU.mult, [ktm, E[i]], [kdec[i]])
            P.tt("dve", vb[i][:], vtm[:], b4c(be[i][:], 128), ALU.mult, [vtm, be[i]], [vb[i]])
            if STOP[0] <= 6:
                continue
            for h in range(4):
                P.matmul(wk_ps[:, h, :], kb[i][:, h, :], Xb[i][:, h, :], True, True, [kb[i], Xb[i]], [wk_ps])
            P.copy("act", wkT[i][:], wk_ps[:], [wk_ps], [wkT[i]])
            for h in range(4):
                P.matmul(uwo[:, h, :], Xb[i][:, h, :], vb[i][:, h, :], True, True, [Xb[i], vb[i]], [uwo])
            P.copy("act", u0[i][:], uwo[:], [uwo], [u0[i]])
            if STOP[0] <= 7:
                continue
            for h in range(4):
                P.matmul(uwo[:, h, :], wkT[i][:, h, :], STb[:, h, :], True, True, [wkT[i], STb], [uwo])
            P.tt("dve", u[i][:], u0[i][:], uwo[:], ALU.subtract, [u0[i], uwo], [u[i]])
            for h in range(4):
                P.matmul(uwo[:, h, :], qdT[i][:, h, :], STb[:, h, :], True, False, [qdT[i], STb], [uwo])
                P.matmul(uwo[:, h, :], qkT[i][:, h, :], u[i][:, h, :], False, True, [qkT[i], u[i]], [uwo])
            P.copy("act", o[i][:], uwo[:], [uwo], [o[i]])
            if STOP[0] <= 8:
                continue
            for h in range(4):
                P.matmul(Sn_ps[:, h, :], kdec[i][:, h, :], u[i][:, h, :], True, True, [kdec[i], u[i]], [Sn_ps])
            P.tt("dve", ST32[:], ST32[:], b4c(cd128[i][:], 128), ALU.mult, [ST32, cd128[i]], [ST32])
            P.tt("dve", ST32[:], ST32[:], Sn_ps[:], ALU.add, [ST32, Sn_ps], [ST32])
            P.copy("act", STb[:], ST32[:], [ST32], [STb])
            if STOP[0] <= 9:
                continue
            P.tt("pool", sq[i][:], o[i][:], o[i][:], ALU.mult, [o[i]], [sq[i]])
            P.op("dve", lambda e, i=i: e.tensor_reduce(ssq[i][:], sq[i][:], AX.X, ALU.add), [sq[i]], [ssq[i]])
            P.act(ssq[i][:], ssq[i][:], AF.Sqrt, [ssq[i], K.eps], [ssq[i]], scale=1.0 / 128, bias=K.eps[0:64, :])
            P.recip(ssq[i][:], ssq[i][:], [ssq[i]], [ssq[i]])
            P.tt("pool", o[i][:], o[i][:], b4c(ssq[i][:], 128), ALU.mult, [o[i], ssq[i]], [o[i]])
            P.tt("pool", o[i][:], o[i][:], bc(ng[:].unsqueeze(1), [64, 4, 128]), ALU.mult, [o[i], ng], [o[i]])
            P.tt("pool", y[i][:], o[i][:].rearrange("c h v -> c (h v)"), gate[i][:], ALU.mult, [o[i], gate[i]], [y[i]])
            if STOP[0] <= 10:
                continue
            for cc in range(4):
                P.transpose(ytp[:, cc, :], y[i][:, cc * 128:(cc + 1) * 128], i64, [y[i], idf], [ytp])
            P.copy("act", ysb[i][:], ytp[:], [ytp], [ysb[i]])
            P.dma("sp", S["ysT"][0][:, cs].rearrange("(cc c) t -> c cc t", c=128), ysb[i][:], reads=[ysb[i]])


def build_program(nc, layers=(0, 1), dbg=False, branches="abcdm"):
    C = declare(nc, dbg=dbg)
    P = Prog(nc)
    K = setup_consts(P, C)
    for l in layers:
        x_src = C.x if l == layers[0] else C.scr["x1"]
        x_dst = C.out if l == layers[-1] else C.scr["x1"]
        with P.scope():
            alloc_hT(P, K)
            phase_norm(P, C, K, l, x_src)
            phase_inproj(P, C, K, l)
        if "a" in branches:
            phase_gdn(P, C, K, l)
        if "b" in branches:
            phase_sg(P, C, K, l)
        if "c" in branches:
            phase_dsa(P, C, K, l)
        if "d" in branches:
            phase_ssd(P, C, K, l)
        if "m" in branches:
            phase_mem(P, C, K, l)
        with P.scope():
            alloc_merged(P, K)
            with P.scope():
                alloc_hT(P, K)
                phase_norm(P, C, K, l, x_src)
                phase_merge(P, C, K, l)
            phase_outproj(P, C, K, l, x_src, x_dst)
    P.finish()
    return C, P


def kernel(**inputs):
    x = np.ascontiguousarray(np.asarray(inputs["x"], dtype=np.float32))
    mem = np.ascontiguousarray(np.asarray(inputs["mem"], dtype=np.float32))
    nb = x.shape[0]
    nc = bass.Bass("TRN2", target_bir_lowering=False)
    build_program(nc)
    prm = {n: np.ascontiguousarray(np.asarray(inputs[n], dtype=np.float32)) for n, _ in PARAMS}
    in_maps = []
    for b in range(nb):
        m = {"x": x[b], "mem": mem[b]}
        m.update(prm)
        in_maps.append(m)
    res = run_bass_kernel_spmd(nc, in_maps, core_ids=list(range(nb)))
    return np.stack([np.asarray(r["out"], dtype=np.float32) for r in res.results], axis=0)
```

```python
from contextlib import ExitStack
import numpy as np
import concourse.bass as bass
import concourse.mybir as mybir
from concourse.bass_utils import run_bass_kernel_spmd

F32 = mybir.dt.float32
BF16 = mybir.dt.bfloat16
AF = mybir.ActivationFunctionType
ALU = mybir.AluOpType
AX = mybir.AxisListType


class Buf:
    __slots__ = ("name", "ap", "w", "r", "excl")

    def __init__(self, name, ap=None):
        self.name = name
        self.ap = ap
        self.excl = False
        self.w = None
        self.r = {}

    def __getitem__(self, idx):
        return self.ap[idx]


class View:
    def __init__(self, parent, ap):
        self.parent = parent
        self.ap = ap
        self.name = parent.name
        self.excl = parent.excl

    def __getitem__(self, idx):
        return self.ap[idx]

    @property
    def w(self):
        return self.parent.w

    @w.setter
    def w(self, v):
        self.parent.w = v

    @property
    def r(self):
        return self.parent.r

    @r.setter
    def r(self, v):
        self.parent.r = v


class Prog:
    ENG = ("pe", "dve", "act", "pool", "sp")
    SEM_LIMIT = 30000
    NDMA = 6

    def __init__(self, nc, same_engine_sync=True):
        self.nc = nc
        self.same = same_engine_sync
        self.stack = ExitStack()
        self.ops = {e: [] for e in self.ENG}
        self.cnt = {e: 0 for e in self.ENG}
        self.owner = {}
        self.nsem = 0
        self.cur = {e: self._newsem(e) for e in self.ENG}
        self.seen = {e: {} for e in self.ENG}
        self.dsem = {}
        self.drr = {}
        self.allsems = []
        self.nbuf = 0

    def _newsem(self, owner):
        s = getattr(self, "semstack", self.stack).enter_context(self.nc.semaphore("s%d_%s" % (self.nsem, owner)))
        self.nsem += 1
        self.owner[id(s)] = owner
        return s

    def sbuf(self, name, shape, dtype):
        self.nbuf += 1
        t = self.stack.enter_context(self.nc.sbuf_tensor("%s_%d" % (name, self.nbuf), list(shape), dtype))
        return Buf(name, t)

    def psum(self, name, shape, dtype):
        self.nbuf += 1
        t = self.stack.enter_context(self.nc.psum_tensor("%s_%d" % (name, self.nbuf), list(shape), dtype))
        b = Buf(name, t)
        b.excl = True
        return b

    def buf(self, name, ap=None):
        return Buf(name, ap)

    def _deps(self, eng, reads, writes):
        deps = {}

        def add(ev):
            if ev is None:
                return
            s, v = ev
            k = id(s)
            if k not in deps or deps[k][1] < v:
                deps[k] = (s, v)

        for b in reads:
            add(b.w)
        for b in writes:
            add(b.w)
            for ev in b.r.values():
                add(ev)
        waits = []
        seen = self.seen[eng]
        for k, (s, v) in deps.items():
            if self.owner.get(k) == eng:
                if eng == "pe" or eng == "sp" or not self.same:
                    continue
            if seen.get(k, 0) >= v:
                continue
            seen[k] = v
            waits.append((s, v))
        return waits

    def _commit(self, ev, reads, writes):
        for b in reads:
            k = id(ev[0])
            b.r[k] = ev
        for b in writes:
            b.w = ev
            b.r = {}

    def op(self, eng, fn, reads=(), writes=()):
        ex = [b for b in reads if getattr(b, "excl", False)]
        if ex:
            writes = list(writes) + ex
        waits = self._deps(eng, reads, writes)
        if self.cnt[eng] >= self.SEM_LIMIT:
            self.cur[eng] = self._newsem(eng)
            self.cnt[eng] = 0
        self.cnt[eng] += 1
        ev = (self.cur[eng], self.cnt[eng])
        self.ops[eng].append((waits, fn, ("c", self.cur[eng], self.cnt[eng])))
        self._commit(ev, reads, writes)
        return ev

    def dma(self, q, out, in_, reads=(), writes=(), **kw):
        waits = self._deps(q, reads, writes)
        if q not in self.dsem:
            self.dsem[q] = [[self._newsem("dma_" + q), 0] for _ in range(self.NDMA)]
            self.drr[q] = 0
        slot = self.dsem[q][self.drr[q] % self.NDMA]
        self.drr[q] += 1
        s, c = slot
        if c > 0:
            seen = self.seen[q]
            if seen.get(id(s), 0) < 16 * c:
                seen[id(s)] = 16 * c
                waits.append((s, 16 * c))
        if 16 * (c + 1) > self.SEM_LIMIT:
            s = self._newsem("dma_" + q)
            slot[0] = s
            c = 0
        slot[1] = c + 1
        ev = (s, 16 * (c + 1))
        self.ops[q].append((waits, lambda e, out=out, in_=in_, kw=kw: e.dma_start(out=out, in_=in_, **kw), ("d", s, 16)))
        self._commit(ev, reads, writes)
        return ev

    def make_identity(self, b, n=128):
        self.op("pool", lambda e: e.memset(b.ap[:], 1.0), writes=[b])
        self.op("pool", lambda e: e.affine_select(out=b.ap[:], in_=b.ap[:], pattern=[[-1, n]], compare_op=ALU.is_ge,
                                                  fill=0.0, base=0, channel_multiplier=1), reads=[b], writes=[b])
        self.op("pool", lambda e: e.affine_select(out=b.ap[:], in_=b.ap[:], pattern=[[1, n]], compare_op=ALU.is_ge,
                                                  fill=0.0, base=0, channel_multiplier=-1), reads=[b], writes=[b])


    def matmul(self, out, lhsT, rhs, start, stop, reads, writes):
        return self.op("pe", lambda e: e.matmul(out, lhsT, rhs, start=start, stop=stop), reads, writes)

    def transpose(self, out, in_, ident, reads, writes):
        return self.op("pe", lambda e: e.transpose(out, in_, ident), reads, writes)

    def act(self, out, in_, func, reads, writes, **kw):
        return self.op("act", lambda e: e.activation(out, in_, func, **kw), reads, writes)

    def tt(self, eng, out, a, b, op, reads, writes):
        return self.op(eng, lambda e: e.tensor_tensor(out, a, b, op), reads, writes)

    def ts(self, eng, out, a, s1, s2, op0, op1, reads, writes, **kw):
        if op1 is None:
            return self.op(eng, lambda e: e.tensor_scalar(out, a, s1, None, op0, **kw), reads, writes)
        return self.op(eng, lambda e: e.tensor_scalar(out, a, s1, s2, op0, op1, **kw), reads, writes)

    def stt(self, eng, out, in0, scalar, in1, op0, op1, reads, writes):
        return self.op(eng, lambda e: e.scalar_tensor_tensor(out, in0, scalar, in1, op0, op1), reads, writes)

    def copy(self, eng, out, in_, reads, writes):
        if eng == "act":
            return self.op(eng, lambda e: e.copy(out, in_), reads, writes)
        return self.op(eng, lambda e: e.tensor_copy(out, in_), reads, writes)

    def memset(self, eng, ap, val, writes):
        return self.op(eng, lambda e: e.memset(ap, val), (), writes)

    def recip(self, out, in_, reads, writes):
        return self.op("dve", lambda e: e.reciprocal(out, in_), reads, writes)

    def barrier(self):
        finals = []
        for e in self.ENG:
            if self.cnt[e] > 0:
                finals.append((self.cur[e], self.cnt[e]))
        for q, slots in self.dsem.items():
            for s, c in slots:
                if c > 0:
                    finals.append((s, 16 * c))
        for e in self.ENG:
            waits = []
            for s, v in finals:
                if self.seen[e].get(id(s), 0) < v:
                    self.seen[e][id(s)] = v
                    waits.append((s, v))
            if waits:
                self.ops[e].append((waits, None, None))

    def scope(self):
        return _Scope(self)

    def flush(self, final=False):
        nc = self.nc
        finals = []
        if final:
            for e in self.ENG:
                if self.cnt[e] > 0:
                    finals.append((self.cur[e], self.cnt[e]))
            for q, slots in self.dsem.items():
                for s, c in slots:
                    if c > 0:
                        finals.append((s, 16 * c))
        if not hasattr(self, "actual"):
            self.actual = {}
            self.amap = {}
        ref = set()
        for e in self.ENG:
            for waits, fn, inc in self.ops[e]:
                for s, v in waits:
                    if self.owner.get(id(s)) in self.ENG:
                        ref.add((id(s), v))
        for s, v in finals:
            if self.owner.get(id(s)) in self.ENG:
                ref.add((id(s), v))
        for e in self.ENG:
            for waits, fn, inc in self.ops[e]:
                if inc is not None and inc[0] == "c":
                    key = (id(inc[1]), inc[2])
                    if key in ref:
                        self.actual[key[0]] = self.actual.get(key[0], 0) + 1
                        self.amap[key] = self.actual[key[0]]

        def tr(s, v):
            if self.owner.get(id(s)) in self.ENG:
                return self.amap[(id(s), v)]
            return v

        engs = {"pe": "tensor", "dve": "vector", "act": "scalar", "pool": "gpsimd", "sp": "sync"}
        with nc.Block() as block:
            for e in self.ENG:
                ops = self.ops[e]
                if not ops and not (final and e == "sp"):
                    continue

                def body(engine, ops=ops, e=e):
                    for waits, fn, inc in ops:
                        for s, v in waits:
                            engine.wait_ge(s, tr(s, v))
                        if fn is not None:
                            ins = fn(engine)
                            if inc[0] == "d":
                                ins.then_inc(inc[1], 16)
                            elif (id(inc[1]), inc[2]) in self.amap:
                                ins.then_inc(inc[1], 1)
                    if final and e == "sp":
                        for s, v in finals:
                            engine.wait_ge(s, tr(s, v))

                getattr(block, engs[e])(body)
        self.nops = getattr(self, "nops", 0) + sum(len(v) for v in self.ops.values())
        self.ops = {e: [] for e in self.ENG}

    def finish(self):
        self.flush(final=True)
        self.stack.close()


class _Scope:
    def __init__(self, P):
        self.P = P

    def __enter__(self):
        self.saved = self.P.stack
        self.P.semstack = getattr(self.P, "semstack", self.saved)
        self.P.stack = ExitStack()
        return self

    def __exit__(self, *a):
        self.P.barrier()
        self.P.flush()
        self.P.stack.close()
        self.P.stack = self.saved
        return False


T = 4096
NT = 32
NTR = [32]
D = 1024
INC = 7896
EPS = 1e-6

O_AQ, O_AK, O_AV = 0, 512, 1024
O_AA, O_AB, O_AG = 1536, 1540, 1544
O_BU, O_BV, O_BG = 2056, 2568, 3080
O_CQ, O_CK, O_CV, O_CIQ, O_CIK, O_CIW, O_CG = 3592, 4104, 4168, 4232, 4744, 4808, 4816
O_DZ, O_DX, O_DDT = 5328, 5840, 6864
O_MQ, O_MG = 6872, 7384

PARAMS = [("norm_g", [2, 1024]), ("w_in", [2, 1024, INC]), ("gdn_conv_w", [2, 4, 1536]), ("gdn_a_log", [2, 4]),
          ("gdn_dt_bias", [2, 4]), ("gdn_norm_g", [2, 128]), ("sg_ln_g", [2, 512]), ("sg_ln_b", [2, 512]),
          ("sg_w", [2, 4, 128, 128]), ("sg_b", [2, 4, 128]), ("dsa_q_norm_g", [2, 64]), ("dsa_k_norm_g", [2, 64]),
          ("ssd_conv_w", [2, 4, 1024]), ("ssd_conv_b", [2, 1024]), ("ssd_a_log", [2, 8]), ("ssd_dt_bias", [2, 8]),
          ("ssd_d", [2, 8]), ("ssd_norm_g", [2, 512]), ("mem_norm_g", [2, 1024]), ("w_mem_kv", [2, 1024, 1024]),
          ("mem_q_norm_g", [2, 128]), ("mem_k_norm_g", [2, 128]), ("w_gate", [2, 5, 1024, 1024]),
          ("w_branch", [2, 5, 512, 1024]), ("w_out", [2, 1024, 1024])]


class Ctx:
    pass


STOP = [99]


def declare(nc, dbg=False, skip=()):
    C = Ctx()
    C.nc = nc
    if "x" not in skip:
        C.x = nc.dram_tensor("x", [T, D], F32, kind="ExternalInput").ap()
    C.mem = nc.dram_tensor("mem", [256, D], F32, kind="ExternalInput").ap()
    C.prm = {}
    for name, shp in PARAMS:
        if name in skip:
            continue
        C.prm[name] = nc.dram_tensor(name, shp, F32, kind="ExternalInput").ap()
    C.out = nc.dram_tensor("out", [T, D], F32, kind="ExternalOutput").ap()
    kind = "ExternalOutput" if dbg else "Internal"
    C.scr = {}

    def scr(name, shape, dt):
        C.scr[name] = nc.dram_tensor("scr_" + name, shape, dt, kind=kind).ap()

    for n in ("gq", "gk", "gv", "cq", "ciq", "mq"):
        scr(n, [512, T], BF16)
    scr("xbc", [1024, T], BF16)
    scr("ck", [64, T], BF16)
    scr("cik", [64, T], BF16)
    for n in ("ag", "bu", "bv", "bg", "cg", "dz", "mg"):
        scr(n, [T, 512], BF16)
    scr("misc", [T, 88], F32)
    scr("ysT", [5, 512, T], BF16)
    scr("x1", [T, D], F32)
    return C


def setup_consts(P, C):
    K = Ctx()
    K.ident = P.sbuf("ident", [128, 128], BF16)
    P.make_identity(K.ident)
    K.ones = P.sbuf("ones", [128, 128], BF16)
    P.memset("pool", K.ones[:], 1.0, [K.ones])
    K.blk2 = P.sbuf("blk2", [128, 128], BF16)
    P.memset("pool", K.blk2[:], 0.0, [K.blk2])
    P.memset("pool", K.blk2[0:64, 0:64], 1.0, [K.blk2])
    P.memset("pool", K.blk2[64:128, 64:128], 1.0, [K.blk2])
    K.eps = P.sbuf("epsc", [128, 1], F32)
    P.memset("pool", K.eps[:], EPS, [K.eps])
    P.barrier()
    return K


def alloc_hT(P, K):
    K.hT = P.sbuf("hT", [128, 8, T], BF16)
    K.hTb = [Buf("hT%d" % t, K.hT.ap) for t in range(NT)]


def alloc_merged(P, K):
    K.mT = P.sbuf("mT", [128, 8, T], BF16)
    K.mTb = [Buf("mT%d" % t, K.mT.ap) for t in range(8)]


def phase_norm(P, C, K, l, x_src):
    with P.scope():
        gb = P.sbuf("gb", [128, D], F32)
        P.dma("sp", gb[:], C.prm["norm_g"][l:l + 1, :].to_broadcast([128, D]), writes=[gb])
        xin = [P.sbuf("xin%d" % i, [128, D], F32) for i in range(2)]
        junk = P.sbuf("junk", [128, D], BF16)
        ss = [P.sbuf("ss%d" % i, [128, 1], F32) for i in range(2)]
        rt = [P.sbuf("rt%d" % i, [128, 1], F32) for i in range(2)]
        hb = [P.sbuf("hb%d" % i, [128, D], BF16) for i in range(2)]
        pt = [P.psum("pt%d" % i, [128, 4, 128], BF16) for i in range(2)]
        for t in range(NT):
            xi, s_, r_, h_ = xin[t % 2], ss[t % 2], rt[t % 2], hb[t % 2]
            P.dma("sp", xi[:], x_src[t * 128:(t + 1) * 128, :], writes=[xi])
            P.act(junk[:], xi[:], AF.Square, [xi], [junk, s_], accum_out=s_[:])
            P.act(r_[:], s_[:], AF.Sqrt, [s_, K.eps], [r_], scale=1.0 / D, bias=K.eps[:])
            P.recip(r_[:], r_[:], [r_], [r_])
            P.stt("dve", h_[:], xi[:], r_[:], gb[:], ALU.mult, ALU.mult, [xi, r_, gb], [h_])
            for half in range(2):
                p_ = pt[half]
                for j in range(4):
                    kc = half * 4 + j
                    P.transpose(p_[:, j, :], h_[:, kc * 128:(kc + 1) * 128], K.ident[:], [h_, K.ident], [p_])
                if half == 0:
                    P.copy("dve", K.hT[:, 0:4, t * 128:(t + 1) * 128], p_[:], [p_], [K.hTb[t]])
                else:
                    P.copy("act", K.hT[:, 4:8, t * 128:(t + 1) * 128], p_[:], [p_], [K.hTb[t]])


def phase_inproj(P, C, K, l):
    w_in = C.prm["w_in"][l]
    S = C.scr
    with P.scope():
        cwg = P.sbuf("cwg", [128, 4, 12], F32)
        cws = P.sbuf("cws", [128, 4, 8], F32)
        for k in range(4):
            P.dma("sp", cwg[:, k, :], C.prm["gdn_conv_w"][l][k].rearrange("(c p) -> p c", p=128), writes=[cwg],
                  allow_slow_non_contiguous=True)
            P.dma("sp", cws[:, k, :], C.prm["ssd_conv_w"][l][k].rearrange("(c p) -> p c", p=128), writes=[cws],
                  allow_slow_non_contiguous=True)
        cbs = P.sbuf("cbs", [128, 8], F32)
        P.dma("sp", cbs[:], C.prm["ssd_conv_b"][l].rearrange("(c p) -> p c", p=128), writes=[cbs],
              allow_slow_non_contiguous=True)
        gq2 = P.sbuf("gq2", [128, 1], F32)
        for i in range(2):
            P.dma("sp", gq2[i * 64:(i + 1) * 64, :], C.prm["dsa_q_norm_g"][l].rearrange("(p o) -> p o", o=1), writes=[gq2])
        gk1 = P.sbuf("gk1", [64, 1], F32)
        P.dma("sp", gk1[:], C.prm["dsa_k_norm_g"][l].rearrange("(p o) -> p o", o=1), writes=[gk1])
        gmq = P.sbuf("gmq", [128, 1], F32)
        P.dma("sp", gmq[:], C.prm["mem_q_norm_g"][l].rearrange("(p o) -> p o", o=1), writes=[gmq])
        P.ts("dve", gq2[:], gq2[:], 0.125, None, ALU.mult, None, [gq2], [gq2])
        P.ts("dve", gmq[:], gmq[:], 128 ** -0.5, None, ALU.mult, None, [gmq], [gmq])

        wb = [P.sbuf("wb%d" % i, [128, 8, 512], BF16) for i in range(2)]
        acc = [P.psum("acc%d" % i, [128, 512], F32) for i in range(3)]
        ssp = [P.psum("ssp%d" % i, [128, 512], F32) for i in range(2)]
        xpad = [P.sbuf("xpad%d" % i, [128, 515], F32) for i in range(4)]
        yb = [P.sbuf("yb%d" % i, [128, 512], F32) for i in range(4)]
        sb = [P.sbuf("sb%d" % i, [128, 512], F32) for i in range(2)]
        sq = [P.sbuf("sq%d" % i, [128, 512], BF16) for i in range(2)]
        rtb = [P.sbuf("rtb%d" % i, [128, 512], F32) for i in range(2)]
        ob = [P.sbuf("ob%d" % i, [128, 512], BF16) for i in range(3)]
        mo = [P.sbuf("mo%d" % i, [128, 88], F32) for i in range(2)]
        st = Ctx()
        st.g = 0
        st.a = 0
        st.e = 0
        st.o = 0

        st.loaded = {}

        def issue_load(gi, spec):
            w = wb[gi % 2]
            for (c0, n, d0) in spec:
                P.dma("pool", w[:, :, d0:d0 + n], w_in[:, c0:c0 + n].rearrange("(kc k) c -> k kc c", k=128), writes=[w])
            st.loaded[gi] = w

        def load_w(col0, ncols):
            w = st.loaded[st.g]
            st.g += 1
            return w

        def fm_group(col0, ncols, kind, dst, cw=None, cb=None, gain=None, scale=1.0):
            w = load_w(col0, ncols)
            nch = (ncols + 127) // 128
            for j in range(nch):
                m = min(128, ncols - j * 128)
                for tg in range(8):
                    a = acc[st.a % 3]
                    st.a += 1
                    for kc in range(8):
                        P.matmul(a[0:m, :], w[:, kc, j * 128:j * 128 + m], K.hT[:, kc, tg * 512:(tg + 1) * 512],
                                 kc == 0, kc == 7, [w] + K.hTb[tg * 4:tg * 4 + 4], [a])
                    e = st.e
                    st.e += 1
                    o = ob[st.o % 3]
                    st.o += 1
                    dsl = dst[j * 128:j * 128 + m, tg * 512:(tg + 1) * 512]
                    if kind == "raw":
                        P.copy("act", o[0:m, :], a[0:m, :], [a], [o])
                        P.dma("sp", dsl, o[0:m, :], reads=[o])
                        continue
                    if kind in ("conv", "conv_l2"):
                        xp, xn = xpad[tg % 4], xpad[(tg + 1) % 4]
                        y = yb[e % 4]
                        ce = "dve"
                        if tg == 0:
                            P.memset("dve", xp[:, 0:3], 0.0, [xp])
                        P.copy("act", xp[:, 3:515], a[:], [a], [xp])
                        cwb, cwo = cw
                        cj = cwo + j
                        P.ts(ce, y[:], xp[:, 0:512], cwb[:, 0, cj:cj + 1], None, ALU.mult, None, [xp, cwb], [y])
                        for k in range(1, 4):
                            P.stt(ce, y[:], xp[:, k:k + 512], cwb[:, k, cj:cj + 1], y[:], ALU.mult, ALU.add, [xp, cwb, y], [y])
                        if tg < 7:
                            P.copy("act", xn[:, 0:3], xp[:, 512:515], [xp], [xn])
                        if kind == "conv":
                            if cb is not None:
                                P.act(o[:], y[:], AF.Silu, [y, cb[0]], [o], bias=cb[0][:, cb[1] + j:cb[1] + j + 1])
                            else:
                                P.act(o[:], y[:], AF.Silu, [y], [o])
                            P.dma("sp", dsl, o[:], reads=[o])
                            continue
                        s_ = sb[e % 2]
                        P.act(s_[:], y[:], AF.Silu, [y], [s_])
                        src_ = s_
                        ones = K.ones
                        nrm_scale = 1.0
                    else:
                        s_ = sb[e % 2]
                        P.copy("act", s_[0:m, :], a[0:m, :], [a], [s_])
                        ones = K.blk2 if kind == "rms64" else K.ones
                        nrm_scale = (1.0 / 64) if kind == "rms64" else (1.0 / 128)
                    q_ = sq[e % 2]
                    P.act(q_[0:m, :], s_[0:m, :], AF.Square, [s_], [q_])
                    sp_ = ssp[e % 2]
                    P.matmul(sp_[0:m, :], ones[0:m, 0:m], q_[0:m, :], True, True, [ones, q_], [sp_])
                    r_ = rtb[e % 2]
                    P.act(r_[0:m, :], sp_[0:m, :], AF.Sqrt, [sp_, K.eps], [r_], scale=nrm_scale, bias=K.eps[0:m, :])
                    P.recip(r_[0:m, :], r_[0:m, :], [r_], [r_])
                    if gain is not None:
                        P.stt("dve", o[0:m, :], s_[0:m, :], gain[0:m, :], r_[0:m, :], ALU.mult, ALU.mult, [s_, gain, r_], [o])
                    else:
                        P.stt("dve", o[0:m, :], s_[0:m, :], scale, r_[0:m, :], ALU.mult, ALU.mult, [s_, r_], [o])
                    P.dma("sp", dsl, o[0:m, :], reads=[o])

        def tm_group(col0, func, dst):
            w = load_w(col0, 512)
            for t in range(NT):
                a = acc[st.a % 3]
                st.a += 1
                for kc in range(8):
                    P.matmul(a[:], K.hT[:, kc, t * 128:(t + 1) * 128], w[:, kc, :], kc == 0, kc == 7,
                             [w, K.hTb[t]], [a])
                o = ob[st.o % 3]
                st.o += 1
                if func is None:
                    P.copy("act", o[:], a[:], [a], [o])
                else:
                    P.act(o[:], a[:], func, [a], [o])
                P.dma("sp", dst[t * 128:(t + 1) * 128, :], o[:], reads=[o])

        def misc_group():
            w = load_w(0, 88)
            for t in range(NT):
                a = acc[st.a % 3]
                st.a += 1
                for kc in range(8):
                    P.matmul(a[:, 0:88], K.hT[:, kc, t * 128:(t + 1) * 128], w[:, kc, 0:88], kc == 0, kc == 7,
                             [w, K.hTb[t]], [a])
                o = mo[t % 2]
                P.copy("act", o[:], a[:, 0:88], [a], [o])
                P.dma("sp", S["misc"][t * 128:(t + 1) * 128, :], o[:], reads=[o])

        groups = [
            (((O_AA, 8, 0), (O_CIW, 8, 8), (O_DDT, 8, 16), (O_CV, 64, 24)), lambda: misc_group()),
            (((O_CIQ, 512, 0),), lambda: fm_group(O_CIQ, 512, "raw", S["ciq"])),
            (((O_CIK, 64, 0),), lambda: fm_group(O_CIK, 64, "raw", S["cik"])),
            (((O_CQ, 512, 0),), lambda: fm_group(O_CQ, 512, "rms64", S["cq"], gain=gq2)),
            (((O_CK, 64, 0),), lambda: fm_group(O_CK, 64, "rms64", S["ck"], gain=gk1)),
            (((O_MQ, 512, 0),), lambda: fm_group(O_MQ, 512, "rms128", S["mq"], gain=gmq)),
            (((O_AQ, 512, 0),), lambda: fm_group(O_AQ, 512, "conv_l2", S["gq"], cw=(cwg, 0), scale=128 ** -0.5)),
            (((O_AK, 512, 0),), lambda: fm_group(O_AK, 512, "conv_l2", S["gk"], cw=(cwg, 4), scale=1.0)),
            (((O_AV, 512, 0),), lambda: fm_group(O_AV, 512, "conv", S["gv"], cw=(cwg, 8))),
            (((O_DX, 512, 0),), lambda: fm_group(O_DX, 512, "conv", S["xbc"][0:512], cw=(cws, 0), cb=(cbs, 0))),
            (((O_DX + 512, 512, 0),), lambda: fm_group(O_DX + 512, 512, "conv", S["xbc"][512:1024], cw=(cws, 4), cb=(cbs, 4))),
            (((O_AG, 512, 0),), lambda: tm_group(O_AG, AF.Silu, S["ag"])),
            (((O_BG, 512, 0),), lambda: tm_group(O_BG, AF.Silu, S["bg"])),
            (((O_CG, 512, 0),), lambda: tm_group(O_CG, AF.Silu, S["cg"])),
            (((O_DZ, 512, 0),), lambda: tm_group(O_DZ, AF.Silu, S["dz"])),
            (((O_MG, 512, 0),), lambda: tm_group(O_MG, AF.Silu, S["mg"])),
            (((O_BU, 512, 0),), lambda: tm_group(O_BU, AF.Gelu, S["bu"])),
            (((O_BV, 512, 0),), lambda: tm_group(O_BV, AF.Gelu, S["bv"])),
        ]
        issue_load(0, groups[0][0])
        for gi, (spec, run) in enumerate(groups):
            if gi + 1 < len(groups):
                issue_load(gi + 1, groups[gi + 1][0])
            run()


def phase_merge(P, C, K, l):
    wgd = C.prm["w_gate"][l]
    wbd = C.prm["w_branch"][l]
    ysT = C.scr["ysT"]
    with P.scope():
        wbuf = [P.sbuf("mw%d" % i, [128, 7680], BF16) for i in range(2)]
        yT = [P.sbuf("yT%d" % i, [128, 4, 512], BF16) for i in range(3)]
        gps = [P.psum("gps%d" % i, [128, 512], F32) for i in range(2)]
        zps = [P.psum("zps%d" % i, [128, 512], F32) for i in range(2)]
        sg = [P.sbuf("sg%d" % i, [128, 512], F32) for i in range(2)]
        tmp = [P.sbuf("tmp%d" % i, [128, 512], F32) for i in range(2)]
        mac = [P.sbuf("mac%d" % i, [128, 512], F32) for i in range(2)]
        cnt = 0

        def views(w):
            return (w[:, 0:5120].rearrange("k (p kc n) -> k p kc n", p=5, kc=8),
                    w[:, 5120:7680].rearrange("k (p cc n) -> k p cc n", p=5, cc=4))

        def load(nch):
            w = wbuf[nch % 2]
            wg, wbr = views(w)
            for p in range(5):
                P.dma("pool", wg[:, p], wgd[p][:, nch * 128:(nch + 1) * 128].rearrange("(kc k) n -> k kc n", k=128), writes=[w])
                P.dma("pool", wbr[:, p], wbd[p][:, nch * 128:(nch + 1) * 128].rearrange("(cc k) n -> k cc n", k=128), writes=[w])

        load(0)
        for nch in range(8):
            w = wbuf[nch % 2]
            wg, wbr = views(w)
            if nch + 1 < 8:
                load(nch + 1)
            for tg in range(8):
                m_ = mac[tg % 2]
                for p in range(5):
                    y = yT[cnt % 3]
                    g_, z_ = gps[cnt % 2], zps[cnt % 2]
                    s_, t_ = sg[cnt % 2], tmp[cnt % 2]
                    cnt += 1
                    P.dma("sp", y[:], ysT[p][:, tg * 512:(tg + 1) * 512].rearrange("(cc c) t -> c cc t", c=128), writes=[y])
                    for kc in range(8):
                        P.matmul(g_[:], wg[:, p, kc, :], K.hT[:, kc, tg * 512:(tg + 1) * 512], kc == 0, kc == 7,
                                 [w] + K.hTb[tg * 4:tg * 4 + 4], [g_])
                    for cc in range(4):
                        P.matmul(z_[:], wbr[:, p, cc, :], y[:, cc, :], cc == 0, cc == 3, [w, y], [z_])
                    P.act(s_[:], g_[:], AF.Sigmoid, [g_], [s_])
                    if p == 0:
                        P.tt("dve", m_[:], z_[:], s_[:], ALU.mult, [z_, s_], [m_])
                    elif p < 4:
                        P.tt("dve", t_[:], z_[:], s_[:], ALU.mult, [z_, s_], [t_])
                        P.tt("pool", m_[:], m_[:], t_[:], ALU.add, [m_, t_], [m_])
                    else:
                        P.tt("dve", t_[:], z_[:], s_[:], ALU.mult, [z_, s_], [t_])
                        P.tt("pool", K.mT[:, nch, tg * 512:(tg + 1) * 512], m_[:], t_[:], ALU.add, [m_, t_], [K.mTb[tg]])


def phase_outproj(P, C, K, l, x_src, x_dst):
    with P.scope():
        wo = P.sbuf("wo", [128, 8, D], BF16)
        P.dma("pool", wo[:], C.prm["w_out"][l].rearrange("(kc k) n -> k kc n", k=128), writes=[wo])
        xin = [P.sbuf("oxin%d" % i, [128, 512], F32) for i in range(2)]
        xo = [P.sbuf("oxo%d" % i, [128, 512], F32) for i in range(2)]
        ops_ = [P.psum("ops%d" % i, [128, 512], F32) for i in range(2)]
        c = 0
        for t in range(NT):
            for hf in range(2):
                xi, o, ps = xin[c % 2], xo[c % 2], ops_[c % 2]
                c += 1
                P.dma("sp", xi[:], x_src[t * 128:(t + 1) * 128, hf * 512:(hf + 1) * 512], writes=[xi])
                for kc in range(8):
                    P.matmul(ps[:], K.mT[:, kc, t * 128:(t + 1) * 128], wo[:, kc, hf * 512:(hf + 1) * 512], kc == 0, kc == 7,
                             [wo, K.mTb[t // 4]], [ps])
                P.tt("dve", o[:], ps[:], xi[:], ALU.add, [ps, xi], [o])
                P.dma("sp", x_dst[t * 128:(t + 1) * 128, hf * 512:(hf + 1) * 512], o[:], reads=[o])


class YOut:
    def __init__(self, P, C, K, p, tag, nps=1):
        self.P, self.C, self.K, self.p = P, C, K, p
        self.ps = [P.psum("yo_ps%s%d" % (tag, i), [128, 4, 128], BF16) for i in range(nps)]
        self.sb = [P.sbuf("yo_sb%s%d" % (tag, i), [128, 4, 128], BF16) for i in range(2)]
        self.n = 0

    def emit(self, y, t, eng="act"):
        P, K = self.P, self.K
        ps, sb = self.ps[self.n % len(self.ps)], self.sb[self.n % 2]
        self.n += 1
        for cc in range(4):
            P.transpose(ps[:, cc, :], y[:, cc * 128:(cc + 1) * 128], K.ident[:], [y, K.ident], [ps])
        P.copy(eng, sb[:], ps[:], [ps], [sb])
        P.dma("sp", self.C.scr["ysT"][self.p][:, t * 128:(t + 1) * 128].rearrange("(cc c) t -> c cc t", c=128), sb[:], reads=[sb])


def phase_mem(P, C, K, l):
    S = C.scr
    with P.scope():
        gb = P.sbuf("m_gb", [128, D], F32)
        P.dma("sp", gb[:], C.prm["mem_norm_g"][l:l + 1, :].to_broadcast([128, D]), writes=[gb])
        gk = P.sbuf("m_gk", [128, 1], F32)
        P.dma("sp", gk[:], C.prm["mem_k_norm_g"][l].rearrange("(p o) -> p o", o=1), writes=[gk])
        wkv = P.sbuf("m_wkv", [128, 8, D], BF16)
        P.dma("pool", wkv[:], C.prm["w_mem_kv"][l].rearrange("(kc k) n -> k kc n", k=128), writes=[wkv])
        memT = P.sbuf("memT", [128, 8, 256], BF16)
        kT = P.sbuf("m_kT", [128, 4, 256], BF16)
        vaug = [P.sbuf("m_va%d" % i, [128, 4, 129], BF16) for i in range(2)]
        xin = P.sbuf("m_x", [128, D], F32)
        junk = P.sbuf("m_junk", [128, D], BF16)
        ss = P.sbuf("m_ss", [128, 1], F32)
        hb = P.sbuf("m_hb", [128, D], BF16)
        pt = P.psum("m_pt", [128, 4, 128], BF16)
        pa = P.psum("m_pa", [128, 512], F32)
        pb = P.psum("m_pb", [128, 512], F32)
        for mt in range(2):
            P.dma("sp", xin[:], C.mem[mt * 128:(mt + 1) * 128, :], writes=[xin])
            P.act(junk[:], xin[:], AF.Square, [xin], [junk, ss], accum_out=ss[:])
            P.act(ss[:], ss[:], AF.Sqrt, [ss, K.eps], [ss], scale=1.0 / D, bias=K.eps[:])
            P.recip(ss[:], ss[:], [ss], [ss])
            P.stt("dve", hb[:], xin[:], ss[:], gb[:], ALU.mult, ALU.mult, [xin, ss, gb], [hb])
            for half in range(2):
                for j in range(4):
                    kc = half * 4 + j
                    P.transpose(pt[:, j, :], hb[:, kc * 128:(kc + 1) * 128], K.ident[:], [hb, K.ident], [pt])
                P.copy("dve", memT[:, half * 4:half * 4 + 4, mt * 128:(mt + 1) * 128], pt[:], [pt], [memT])
        sq = P.sbuf("m_sq", [128, 256], BF16)
        kf = P.sbuf("m_kf", [128, 256], F32)
        rr = P.sbuf("m_rr", [128, 256], F32)
        for h in range(4):
            for kc in range(8):
                P.matmul(pa[:, 0:256], wkv[:, kc, h * 128:(h + 1) * 128], memT[:, kc, :], kc == 0, kc == 7, [wkv, memT], [pa])
            P.copy("act", kf[:], pa[:, 0:256], [pa], [kf])
            P.act(sq[:], kf[:], AF.Square, [kf], [sq])
            P.matmul(pb[:, 0:256], K.ones[:], sq[:], True, True, [K.ones, sq], [pb])
            P.act(rr[:], pb[:, 0:256], AF.Sqrt, [pb, K.eps], [rr], scale=1.0 / 128, bias=K.eps[:])
            P.recip(rr[:], rr[:], [rr], [rr])
            P.stt("dve", kT[:, h, :], kf[:], gk[:], rr[:], ALU.mult, ALU.mult, [kf, gk, rr], [kT])
        for mt in range(2):
            for kc in range(8):
                P.matmul(pa[:], memT[:, kc, mt * 128:(mt + 1) * 128], wkv[:, kc, 512:1024], kc == 0, kc == 7, [wkv, memT], [pa])
            P.memset("pool", vaug[mt][:, :, 128:129], 1.0, [vaug[mt]])
            P.copy("act", vaug[mt][:, :, 0:128], pa[:].rearrange("m (h d) -> m h d", h=4), [pa], [vaug[mt]])
        qT = [P.sbuf("m_qT%d" % i, [128, 4, 128], BF16) for i in range(2)]
        gt = [P.sbuf("m_gt%d" % i, [128, 512], BF16) for i in range(2)]
        lg = [P.psum("m_lg%d" % i, [128, 4, 128], F32) for i in range(2)]
        pT = [P.sbuf("m_pT%d" % i, [128, 4, 128], BF16) for i in range(2)]
        po = P.psum("m_po", [128, 4, 256], F32)
        rd = [P.sbuf("m_rd%d" % i, [128, 4], F32) for i in range(2)]
        yb = [P.sbuf("m_y%d" % i, [128, 512], BF16) for i in range(2)]
        yo = YOut(P, C, K, 4, "m")
        for t in range(NTR[0]):
            q, g = qT[t % 2], gt[t % 2]
            P.dma("sp", q[:], S["mq"][:, t * 128:(t + 1) * 128].rearrange("(h d) t -> d h t", d=128), writes=[q])
            P.dma("sp", g[:], S["mg"][t * 128:(t + 1) * 128, :], writes=[g])
            for mt in range(2):
                for h in range(4):
                    P.matmul(lg[mt][:, h, :], kT[:, h, mt * 128:(mt + 1) * 128], q[:, h, :], True, True, [kT, q], [lg[mt]])
                P.act(pT[mt][:], lg[mt][:], AF.Exp, [lg[mt]], [pT[mt]])
            for h in range(4):
                for mt in range(2):
                    P.matmul(po[:, h, 0:129], pT[mt][:, h, :], vaug[mt][:, h, :], mt == 0, mt == 1, [pT[mt], vaug[mt]], [po])
            r_ = rd[t % 2]
            y = yb[t % 2]
            P.recip(r_[:], po[:, :, 128], [po], [r_])
            for h in range(4):
                P.stt("dve", y[:, h * 128:(h + 1) * 128], po[:, h, 0:128], r_[:, h:h + 1], g[:, h * 128:(h + 1) * 128],
                      ALU.mult, ALU.mult, [po, r_, g], [y])
            yo.emit(y, t)


def phase_sg(P, C, K, l):
    S = C.scr
    with P.scope():
        lng = P.sbuf("b_lng", [128, 512], F32)
        lnb = P.sbuf("b_lnb", [128, 512], F32)
        P.dma("sp", lng[:], C.prm["sg_ln_g"][l:l + 1, :].to_broadcast([128, 512]), writes=[lng])
        P.dma("sp", lnb[:], C.prm["sg_ln_b"][l:l + 1, :].to_broadcast([128, 512]), writes=[lnb])
        bsT = P.sbuf("b_bsT", [128, 4], F32)
        P.dma("sp", bsT[:], C.prm["sg_b"][l].rearrange("g t -> t g"), writes=[bsT], allow_slow_non_contiguous=True)
        eps5 = P.sbuf("b_eps5", [128, 1], F32)
        P.memset("pool", eps5[:], 1e-5, [eps5])
        wf = P.sbuf("b_wf", [128, 4, 128], F32)
        wbf = P.sbuf("b_wbf", [128, 4, 128], BF16)
        WcT = P.sbuf("b_WcT", [128, 4, 128], BF16)
        pt = P.psum("b_pt", [128, 4, 128], BF16)
        P.dma("sp", wf[:], C.prm["sg_w"][l].rearrange("g t s -> t g s"), writes=[wf])
        for g in range(4):
            P.op("pool", lambda e, g=g: e.affine_select(out=wf[:, g, :], in_=wf[:, g, :], pattern=[[-1, 128]], compare_op=ALU.is_ge,
                                                        fill=0.0, base=0, channel_multiplier=1), [wf], [wf])
        P.barrier()
        P.copy("dve", wbf[:], wf[:], [wf], [wbf])
        for g in range(4):
            P.transpose(pt[:, g, :], wbf[:, g, :], K.ident[:], [wbf, K.ident], [pt])
        P.copy("dve", WcT[:], pt[:], [pt], [WcT])

        vin = [P.sbuf("b_v%d" % i, [128, 512], BF16) for i in range(2)]
        uin = [P.sbuf("b_u%d" % i, [128, 512], BF16) for i in range(2)]
        gin = [P.sbuf("b_g%d" % i, [128, 512], BF16) for i in range(2)]
        st6 = [P.sbuf("b_st%d" % i, [128, 6], F32) for i in range(2)]
        mv = [P.sbuf("b_mv%d" % i, [128, 2], F32) for i in range(2)]
        rs = [P.sbuf("b_rs%d" % i, [128, 1], F32) for i in range(2)]
        vn = [P.sbuf("b_vn%d" % i, [128, 512], F32) for i in range(2)]
        vnb = [P.sbuf("b_vnb%d" % i, [128, 512], BF16) for i in range(2)]
        mx = [P.psum("b_mx%d" % i, [128, 512], F32) for i in range(2)]
        tm = [P.sbuf("b_tm%d" % i, [128, 512], F32) for i in range(2)]
        yb = [P.sbuf("b_y%d" % i, [128, 512], BF16) for i in range(2)]
        yo = YOut(P, C, K, 1, "b")
        for t in range(NTR[0]):
            i = t % 2
            v, u, g = vin[i], uin[i], gin[i]
            sl = slice(t * 128, (t + 1) * 128)
            P.dma("sp", v[:], S["bv"][sl, :], writes=[v])
            P.dma("sp", u[:], S["bu"][sl, :], writes=[u])
            P.dma("sp", g[:], S["bg"][sl, :], writes=[g])
            P.op("dve", lambda e, i=i: e.bn_stats(st6[i][:], vin[i][:]), [v], [st6[i]])
            P.op("dve", lambda e, i=i: e.bn_aggr(mv[i][:], st6[i][:]), [st6[i]], [mv[i]])
            P.act(rs[i][:], mv[i][:, 1:2], AF.Sqrt, [mv[i], eps5], [rs[i]], bias=eps5[:])
            P.recip(rs[i][:], rs[i][:], [rs[i]], [rs[i]])
            P.ts("dve", vn[i][:], v[:], mv[i][:, 0:1], rs[i][:], ALU.subtract, ALU.mult, [v, mv[i], rs[i]], [vn[i]])
            P.tt("pool", vn[i][:], vn[i][:], lng[:], ALU.mult, [vn[i], lng], [vn[i]])
            P.tt("pool", vnb[i][:], vn[i][:], lnb[:], ALU.add, [vn[i], lnb], [vnb[i]])
            for gg in range(4):
                P.matmul(mx[i][:, gg * 128:(gg + 1) * 128], WcT[:, gg, :], vnb[i][:, gg * 128:(gg + 1) * 128], True, True,
                         [WcT, vnb[i]], [mx[i]])
            for gg in range(4):
                c = slice(gg * 128, (gg + 1) * 128)
                P.stt("dve", tm[i][:, c], mx[i][:, c], bsT[:, gg:gg + 1], u[:, c], ALU.add, ALU.mult, [mx[i], bsT, u], [tm[i]])
            P.tt("pool", yb[i][:], tm[i][:], g[:], ALU.mult, [tm[i], g], [yb[i]])
            yo.emit(yb[i], t)


def bc(ap, shape):
    return ap.to_broadcast(list(shape))


def phase_ssd(P, C, K, l):
    S = C.scr
    with P.scope():
        uinc = P.sbuf("d_uinc", [128, 128], F32)
        P.memset("pool", uinc[:], 1.0, [uinc])
        P.op("pool", lambda e: e.affine_select(out=uinc[:], in_=uinc[:], pattern=[[1, 128]], compare_op=ALU.is_ge,
                                               fill=0.0, base=0, channel_multiplier=-1), [uinc], [uinc])
        onesf = P.sbuf("d_onesf", [128, 128], F32)
        P.memset("pool", onesf[:], 1.0, [onesf])
        P.barrier()
        aB = P.sbuf("d_aB", [128, 8], F32)
        P.dma("sp", aB[:], C.prm["ssd_a_log"][l:l + 1, :].to_broadcast([128, 8]), writes=[aB])
        P.act(aB[:], aB[:], AF.Exp, [aB], [aB])
        P.ts("dve", aB[:], aB[:], -1.0, None, ALU.mult, None, [aB], [aB])
        dtb = P.sbuf("d_dtb", [128, 8], F32)
        P.dma("sp", dtb[:], C.prm["ssd_dt_bias"][l:l + 1, :].to_broadcast([128, 8]), writes=[dtb])
        dsk = P.sbuf("d_dsk", [128, 8], F32)
        P.dma("sp", dsk[:], C.prm["ssd_d"][l:l + 1, :].to_broadcast([128, 8]), writes=[dsk])
        ngB = P.sbuf("d_ngB", [128, 512], F32)
        P.dma("sp", ngB[:], C.prm["ssd_norm_g"][l:l + 1, :].to_broadcast([128, 512]), writes=[ngB])
        S32 = P.sbuf("d_S32", [128, 512], F32)
        Sbf = P.sbuf("d_Sbf", [128, 512], BF16)
        P.memset("pool", S32[:], 0.0, [S32])
        P.memset("pool", Sbf[:], 0.0, [Sbf])

        xf = [P.sbuf("d_xf%d" % i, [128, 8, 128], BF16) for i in range(2)]
        dtin = [P.sbuf("d_dtin%d" % i, [128, 8], F32) for i in range(2)]
        zg = [P.sbuf("d_zg%d" % i, [128, 512], BF16) for i in range(2)]
        dt = P.sbuf("d_dt", [128, 8], F32)
        la = P.sbuf("d_la", [128, 8], F32)
        lab = P.sbuf("d_lab", [128, 8, 128], F32)
        cs = P.sbuf("d_cs", [128, 16], F32)
        pre = P.sbuf("d_pre", [128, 24], F32)
        E = P.sbuf("d_E", [128, 24], F32)
        seg = P.sbuf("d_seg", [128, 8, 128], F32)
        Lm = P.sbuf("d_L", [128, 8, 128], F32)
        MT = P.sbuf("d_MT", [128, 8, 128], BF16)
        cbm = P.sbuf("d_cbm", [128, 2, 128], F32)
        xs = P.sbuf("d_xs", [128, 512], BF16)
        btm = P.sbuf("d_btm", [128, 2, 128], BF16)
        xdt = P.sbuf("d_xdt", [128, 512], BF16)
        xdtd = P.sbuf("d_xdtd", [128, 512], BF16)
        t1 = P.sbuf("d_t1", [128, 512], F32)
        y1 = P.sbuf("d_y1", [128, 512], F32)
        y2 = P.sbuf("d_y2", [128, 512], F32)
        junk = P.sbuf("d_junk", [128, 512], BF16)
        ssq = P.sbuf("d_ssq", [128, 1], F32)
        yb = [P.sbuf("d_yb%d" % i, [128, 512], BF16) for i in range(2)]

        small = P.psum("d_small", [128, 512], F32)
        csps = View(small, small[:, 0:16])
        cbps = View(small, small[:, 128:384])
        csrow = P.psum("d_csrow", [128, 8, 128], F32)
        tp = P.psum("d_tp", [128, 768], BF16)
        yps = P.psum("d_yps", [128, 512], F32)
        yoff = P.psum("d_yoff", [128, 512], F32)
        stp = P.psum("d_stp", [128, 512], F32)
        yo = YOut(P, C, K, 3, "d")

        for t in range(NTR[0]):
            i = t % 2
            sl = slice(t * 128, (t + 1) * 128)
            x_ = xf[i]
            P.dma("sp", x_[:], S["xbc"][:, sl].rearrange("(c p) t -> p c t", p=128), writes=[x_])
            P.dma("sp", dtin[i][:], S["misc"][sl, 16:24], writes=[dtin[i]])
            P.dma("sp", zg[i][:], S["dz"][sl, :], writes=[zg[i]])
            P.tt("dve", dt[:], dtin[i][:], dtb[:], ALU.add, [dtin[i], dtb], [dt])
            P.act(dt[:], dt[:], AF.Exp, [dt], [dt])
            P.act(dt[:], dt[:], AF.Ln, [dt], [dt], bias=1.0)
            P.tt("dve", la[:], dt[:], aB[:], ALU.mult, [dt, aB], [la])
            if STOP[0] <= 1:
                continue
            P.matmul(csps[:, 0:8], uinc[:], la[:], True, True, [uinc, la], [csps])
            P.matmul(csps[:, 8:16], onesf[:], la[:], True, True, [onesf, la], [csps])
            P.copy("dve", cs[:], csps[:], [csps], [cs])
            if STOP[0] <= 2:
                continue
            P.copy("dve", lab[:], bc(la[:].unsqueeze(2), [128, 8, 128]), [la], [lab])
            for h in range(8):
                P.matmul(csrow[:, h, :], lab[:, h, :], uinc[:], True, True, [lab, uinc], [csrow])
            if STOP[0] <= 3:
                continue
            P.tt("dve", pre[:, 0:8], cs[:, 8:16], cs[:, 0:8], ALU.subtract, [cs], [pre])
            P.copy("dve", pre[:, 8:16], cs[:, 8:16], [cs], [pre])
            P.copy("dve", pre[:, 16:24], cs[:, 0:8], [cs], [pre])
            P.act(E[:], pre[:], AF.Exp, [pre], [E])
            if STOP[0] <= 4:
                continue
            P.tt("dve", seg[:], csrow[:], bc(cs[:, 0:8].unsqueeze(2), [128, 8, 128]), ALU.subtract, [csrow, cs], [seg])
            P.ts("dve", seg[:], seg[:], 0.0, None, ALU.min, None, [seg], [seg])
            P.act(Lm[:], seg[:], AF.Exp, [seg], [Lm])
            if STOP[0] <= 5:
                continue
            for g in range(2):
                P.matmul(cbps[:, g * 128:(g + 1) * 128], x_[:, 4 + g, :], x_[:, 6 + g, :], True, True, [x_], [cbps])
            P.tt("dve", cbm[:], cbps[:].rearrange("s (g c) -> s g c", g=2), bc(uinc[:].unsqueeze(1), [128, 2, 128]), ALU.mult,
                 [cbps, uinc], [cbm])
            for g in range(2):
                P.tt("dve", MT[:, g * 4:(g + 1) * 4, :], Lm[:, g * 4:(g + 1) * 4, :], bc(cbm[:, g:g + 1, :], [128, 4, 128]), ALU.mult,
                     [Lm, cbm], [MT])
            if STOP[0] <= 6:
                continue
            for c in range(4):
                P.transpose(tp[:, c * 128:(c + 1) * 128], x_[:, c, :], K.ident[:], [x_, K.ident], [tp])
            for g in range(2):
                P.transpose(tp[:, 512 + g * 128:512 + (g + 1) * 128], x_[:, 4 + g, :], K.ident[:], [x_, K.ident], [tp])
            P.copy("act", xs[:], tp[:, 0:512], [tp], [xs])
            P.copy("act", btm[:], tp[:, 512:768].rearrange("s (g d) -> s g d", g=2), [tp], [btm])
            xs3 = xs[:].rearrange("s (h p) -> s h p", h=8)
            P.tt("dve", xdt[:].rearrange("s (h p) -> s h p", h=8), xs3, bc(dt[:].unsqueeze(2), [128, 8, 64]), ALU.mult, [xs, dt], [xdt])
            P.tt("dve", xdtd[:].rearrange("s (h p) -> s h p", h=8), xdt[:].rearrange("s (h p) -> s h p", h=8),
                 bc(E[:, 0:8].unsqueeze(2), [128, 8, 64]), ALU.mult, [xdt, E], [xdtd])
            if STOP[0] <= 7:
                continue
            for h in range(8):
                P.matmul(yps[:, h * 64:(h + 1) * 64], MT[:, h, :], xdt[:, h * 64:(h + 1) * 64], True, True, [MT, xdt], [yps])
            for g in range(2):
                P.matmul(yoff[:, g * 256:(g + 1) * 256], x_[:, 6 + g, :], Sbf[:, g * 256:(g + 1) * 256], True, True, [x_, Sbf], [yoff])
            P.tt("dve", t1[:].rearrange("s (h p) -> s h p", h=8), yoff[:].rearrange("s (h p) -> s h p", h=8),
                 bc(E[:, 16:24].unsqueeze(2), [128, 8, 64]), ALU.mult, [yoff, E], [t1])
            P.tt("dve", y1[:], yps[:], t1[:], ALU.add, [yps, t1], [y1])
            P.tt("pool", t1[:].rearrange("s (h p) -> s h p", h=8), xs3, bc(dsk[:].unsqueeze(2), [128, 8, 64]), ALU.mult, [xs, dsk], [t1])
            P.tt("pool", y1[:], y1[:], t1[:], ALU.add, [y1, t1], [y1])
            P.tt("pool", y2[:], y1[:], zg[i][:], ALU.mult, [y1, zg[i]], [y2])
            P.act(junk[:], y2[:], AF.Square, [y2], [junk, ssq], accum_out=ssq[:])
            P.act(ssq[:], ssq[:], AF.Sqrt, [ssq, K.eps], [ssq], scale=1.0 / 512, bias=K.eps[:])
            P.recip(ssq[:], ssq[:], [ssq], [ssq])
            P.stt("dve", yb[i][:], y2[:], ssq[:], ngB[:], ALU.mult, ALU.mult, [y2, ssq, ngB], [yb[i]])
            yo.emit(yb[i], t)
            if STOP[0] <= 8:
                continue
            for g in range(2):
                P.matmul(stp[:, g * 256:(g + 1) * 256], btm[:, g, :], xdtd[:, g * 256:(g + 1) * 256], True, True, [btm, xdtd], [stp])
            P.tt("dve", S32[:].rearrange("d (h p) -> d h p", h=8), S32[:].rearrange("d (h p) -> d h p", h=8),
                 bc(E[:, 8:16].unsqueeze(2), [128, 8, 64]), ALU.mult, [S32, E], [S32])
            P.tt("dve", S32[:], S32[:], stp[:], ALU.add, [S32, stp], [S32])
            P.copy("act", Sbf[:], S32[:], [S32], [Sbf])


NBIS = 18


def phase_dsa(P, C, K, l):
    S = C.scr
    I32 = mybir.dt.int32
    with P.scope():
        rb = [P.sbuf("c_rb%d" % i, [1, T], BF16) for i in range(3)]
        P.op("pool", lambda e: e.iota(rb[0][:], pattern=[[0, NT], [1, 128]], base=0, channel_multiplier=0,
                                      allow_small_or_imprecise_dtypes=True), (), [rb[0]])
        P.op("pool", lambda e: e.iota(rb[1][:], pattern=[[1, NT], [0, 128]], base=0, channel_multiplier=0,
                                      allow_small_or_imprecise_dtypes=True), (), [rb[1]])
        P.memset("pool", rb[2][:], 1.0, [rb[2]])
        posrow = P.sbuf("c_posrow", [128, T], F32)
        P.op("pool", lambda e: e.iota(posrow[:], pattern=[[1, T]], base=0, channel_multiplier=0,
                                      allow_small_or_imprecise_dtypes=True), (), [posrow])
        sl1 = P.sbuf("c_sl1", [1, 8, 128], BF16)
        sl128 = P.sbuf("c_sl128", [1, 8, 128], BF16)
        nsl64 = P.sbuf("c_nsl64", [1, 8, 128], F32)
        nsl1 = P.sbuf("c_nsl1", [1, 8, 128], F32)
        for h in range(8):
            sl = 2.0 ** -(h + 1)
            P.memset("pool", sl1[:, h, :], sl, [sl1])
            P.memset("pool", sl128[:, h, :], 128 * sl, [sl128])
            P.memset("pool", nsl64[:, h, :], -64 * sl, [nsl64])
            P.memset("pool", nsl1[:, h, :], -sl, [nsl1])
        idf = P.sbuf("c_idf", [128, 128], F32)
        P.make_identity(idf)
        clow = P.sbuf("c_clow", [128, 128], BF16)
        P.memset("pool", clow[:], 1.0, [clow])
        P.op("pool", lambda e: e.affine_select(out=clow[:], in_=clow[:], pattern=[[-1, 128]], compare_op=ALU.is_ge,
                                               fill=0.0, base=0, channel_multiplier=1), [clow], [clow])
        cneg = P.sbuf("c_cneg", [128, 128], F32)
        P.memset("pool", cneg[:], 0.0, [cneg])
        P.op("pool", lambda e: e.affine_select(out=cneg[:], in_=cneg[:], pattern=[[-1, 128]], compare_op=ALU.is_ge,
                                               fill=-1e30, base=0, channel_multiplier=1), [cneg], [cneg])
        pw = P.sbuf("c_pw", [128, NBIS], F32)
        for k in range(NBIS):
            P.memset("pool", pw[:, k:k + 1], 2.0 ** -(k + 1), [pw])
        vaug = P.sbuf("c_vaug", [128, NT, 65], BF16)
        P.memset("pool", vaug[:, :, 64:65], 1.0, [vaug])
        P.barrier()
        kTa = P.sbuf("c_kTa", [68, T], BF16)
        ikT = P.sbuf("c_ikT", [64, T], BF16)
        P.dma("sp", kTa[0:64, :], S["ck"], writes=[kTa])
        P.dma("sp", ikT[:], S["cik"], writes=[ikT])
        P.dma("sp", kTa[64:65, :], rb[0][:], reads=[rb[0]], writes=[kTa])
        P.dma("sp", kTa[65:66, :], rb[1][:], reads=[rb[1]], writes=[kTa])
        P.dma("sp", kTa[66:67, :], rb[2][:], reads=[rb[2]], writes=[kTa])
        P.dma("sp", kTa[67:68, :], rb[2][:], reads=[rb[2]], writes=[kTa])
        for s4 in range(0, NT, 4):
            P.dma("pool", vaug[:, s4:s4 + 4, 0:64], S["misc"][s4 * 128:(s4 + 4) * 128, 24:88].rearrange("(st s) d -> s st d", s=128),
                  writes=[vaug])

        iqT = [P.sbuf("c_iqT%d" % i, [64, 8, 128], BF16) for i in range(2)]
        qTa = [P.sbuf("c_qTa%d" % i, [68, 8, 128], BF16) for i in range(2)]
        for i in range(2):
            P.dma("sp", qTa[i][64:65], sl1[:], reads=[sl1], writes=[qTa[i]])
            P.dma("sp", qTa[i][65:66], sl128[:], reads=[sl128], writes=[qTa[i]])
        iw = [P.sbuf("c_iw%d" % i, [128, 8], F32) for i in range(2)]
        gt = [P.sbuf("c_gt%d" % i, [128, 512], BF16) for i in range(2)]
        iwa = P.sbuf("c_iwa", [128, 8], F32)
        iws = P.sbuf("c_iws", [128, 8], F32)
        sidx = P.sbuf("c_sidx", [128, T], F32)
        junk = P.sbuf("c_junk", [128, T], F32)
        M01 = P.sbuf("c_M01", [128, T], BF16)
        M01T = P.sbuf("c_M01T", [128, NT, 128], BF16)
        rl = [P.sbuf("c_rl%d" % i, [128, 512], F32) for i in range(4)]
        ex = [P.sbuf("c_ex%d" % i, [128, 8, 128], BF16) for i in range(3)]
        pT = [P.sbuf("c_pT%d" % i, [128, 8, 128], BF16) for i in range(3)]
        mn = P.sbuf("c_mn", [128, 1], F32)
        mxv = P.sbuf("c_mx", [128, 1], F32)
        W = P.sbuf("c_W", [128, NBIS], F32)
        lo = P.sbuf("c_lo", [128, 1], F32)
        mid = P.sbuf("c_mid", [128, 1], F32)
        cnt = P.sbuf("c_cnt", [128, 1], F32)
        dd = P.sbuf("c_dd", [128, 1], F32)
        smax = P.sbuf("c_smax", [128, 1], F32)
        smaxb = P.sbuf("c_smaxb", [128, 32], F32)
        srow_i = P.sbuf("c_srow_i", [1, 128], I32)
        nhi_i = P.sbuf("c_nhi_i", [1, 128], I32)
        nlo_i = P.sbuf("c_nlo_i", [1, 128], I32)
        nhi_f = P.sbuf("c_nhi_f", [1, 128], F32)
        nlo_f = P.sbuf("c_nlo_f", [1, 128], F32)
        r2 = [P.sbuf("c_r2%d" % i, [1, 8, 128], BF16) for i in range(2)]
        r3 = [P.sbuf("c_r3%d" % i, [1, 8, 128], BF16) for i in range(2)]
        rden = P.sbuf("c_rden", [128, 8], F32)
        ot = P.sbuf("c_ot", [128, 512], F32)
        yb = [P.sbuf("c_yb%d" % i, [128, 512], BF16) for i in range(2)]

        sps = [P.psum("c_sps%d" % i, [128, 512], F32) for i in range(2)]
        lg = P.psum("c_lg", [128, 8, 128], F32)
        ops_ = P.psum("c_ops", [128, 8, 128], F32)
        mtp = P.psum("c_mtp", [128, 4, 128], BF16)
        yo = YOut(P, C, K, 2, "c")
        IWS = (8 ** -0.5) * (64 ** -0.5)
        nsp = 0
        for t in range(NTR[0]):
            i = t % 2
            sl = slice(t * 128, (t + 1) * 128)
            nk = t + 1
            Lk = nk * 128
            P.dma("sp", iqT[i][:], S["ciq"][:, sl].rearrange("(h d) t -> d h t", d=64), writes=[iqT[i]])
            P.dma("sp", qTa[i][0:64], S["cq"][:, sl].rearrange("(h d) t -> d h t", d=64), writes=[qTa[i]])
            P.dma("sp", iw[i][:], S["misc"][sl, 8:16], writes=[iw[i]])
            P.dma("sp", gt[i][:], S["cg"][sl, :], writes=[gt[i]])
            if STOP[0] <= 1:
                continue
            if t >= 2:
                P.act(iwa[:], iw[i][:], AF.Abs, [iw[i]], [iwa], scale=IWS)
                P.act(iws[:], iw[i][:], AF.Sign, [iw[i]], [iws])
                for c0 in range(0, Lk, 512):
                    w_ = min(512, Lk - c0)
                    for h in range(8):
                        sp_ = sps[nsp % 2]
                        r_ = rl[nsp % 4]
                        nsp += 1
                        P.matmul(sp_[:, 0:w_], iqT[i][:, h, :], ikT[:, c0:c0 + w_], True, True, [iqT[i], ikT], [sp_])
                        P.act(r_[:, 0:w_], sp_[:, 0:w_], AF.Relu, [sp_, iwa], [r_], scale=iwa[:, h:h + 1])
                        if h == 0:
                            P.ts("dve", sidx[:, c0:c0 + w_], r_[:, 0:w_], iws[:, 0:1], None, ALU.mult, None, [r_, iws], [sidx])
                        else:
                            P.stt("dve", sidx[:, c0:c0 + w_], r_[:, 0:w_], iws[:, h:h + 1], sidx[:, c0:c0 + w_], ALU.mult, ALU.add,
                                  [r_, iws, sidx], [sidx])
                P.op("dve", lambda e, Lk=Lk: e.tensor_reduce(mn[:], sidx[:, 0:Lk], AX.X, ALU.min), [sidx], [mn])
                P.op("dve", lambda e, Lk=Lk: e.tensor_reduce(mxv[:], sidx[:, 0:Lk], AX.X, ALU.max), [sidx], [mxv])
                P.tt("dve", sidx[:, t * 128:Lk], sidx[:, t * 128:Lk], cneg[:], ALU.add, [sidx, cneg], [sidx])
                P.tt("dve", mxv[:], mxv[:], mn[:], ALU.subtract, [mxv, mn], [mxv])
                P.ts("dve", W[:], pw[:], mxv[:], None, ALU.mult, None, [pw, mxv], [W])
                P.tt("dve", mid[:], mn[:], W[:, 0:1], ALU.add, [mn, W], [mid])
                for k in range(NBIS):
                    P.ts("dve", junk[:, 0:Lk], sidx[:, 0:Lk], mid[:], None, ALU.is_ge, ALU.add, [sidx, mid], [junk, cnt],
                         accum_out=cnt[:])
                    P.ts("dve", dd[:], cnt[:], 255.5, 0.5, ALU.is_ge, ALU.subtract, [cnt], [dd])
                    if k < NBIS - 1:
                        P.stt("dve", mid[:], dd[:], W[:, k:k + 1], mid[:], ALU.mult, ALU.add, [dd, W, mid], [mid])
                    else:
                        P.ts("dve", dd[:], dd[:], 0.5, None, ALU.subtract, None, [dd], [dd])
                        P.stt("dve", lo[:], dd[:], W[:, k:k + 1], mid[:], ALU.mult, ALU.add, [dd, W, mid], [lo])
                P.ts("dve", M01[:, 0:Lk], sidx[:, 0:Lk], lo[:], None, ALU.is_ge, None, [sidx, lo], [M01])
            else:
                if t > 0:
                    P.memset("pool", M01[:, 0:t * 128], 1.0, [M01])
                P.copy("pool", M01[:, t * 128:Lk], clow[:], [clow], [M01])
            if STOP[0] <= 2:
                continue
            P.tt("dve", junk[:, 0:Lk], M01[:, 0:Lk], posrow[:, 0:Lk], ALU.mult, [M01, posrow], [junk])
            P.op("dve", lambda e, Lk=Lk: e.tensor_reduce(smax[:], junk[:, 0:Lk], AX.X, ALU.max), [junk], [smax])
            srp = sps[nsp % 2]
            nsp += 1
            P.copy("dve", smaxb[:], bc(smax[:], [128, 32]), [smax], [smaxb])
            P.matmul(srp[0:32, 0:128], smaxb[:], idf[:], True, True, [smaxb, idf], [srp])
            if STOP[0] <= 2.5:
                continue
            P.copy("dve", srow_i[:], srp[0:1, 0:128], [srp], [srow_i])
            P.ts("dve", nhi_i[:], srow_i[:], 6, None, ALU.arith_shift_right, None, [srow_i], [nhi_i])
            P.ts("dve", nlo_i[:], srow_i[:], 63, None, ALU.bitwise_and, None, [srow_i], [nlo_i])
            P.copy("dve", nhi_f[:], nhi_i[:], [nhi_i], [nhi_f])
            P.copy("dve", nlo_f[:], nlo_i[:], [nlo_i], [nlo_f])
            P.tt("dve", r2[i][:], nsl64[:], bc(nhi_f[:].unsqueeze(1), [1, 8, 128]), ALU.mult, [nsl64, nhi_f], [r2[i]])
            P.tt("dve", r3[i][:], nsl1[:], bc(nlo_f[:].unsqueeze(1), [1, 8, 128]), ALU.mult, [nsl1, nlo_f], [r3[i]])
            if STOP[0] <= 2.7:
                continue
            P.dma("sp", qTa[i][66:67], r2[i][:], reads=[r2[i]], writes=[qTa[i]])
            P.dma("sp", qTa[i][67:68], r3[i][:], reads=[r3[i]], writes=[qTa[i]])
            if STOP[0] <= 3:
                continue
            for k0 in range(0, nk, 4):
                n4 = min(4, nk - k0)
                for j in range(n4):
                    P.transpose(mtp[:, j, :], M01[:, (k0 + j) * 128:(k0 + j + 1) * 128], K.ident[:], [M01, K.ident], [mtp])
                P.copy("act", M01T[:, k0:k0 + n4, :], mtp[:, 0:n4, :], [mtp], [M01T])
            if STOP[0] <= 4:
                continue
            for kt in range(nk):
                e_ = ex[kt % 3]
                p_ = pT[kt % 3]
                for half in range(2):
                    P.matmul(lg[:, half * 4:(half + 1) * 4, :], kTa[:, kt * 128:(kt + 1) * 128],
                             qTa[i][:, half * 4:(half + 1) * 4, :], True, True, [kTa, qTa[i]], [lg])
                P.act(e_[:], lg[:], AF.Exp, [lg], [e_])
                P.stt("dve", p_[:], e_[:], 3.0e38, bc(M01T[:, kt:kt + 1, :], [128, 8, 128]), ALU.min, ALU.mult,
                      [e_, M01T], [p_])
                for h in range(8):
                    P.matmul(ops_[:, h, 0:65], p_[:, h, :], vaug[:, kt, :], kt == 0 and h % 4 == 0, kt == nk - 1 and h % 4 == 3,
                             [p_, vaug], [ops_])
            if STOP[0] <= 5:
                continue
            P.recip(rden[:], ops_[:, :, 64], [ops_], [rden])
            P.tt("dve", ot[:].rearrange("t (h d) -> t h d", h=8), ops_[:, :, 0:64], bc(rden[:].unsqueeze(2), [128, 8, 64]), ALU.mult,
                 [ops_, rden], [ot])
            P.tt("pool", yb[i][:], ot[:], gt[i][:], ALU.mult, [ot, gt[i]], [yb[i]])
            yo.emit(yb[i], t)


def phase_gdn(P, C, K, l):
    S = C.scr
    NCH = NTR[0] * 2
    with P.scope():
        uinc = P.sbuf("a_uinc", [128, 128], F32)
        P.memset("pool", uinc[:], 1.0, [uinc])
        P.op("pool", lambda e: e.affine_select(out=uinc[:], in_=uinc[:], pattern=[[1, 128]], compare_op=ALU.is_ge,
                                               fill=0.0, base=0, channel_multiplier=-1), [uinc], [uinc])
        lstr = P.sbuf("a_lstr", [128, 128], F32)
        P.memset("pool", lstr[:], 1.0, [lstr])
        P.op("pool", lambda e: e.affine_select(out=lstr[:], in_=lstr[:], pattern=[[-1, 128]], compare_op=ALU.is_gt,
                                               fill=0.0, base=0, channel_multiplier=1), [lstr], [lstr])
        idf = P.sbuf("a_idf", [128, 128], F32)
        P.make_identity(idf)
        onesf = P.sbuf("a_onesf", [128, 128], F32)
        P.memset("pool", onesf[:], 1.0, [onesf])
        P.barrier()
        aB = P.sbuf("a_aB", [64, 4], F32)
        P.dma("sp", aB[:], C.prm["gdn_a_log"][l:l + 1, :].to_broadcast([64, 4]), writes=[aB])
        P.act(aB[:], aB[:], AF.Exp, [aB], [aB])
        P.ts("dve", aB[:], aB[:], -1.0, None, ALU.mult, None, [aB], [aB])
        dtb = P.sbuf("a_dtb", [64, 4], F32)
        P.dma("sp", dtb[:], C.prm["gdn_dt_bias"][l:l + 1, :].to_broadcast([64, 4]), writes=[dtb])
        ng = P.sbuf("a_ng", [64, 128], F32)
        P.dma("sp", ng[:], C.prm["gdn_norm_g"][l:l + 1, :].to_broadcast([64, 128]), writes=[ng])
        ST32 = P.sbuf("a_ST32", [128, 4, 128], F32)
        STb = P.sbuf("a_STb", [128, 4, 128], BF16)
        P.memset("pool", ST32[:], 0.0, [ST32])
        P.memset("pool", STb[:], 0.0, [STb])

        def dbl(name, shape, dt):
            return [P.sbuf("%s%d" % (name, i), shape, dt) for i in range(2)]

        qT, kT, vT = dbl("a_qT", [128, 4, 64], BF16), dbl("a_kT", [128, 4, 64], BF16), dbl("a_vT", [128, 4, 64], BF16)
        ab, gate = dbl("a_ab", [64, 8], F32), dbl("a_gate", [64, 512], BF16)
        sp_, gl, be = dbl("a_sp", [64, 4], F32), dbl("a_gl", [64, 4], F32), dbl("a_be", [64, 4], F32)
        gc, gt128, cd128 = dbl("a_gc", [64, 4], F32), dbl("a_gt", [128, 4], F32), dbl("a_cd", [128, 4], F32)
        lab = dbl("a_lab", [64, 4, 128], F32)
        pre, E, bE = dbl("a_pre", [64, 8], F32), dbl("a_E", [64, 8], F32), dbl("a_bE", [64, 4], F32)
        seg, nseg = dbl("a_seg", [64, 4, 64], F32), dbl("a_nseg", [64, 4, 64], F32)
        dI, dL = dbl("a_dI", [64, 4, 64], F32), dbl("a_dL", [64, 4, 64], F32)
        egr = dbl("a_egr", [128, 4, 64], F32)
        qdT = dbl("a_qdT", [128, 4, 64], BF16)
        qkT = dbl("a_qkT", [64, 4, 64], BF16)
        Nm = [dbl("a_N", [64, 4, 64], F32), dbl("a_N2", [64, 4, 64], F32)]
        Mm = [dbl("a_M", [64, 4, 64], F32), dbl("a_M2", [64, 4, 64], F32)]
        X, Xb = dbl("a_X", [64, 4, 64], F32), dbl("a_Xb", [64, 4, 64], BF16)
        kb, kdec, vb = dbl("a_kb", [64, 4, 128], BF16), dbl("a_kdec", [64, 4, 128], BF16), dbl("a_vb", [64, 4, 128], BF16)
        wkT, u0 = dbl("a_wkT", [128, 4, 64], BF16), dbl("a_u0", [64, 4, 128], F32)
        u = dbl("a_u", [64, 4, 128], BF16)
        o, sq = dbl("a_o", [64, 4, 128], F32), dbl("a_sq", [64, 4, 128], F32)
        ssq = dbl("a_ssq", [64, 4], F32)
        y = dbl("a_y", [64, 512], F32)
        ysb = dbl("a_ysb", [128, 4, 64], BF16)

        b0 = P.psum("a_b0", [128, 512], F32)
        gcrow, wk_ps = View(b0, b0[:, 0:256].rearrange("p (h c) -> p h c", h=4)), View(b0, b0[:, 256:512].rearrange("p (h c) -> p h c", h=4))
        b1 = P.psum("a_b1", [128, 512], F32)
        sm_ps, ytp = View(b1, b1[:, 0:16]), View(b1, b1[:, 256:512].rearrange("p (h c) -> p h c", h=4))
        b2 = P.psum("a_b2", [64, 512], F32)
        kk_ps, qk_ps = View(b2, b2[:, 0:256].rearrange("p (h c) -> p h c", h=4)), View(b2, b2[:, 256:512].rearrange("p (h c) -> p h c", h=4))
        b3 = P.psum("a_b3", [64, 512], F32)
        P_ps, Q_ps = View(b3, b3[:, 0:256].rearrange("p (h c) -> p h c", h=4)), View(b3, b3[:, 256:512].rearrange("p (h c) -> p h c", h=4))
        b4 = P.psum("a_b4", [64, 512], F32)
        XP_ps, M_ps = View(b4, b4[:, 0:256].rearrange("p (h c) -> p h c", h=4)), View(b4, b4[:, 256:512].rearrange("p (h c) -> p h c", h=4))
        kv_ps = P.psum("a_kvps", [64, 2, 512], BF16)
        ktm, vtm = View(kv_ps, kv_ps[:, 0, :].rearrange("p (h d) -> p h d", h=4)), View(kv_ps, kv_ps[:, 1, :].rearrange("p (h d) -> p h d", h=4))
        uwo = P.psum("a_uwo", [64, 4, 128], F32)
        Sn_ps = P.psum("a_Sn", [128, 4, 128], F32)
        u64, i64 = uinc[0:64, 0:64], idf[0:64, 0:64]

        def b4c(ap, n):
            return bc(ap.unsqueeze(2), [ap.shape[0], 4, n])

        for ch in range(NCH):
            i = ch % 2
            cs = slice(ch * 64, (ch + 1) * 64)
            for dst, nm in ((qT[i], "gq"), (kT[i], "gk"), (vT[i], "gv")):
                P.dma("sp", dst[:], S[nm][:, cs].rearrange("(h d) t -> d h t", d=128), writes=[dst])
            P.dma("sp", ab[i][:], S["misc"][cs, 0:8], writes=[ab[i]])
            P.dma("sp", gate[i][:], S["ag"][cs, :], writes=[gate[i]])
            P.tt("dve", sp_[i][:], ab[i][:, 0:4], dtb[:], ALU.add, [ab[i], dtb], [sp_[i]])
            P.act(sp_[i][:], sp_[i][:], AF.Exp, [sp_[i]], [sp_[i]])
            P.act(sp_[i][:], sp_[i][:], AF.Ln, [sp_[i]], [sp_[i]], bias=1.0)
            P.tt("dve", gl[i][:], sp_[i][:], aB[:], ALU.mult, [sp_[i], aB], [gl[i]])
            P.act(be[i][:], ab[i][:, 4:8], AF.Exp, [ab[i]], [be[i]], scale=-1.0)
            P.ts("dve", be[i][:], be[i][:], 1.0, None, ALU.add, None, [be[i]], [be[i]])
            P.recip(be[i][:], be[i][:], [be[i]], [be[i]])
            P.matmul(sm_ps[0:64, 0:4], u64, gl[i][:], True, True, [uinc, gl[i]], [sm_ps])
            P.matmul(sm_ps[:, 4:8], onesf[0:64, :], gl[i][:], True, True, [onesf, gl[i]], [sm_ps])
            P.copy("dve", gc[i][:], sm_ps[0:64, 0:4], [sm_ps], [gc[i]])
            P.copy("dve", gt128[i][:], sm_ps[:, 4:8], [sm_ps], [gt128[i]])
            P.copy("dve", lab[i][:], b4c(gl[i][:], 128), [gl[i]], [lab[i]])
            for h in range(4):
                P.matmul(gcrow[:, h, :], lab[i][:, h, :], u64, True, True, [lab[i], uinc], [gcrow])
            P.copy("dve", pre[i][:, 0:4], gc[i][:], [gc[i]], [pre[i]])
            P.tt("dve", pre[i][:, 4:8], gt128[i][0:64, :], gc[i][:], ALU.subtract, [gt128[i], gc[i]], [pre[i]])
            P.act(E[i][:], pre[i][:], AF.Exp, [pre[i]], [E[i]])
            P.act(cd128[i][:], gt128[i][:], AF.Exp, [gt128[i]], [cd128[i]])
            P.tt("dve", bE[i][:], be[i][:], E[i][:, 0:4], ALU.mult, [be[i], E[i]], [bE[i]])
            if STOP[0] <= 1:
                continue
            P.tt("dve", seg[i][:], gcrow[0:64], b4c(gc[i][:], 64), ALU.subtract, [gcrow, gc[i]], [seg[i]])
            P.ts("dve", nseg[i][:], seg[i][:], -1.0, 0.0, ALU.mult, ALU.min, [seg[i]], [nseg[i]])
            P.ts("dve", seg[i][:], seg[i][:], 0.0, None, ALU.min, None, [seg[i]], [seg[i]])
            P.act(dI[i][:], seg[i][:], AF.Exp, [seg[i]], [dI[i]])
            P.act(dL[i][:], nseg[i][:], AF.Exp, [nseg[i]], [dL[i]])
            P.tt("dve", dI[i][:], dI[i][:], bc(u64.unsqueeze(1), [64, 4, 64]), ALU.mult, [dI[i], uinc], [dI[i]])
            P.tt("dve", dL[i][:], dL[i][:], bc(lstr[0:64, 0:64].unsqueeze(1), [64, 4, 64]), ALU.mult, [dL[i], lstr], [dL[i]])
            P.act(egr[i][:], gcrow[:], AF.Exp, [gcrow], [egr[i]])
            P.tt("dve", qdT[i][:], qT[i][:], egr[i][:], ALU.mult, [qT[i], egr[i]], [qdT[i]])
            if STOP[0] <= 2:
                continue
            for h in range(4):
                P.matmul(kk_ps[:, h, :], kT[i][:, h, :], kT[i][:, h, :], True, True, [kT[i]], [kk_ps])
            for h in range(4):
                P.matmul(qk_ps[:, h, :], kT[i][:, h, :], qT[i][:, h, :], True, True, [kT[i], qT[i]], [qk_ps])
            N0, M0 = Nm[0][i], Mm[0][i]
            P.tt("dve", N0[:], kk_ps[:], dL[i][:], ALU.mult, [kk_ps, dL[i]], [N0])
            P.tt("dve", N0[:], N0[:], b4c(be[i][:], 64), ALU.mult, [N0, be[i]], [N0])
            P.tt("dve", qkT[i][:], qk_ps[:], dI[i][:], ALU.mult, [qk_ps, dI[i]], [qkT[i]])
            if STOP[0] <= 3:
                continue
            for h in range(4):
                P.transpose(M_ps[:, h, :], N0[:, h, :], i64, [N0, idf], [M_ps])
            P.copy("act", M0[:], M_ps[:], [M_ps], [M0])
            if STOP[0] <= 4:
                continue
            P.tt("dve", X[i][:], bc(i64.unsqueeze(1), [64, 4, 64]), M0[:], ALU.subtract, [idf, M0], [X[i]])
            Pc, Qc = N0, M0
            if STOP[0] <= 4.1:
                continue
            for st in range(1, 6):
                if STOP[0] <= 4.2 and st > 1:
                    break
                Pn, Qn = Nm[st % 2][i], Mm[st % 2][i]
                for h in range(4):
                    P.matmul(P_ps[:, h, :], Qc[:, h, :], Pc[:, h, :], True, True, [Qc, Pc], [P_ps])
                if st < 5:
                    for h in range(4):
                        P.matmul(Q_ps[:, h, :], Pc[:, h, :], Qc[:, h, :], True, True, [Qc, Pc], [Q_ps])
                if STOP[0] <= 4.15:
                    break
                P.copy("act", Pn[:], P_ps[:], [P_ps], [Pn])
                if st < 5:
                    P.copy("dve", Qn[:], Q_ps[:], [Q_ps], [Qn])
                if STOP[0] <= 4.17:
                    break
                for h in range(4):
                    P.matmul(XP_ps[:, h, :], Pn[:, h, :], X[i][:, h, :], True, True, [Pn, X[i]], [XP_ps])
                P.tt("dve", X[i][:], X[i][:], XP_ps[:], ALU.add, [X[i], XP_ps], [X[i]])
                Pc, Qc = Pn, Qn
            if STOP[0] <= 4.5:
                continue
            P.copy("act", Xb[i][:], X[i][:], [X[i]], [Xb[i]])
            if STOP[0] <= 5:
                continue
            for h in range(4):
                P.transpose(ktm[:, h, :], kT[i][:, h, :], K.ident[:], [kT[i], K.ident], [ktm])
            for h in range(4):
                P.transpose(vtm[:, h, :], vT[i][:, h, :], K.ident[:], [vT[i], K.ident], [vtm])
            P.tt("dve", kb[i][:], ktm[:], b4c(bE[i][:], 128), ALU.mult, [ktm, bE[i]], [kb[i]])
            P.tt("dve", kdec[i][:], ktm[:], b4c(E[i][:, 4:8], 128), ALU.mult, [ktm, E[i]], [kdec[i]])
            P.tt("dve", vb[i][:], vtm[:], b4c(be[i][:], 128), ALU.mult, [vtm, be[i]], [vb[i]])
            if STOP[0] <= 6:
                continue
            for h in range(4):
                P.matmul(wk_ps[:, h, :], kb[i][:, h, :], Xb[i][:, h, :], True, True, [kb[i], Xb[i]], [wk_ps])
            P.copy("act", wkT[i][:], wk_ps[:], [wk_ps], [wkT[i]])
            for h in range(4):
                P.matmul(uwo[:, h, :], Xb[i][:, h, :], vb[i][:, h, :], True, True, [Xb[i], vb[i]], [uwo])
            P.copy("act", u0[i][:], uwo[:], [uwo], [u0[i]])
            if STOP[0] <= 7:
                continue
            for h in range(4):
                P.matmul(uwo[:, h, :], wkT[i][:, h, :], STb[:, h, :], True, True, [wkT[i], STb], [uwo])
            P.tt("dve", u[i][:], u0[i][:], uwo[:], ALU.subtract, [u0[i], uwo], [u[i]])
            for h in range(4):
                P.matmul(uwo[:, h, :], qdT[i][:, h, :], STb[:, h, :], True, False, [qdT[i], STb], [uwo])
                P.matmul(uwo[:, h, :], qkT[i][:, h, :], u[i][:, h, :], False, True, [qkT[i], u[i]], [uwo])
            P.copy("act", o[i][:], uwo[:], [uwo], [o[i]])
            if STOP[0] <= 8:
                continue
            for h in range(4):
                P.matmul(Sn_ps[:, h, :], kdec[i][:, h, :], u[i][:, h, :], True, True, [kdec[i], u[i]], [Sn_ps])
            P.tt("dve", ST32[:], ST32[:], b4c(cd128[i][:], 128), ALU.mult, [ST32, cd128[i]], [ST32])
            P.tt("dve", ST32[:], ST32[:], Sn_ps[:], ALU.add, [ST32, Sn_ps], [ST32])
            P.copy("act", STb[:], ST32[:], [ST32], [STb])
            if STOP[0] <= 9:
                continue
            P.tt("pool", sq[i][:], o[i][:], o[i][:], ALU.mult, [o[i]], [sq[i]])
            P.op("dve", lambda e, i=i: e.tensor_reduce(ssq[i][:], sq[i][:], AX.X, ALU.add), [sq[i]], [ssq[i]])
            P.act(ssq[i][:], ssq[i][:], AF.Sqrt, [ssq[i], K.eps], [ssq[i]], scale=1.0 / 128, bias=K.eps[0:64, :])
            P.recip(ssq[i][:], ssq[i][:], [ssq[i]], [ssq[i]])
            P.tt("pool", o[i][:], o[i][:], b4c(ssq[i][:], 128), ALU.mult, [o[i], ssq[i]], [o[i]])
            P.tt("pool", o[i][:], o[i][:], bc(ng[:].unsqueeze(1), [64, 4, 128]), ALU.mult, [o[i], ng], [o[i]])
            P.tt("pool", y[i][:], o[i][:].rearrange("c h v -> c (h v)"), gate[i][:], ALU.mult, [o[i], gate[i]], [y[i]])
            if STOP[0] <= 10:
                continue
            for cc in range(4):
                P.transpose(ytp[:, cc, :], y[i][:, cc * 128:(cc + 1) * 128], i64, [y[i], idf], [ytp])
            P.copy("act", ysb[i][:], ytp[:], [ytp], [ysb[i]])
            P.dma("sp", S["ysT"][0][:, cs].rearrange("(cc c) t -> c cc t", c=128), ysb[i][:], reads=[ysb[i]])


def build_program(nc, layers=(0, 1), dbg=False, branches="abcdm"):
    C = declare(nc, dbg=dbg)
    P = Prog(nc)
    K = setup_consts(P, C)
    for l in layers:
        x_src = C.x if l == layers[0] else C.scr["x1"]
        x_dst = C.out if l == layers[-1] else C.scr["x1"]
        with P.scope():
            alloc_hT(P, K)
            phase_norm(P, C, K, l, x_src)
            phase_inproj(P, C, K, l)
        if "a" in branches:
            phase_gdn(P, C, K, l)
        if "b" in branches:
            phase_sg(P, C, K, l)
        if "c" in branches:
            phase_dsa(P, C, K, l)
        if "d" in branches:
            phase_ssd(P, C, K, l)
        if "m" in branches:
            phase_mem(P, C, K, l)
        with P.scope():
            alloc_merged(P, K)
            with P.scope():
                alloc_hT(P, K)
                phase_norm(P, C, K, l, x_src)
                phase_merge(P, C, K, l)
            phase_outproj(P, C, K, l, x_src, x_dst)
    P.finish()
    return C, P


def kernel(**inputs):
    x = np.ascontiguousarray(np.asarray(inputs["x"], dtype=np.float32))
    mem = np.ascontiguousarray(np.asarray(inputs["mem"], dtype=np.float32))
    nb = x.shape[0]
    nc = bass.Bass("TRN2", target_bir_lowering=False)
    build_program(nc)
    prm = {n: np.ascontiguousarray(np.asarray(inputs[n], dtype=np.float32)) for n, _ in PARAMS}
    in_maps = []
    for b in range(nb):
        m = {"x": x[b], "mem": mem[b]}
        m.update(prm)
        in_maps.append(m)
    res = run_bass_kernel_spmd(nc, in_maps, core_ids=list(range(nb)))
    return np.stack([np.asarray(r["out"], dtype=np.float32) for r in res.results], axis=0)
```

```python
from contextlib import ExitStack
import numpy as np
import concourse.bass as bass
import concourse.mybir as mybir
from concourse.bass_utils import run_bass_kernel_spmd

F32 = mybir.dt.float32
BF16 = mybir.dt.bfloat16
AF = mybir.ActivationFunctionType
ALU = mybir.AluOpType
AX = mybir.AxisListType


class Buf:
    __slots__ = ("name", "ap", "w", "r", "excl")

    def __init__(self, name, ap=None):
        self.name = name
        self.ap = ap
        self.excl = False
        self.w = None
        self.r = {}

    def __getitem__(self, idx):
        return self.ap[idx]


class View:
    def __init__(self, parent, ap):
        self.parent = parent
        self.ap = ap
        self.name = parent.name
        self.excl = parent.excl

    def __getitem__(self, idx):
        return self.ap[idx]

    @property
    def w(self):
        return self.parent.w

    @w.setter
    def w(self, v):
        self.parent.w = v

    @property
    def r(self):
        return self.parent.r

    @r.setter
    def r(self, v):
        self.parent.r = v


class Prog:
    ENG = ("pe", "dve", "act", "pool", "sp")
    SEM_LIMIT = 30000
    NDMA = 6

    def __init__(self, nc, same_engine_sync=True):
        self.nc = nc
        self.same = same_engine_sync
        self.stack = ExitStack()
        self.ops = {e: [] for e in self.ENG}
        self.cnt = {e: 0 for e in self.ENG}
        self.owner = {}
        self.nsem = 0
        self.cur = {e: self._newsem(e) for e in self.ENG}
        self.seen = {e: {} for e in self.ENG}
        self.dsem = {}
        self.drr = {}
        self.allsems = []
        self.nbuf = 0

    def _newsem(self, owner):
        s = getattr(self, "semstack", self.stack).enter_context(self.nc.semaphore("s%d_%s" % (self.nsem, owner)))
        self.nsem += 1
        self.owner[id(s)] = owner
        return s

    def sbuf(self, name, shape, dtype):
        self.nbuf += 1
        t = self.stack.enter_context(self.nc.sbuf_tensor("%s_%d" % (name, self.nbuf), list(shape), dtype))
        return Buf(name, t)

    def psum(self, name, shape, dtype):
        self.nbuf += 1
        t = self.stack.enter_context(self.nc.psum_tensor("%s_%d" % (name, self.nbuf), list(shape), dtype))
        b = Buf(name, t)
        b.excl = True
        return b

    def buf(self, name, ap=None):
        return Buf(name, ap)

    def _deps(self, eng, reads, writes):
        deps = {}

        def add(ev):
            if ev is None:
                return
            s, v = ev
            k = id(s)
            if k not in deps or deps[k][1] < v:
                deps[k] = (s, v)

        for b in reads:
            add(b.w)
        for b in writes:
            add(b.w)
            for ev in b.r.values():
                add(ev)
        waits = []
        seen = self.seen[eng]
        for k, (s, v) in deps.items():
            if self.owner.get(k) == eng:
                if eng == "pe" or eng == "sp" or not self.same:
                    continue
            if seen.get(k, 0) >= v:
                continue
            seen[k] = v
            waits.append((s, v))
        return waits

    def _commit(self, ev, reads, writes):
        for b in reads:
            k = id(ev[0])
            b.r[k] = ev
        for b in writes:
            b.w = ev
            b.r = {}

    def op(self, eng, fn, reads=(), writes=()):
        ex = [b for b in reads if getattr(b, "excl", False)]
        if ex:
            writes = list(writes) + ex
        waits = self._deps(eng, reads, writes)
        if self.cnt[eng] >= self.SEM_LIMIT:
            self.cur[eng] = self._newsem(eng)
            self.cnt[eng] = 0
        self.cnt[eng] += 1
        ev = (self.cur[eng], self.cnt[eng])
        self.ops[eng].append((waits, fn, ("c", self.cur[eng], self.cnt[eng])))
        self._commit(ev, reads, writes)
        return ev

    def dma(self, q, out, in_, reads=(), writes=(), **kw):
        waits = self._deps(q, reads, writes)
        if q not in self.dsem:
            self.dsem[q] = [[self._newsem("dma_" + q), 0] for _ in range(self.NDMA)]
            self.drr[q] = 0
        slot = self.dsem[q][self.drr[q] % self.NDMA]
        self.drr[q] += 1
        s, c = slot
        if c > 0:
            seen = self.seen[q]
            if seen.get(id(s), 0) < 16 * c:
                seen[id(s)] = 16 * c
                waits.append((s, 16 * c))
        if 16 * (c + 1) > self.SEM_LIMIT:
            s = self._newsem("dma_" + q)
            slot[0] = s
            c = 0
        slot[1] = c + 1
        ev = (s, 16 * (c + 1))
        self.ops[q].append((waits, lambda e, out=out, in_=in_, kw=kw: e.dma_start(out=out, in_=in_, **kw), ("d", s, 16)))
        self._commit(ev, reads, writes)
        return ev

    def make_identity(self, b, n=128):
        self.op("pool", lambda e: e.memset(b.ap[:], 1.0), writes=[b])
        self.op("pool", lambda e: e.affine_select(out=b.ap[:], in_=b.ap[:], pattern=[[-1, n]], compare_op=ALU.is_ge,
                                                  fill=0.0, base=0, channel_multiplier=1), reads=[b], writes=[b])
        self.op("pool", lambda e: e.affine_select(out=b.ap[:], in_=b.ap[:], pattern=[[1, n]], compare_op=ALU.is_ge,
                                                  fill=0.0, base=0, channel_multiplier=-1), reads=[b], writes=[b])


    def matmul(self, out, lhsT, rhs, start, stop, reads, writes):
        return self.op("pe", lambda e: e.matmul(out, lhsT, rhs, start=start, stop=stop), reads, writes)

    def transpose(self, out, in_, ident, reads, writes):
        return self.op("pe", lambda e: e.transpose(out, in_, ident), reads, writes)

    def act(self, out, in_, func, reads, writes, **kw):
        return self.op("act", lambda e: e.activation(out, in_, func, **kw), reads, writes)

    def tt(self, eng, out, a, b, op, reads, writes):
        return self.op(eng, lambda e: e.tensor_tensor(out, a, b, op), reads, writes)

    def ts(self, eng, out, a, s1, s2, op0, op1, reads, writes, **kw):
        if op1 is None:
            return self.op(eng, lambda e: e.tensor_scalar(out, a, s1, None, op0, **kw), reads, writes)
        return self.op(eng, lambda e: e.tensor_scalar(out, a, s1, s2, op0, op1, **kw), reads, writes)

    def stt(self, eng, out, in0, scalar, in1, op0, op1, reads, writes):
        return self.op(eng, lambda e: e.scalar_tensor_tensor(out, in0, scalar, in1, op0, op1), reads, writes)

    def copy(self, eng, out, in_, reads, writes):
        if eng == "act":
            return self.op(eng, lambda e: e.copy(out, in_), reads, writes)
        return self.op(eng, lambda e: e.tensor_copy(out, in_), reads, writes)

    def memset(self, eng, ap, val, writes):
        return self.op(eng, lambda e: e.memset(ap, val), (), writes)

    def recip(self, out, in_, reads, writes):
        return self.op("dve", lambda e: e.reciprocal(out, in_), reads, writes)

    def barrier(self):
        finals = []
        for e in self.ENG:
            if self.cnt[e] > 0:
                finals.append((self.cur[e], self.cnt[e]))
        for q, slots in self.dsem.items():
            for s, c in slots:
                if c > 0:
                    finals.append((s, 16 * c))
        for e in self.ENG:
            waits = []
            for s, v in finals:
                if self.seen[e].get(id(s), 0) < v:
                    self.seen[e][id(s)] = v
                    waits.append((s, v))
            if waits:
                self.ops[e].append((waits, None, None))

    def scope(self):
        return _Scope(self)

    def flush(self, final=False):
        nc = self.nc
        finals = []
        if final:
            for e in self.ENG:
                if self.cnt[e] > 0:
                    finals.append((self.cur[e], self.cnt[e]))
            for q, slots in self.dsem.items():
                for s, c in slots:
                    if c > 0:
                        finals.append((s, 16 * c))
        if not hasattr(self, "actual"):
            self.actual = {}
            self.amap = {}
        ref = set()
        for e in self.ENG:
            for waits, fn, inc in self.ops[e]:
                for s, v in waits:
                    if self.owner.get(id(s)) in self.ENG:
                        ref.add((id(s), v))
        for s, v in finals:
            if self.owner.get(id(s)) in self.ENG:
                ref.add((id(s), v))
        for e in self.ENG:
            for waits, fn, inc in self.ops[e]:
                if inc is not None and inc[0] == "c":
                    key = (id(inc[1]), inc[2])
                    if key in ref:
                        self.actual[key[0]] = self.actual.get(key[0], 0) + 1
                        self.amap[key] = self.actual[key[0]]

        def tr(s, v):
            if self.owner.get(id(s)) in self.ENG:
                return self.amap[(id(s), v)]
            return v

        engs = {"pe": "tensor", "dve": "vector", "act": "scalar", "pool": "gpsimd", "sp": "sync"}
        with nc.Block() as block:
            for e in self.ENG:
                ops = self.ops[e]
                if not ops and not (final and e == "sp"):
                    continue

                def body(engine, ops=ops, e=e):
                    for waits, fn, inc in ops:
                        for s, v in waits:
                            engine.wait_ge(s, tr(s, v))
                        if fn is not None:
                            ins = fn(engine)
                            if inc[0] == "d":
                                ins.then_inc(inc[1], 16)
                            elif (id(inc[1]), inc[2]) in self.amap:
                                ins.then_inc(inc[1], 1)
                    if final and e == "sp":
                        for s, v in finals:
                            engine.wait_ge(s, tr(s, v))

                getattr(block, engs[e])(body)
        self.nops = getattr(self, "nops", 0) + sum(len(v) for v in self.ops.values())
        self.ops = {e: [] for e in self.ENG}

    def finish(self):
        self.flush(final=True)
        self.stack.close()


class _Scope:
    def __init__(self, P):
        self.P = P

    def __enter__(self):
        self.saved = self.P.stack
        self.P.semstack = getattr(self.P, "semstack", self.saved)
        self.P.stack = ExitStack()
        return self

    def __exit__(self, *a):
        self.P.barrier()
        self.P.flush()
        self.P.stack.close()
        self.P.stack = self.saved
        return False


T = 4096
NT = 32
NTR = [32]
D = 1024
INC = 7896
EPS = 1e-6

O_AQ, O_AK, O_AV = 0, 512, 1024
O_AA, O_AB, O_AG = 1536, 1540, 1544
O_BU, O_BV, O_BG = 2056, 2568, 3080
O_CQ, O_CK, O_CV, O_CIQ, O_CIK, O_CIW, O_CG = 3592, 4104, 4168, 4232, 4744, 4808, 4816
O_DZ, O_DX, O_DDT = 5328, 5840, 6864
O_MQ, O_MG = 6872, 7384

PARAMS = [("norm_g", [2, 1024]), ("w_in", [2, 1024, INC]), ("gdn_conv_w", [2, 4, 1536]), ("gdn_a_log", [2, 4]),
          ("gdn_dt_bias", [2, 4]), ("gdn_norm_g", [2, 128]), ("sg_ln_g", [2, 512]), ("sg_ln_b", [2, 512]),
          ("sg_w", [2, 4, 128, 128]), ("sg_b", [2, 4, 128]), ("dsa_q_norm_g", [2, 64]), ("dsa_k_norm_g", [2, 64]),
          ("ssd_conv_w", [2, 4, 1024]), ("ssd_conv_b", [2, 1024]), ("ssd_a_log", [2, 8]), ("ssd_dt_bias", [2, 8]),
          ("ssd_d", [2, 8]), ("ssd_norm_g", [2, 512]), ("mem_norm_g", [2, 1024]), ("w_mem_kv", [2, 1024, 1024]),
          ("mem_q_norm_g", [2, 128]), ("mem_k_norm_g", [2, 128]), ("w_gate", [2, 5, 1024, 1024]),
          ("w_branch", [2, 5, 512, 1024]), ("w_out", [2, 1024, 1024])]


class Ctx:
    pass


STOP = [99]


def declare(nc, dbg=False, skip=()):
    C = Ctx()
    C.nc = nc
    if "x" not in skip:
        C.x = nc.dram_tensor("x", [T, D], F32, kind="ExternalInput").ap()
    C.mem = nc.dram_tensor("mem", [256, D], F32, kind="ExternalInput").ap()
    C.prm = {}
    for name, shp in PARAMS:
        if name in skip:
            continue
        C.prm[name] = nc.dram_tensor(name, shp, F32, kind="ExternalInput").ap()
    C.out = nc.dram_tensor("out", [T, D], F32, kind="ExternalOutput").ap()
    kind = "ExternalOutput" if dbg else "Internal"
    C.scr = {}

    def scr(name, shape, dt):
        C.scr[name] = nc.dram_tensor("scr_" + name, shape, dt, kind=kind).ap()

    for n in ("gq", "gk", "gv", "cq", "ciq", "mq"):
        scr(n, [512, T], BF16)
    scr("xbc", [1024, T], BF16)
    scr("ck", [64, T], BF16)
    scr("cik", [64, T], BF16)
    for n in ("ag", "bu", "bv", "bg", "cg", "dz", "mg"):
        scr(n, [T, 512], BF16)
    scr("misc", [T, 88], F32)
    scr("ysT", [5, 512, T], BF16)
    scr("x1", [T, D], F32)
    return C


def setup_consts(P, C):
    K = Ctx()
    K.ident = P.sbuf("ident", [128, 128], BF16)
    P.make_identity(K.ident)
    K.ones = P.sbuf("ones", [128, 128], BF16)
    P.memset("pool", K.ones[:], 1.0, [K.ones])
    K.blk2 = P.sbuf("blk2", [128, 128], BF16)
    P.memset("pool", K.blk2[:], 0.0, [K.blk2])
    P.memset("pool", K.blk2[0:64, 0:64], 1.0, [K.blk2])
    P.memset("pool", K.blk2[64:128, 64:128], 1.0, [K.blk2])
    K.eps = P.sbuf("epsc", [128, 1], F32)
    P.memset("pool", K.eps[:], EPS, [K.eps])
    P.barrier()
    return K


def alloc_hT(P, K):
    K.hT = P.sbuf("hT", [128, 8, T], BF16)
    K.hTb = [Buf("hT%d" % t, K.hT.ap) for t in range(NT)]


def alloc_merged(P, K):
    K.mT = P.sbuf("mT", [128, 8, T], BF16)
    K.mTb = [Buf("mT%d" % t, K.mT.ap) for t in range(8)]


def phase_norm(P, C, K, l, x_src):
    with P.scope():
        gb = P.sbuf("gb", [128, D], F32)
        P.dma("sp", gb[:], C.prm["norm_g"][l:l + 1, :].to_broadcast([128, D]), writes=[gb])
        xin = [P.sbuf("xin%d" % i, [128, D], F32) for i in range(2)]
        junk = P.sbuf("junk", [128, D], BF16)
        ss = [P.sbuf("ss%d" % i, [128, 1], F32) for i in range(2)]
        rt = [P.sbuf("rt%d" % i, [128, 1], F32) for i in range(2)]
        hb = [P.sbuf("hb%d" % i, [128, D], BF16) for i in range(2)]
        pt = [P.psum("pt%d" % i, [128, 4, 128], BF16) for i in range(2)]
        for t in range(NT):
            xi, s_, r_, h_ = xin[t % 2], ss[t % 2], rt[t % 2], hb[t % 2]
            P.dma("sp", xi[:], x_src[t * 128:(t + 1) * 128, :], writes=[xi])
            P.act(junk[:], xi[:], AF.Square, [xi], [junk, s_], accum_out=s_[:])
            P.act(r_[:], s_[:], AF.Sqrt, [s_, K.eps], [r_], scale=1.0 / D, bias=K.eps[:])
            P.recip(r_[:], r_[:], [r_], [r_])
            P.stt("dve", h_[:], xi[:], r_[:], gb[:], ALU.mult, ALU.mult, [xi, r_, gb], [h_])
            for half in range(2):
                p_ = pt[half]
                for j in range(4):
                    kc = half * 4 + j
                    P.transpose(p_[:, j, :], h_[:, kc * 128:(kc + 1) * 128], K.ident[:], [h_, K.ident], [p_])
                if half == 0:
                    P.copy("dve", K.hT[:, 0:4, t * 128:(t + 1) * 128], p_[:], [p_], [K.hTb[t]])
                else:
                    P.copy("act", K.hT[:, 4:8, t * 128:(t + 1) * 128], p_[:], [p_], [K.hTb[t]])


def phase_inproj(P, C, K, l):
    w_in = C.prm["w_in"][l]
    S = C.scr
    with P.scope():
        cwg = P.sbuf("cwg", [128, 4, 12], F32)
        cws = P.sbuf("cws", [128, 4, 8], F32)
        for k in range(4):
            P.dma("sp", cwg[:, k, :], C.prm["gdn_conv_w"][l][k].rearrange("(c p) -> p c", p=128), writes=[cwg],
                  allow_slow_non_contiguous=True)
            P.dma("sp", cws[:, k, :], C.prm["ssd_conv_w"][l][k].rearrange("(c p) -> p c", p=128), writes=[cws],
                  allow_slow_non_contiguous=True)
        cbs = P.sbuf("cbs", [128, 8], F32)
        P.dma("sp", cbs[:], C.prm["ssd_conv_b"][l].rearrange("(c p) -> p c", p=128), writes=[cbs],
              allow_slow_non_contiguous=True)
        gq2 = P.sbuf("gq2", [128, 1], F32)
        for i in range(2):
            P.dma("sp", gq2[i * 64:(i + 1) * 64, :], C.prm["dsa_q_norm_g"][l].rearrange("(p o) -> p o", o=1), writes=[gq2])
        gk1 = P.sbuf("gk1", [64, 1], F32)
        P.dma("sp", gk1[:], C.prm["dsa_k_norm_g"][l].rearrange("(p o) -> p o", o=1), writes=[gk1])
        gmq = P.sbuf("gmq", [128, 1], F32)
        P.dma("sp", gmq[:], C.prm["mem_q_norm_g"][l].rearrange("(p o) -> p o", o=1), writes=[gmq])
        P.ts("dve", gq2[:], gq2[:], 0.125, None, ALU.mult, None, [gq2], [gq2])
        P.ts("dve", gmq[:], gmq[:], 128 ** -0.5, None, ALU.mult, None, [gmq], [gmq])

        wb = [P.sbuf("wb%d" % i, [128, 8, 512], BF16) for i in range(2)]
        acc = [P.psum("acc%d" % i, [128, 512], F32) for i in range(3)]
        ssp = [P.psum("ssp%d" % i, [128, 512], F32) for i in range(2)]
        xpad = [P.sbuf("xpad%d" % i, [128, 515], F32) for i in range(4)]
        yb = [P.sbuf("yb%d" % i, [128, 512], F32) for i in range(4)]
        sb = [P.sbuf("sb%d" % i, [128, 512], F32) for i in range(2)]
        sq = [P.sbuf("sq%d" % i, [128, 512], BF16) for i in range(2)]
        rtb = [P.sbuf("rtb%d" % i, [128, 512], F32) for i in range(2)]
        ob = [P.sbuf("ob%d" % i, [128, 512], BF16) for i in range(3)]
        mo = [P.sbuf("mo%d" % i, [128, 88], F32) for i in range(2)]
        st = Ctx()
        st.g = 0
        st.a = 0
        st.e = 0
        st.o = 0

        st.loaded = {}

        def issue_load(gi, spec):
            w = wb[gi % 2]
            for (c0, n, d0) in spec:
                P.dma("pool", w[:, :, d0:d0 + n], w_in[:, c0:c0 + n].rearrange("(kc k) c -> k kc c", k=128), writes=[w])
            st.loaded[gi] = w

        def load_w(col0, ncols):
            w = st.loaded[st.g]
            st.g += 1
            return w

        def fm_group(col0, ncols, kind, dst, cw=None, cb=None, gain=None, scale=1.0):
            w = load_w(col0, ncols)
            nch = (ncols + 127) // 128
            for j in range(nch):
                m = min(128, ncols - j * 128)
                for tg in range(8):
                    a = acc[st.a % 3]
                    st.a += 1
                    for kc in range(8):
                        P.matmul(a[0:m, :], w[:, kc, j * 128:j * 128 + m], K.hT[:, kc, tg * 512:(tg + 1) * 512],
                                 kc == 0, kc == 7, [w] + K.hTb[tg * 4:tg * 4 + 4], [a])
                    e = st.e
                    st.e += 1
                    o = ob[st.o % 3]
                    st.o += 1
                    dsl = dst[j * 128:j * 128 + m, tg * 512:(tg + 1) * 512]
                    if kind == "raw":
                        P.copy("act", o[0:m, :], a[0:m, :], [a], [o])
                        P.dma("sp", dsl, o[0:m, :], reads=[o])
                        continue
                    if kind in ("conv", "conv_l2"):
                        xp, xn = xpad[tg % 4], xpad[(tg + 1) % 4]
                        y = yb[e % 4]
                        ce = "dve"
                        if tg == 0:
                            P.memset("dve", xp[:, 0:3], 0.0, [xp])
                        P.copy("act", xp[:, 3:515], a[:], [a], [xp])
                        cwb, cwo = cw
                        cj = cwo + j
                        P.ts(ce, y[:], xp[:, 0:512], cwb[:, 0, cj:cj + 1], None, ALU.mult, None, [xp, cwb], [y])
                        for k in range(1, 4):
                            P.stt(ce, y[:], xp[:, k:k + 512], cwb[:, k, cj:cj + 1], y[:], ALU.mult, ALU.add, [xp, cwb, y], [y])
                        if tg < 7:
                            P.copy("act", xn[:, 0:3], xp[:, 512:515], [xp], [xn])
                        if kind == "conv":
                            if cb is not None:
                                P.act(o[:], y[:], AF.Silu, [y, cb[0]], [o], bias=cb[0][:, cb[1] + j:cb[1] + j + 1])
                            else:
                                P.act(o[:], y[:], AF.Silu, [y], [o])
                            P.dma("sp", dsl, o[:], reads=[o])
                            continue
                        s_ = sb[e % 2]
                        P.act(s_[:], y[:], AF.Silu, [y], [s_])
                        src_ = s_
                        ones = K.ones
                        nrm_scale = 1.0
                    else:
                        s_ = sb[e % 2]
                        P.copy("act", s_[0:m, :], a[0:m, :], [a], [s_])
                        ones = K.blk2 if kind == "rms64" else K.ones
                        nrm_scale = (1.0 / 64) if kind == "rms64" else (1.0 / 128)
                    q_ = sq[e % 2]
                    P.act(q_[0:m, :], s_[0:m, :], AF.Square, [s_], [q_])
                    sp_ = ssp[e % 2]
                    P.matmul(sp_[0:m, :], ones[0:m, 0:m], q_[0:m, :], True, True, [ones, q_], [sp_])
                    r_ = rtb[e % 2]
                    P.act(r_[0:m, :], sp_[0:m, :], AF.Sqrt, [sp_, K.eps], [r_], scale=nrm_scale, bias=K.eps[0:m, :])
                    P.recip(r_[0:m, :], r_[0:m, :], [r_], [r_])
                    if gain is not None:
                        P.stt("dve", o[0:m, :], s_[0:m, :], gain[0:m, :], r_[0:m, :], ALU.mult, ALU.mult, [s_, gain, r_], [o])
                    else:
                        P.stt("dve", o[0:m, :], s_[0:m, :], scale, r_[0:m, :], ALU.mult, ALU.mult, [s_, r_], [o])
                    P.dma("sp", dsl, o[0:m, :], reads=[o])

        def tm_group(col0, func, dst):
            w = load_w(col0, 512)
            for t in range(NT):
                a = acc[st.a % 3]
                st.a += 1
                for kc in range(8):
                    P.matmul(a[:], K.hT[:, kc, t * 128:(t + 1) * 128], w[:, kc, :], kc == 0, kc == 7,
                             [w, K.hTb[t]], [a])
                o = ob[st.o % 3]
                st.o += 1
                if func is None:
                    P.copy("act", o[:], a[:], [a], [o])
                else:
                    P.act(o[:], a[:], func, [a], [o])
                P.dma("sp", dst[t * 128:(t + 1) * 128, :], o[:], reads=[o])

        def misc_group():
            w = load_w(0, 88)
            for t in range(NT):
                a = acc[st.a % 3]
                st.a += 1
                for kc in range(8):
                    P.matmul(a[:, 0:88], K.hT[:, kc, t * 128:(t + 1) * 128], w[:, kc, 0:88], kc == 0, kc == 7,
                             [w, K.hTb[t]], [a])
                o = mo[t % 2]
                P.copy("act", o[:], a[:, 0:88], [a], [o])
                P.dma("sp", S["misc"][t * 128:(t + 1) * 128, :], o[:], reads=[o])

        groups = [
            (((O_AA, 8, 0), (O_CIW, 8, 8), (O_DDT, 8, 16), (O_CV, 64, 24)), lambda: misc_group()),
            (((O_CIQ, 512, 0),), lambda: fm_group(O_CIQ, 512, "raw", S["ciq"])),
            (((O_CIK, 64, 0),), lambda: fm_group(O_CIK, 64, "raw", S["cik"])),
            (((O_CQ, 512, 0),), lambda: fm_group(O_CQ, 512, "rms64", S["cq"], gain=gq2)),
            (((O_CK, 64, 0),), lambda: fm_group(O_CK, 64, "rms64", S["ck"], gain=gk1)),
            (((O_MQ, 512, 0),), lambda: fm_group(O_MQ, 512, "rms128", S["mq"], gain=gmq)),
            (((O_AQ, 512, 0),), lambda: fm_group(O_AQ, 512, "conv_l2", S["gq"], cw=(cwg, 0), scale=128 ** -0.5)),
            (((O_AK, 512, 0),), lambda: fm_group(O_AK, 512, "conv_l2", S["gk"], cw=(cwg, 4), scale=1.0)),
            (((O_AV, 512, 0),), lambda: fm_group(O_AV, 512, "conv", S["gv"], cw=(cwg, 8))),
            (((O_DX, 512, 0),), lambda: fm_group(O_DX, 512, "conv", S["xbc"][0:512], cw=(cws, 0), cb=(cbs, 0))),
            (((O_DX + 512, 512, 0),), lambda: fm_group(O_DX + 512, 512, "conv", S["xbc"][512:1024], cw=(cws, 4), cb=(cbs, 4))),
            (((O_AG, 512, 0),), lambda: tm_group(O_AG, AF.Silu, S["ag"])),
            (((O_BG, 512, 0),), lambda: tm_group(O_BG, AF.Silu, S["bg"])),
            (((O_CG, 512, 0),), lambda: tm_group(O_CG, AF.Silu, S["cg"])),
            (((O_DZ, 512, 0),), lambda: tm_group(O_DZ, AF.Silu, S["dz"])),
            (((O_MG, 512, 0),), lambda: tm_group(O_MG, AF.Silu, S["mg"])),
            (((O_BU, 512, 0),), lambda: tm_group(O_BU, AF.Gelu, S["bu"])),
            (((O_BV, 512, 0),), lambda: tm_group(O_BV, AF.Gelu, S["bv"])),
        ]
        issue_load(0, groups[0][0])
        for gi, (spec, run) in enumerate(groups):
            if gi + 1 < len(groups):
                issue_load(gi + 1, groups[gi + 1][0])
            run()


def phase_merge(P, C, K, l):
    wgd = C.prm["w_gate"][l]
    wbd = C.prm["w_branch"][l]
    ysT = C.scr["ysT"]
    with P.scope():
        wbuf = [P.sbuf("mw%d" % i, [128, 7680], BF16) for i in range(2)]
        yT = [P.sbuf("yT%d" % i, [128, 4, 512], BF16) for i in range(3)]
        gps = [P.psum("gps%d" % i, [128, 512], F32) for i in range(2)]
        zps = [P.psum("zps%d" % i, [128, 512], F32) for i in range(2)]
        sg = [P.sbuf("sg%d" % i, [128, 512], F32) for i in range(2)]
        tmp = [P.sbuf("tmp%d" % i, [128, 512], F32) for i in range(2)]
        mac = [P.sbuf("mac%d" % i, [128, 512], F32) for i in range(2)]
        cnt = 0

        def views(w):
            return (w[:, 0:5120].rearrange("k (p kc n) -> k p kc n", p=5, kc=8),
                    w[:, 5120:7680].rearrange("k (p cc n) -> k p cc n", p=5, cc=4))

        def load(nch):
            w = wbuf[nch % 2]
            wg, wbr = views(w)
            for p in range(5):
                P.dma("pool", wg[:, p], wgd[p][:, nch * 128:(nch + 1) * 128].rearrange("(kc k) n -> k kc n", k=128), writes=[w])
                P.dma("pool", wbr[:, p], wbd[p][:, nch * 128:(nch + 1) * 128].rearrange("(cc k) n -> k cc n", k=128), writes=[w])

        load(0)
        for nch in range(8):
            w = wbuf[nch % 2]
            wg, wbr = views(w)
            if nch + 1 < 8:
                load(nch + 1)
            for tg in range(8):
                m_ = mac[tg % 2]
                for p in range(5):
                    y = yT[cnt % 3]
                    g_, z_ = gps[cnt % 2], zps[cnt % 2]
                    s_, t_ = sg[cnt % 2], tmp[cnt % 2]
                    cnt += 1
                    P.dma("sp", y[:], ysT[p][:, tg * 512:(tg + 1) * 512].rearrange("(cc c) t -> c cc t", c=128), writes=[y])
                    for kc in range(8):
                        P.matmul(g_[:], wg[:, p, kc, :], K.hT[:, kc, tg * 512:(tg + 1) * 512], kc == 0, kc == 7,
                                 [w] + K.hTb[tg * 4:tg * 4 + 4], [g_])
                    for cc in range(4):
                        P.matmul(z_[:], wbr[:, p, cc, :], y[:, cc, :], cc == 0, cc == 3, [w, y], [z_])
                    P.act(s_[:], g_[:], AF.Sigmoid, [g_], [s_])
                    if p == 0:
                        P.tt("dve", m_[:], z_[:], s_[:], ALU.mult, [z_, s_], [m_])
                    elif p < 4:
                        P.tt("dve", t_[:], z_[:], s_[:], ALU.mult, [z_, s_], [t_])
                        P.tt("pool", m_[:], m_[:], t_[:], ALU.add, [m_, t_], [m_])
                    else:
                        P.tt("dve", t_[:], z_[:], s_[:], ALU.mult, [z_, s_], [t_])
                        P.tt("pool", K.mT[:, nch, tg * 512:(tg + 1) * 512], m_[:], t_[:], ALU.add, [m_, t_], [K.mTb[tg]])


def phase_outproj(P, C, K, l, x_src, x_dst):
    with P.scope():
        wo = P.sbuf("wo", [128, 8, D], BF16)
        P.dma("pool", wo[:], C.prm["w_out"][l].rearrange("(kc k) n -> k kc n", k=128), writes=[wo])
        xin = [P.sbuf("oxin%d" % i, [128, 512], F32) for i in range(2)]
        xo = [P.sbuf("oxo%d" % i, [128, 512], F32) for i in range(2)]
        ops_ = [P.psum("ops%d" % i, [128, 512], F32) for i in range(2)]
        c = 0
        for t in range(NT):
            for hf in range(2):
                xi, o, ps = xin[c % 2], xo[c % 2], ops_[c % 2]
                c += 1
                P.dma("sp", xi[:], x_src[t * 128:(t + 1) * 128, hf * 512:(hf + 1) * 512], writes=[xi])
                for kc in range(8):
                    P.matmul(ps[:], K.mT[:, kc, t * 128:(t + 1) * 128], wo[:, kc, hf * 512:(hf + 1) * 512], kc == 0, kc == 7,
                             [wo, K.mTb[t // 4]], [ps])
                P.tt("dve", o[:], ps[:], xi[:], ALU.add, [ps, xi], [o])
                P.dma("sp", x_dst[t * 128:(t + 1) * 128, hf * 512:(hf + 1) * 512], o[:], reads=[o])


class YOut:
    def __init__(self, P, C, K, p, tag, nps=1, ps=None):
        self.P, self.C, self.K, self.p = P, C, K, p
        self.ps = ps if ps is not None else [P.psum("yo_ps%s%d" % (tag, i), [128, 4, 128], BF16) for i in range(nps)]
        self.sb = [P.sbuf("yo_sb%s%d" % (tag, i), [128, 4, 128], BF16) for i in range(2)]
        self.n = 0

    def emit(self, y, t, eng="act"):
        P, K = self.P, self.K
        ps, sb = self.ps[self.n % len(self.ps)], self.sb[self.n % 2]
        self.n += 1
        for cc in range(4):
            P.transpose(ps[:, cc, :], y[:, cc * 128:(cc + 1) * 128], K.ident[:], [y, K.ident], [ps])
        P.copy(eng, sb[:], ps[:], [ps], [sb])
        P.dma("sp", self.C.scr["ysT"][self.p][:, t * 128:(t + 1) * 128].rearrange("(cc c) t -> c cc t", c=128), sb[:], reads=[sb])


def phase_mem(P, C, K, l):
    S = C.scr
    with P.scope():
        gb = P.sbuf("m_gb", [128, D], F32)
        P.dma("sp", gb[:], C.prm["mem_norm_g"][l:l + 1, :].to_broadcast([128, D]), writes=[gb])
        gk = P.sbuf("m_gk", [128, 1], F32)
        P.dma("sp", gk[:], C.prm["mem_k_norm_g"][l].rearrange("(p o) -> p o", o=1), writes=[gk])
        wkv = P.sbuf("m_wkv", [128, 8, D], BF16)
        P.dma("pool", wkv[:], C.prm["w_mem_kv"][l].rearrange("(kc k) n -> k kc n", k=128), writes=[wkv])
        memT = P.sbuf("memT", [128, 8, 256], BF16)
        kT = P.sbuf("m_kT", [128, 4, 256], BF16)
        vaug = [P.sbuf("m_va%d" % i, [128, 4, 129], BF16) for i in range(2)]
        xin = P.sbuf("m_x", [128, D], F32)
        junk = P.sbuf("m_junk", [128, D], BF16)
        ss = P.sbuf("m_ss", [128, 1], F32)
        hb = P.sbuf("m_hb", [128, D], BF16)
        pt = P.psum("m_pt", [128, 4, 128], BF16)
        pa = P.psum("m_pa", [128, 512], F32)
        pb = P.psum("m_pb", [128, 512], F32)
        for mt in range(2):
            P.dma("sp", xin[:], C.mem[mt * 128:(mt + 1) * 128, :], writes=[xin])
            P.act(junk[:], xin[:], AF.Square, [xin], [junk, ss], accum_out=ss[:])
            P.act(ss[:], ss[:], AF.Sqrt, [ss, K.eps], [ss], scale=1.0 / D, bias=K.eps[:])
            P.recip(ss[:], ss[:], [ss], [ss])
            P.stt("dve", hb[:], xin[:], ss[:], gb[:], ALU.mult, ALU.mult, [xin, ss, gb], [hb])
            for half in range(2):
                for j in range(4):
                    kc = half * 4 + j
                    P.transpose(pt[:, j, :], hb[:, kc * 128:(kc + 1) * 128], K.ident[:], [hb, K.ident], [pt])
                P.copy("dve", memT[:, half * 4:half * 4 + 4, mt * 128:(mt + 1) * 128], pt[:], [pt], [memT])
        sq = P.sbuf("m_sq", [128, 256], BF16)
        kf = P.sbuf("m_kf", [128, 256], F32)
        rr = P.sbuf("m_rr", [128, 256], F32)
        for h in range(4):
            for kc in range(8):
                P.matmul(pa[:, 0:256], wkv[:, kc, h * 128:(h + 1) * 128], memT[:, kc, :], kc == 0, kc == 7, [wkv, memT], [pa])
            P.copy("act", kf[:], pa[:, 0:256], [pa], [kf])
            P.act(sq[:], kf[:], AF.Square, [kf], [sq])
            P.matmul(pb[:, 0:256], K.ones[:], sq[:], True, True, [K.ones, sq], [pb])
            P.act(rr[:], pb[:, 0:256], AF.Sqrt, [pb, K.eps], [rr], scale=1.0 / 128, bias=K.eps[:])
            P.recip(rr[:], rr[:], [rr], [rr])
            P.stt("dve", kT[:, h, :], kf[:], gk[:], rr[:], ALU.mult, ALU.mult, [kf, gk, rr], [kT])
        for mt in range(2):
            for kc in range(8):
                P.matmul(pa[:], memT[:, kc, mt * 128:(mt + 1) * 128], wkv[:, kc, 512:1024], kc == 0, kc == 7, [wkv, memT], [pa])
            P.memset("pool", vaug[mt][:, :, 128:129], 1.0, [vaug[mt]])
            P.copy("act", vaug[mt][:, :, 0:128], pa[:].rearrange("m (h d) -> m h d", h=4), [pa], [vaug[mt]])
        qT = [P.sbuf("m_qT%d" % i, [128, 4, 128], BF16) for i in range(2)]
        gt = [P.sbuf("m_gt%d" % i, [128, 512], BF16) for i in range(2)]
        lg = [P.psum("m_lg%d" % i, [128, 4, 128], F32) for i in range(2)]
        pT = [P.sbuf("m_pT%d" % i, [128, 4, 128], BF16) for i in range(2)]
        po = P.psum("m_po", [128, 4, 256], F32)
        rd = [P.sbuf("m_rd%d" % i, [128, 4], F32) for i in range(2)]
        yb = [P.sbuf("m_y%d" % i, [128, 512], BF16) for i in range(2)]
        yo = YOut(P, C, K, 4, "m")
        for t in range(NTR[0]):
            q, g = qT[t % 2], gt[t % 2]
            P.dma("sp", q[:], S["mq"][:, t * 128:(t + 1) * 128].rearrange("(h d) t -> d h t", d=128), writes=[q])
            P.dma("sp", g[:], S["mg"][t * 128:(t + 1) * 128, :], writes=[g])
            for mt in range(2):
                for h in range(4):
                    P.matmul(lg[mt][:, h, :], kT[:, h, mt * 128:(mt + 1) * 128], q[:, h, :], True, True, [kT, q], [lg[mt]])
                P.act(pT[mt][:], lg[mt][:], AF.Exp, [lg[mt]], [pT[mt]])
            for h in range(4):
                for mt in range(2):
                    P.matmul(po[:, h, 0:129], pT[mt][:, h, :], vaug[mt][:, h, :], mt == 0, mt == 1, [pT[mt], vaug[mt]], [po])
            r_ = rd[t % 2]
            y = yb[t % 2]
            P.recip(r_[:], po[:, :, 128], [po], [r_])
            for h in range(4):
                P.stt("dve", y[:, h * 128:(h + 1) * 128], po[:, h, 0:128], r_[:, h:h + 1], g[:, h * 128:(h + 1) * 128],
                      ALU.mult, ALU.mult, [po, r_, g], [y])
            yo.emit(y, t)


def phase_sg(P, C, K, l):
    S = C.scr
    with P.scope():
        lng = P.sbuf("b_lng", [128, 512], F32)
        lnb = P.sbuf("b_lnb", [128, 512], F32)
        P.dma("sp", lng[:], C.prm["sg_ln_g"][l:l + 1, :].to_broadcast([128, 512]), writes=[lng])
        P.dma("sp", lnb[:], C.prm["sg_ln_b"][l:l + 1, :].to_broadcast([128, 512]), writes=[lnb])
        bsT = P.sbuf("b_bsT", [128, 4], F32)
        P.dma("sp", bsT[:], C.prm["sg_b"][l].rearrange("g t -> t g"), writes=[bsT], allow_slow_non_contiguous=True)
        eps5 = P.sbuf("b_eps5", [128, 1], F32)
        P.memset("pool", eps5[:], 1e-5, [eps5])
        wf = P.sbuf("b_wf", [128, 4, 128], F32)
        wbf = P.sbuf("b_wbf", [128, 4, 128], BF16)
        WcT = P.sbuf("b_WcT", [128, 4, 128], BF16)
        pt = P.psum("b_pt", [128, 4, 128], BF16)
        P.dma("sp", wf[:], C.prm["sg_w"][l].rearrange("g t s -> t g s"), writes=[wf])
        for g in range(4):
            P.op("pool", lambda e, g=g: e.affine_select(out=wf[:, g, :], in_=wf[:, g, :], pattern=[[-1, 128]], compare_op=ALU.is_ge,
                                                        fill=0.0, base=0, channel_multiplier=1), [wf], [wf])
        P.barrier()
        P.copy("dve", wbf[:], wf[:], [wf], [wbf])
        for g in range(4):
            P.transpose(pt[:, g, :], wbf[:, g, :], K.ident[:], [wbf, K.ident], [pt])
        P.copy("dve", WcT[:], pt[:], [pt], [WcT])

        vin = [P.sbuf("b_v%d" % i, [128, 512], BF16) for i in range(2)]
        uin = [P.sbuf("b_u%d" % i, [128, 512], BF16) for i in range(2)]
        gin = [P.sbuf("b_g%d" % i, [128, 512], BF16) for i in range(2)]
        st6 = [P.sbuf("b_st%d" % i, [128, 6], F32) for i in range(2)]
        mv = [P.sbuf("b_mv%d" % i, [128, 2], F32) for i in range(2)]
        rs = [P.sbuf("b_rs%d" % i, [128, 1], F32) for i in range(2)]
        vn = [P.sbuf("b_vn%d" % i, [128, 512], F32) for i in range(2)]
        vnb = [P.sbuf("b_vnb%d" % i, [128, 512], BF16) for i in range(2)]
        mx = [P.psum("b_mx%d" % i, [128, 512], F32) for i in range(2)]
        tm = [P.sbuf("b_tm%d" % i, [128, 512], F32) for i in range(2)]
        yb = [P.sbuf("b_y%d" % i, [128, 512], BF16) for i in range(2)]
        yo = YOut(P, C, K, 1, "b")
        for t in range(NTR[0]):
            i = t % 2
            v, u, g = vin[i], uin[i], gin[i]
            sl = slice(t * 128, (t + 1) * 128)
            P.dma("sp", v[:], S["bv"][sl, :], writes=[v])
            P.dma("sp", u[:], S["bu"][sl, :], writes=[u])
            P.dma("sp", g[:], S["bg"][sl, :], writes=[g])
            P.op("dve", lambda e, i=i: e.bn_stats(st6[i][:], vin[i][:]), [v], [st6[i]])
            P.op("dve", lambda e, i=i: e.bn_aggr(mv[i][:], st6[i][:]), [st6[i]], [mv[i]])
            P.act(rs[i][:], mv[i][:, 1:2], AF.Sqrt, [mv[i], eps5], [rs[i]], bias=eps5[:])
            P.recip(rs[i][:], rs[i][:], [rs[i]], [rs[i]])
            P.ts("dve", vn[i][:], v[:], mv[i][:, 0:1], rs[i][:], ALU.subtract, ALU.mult, [v, mv[i], rs[i]], [vn[i]])
            P.tt("pool", vn[i][:], vn[i][:], lng[:], ALU.mult, [vn[i], lng], [vn[i]])
            P.tt("pool", vnb[i][:], vn[i][:], lnb[:], ALU.add, [vn[i], lnb], [vnb[i]])
            for gg in range(4):
                P.matmul(mx[i][:, gg * 128:(gg + 1) * 128], WcT[:, gg, :], vnb[i][:, gg * 128:(gg + 1) * 128], True, True,
                         [WcT, vnb[i]], [mx[i]])
            for gg in range(4):
                c = slice(gg * 128, (gg + 1) * 128)
                P.stt("dve", tm[i][:, c], mx[i][:, c], bsT[:, gg:gg + 1], u[:, c], ALU.add, ALU.mult, [mx[i], bsT, u], [tm[i]])
            P.tt("pool", yb[i][:], tm[i][:], g[:], ALU.mult, [tm[i], g], [yb[i]])
            yo.emit(yb[i], t)


def bc(ap, shape):
    return ap.to_broadcast(list(shape))


def phase_ssd(P, C, K, l):
    S = C.scr
    with P.scope():
        uinc = P.sbuf("d_uinc", [128, 128], F32)
        P.memset("pool", uinc[:], 1.0, [uinc])
        P.op("pool", lambda e: e.affine_select(out=uinc[:], in_=uinc[:], pattern=[[1, 128]], compare_op=ALU.is_ge,
                                               fill=0.0, base=0, channel_multiplier=-1), [uinc], [uinc])
        onesf = P.sbuf("d_onesf", [128, 128], F32)
        P.memset("pool", onesf[:], 1.0, [onesf])
        P.barrier()
        aB = P.sbuf("d_aB", [128, 8], F32)
        P.dma("sp", aB[:], C.prm["ssd_a_log"][l:l + 1, :].to_broadcast([128, 8]), writes=[aB])
        P.act(aB[:], aB[:], AF.Exp, [aB], [aB])
        P.ts("dve", aB[:], aB[:], -1.0, None, ALU.mult, None, [aB], [aB])
        dtb = P.sbuf("d_dtb", [128, 8], F32)
        P.dma("sp", dtb[:], C.prm["ssd_dt_bias"][l:l + 1, :].to_broadcast([128, 8]), writes=[dtb])
        dsk = P.sbuf("d_dsk", [128, 8], F32)
        P.dma("sp", dsk[:], C.prm["ssd_d"][l:l + 1, :].to_broadcast([128, 8]), writes=[dsk])
        ngB = P.sbuf("d_ngB", [128, 512], F32)
        P.dma("sp", ngB[:], C.prm["ssd_norm_g"][l:l + 1, :].to_broadcast([128, 512]), writes=[ngB])
        S32 = P.sbuf("d_S32", [128, 512], F32)
        Sbf = P.sbuf("d_Sbf", [128, 512], BF16)
        P.memset("pool", S32[:], 0.0, [S32])
        P.memset("pool", Sbf[:], 0.0, [Sbf])

        xf = [P.sbuf("d_xf%d" % i, [128, 8, 128], BF16) for i in range(2)]
        dtin = [P.sbuf("d_dtin%d" % i, [128, 8], F32) for i in range(2)]
        zg = [P.sbuf("d_zg%d" % i, [128, 512], BF16) for i in range(2)]
        dt = P.sbuf("d_dt", [128, 8], F32)
        la = P.sbuf("d_la", [128, 8], F32)
        lab = P.sbuf("d_lab", [128, 8, 128], F32)
        cs = P.sbuf("d_cs", [128, 16], F32)
        pre = P.sbuf("d_pre", [128, 24], F32)
        E = P.sbuf("d_E", [128, 24], F32)
        seg = P.sbuf("d_seg", [128, 8, 128], F32)
        Lm = P.sbuf("d_L", [128, 8, 128], F32)
        MT = P.sbuf("d_MT", [128, 8, 128], BF16)
        cbm = P.sbuf("d_cbm", [128, 2, 128], F32)
        xs = P.sbuf("d_xs", [128, 512], BF16)
        btm = P.sbuf("d_btm", [128, 2, 128], BF16)
        xdt = P.sbuf("d_xdt", [128, 512], BF16)
        xdtd = P.sbuf("d_xdtd", [128, 512], BF16)
        t1 = P.sbuf("d_t1", [128, 512], F32)
        y1 = P.sbuf("d_y1", [128, 512], F32)
        y2 = P.sbuf("d_y2", [128, 512], F32)
        junk = P.sbuf("d_junk", [128, 512], BF16)
        ssq = P.sbuf("d_ssq", [128, 1], F32)
        yb = [P.sbuf("d_yb%d" % i, [128, 512], BF16) for i in range(2)]

        small = P.psum("d_small", [128, 512], F32)
        csps = View(small, small[:, 0:16])
        cbps = View(small, small[:, 128:384])
        csrow = P.psum("d_csrow", [128, 8, 128], F32)
        tp = P.psum("d_tp", [128, 768], BF16)
        yps = P.psum("d_yps", [128, 512], F32)
        yoff = P.psum("d_yoff", [128, 512], F32)
        stp = P.psum("d_stp", [128, 512], F32)
        yo = YOut(P, C, K, 3, "d")

        for t in range(NTR[0]):
            i = t % 2
            sl = slice(t * 128, (t + 1) * 128)
            x_ = xf[i]
            P.dma("sp", x_[:], S["xbc"][:, sl].rearrange("(c p) t -> p c t", p=128), writes=[x_])
            P.dma("sp", dtin[i][:], S["misc"][sl, 16:24], writes=[dtin[i]])
            P.dma("sp", zg[i][:], S["dz"][sl, :], writes=[zg[i]])
            P.tt("dve", dt[:], dtin[i][:], dtb[:], ALU.add, [dtin[i], dtb], [dt])
            P.act(dt[:], dt[:], AF.Exp, [dt], [dt])
            P.act(dt[:], dt[:], AF.Ln, [dt], [dt], bias=1.0)
            P.tt("dve", la[:], dt[:], aB[:], ALU.mult, [dt, aB], [la])
            if STOP[0] <= 1:
                continue
            P.matmul(csps[:, 0:8], uinc[:], la[:], True, True, [uinc, la], [csps])
            P.matmul(csps[:, 8:16], onesf[:], la[:], True, True, [onesf, la], [csps])
            P.copy("dve", cs[:], csps[:], [csps], [cs])
            if STOP[0] <= 2:
                continue
            P.copy("dve", lab[:], bc(la[:].unsqueeze(2), [128, 8, 128]), [la], [lab])
            for h in range(8):
                P.matmul(csrow[:, h, :], lab[:, h, :], uinc[:], True, True, [lab, uinc], [csrow])
            if STOP[0] <= 3:
                continue
            P.tt("dve", pre[:, 0:8], cs[:, 8:16], cs[:, 0:8], ALU.subtract, [cs], [pre])
            P.copy("dve", pre[:, 8:16], cs[:, 8:16], [cs], [pre])
            P.copy("dve", pre[:, 16:24], cs[:, 0:8], [cs], [pre])
            P.act(E[:], pre[:], AF.Exp, [pre], [E])
            if STOP[0] <= 4:
                continue
            P.tt("dve", seg[:], csrow[:], bc(cs[:, 0:8].unsqueeze(2), [128, 8, 128]), ALU.subtract, [csrow, cs], [seg])
            P.ts("dve", seg[:], seg[:], 0.0, None, ALU.min, None, [seg], [seg])
            P.act(Lm[:], seg[:], AF.Exp, [seg], [Lm])
            if STOP[0] <= 5:
                continue
            for g in range(2):
                P.matmul(cbps[:, g * 128:(g + 1) * 128], x_[:, 4 + g, :], x_[:, 6 + g, :], True, True, [x_], [cbps])
            P.tt("dve", cbm[:], cbps[:].rearrange("s (g c) -> s g c", g=2), bc(uinc[:].unsqueeze(1), [128, 2, 128]), ALU.mult,
                 [cbps, uinc], [cbm])
            for g in range(2):
                P.tt("dve", MT[:, g * 4:(g + 1) * 4, :], Lm[:, g * 4:(g + 1) * 4, :], bc(cbm[:, g:g + 1, :], [128, 4, 128]), ALU.mult,
                     [Lm, cbm], [MT])
            if STOP[0] <= 6:
                continue
            for c in range(4):
                P.transpose(tp[:, c * 128:(c + 1) * 128], x_[:, c, :], K.ident[:], [x_, K.ident], [tp])
            for g in range(2):
                P.transpose(tp[:, 512 + g * 128:512 + (g + 1) * 128], x_[:, 4 + g, :], K.ident[:], [x_, K.ident], [tp])
            P.copy("act", xs[:], tp[:, 0:512], [tp], [xs])
            P.copy("act", btm[:], tp[:, 512:768].rearrange("s (g d) -> s g d", g=2), [tp], [btm])
            xs3 = xs[:].rearrange("s (h p) -> s h p", h=8)
            P.tt("dve", xdt[:].rearrange("s (h p) -> s h p", h=8), xs3, bc(dt[:].unsqueeze(2), [128, 8, 64]), ALU.mult, [xs, dt], [xdt])
            P.tt("dve", xdtd[:].rearrange("s (h p) -> s h p", h=8), xdt[:].rearrange("s (h p) -> s h p", h=8),
                 bc(E[:, 0:8].unsqueeze(2), [128, 8, 64]), ALU.mult, [xdt, E], [xdtd])
            if STOP[0] <= 7:
                continue
            for h in range(8):
                P.matmul(yps[:, h * 64:(h + 1) * 64], MT[:, h, :], xdt[:, h * 64:(h + 1) * 64], True, True, [MT, xdt], [yps])
            for g in range(2):
                P.matmul(yoff[:, g * 256:(g + 1) * 256], x_[:, 6 + g, :], Sbf[:, g * 256:(g + 1) * 256], True, True, [x_, Sbf], [yoff])
            P.tt("dve", t1[:].rearrange("s (h p) -> s h p", h=8), yoff[:].rearrange("s (h p) -> s h p", h=8),
                 bc(E[:, 16:24].unsqueeze(2), [128, 8, 64]), ALU.mult, [yoff, E], [t1])
            P.tt("dve", y1[:], yps[:], t1[:], ALU.add, [yps, t1], [y1])
            P.tt("pool", t1[:].rearrange("s (h p) -> s h p", h=8), xs3, bc(dsk[:].unsqueeze(2), [128, 8, 64]), ALU.mult, [xs, dsk], [t1])
            P.tt("pool", y1[:], y1[:], t1[:], ALU.add, [y1, t1], [y1])
            P.tt("pool", y2[:], y1[:], zg[i][:], ALU.mult, [y1, zg[i]], [y2])
            P.act(junk[:], y2[:], AF.Square, [y2], [junk, ssq], accum_out=ssq[:])
            P.act(ssq[:], ssq[:], AF.Sqrt, [ssq, K.eps], [ssq], scale=1.0 / 512, bias=K.eps[:])
            P.recip(ssq[:], ssq[:], [ssq], [ssq])
            P.stt("dve", yb[i][:], y2[:], ssq[:], ngB[:], ALU.mult, ALU.mult, [y2, ssq, ngB], [yb[i]])
            yo.emit(yb[i], t)
            if STOP[0] <= 8:
                continue
            for g in range(2):
                P.matmul(stp[:, g * 256:(g + 1) * 256], btm[:, g, :], xdtd[:, g * 256:(g + 1) * 256], True, True, [btm, xdtd], [stp])
            P.tt("dve", S32[:].rearrange("d (h p) -> d h p", h=8), S32[:].rearrange("d (h p) -> d h p", h=8),
                 bc(E[:, 8:16].unsqueeze(2), [128, 8, 64]), ALU.mult, [S32, E], [S32])
            P.tt("dve", S32[:], S32[:], stp[:], ALU.add, [S32, stp], [S32])
            P.copy("act", Sbf[:], S32[:], [S32], [Sbf])


NBIS = 18


def phase_dsa(P, C, K, l):
    S = C.scr
    I32 = mybir.dt.int32
    with P.scope():
        rb = [P.sbuf("c_rb%d" % i, [1, T], BF16) for i in range(3)]
        P.op("pool", lambda e: e.iota(rb[0][:], pattern=[[0, NT], [1, 128]], base=0, channel_multiplier=0,
                                      allow_small_or_imprecise_dtypes=True), (), [rb[0]])
        P.op("pool", lambda e: e.iota(rb[1][:], pattern=[[1, NT], [0, 128]], base=0, channel_multiplier=0,
                                      allow_small_or_imprecise_dtypes=True), (), [rb[1]])
        P.memset("pool", rb[2][:], 1.0, [rb[2]])
        posrow = P.sbuf("c_posrow", [128, T], F32)
        P.op("pool", lambda e: e.iota(posrow[:], pattern=[[1, T]], base=0, channel_multiplier=0,
                                      allow_small_or_imprecise_dtypes=True), (), [posrow])
        sl1 = P.sbuf("c_sl1", [1, 8, 128], BF16)
        sl128 = P.sbuf("c_sl128", [1, 8, 128], BF16)
        nsl64 = P.sbuf("c_nsl64", [1, 8, 128], F32)
        nsl1 = P.sbuf("c_nsl1", [1, 8, 128], F32)
        for h in range(8):
            sl = 2.0 ** -(h + 1)
            P.memset("pool", sl1[:, h, :], sl, [sl1])
            P.memset("pool", sl128[:, h, :], 128 * sl, [sl128])
            P.memset("pool", nsl64[:, h, :], -64 * sl, [nsl64])
            P.memset("pool", nsl1[:, h, :], -sl, [nsl1])
        idf = P.sbuf("c_idf", [128, 128], F32)
        P.make_identity(idf)
        clow = P.sbuf("c_clow", [128, 128], BF16)
        P.memset("pool", clow[:], 1.0, [clow])
        P.op("pool", lambda e: e.affine_select(out=clow[:], in_=clow[:], pattern=[[-1, 128]], compare_op=ALU.is_ge,
                                               fill=0.0, base=0, channel_multiplier=1), [clow], [clow])
        cneg = P.sbuf("c_cneg", [128, 128], F32)
        P.memset("pool", cneg[:], 0.0, [cneg])
        P.op("pool", lambda e: e.affine_select(out=cneg[:], in_=cneg[:], pattern=[[-1, 128]], compare_op=ALU.is_ge,
                                               fill=-1e30, base=0, channel_multiplier=1), [cneg], [cneg])
        pw = P.sbuf("c_pw", [128, NBIS], F32)
        for k in range(NBIS):
            P.memset("pool", pw[:, k:k + 1], 2.0 ** -(k + 1), [pw])
        vaug = P.sbuf("c_vaug", [128, NT, 65], BF16)
        P.memset("pool", vaug[:, :, 64:65], 1.0, [vaug])
        P.barrier()
        kTa = P.sbuf("c_kTa", [68, T], BF16)
        ikT = P.sbuf("c_ikT", [64, T], BF16)
        P.dma("sp", kTa[0:64, :], S["ck"], writes=[kTa])
        P.dma("sp", ikT[:], S["cik"], writes=[ikT])
        P.dma("sp", kTa[64:65, :], rb[0][:], reads=[rb[0]], writes=[kTa])
        P.dma("sp", kTa[65:66, :], rb[1][:], reads=[rb[1]], writes=[kTa])
        P.dma("sp", kTa[66:67, :], rb[2][:], reads=[rb[2]], writes=[kTa])
        P.dma("sp", kTa[67:68, :], rb[2][:], reads=[rb[2]], writes=[kTa])
        for s4 in range(0, NT, 4):
            P.dma("pool", vaug[:, s4:s4 + 4, 0:64], S["misc"][s4 * 128:(s4 + 4) * 128, 24:88].rearrange("(st s) d -> s st d", s=128),
                  writes=[vaug])

        iqT = [P.sbuf("c_iqT%d" % i, [64, 8, 128], BF16) for i in range(2)]
        qTa = [P.sbuf("c_qTa%d" % i, [68, 8, 128], BF16) for i in range(2)]
        for i in range(2):
            P.dma("sp", qTa[i][64:65], sl1[:], reads=[sl1], writes=[qTa[i]])
            P.dma("sp", qTa[i][65:66], sl128[:], reads=[sl128], writes=[qTa[i]])
        iw = [P.sbuf("c_iw%d" % i, [128, 8], F32) for i in range(2)]
        gt = [P.sbuf("c_gt%d" % i, [128, 512], BF16) for i in range(2)]
        iwa = P.sbuf("c_iwa", [128, 8], F32)
        iws = P.sbuf("c_iws", [128, 8], F32)
        sidx = P.sbuf("c_sidx", [128, T], F32)
        junk = P.sbuf("c_junk", [128, T], F32)
        M01 = P.sbuf("c_M01", [128, T], BF16)
        M01T = P.sbuf("c_M01T", [128, NT, 128], BF16)
        rl = [P.sbuf("c_rl%d" % i, [128, 512], F32) for i in range(4)]
        ex = [P.sbuf("c_ex%d" % i, [128, 8, 128], BF16) for i in range(3)]
        pT = [P.sbuf("c_pT%d" % i, [128, 8, 128], BF16) for i in range(3)]
        mn = P.sbuf("c_mn", [128, 1], F32)
        mxv = P.sbuf("c_mx", [128, 1], F32)
        W = P.sbuf("c_W", [128, NBIS], F32)
        lo = P.sbuf("c_lo", [128, 1], F32)
        mid = P.sbuf("c_mid", [128, 1], F32)
        cnt = P.sbuf("c_cnt", [128, 1], F32)
        dd = P.sbuf("c_dd", [128, 1], F32)
        smax = P.sbuf("c_smax", [128, 1], F32)
        smaxb = P.sbuf("c_smaxb", [128, 32], F32)
        srow_i = P.sbuf("c_srow_i", [1, 128], I32)
        nhi_i = P.sbuf("c_nhi_i", [1, 128], I32)
        nlo_i = P.sbuf("c_nlo_i", [1, 128], I32)
        nhi_f = P.sbuf("c_nhi_f", [1, 128], F32)
        nlo_f = P.sbuf("c_nlo_f", [1, 128], F32)
        r2 = [P.sbuf("c_r2%d" % i, [1, 8, 128], BF16) for i in range(2)]
        r3 = [P.sbuf("c_r3%d" % i, [1, 8, 128], BF16) for i in range(2)]
        rden = P.sbuf("c_rden", [128, 8], F32)
        ot = P.sbuf("c_ot", [128, 512], F32)
        yb = [P.sbuf("c_yb%d" % i, [128, 512], BF16) for i in range(2)]

        sps = [P.psum("c_sps%d" % i, [128, 512], F32) for i in range(2)]
        lgh = [P.psum("c_lg%d" % i, [128, 4, 128], F32) for i in range(3)]
        ops_ = P.psum("c_ops", [128, 8, 128], F32)
        mty = P.psum("c_mty", [128, 8, 128], BF16)
        mtp = View(mty, mty[:, 0:4, :])
        yo = YOut(P, C, K, 2, "c", ps=[View(mty, mty[:, 4:8, :])])
        IWS = (8 ** -0.5) * (64 ** -0.5)
        nsp = 0
        for t in range(NTR[0]):
            i = t % 2
            sl = slice(t * 128, (t + 1) * 128)
            nk = t + 1
            Lk = nk * 128
            P.dma("sp", iqT[i][:], S["ciq"][:, sl].rearrange("(h d) t -> d h t", d=64), writes=[iqT[i]])
            P.dma("sp", qTa[i][0:64], S["cq"][:, sl].rearrange("(h d) t -> d h t", d=64), writes=[qTa[i]])
            P.dma("sp", iw[i][:], S["misc"][sl, 8:16], writes=[iw[i]])
            P.dma("sp", gt[i][:], S["cg"][sl, :], writes=[gt[i]])
            if STOP[0] <= 1:
                continue
            if t >= 2:
                P.act(iwa[:], iw[i][:], AF.Abs, [iw[i]], [iwa], scale=IWS)
                P.act(iws[:], iw[i][:], AF.Sign, [iw[i]], [iws])
                for c0 in range(0, Lk, 512):
                    w_ = min(512, Lk - c0)
                    for h in range(8):
                        sp_ = sps[nsp % 2]
                        r_ = rl[nsp % 4]
                        nsp += 1
                        P.matmul(sp_[:, 0:w_], iqT[i][:, h, :], ikT[:, c0:c0 + w_], True, True, [iqT[i], ikT], [sp_])
                        P.act(r_[:, 0:w_], sp_[:, 0:w_], AF.Relu, [sp_, iwa], [r_], scale=iwa[:, h:h + 1])
                        if h == 0:
                            P.ts("dve", sidx[:, c0:c0 + w_], r_[:, 0:w_], iws[:, 0:1], None, ALU.mult, None, [r_, iws], [sidx])
                        else:
                            P.stt("dve", sidx[:, c0:c0 + w_], r_[:, 0:w_], iws[:, h:h + 1], sidx[:, c0:c0 + w_], ALU.mult, ALU.add,
                                  [r_, iws, sidx], [sidx])
                P.op("dve", lambda e, Lk=Lk: e.tensor_reduce(mn[:], sidx[:, 0:Lk], AX.X, ALU.min), [sidx], [mn])
                P.op("dve", lambda e, Lk=Lk: e.tensor_reduce(mxv[:], sidx[:, 0:Lk], AX.X, ALU.max), [sidx], [mxv])
                P.tt("dve", sidx[:, t * 128:Lk], sidx[:, t * 128:Lk], cneg[:], ALU.add, [sidx, cneg], [sidx])
                P.tt("dve", mxv[:], mxv[:], mn[:], ALU.subtract, [mxv, mn], [mxv])
                P.ts("dve", W[:], pw[:], mxv[:], None, ALU.mult, None, [pw, mxv], [W])
                P.tt("dve", mid[:], mn[:], W[:, 0:1], ALU.add, [mn, W], [mid])
                for k in range(NBIS):
                    P.ts("dve", junk[:, 0:Lk], sidx[:, 0:Lk], mid[:], None, ALU.is_ge, ALU.add, [sidx, mid], [junk, cnt],
                         accum_out=cnt[:])
                    P.ts("dve", dd[:], cnt[:], 255.5, 0.5, ALU.is_ge, ALU.subtract, [cnt], [dd])
                    if k < NBIS - 1:
                        P.stt("dve", mid[:], dd[:], W[:, k:k + 1], mid[:], ALU.mult, ALU.add, [dd, W, mid], [mid])
                    else:
                        P.ts("dve", dd[:], dd[:], 0.5, None, ALU.subtract, None, [dd], [dd])
                        P.stt("dve", lo[:], dd[:], W[:, k:k + 1], mid[:], ALU.mult, ALU.add, [dd, W, mid], [lo])
                P.ts("dve", M01[:, 0:Lk], sidx[:, 0:Lk], lo[:], None, ALU.is_ge, None, [sidx, lo], [M01])
            else:
                if t > 0:
                    P.memset("pool", M01[:, 0:t * 128], 1.0, [M01])
                P.copy("pool", M01[:, t * 128:Lk], clow[:], [clow], [M01])
            if STOP[0] <= 2:
                continue
            P.tt("dve", junk[:, 0:Lk], M01[:, 0:Lk], posrow[:, 0:Lk], ALU.mult, [M01, posrow], [junk])
            P.op("dve", lambda e, Lk=Lk: e.tensor_reduce(smax[:], junk[:, 0:Lk], AX.X, ALU.max), [junk], [smax])
            srp = sps[nsp % 2]
            nsp += 1
            P.copy("dve", smaxb[:], bc(smax[:], [128, 32]), [smax], [smaxb])
            P.matmul(srp[0:32, 0:128], smaxb[:], idf[:], True, True, [smaxb, idf], [srp])
            if STOP[0] <= 2.5:
                continue
            P.copy("dve", srow_i[:], srp[0:1, 0:128], [srp], [srow_i])
            P.ts("dve", nhi_i[:], srow_i[:], 6, None, ALU.arith_shift_right, None, [srow_i], [nhi_i])
            P.ts("dve", nlo_i[:], srow_i[:], 63, None, ALU.bitwise_and, None, [srow_i], [nlo_i])
            P.copy("dve", nhi_f[:], nhi_i[:], [nhi_i], [nhi_f])
            P.copy("dve", nlo_f[:], nlo_i[:], [nlo_i], [nlo_f])
            P.tt("dve", r2[i][:], nsl64[:], bc(nhi_f[:].unsqueeze(1), [1, 8, 128]), ALU.mult, [nsl64, nhi_f], [r2[i]])
            P.tt("dve", r3[i][:], nsl1[:], bc(nlo_f[:].unsqueeze(1), [1, 8, 128]), ALU.mult, [nsl1, nlo_f], [r3[i]])
            if STOP[0] <= 2.7:
                continue
            P.dma("sp", qTa[i][66:67], r2[i][:], reads=[r2[i]], writes=[qTa[i]])
            P.dma("sp", qTa[i][67:68], r3[i][:], reads=[r3[i]], writes=[qTa[i]])
            if STOP[0] <= 3:
                continue
            for k0 in range(0, nk, 4):
                n4 = min(4, nk - k0)
                for j in range(n4):
                    P.transpose(mtp[:, j, :], M01[:, (k0 + j) * 128:(k0 + j + 1) * 128], K.ident[:], [M01, K.ident], [mtp])
                P.copy("act", M01T[:, k0:k0 + n4, :], mtp[:, 0:n4, :], [mtp], [M01T])
            if STOP[0] <= 4:
                continue
            def qk(kt_, half):
                lb = lgh[(2 * kt_ + half) % 3]
                P.matmul(lb[:], kTa[:, kt_ * 128:(kt_ + 1) * 128],
                         qTa[i][:, half * 4:(half + 1) * 4, :], True, True, [kTa, qTa[i]], [lb])

            qk(0, 0)
            qk(0, 1)
            for kt in range(nk):
                e_ = ex[kt % 3]
                p_ = pT[kt % 3]
                if kt + 1 < nk:
                    qk(kt + 1, 0)
                for half in range(2):
                    lb = lgh[(2 * kt + half) % 3]
                    P.act(e_[:, half * 4:(half + 1) * 4, :], lb[:], AF.Exp, [lb], [e_])
                if kt + 1 < nk:
                    qk(kt + 1, 1)
                P.stt("dve", p_[:], e_[:], 3.0e38, bc(M01T[:, kt:kt + 1, :], [128, 8, 128]), ALU.min, ALU.mult,
                      [e_, M01T], [p_])
                for h in range(8):
                    P.matmul(ops_[:, h, 0:65], p_[:, h, :], vaug[:, kt, :], kt == 0 and h % 4 == 0, kt == nk - 1 and h % 4 == 3,
                             [p_, vaug], [ops_])
            if STOP[0] <= 5:
                continue
            P.recip(rden[:], ops_[:, :, 64], [ops_], [rden])
            P.tt("dve", ot[:].rearrange("t (h d) -> t h d", h=8), ops_[:, :, 0:64], bc(rden[:].unsqueeze(2), [128, 8, 64]), ALU.mult,
                 [ops_, rden], [ot])
            P.tt("pool", yb[i][:], ot[:], gt[i][:], ALU.mult, [ot, gt[i]], [yb[i]])
            yo.emit(yb[i], t)


def phase_gdn(P, C, K, l):
    S = C.scr
    NCH = NTR[0] * 2
    with P.scope():
        uinc = P.sbuf("a_uinc", [128, 128], F32)
        P.memset("pool", uinc[:], 1.0, [uinc])
        P.op("pool", lambda e: e.affine_select(out=uinc[:], in_=uinc[:], pattern=[[1, 128]], compare_op=ALU.is_ge,
                                               fill=0.0, base=0, channel_multiplier=-1), [uinc], [uinc])
        lstr = P.sbuf("a_lstr", [128, 128], F32)
        P.memset("pool", lstr[:], 1.0, [lstr])
        P.op("pool", lambda e: e.affine_select(out=lstr[:], in_=lstr[:], pattern=[[-1, 128]], compare_op=ALU.is_gt,
                                               fill=0.0, base=0, channel_multiplier=1), [lstr], [lstr])
        idf = P.sbuf("a_idf", [128, 128], F32)
        P.make_identity(idf)
        onesf = P.sbuf("a_onesf", [128, 128], F32)
        P.memset("pool", onesf[:], 1.0, [onesf])
        P.barrier()
        aB = P.sbuf("a_aB", [64, 4], F32)
        P.dma("sp", aB[:], C.prm["gdn_a_log"][l:l + 1, :].to_broadcast([64, 4]), writes=[aB])
        P.act(aB[:], aB[:], AF.Exp, [aB], [aB])
        P.ts("dve", aB[:], aB[:], -1.0, None, ALU.mult, None, [aB], [aB])
        dtb = P.sbuf("a_dtb", [64, 4], F32)
        P.dma("sp", dtb[:], C.prm["gdn_dt_bias"][l:l + 1, :].to_broadcast([64, 4]), writes=[dtb])
        ng = P.sbuf("a_ng", [64, 128], F32)
        P.dma("sp", ng[:], C.prm["gdn_norm_g"][l:l + 1, :].to_broadcast([64, 128]), writes=[ng])
        ST32 = P.sbuf("a_ST32", [128, 4, 128], F32)
        STb = P.sbuf("a_STb", [128, 4, 128], BF16)
        P.memset("pool", ST32[:], 0.0, [ST32])
        P.memset("pool", STb[:], 0.0, [STb])

        def dbl(name, shape, dt):
            return [P.sbuf("%s%d" % (name, i), shape, dt) for i in range(2)]

        qT, kT, vT = dbl("a_qT", [128, 4, 64], BF16), dbl("a_kT", [128, 4, 64], BF16), dbl("a_vT", [128, 4, 64], BF16)
        ab, gate = dbl("a_ab", [64, 8], F32), dbl("a_gate", [64, 512], BF16)
        sp_, gl, be = dbl("a_sp", [64, 4], F32), dbl("a_gl", [64, 4], F32), dbl("a_be", [64, 4], F32)
        gc, gt128, cd128 = dbl("a_gc", [64, 4], F32), dbl("a_gt", [128, 4], F32), dbl("a_cd", [128, 4], F32)
        lab = dbl("a_lab", [64, 4, 128], F32)
        pre, E, bE = dbl("a_pre", [64, 8], F32), dbl("a_E", [64, 8], F32), dbl("a_bE", [64, 4], F32)
        seg, nseg = dbl("a_seg", [64, 4, 64], F32), dbl("a_nseg", [64, 4, 64], F32)
        dI, dL = dbl("a_dI", [64, 4, 64], F32), dbl("a_dL", [64, 4, 64], F32)
        egr = dbl("a_egr", [128, 4, 64], F32)
        qdT = dbl("a_qdT", [128, 4, 64], BF16)
        qkT = dbl("a_qkT", [64, 4, 64], BF16)
        Nm = [dbl("a_N", [64, 4, 64], F32), dbl("a_N2", [64, 4, 64], F32)]
        Mm = [dbl("a_M", [64, 4, 64], F32), dbl("a_M2", [64, 4, 64], F32)]
        X, Xb = dbl("a_X", [64, 4, 64], F32), dbl("a_Xb", [64, 4, 64], BF16)
        kb, kdec, vb = dbl("a_kb", [64, 4, 128], BF16), dbl("a_kdec", [64, 4, 128], BF16), dbl("a_vb", [64, 4, 128], BF16)
        wkT, u0 = dbl("a_wkT", [128, 4, 64], BF16), dbl("a_u0", [64, 4, 128], F32)
        u = dbl("a_u", [64, 4, 128], BF16)
        o, sq = dbl("a_o", [64, 4, 128], F32), dbl("a_sq", [64, 4, 128], F32)
        ssq = dbl("a_ssq", [64, 4], F32)
        y = dbl("a_y", [64, 512], F32)
        ysb = dbl("a_ysb", [128, 4, 64], BF16)

        b0 = P.psum("a_b0", [128, 512], F32)
        gcrow, wk_ps = View(b0, b0[:, 0:256].rearrange("p (h c) -> p h c", h=4)), View(b0, b0[:, 256:512].rearrange("p (h c) -> p h c", h=4))
        b1 = P.psum("a_b1", [128, 512], F32)
        sm_ps, ytp = View(b1, b1[:, 0:16]), View(b1, b1[:, 256:512].rearrange("p (h c) -> p h c", h=4))
        b2 = P.psum("a_b2", [64, 512], F32)
        kk_ps, qk_ps = View(b2, b2[:, 0:256].rearrange("p (h c) -> p h c", h=4)), View(b2, b2[:, 256:512].rearrange("p (h c) -> p h c", h=4))
        b3 = P.psum("a_b3", [64, 512], F32)
        P_ps, Q_ps = View(b3, b3[:, 0:256].rearrange("p (h c) -> p h c", h=4)), View(b3, b3[:, 256:512].rearrange("p (h c) -> p h c", h=4))
        b4 = P.psum("a_b4", [64, 512], F32)
        XP_ps, M_ps = View(b4, b4[:, 0:256].rearrange("p (h c) -> p h c", h=4)), View(b4, b4[:, 256:512].rearrange("p (h c) -> p h c", h=4))
        kv_ps = P.psum("a_kvps", [64, 2, 512], BF16)
        ktm, vtm = View(kv_ps, kv_ps[:, 0, :].rearrange("p (h d) -> p h d", h=4)), View(kv_ps, kv_ps[:, 1, :].rearrange("p (h d) -> p h d", h=4))
        uwo = P.psum("a_uwo", [64, 4, 128], F32)
        Sn_ps = P.psum("a_Sn", [128, 4, 128], F32)
        u64, i64 = uinc[0:64, 0:64], idf[0:64, 0:64]

        def b4c(ap, n):
            return bc(ap.unsqueeze(2), [ap.shape[0], 4, n])

        def pre_fn(ch):
            i = ch % 2
            cs = slice(ch * 64, (ch + 1) * 64)
            for dst, nm in ((qT[i], "gq"), (kT[i], "gk"), (vT[i], "gv")):
                P.dma("sp", dst[:], S[nm][:, cs].rearrange("(h d) t -> d h t", d=128), writes=[dst])
            P.dma("sp", ab[i][:], S["misc"][cs, 0:8], writes=[ab[i]])
            P.dma("sp", gate[i][:], S["ag"][cs, :], writes=[gate[i]])
            P.tt("dve", sp_[i][:], ab[i][:, 0:4], dtb[:], ALU.add, [ab[i], dtb], [sp_[i]])
            P.act(sp_[i][:], sp_[i][:], AF.Exp, [sp_[i]], [sp_[i]])
            P.act(sp_[i][:], sp_[i][:], AF.Ln, [sp_[i]], [sp_[i]], bias=1.0)
            P.tt("dve", gl[i][:], sp_[i][:], aB[:], ALU.mult, [sp_[i], aB], [gl[i]])
            P.act(be[i][:], ab[i][:, 4:8], AF.Exp, [ab[i]], [be[i]], scale=-1.0)
            P.ts("dve", be[i][:], be[i][:], 1.0, None, ALU.add, None, [be[i]], [be[i]])
            P.recip(be[i][:], be[i][:], [be[i]], [be[i]])
            P.matmul(sm_ps[0:64, 0:4], u64, gl[i][:], True, True, [uinc, gl[i]], [sm_ps])
            P.matmul(sm_ps[:, 4:8], onesf[0:64, :], gl[i][:], True, True, [onesf, gl[i]], [sm_ps])
            P.copy("dve", gc[i][:], sm_ps[0:64, 0:4], [sm_ps], [gc[i]])
            P.copy("dve", gt128[i][:], sm_ps[:, 4:8], [sm_ps], [gt128[i]])
            P.copy("dve", lab[i][:], b4c(gl[i][:], 128), [gl[i]], [lab[i]])
            for h in range(4):
                P.matmul(gcrow[:, h, :], lab[i][:, h, :], u64, True, True, [lab[i], uinc], [gcrow])
            P.copy("dve", pre[i][:, 0:4], gc[i][:], [gc[i]], [pre[i]])
            P.tt("dve", pre[i][:, 4:8], gt128[i][0:64, :], gc[i][:], ALU.subtract, [gt128[i], gc[i]], [pre[i]])
            P.act(E[i][:], pre[i][:], AF.Exp, [pre[i]], [E[i]])
            P.act(cd128[i][:], gt128[i][:], AF.Exp, [gt128[i]], [cd128[i]])
            P.tt("dve", bE[i][:], be[i][:], E[i][:, 0:4], ALU.mult, [be[i], E[i]], [bE[i]])
            if STOP[0] <= 1:
                return
            P.tt("dve", seg[i][:], gcrow[0:64], b4c(gc[i][:], 64), ALU.subtract, [gcrow, gc[i]], [seg[i]])
            P.ts("dve", nseg[i][:], seg[i][:], -1.0, 0.0, ALU.mult, ALU.min, [seg[i]], [nseg[i]])
            P.ts("dve", seg[i][:], seg[i][:], 0.0, None, ALU.min, None, [seg[i]], [seg[i]])
            P.act(dI[i][:], seg[i][:], AF.Exp, [seg[i]], [dI[i]])
            P.act(dL[i][:], nseg[i][:], AF.Exp, [nseg[i]], [dL[i]])
            P.tt("dve", dI[i][:], dI[i][:], bc(u64.unsqueeze(1), [64, 4, 64]), ALU.mult, [dI[i], uinc], [dI[i]])
            P.tt("dve", dL[i][:], dL[i][:], bc(lstr[0:64, 0:64].unsqueeze(1), [64, 4, 64]), ALU.mult, [dL[i], lstr], [dL[i]])
            P.act(egr[i][:], gcrow[:], AF.Exp, [gcrow], [egr[i]])
            P.tt("dve", qdT[i][:], qT[i][:], egr[i][:], ALU.mult, [qT[i], egr[i]], [qdT[i]])
            if STOP[0] <= 2:
                return
            for h in range(4):
                P.matmul(kk_ps[:, h, :], kT[i][:, h, :], kT[i][:, h, :], True, True, [kT[i]], [kk_ps])
            for h in range(4):
                P.matmul(qk_ps[:, h, :], kT[i][:, h, :], qT[i][:, h, :], True, True, [kT[i], qT[i]], [qk_ps])
            N0, M0 = Nm[0][i], Mm[0][i]
            P.tt("dve", N0[:], kk_ps[:], dL[i][:], ALU.mult, [kk_ps, dL[i]], [N0])
            P.tt("dve", N0[:], N0[:], b4c(be[i][:], 64), ALU.mult, [N0, be[i]], [N0])
            P.tt("dve", qkT[i][:], qk_ps[:], dI[i][:], ALU.mult, [qk_ps, dI[i]], [qkT[i]])
            if STOP[0] <= 3:
                return
            for h in range(4):
                P.transpose(M_ps[:, h, :], N0[:, h, :], i64, [N0, idf], [M_ps])
            P.copy("act", M0[:], M_ps[:], [M_ps], [M0])
            if STOP[0] <= 4:
                return
            P.tt("dve", X[i][:], bc(i64.unsqueeze(1), [64, 4, 64]), M0[:], ALU.subtract, [idf, M0], [X[i]])
            Pc, Qc = N0, M0
            if STOP[0] <= 4.1:
                return
            for st in range(1, 6):
                if STOP[0] <= 4.2 and st > 1:
                    break
                Pn, Qn = Nm[st % 2][i], Mm[st % 2][i]
                for h in range(4):
                    P.matmul(P_ps[:, h, :], Qc[:, h, :], Pc[:, h, :], True, True, [Qc, Pc], [P_ps])
                if st < 5:
                    for h in range(4):
                        P.matmul(Q_ps[:, h, :], Pc[:, h, :], Qc[:, h, :], True, True, [Qc, Pc], [Q_ps])
                if STOP[0] <= 4.15:
                    break
                P.copy("act", Pn[:], P_ps[:], [P_ps], [Pn])
                if st < 5:
                    P.copy("dve", Qn[:], Q_ps[:], [Q_ps], [Qn])
                if STOP[0] <= 4.17:
                    break
                for h in range(4):
                    P.matmul(XP_ps[:, h, :], Pn[:, h, :], X[i][:, h, :], True, True, [Pn, X[i]], [XP_ps])
                P.tt("dve", X[i][:], X[i][:], XP_ps[:], ALU.add, [X[i], XP_ps], [X[i]])
                Pc, Qc = Pn, Qn
            if STOP[0] <= 4.5:
                return
            P.copy("act", Xb[i][:], X[i][:], [X[i]], [Xb[i]])
            if STOP[0] <= 5:
                return
            for h in range(4):
                P.transpose(ktm[:, h, :], kT[i][:, h, :], K.ident[:], [kT[i], K.ident], [ktm])
            for h in range(4):
                P.transpose(vtm[:, h, :], vT[i][:, h, :], K.ident[:], [vT[i], K.ident], [vtm])
            P.tt("dve", kb[i][:], ktm[:], b4c(bE[i][:], 128), ALU.mult, [ktm, bE[i]], [kb[i]])
            P.tt("dve", kdec[i][:], ktm[:], b4c(E[i][:, 4:8], 128), ALU.mult, [ktm, E[i]], [kdec[i]])
            P.tt("dve", vb[i][:], vtm[:], b4c(be[i][:], 128), ALU.mult, [vtm, be[i]], [vb[i]])
            if STOP[0] <= 6:
                return
            for h in range(4):
                P.matmul(wk_ps[:, h, :], kb[i][:, h, :], Xb[i][:, h, :], True, True, [kb[i], Xb[i]], [wk_ps])
            P.copy("act", wkT[i][:], wk_ps[:], [wk_ps], [wkT[i]])
            for h in range(4):
                P.matmul(uwo[:, h, :], Xb[i][:, h, :], vb[i][:, h, :], True, True, [Xb[i], vb[i]], [uwo])
            P.copy("act", u0[i][:], uwo[:], [uwo], [u0[i]])
            if STOP[0] <= 7:
                return
        def seq_fn(ch):
            i = ch % 2
            cs = slice(ch * 64, (ch + 1) * 64)
            for h in range(4):
                P.matmul(uwo[:, h, :], wkT[i][:, h, :], STb[:, h, :], True, True, [wkT[i], STb], [uwo])
            P.tt("dve", u[i][:], u0[i][:], uwo[:], ALU.subtract, [u0[i], uwo], [u[i]])
            for h in range(4):
                P.matmul(uwo[:, h, :], qdT[i][:, h, :], STb[:, h, :], True, False, [qdT[i], STb], [uwo])
                P.matmul(uwo[:, h, :], qkT[i][:, h, :], u[i][:, h, :], False, True, [qkT[i], u[i]], [uwo])
            P.copy("act", o[i][:], uwo[:], [uwo], [o[i]])
            if STOP[0] <= 8:
                return
            for h in range(4):
                P.matmul(Sn_ps[:, h, :], kdec[i][:, h, :], u[i][:, h, :], True, True, [kdec[i], u[i]], [Sn_ps])
            P.tt("dve", ST32[:], ST32[:], b4c(cd128[i][:], 128), ALU.mult, [ST32, cd128[i]], [ST32])
            P.tt("dve", ST32[:], ST32[:], Sn_ps[:], ALU.add, [ST32, Sn_ps], [ST32])
            P.copy("act", STb[:], ST32[:], [ST32], [STb])
            if STOP[0] <= 9:
                return
            P.tt("pool", sq[i][:], o[i][:], o[i][:], ALU.mult, [o[i]], [sq[i]])
            P.op("dve", lambda e, i=i: e.tensor_reduce(ssq[i][:], sq[i][:], AX.X, ALU.add), [sq[i]], [ssq[i]])
            P.act(ssq[i][:], ssq[i][:], AF.Sqrt, [ssq[i], K.eps], [ssq[i]], scale=1.0 / 128, bias=K.eps[0:64, :])
            P.recip(ssq[i][:], ssq[i][:], [ssq[i]], [ssq[i]])
            P.tt("pool", o[i][:], o[i][:], b4c(ssq[i][:], 128), ALU.mult, [o[i], ssq[i]], [o[i]])
            P.tt("pool", o[i][:], o[i][:], bc(ng[:].unsqueeze(1), [64, 4, 128]), ALU.mult, [o[i], ng], [o[i]])
            P.tt("pool", y[i][:], o[i][:].rearrange("c h v -> c (h v)"), gate[i][:], ALU.mult, [o[i], gate[i]], [y[i]])
            if STOP[0] <= 10:
                return
            for cc in range(4):
                P.transpose(ytp[:, cc, :], y[i][:, cc * 128:(cc + 1) * 128], i64, [y[i], idf], [ytp])
            P.copy("act", ysb[i][:], ytp[:], [ytp], [ysb[i]])
            P.dma("sp", S["ysT"][0][:, cs].rearrange("(cc c) t -> c cc t", c=128), ysb[i][:], reads=[ysb[i]])


        pre_fn(0)
        for ch in range(NCH):
            if ch + 1 < NCH:
                pre_fn(ch + 1)
            seq_fn(ch)


def build_program(nc, layers=(0, 1), dbg=False, branches="abcdm"):
    C = declare(nc, dbg=dbg)
    P = Prog(nc)
    K = setup_consts(P, C)
    for l in layers:
        x_src = C.x if l == layers[0] else C.scr["x1"]
        x_dst = C.out if l == layers[-1] else C.scr["x1"]
        with P.scope():
            alloc_hT(P, K)
            phase_norm(P, C, K, l, x_src)
            phase_inproj(P, C, K, l)
        if "a" in branches:
            phase_gdn(P, C, K, l)
        if "b" in branches:
            phase_sg(P, C, K, l)
        if "c" in branches:
            phase_dsa(P, C, K, l)
        if "d" in branches:
            phase_ssd(P, C, K, l)
        if "m" in branches:
            phase_mem(P, C, K, l)
        with P.scope():
            alloc_merged(P, K)
            with P.scope():
                alloc_hT(P, K)
                phase_norm(P, C, K, l, x_src)
                phase_merge(P, C, K, l)
            phase_outproj(P, C, K, l, x_src, x_dst)
    P.finish()
    return C, P


def kernel(**inputs):
    x = np.ascontiguousarray(np.asarray(inputs["x"], dtype=np.float32))
    mem = np.ascontiguousarray(np.asarray(inputs["mem"], dtype=np.float32))
    nb = x.shape[0]
    nc = bass.Bass("TRN2", target_bir_lowering=False)
    build_program(nc)
    prm = {n: np.ascontiguousarray(np.asarray(inputs[n], dtype=np.float32)) for n, _ in PARAMS}
    in_maps = []
    for b in range(nb):
        m = {"x": x[b], "mem": mem[b]}
        m.update(prm)
        in_maps.append(m)
    res = run_bass_kernel_spmd(nc, in_maps, core_ids=list(range(nb)))
    return np.stack([np.asarray(r["out"], dtype=np.float32) for r in res.results], axis=0)
```

```python
from contextlib import ExitStack
import numpy as np
import concourse.bass as bass
import concourse.mybir as mybir
from concourse.bass_utils import run_bass_kernel_spmd

F32 = mybir.dt.float32
BF16 = mybir.dt.bfloat16
AF = mybir.ActivationFunctionType
ALU = mybir.AluOpType
AX = mybir.AxisListType


class Buf:
    __slots__ = ("name", "ap", "w", "r", "excl")

    def __init__(self, name, ap=None):
        self.name = name
        self.ap = ap
        self.excl = False
        self.w = None
        self.r = {}

    def __getitem__(self, idx):
        return self.ap[idx]


class View:
    def __init__(self, parent, ap):
        self.parent = parent
        self.ap = ap
        self.name = parent.name
        self.excl = parent.excl

    def __getitem__(self, idx):
        return self.ap[idx]

    @property
    def w(self):
        return self.parent.w

    @w.setter
    def w(self, v):
        self.parent.w = v

    @property
    def r(self):
        return self.parent.r

    @r.setter
    def r(self, v):
        self.parent.r = v


class Prog:
    ENG = ("pe", "dve", "act", "pool", "sp")
    SEM_LIMIT = 30000
    NDMA = 6

    def __init__(self, nc, same_engine_sync=True):
        self.nc = nc
        self.same = same_engine_sync
        self.stack = ExitStack()
        self.ops = {e: [] for e in self.ENG}
        self.cnt = {e: 0 for e in self.ENG}
        self.owner = {}
        self.nsem = 0
        self.cur = {e: self._newsem(e) for e in self.ENG}
        self.seen = {e: {} for e in self.ENG}
        self.dsem = {}
        self.drr = {}
        self.allsems = []
        self.nbuf = 0

    def _newsem(self, owner):
        s = getattr(self, "semstack", self.stack).enter_context(self.nc.semaphore("s%d_%s" % (self.nsem, owner)))
        self.nsem += 1
        self.owner[id(s)] = owner
        return s

    def sbuf(self, name, shape, dtype):
        self.nbuf += 1
        t = self.stack.enter_context(self.nc.sbuf_tensor("%s_%d" % (name, self.nbuf), list(shape), dtype))
        return Buf(name, t)

    def psum(self, name, shape, dtype):
        self.nbuf += 1
        t = self.stack.enter_context(self.nc.psum_tensor("%s_%d" % (name, self.nbuf), list(shape), dtype))
        b = Buf(name, t)
        b.excl = True
        return b

    def buf(self, name, ap=None):
        return Buf(name, ap)

    def _deps(self, eng, reads, writes):
        deps = {}

        def add(ev):
            if ev is None:
                return
            s, v = ev
            k = id(s)
            if k not in deps or deps[k][1] < v:
                deps[k] = (s, v)

        for b in reads:
            add(b.w)
        for b in writes:
            add(b.w)
            for ev in b.r.values():
                add(ev)
        waits = []
        seen = self.seen[eng]
        for k, (s, v) in deps.items():
            if self.owner.get(k) == eng:
                if eng == "pe" or eng == "sp" or not self.same:
                    continue
            if seen.get(k, 0) >= v:
                continue
            seen[k] = v
            waits.append((s, v))
        return waits

    def _commit(self, ev, reads, writes):
        for b in reads:
            k = id(ev[0])
            b.r[k] = ev
        for b in writes:
            b.w = ev
            b.r = {}

    def op(self, eng, fn, reads=(), writes=()):
        ex = [b for b in reads if getattr(b, "excl", False)]
        if ex:
            writes = list(writes) + ex
        waits = self._deps(eng, reads, writes)
        if self.cnt[eng] >= self.SEM_LIMIT:
            self.cur[eng] = self._newsem(eng)
            self.cnt[eng] = 0
        self.cnt[eng] += 1
        ev = (self.cur[eng], self.cnt[eng])
        self.ops[eng].append((waits, fn, ("c", self.cur[eng], self.cnt[eng])))
        self._commit(ev, reads, writes)
        return ev

    def dma(self, q, out, in_, reads=(), writes=(), **kw):
        waits = self._deps(q, reads, writes)
        if q not in self.dsem:
            self.dsem[q] = [[self._newsem("dma_" + q), 0] for _ in range(self.NDMA)]
            self.drr[q] = 0
        slot = self.dsem[q][self.drr[q] % self.NDMA]
        self.drr[q] += 1
        s, c = slot
        if c > 0:
            seen = self.seen[q]
            if seen.get(id(s), 0) < 16 * c:
                seen[id(s)] = 16 * c
                waits.append((s, 16 * c))
        if 16 * (c + 1) > self.SEM_LIMIT:
            s = self._newsem("dma_" + q)
            slot[0] = s
            c = 0
        slot[1] = c + 1
        ev = (s, 16 * (c + 1))
        self.ops[q].append((waits, lambda e, out=out, in_=in_, kw=kw: e.dma_start(out=out, in_=in_, **kw), ("d", s, 16)))
        self._commit(ev, reads, writes)
        return ev

    def make_identity(self, b, n=128):
        self.op("pool", lambda e: e.memset(b.ap[:], 1.0), writes=[b])
        self.op("pool", lambda e: e.affine_select(out=b.ap[:], in_=b.ap[:], pattern=[[-1, n]], compare_op=ALU.is_ge,
                                                  fill=0.0, base=0, channel_multiplier=1), reads=[b], writes=[b])
        self.op("pool", lambda e: e.affine_select(out=b.ap[:], in_=b.ap[:], pattern=[[1, n]], compare_op=ALU.is_ge,
                                                  fill=0.0, base=0, channel_multiplier=-1), reads=[b], writes=[b])


    def matmul(self, out, lhsT, rhs, start, stop, reads, writes):
        return self.op("pe", lambda e: e.matmul(out, lhsT, rhs, start=start, stop=stop), reads, writes)

    def transpose(self, out, in_, ident, reads, writes):
        return self.op("pe", lambda e: e.transpose(out, in_, ident), reads, writes)

    def act(self, out, in_, func, reads, writes, **kw):
        return self.op("act", lambda e: e.activation(out, in_, func, **kw), reads, writes)

    def tt(self, eng, out, a, b, op, reads, writes):
        return self.op(eng, lambda e: e.tensor_tensor(out, a, b, op), reads, writes)

    def ts(self, eng, out, a, s1, s2, op0, op1, reads, writes, **kw):
        if op1 is None:
            return self.op(eng, lambda e: e.tensor_scalar(out, a, s1, None, op0, **kw), reads, writes)
        return self.op(eng, lambda e: e.tensor_scalar(out, a, s1, s2, op0, op1, **kw), reads, writes)

    def stt(self, eng, out, in0, scalar, in1, op0, op1, reads, writes):
        return self.op(eng, lambda e: e.scalar_tensor_tensor(out, in0, scalar, in1, op0, op1), reads, writes)

    def copy(self, eng, out, in_, reads, writes):
        if eng == "act":
            return self.op(eng, lambda e: e.copy(out, in_), reads, writes)
        return self.op(eng, lambda e: e.tensor_copy(out, in_), reads, writes)

    def memset(self, eng, ap, val, writes):
        return self.op(eng, lambda e: e.memset(ap, val), (), writes)

    def recip(self, out, in_, reads, writes):
        return self.op("dve", lambda e: e.reciprocal(out, in_), reads, writes)

    def barrier(self):
        finals = []
        for e in self.ENG:
            if self.cnt[e] > 0:
                finals.append((self.cur[e], self.cnt[e]))
        for q, slots in self.dsem.items():
            for s, c in slots:
                if c > 0:
                    finals.append((s, 16 * c))
        for e in self.ENG:
            waits = []
            for s, v in finals:
                if self.seen[e].get(id(s), 0) < v:
                    self.seen[e][id(s)] = v
                    waits.append((s, v))
            if waits:
                self.ops[e].append((waits, None, None))

    def scope(self):
        return _Scope(self)

    def flush(self, final=False):
        nc = self.nc
        finals = []
        if final:
            for e in self.ENG:
                if self.cnt[e] > 0:
                    finals.append((self.cur[e], self.cnt[e]))
            for q, slots in self.dsem.items():
                for s, c in slots:
                    if c > 0:
                        finals.append((s, 16 * c))
        if not hasattr(self, "actual"):
            self.actual = {}
            self.amap = {}
        ref = set()
        for e in self.ENG:
            for waits, fn, inc in self.ops[e]:
                for s, v in waits:
                    if self.owner.get(id(s)) in self.ENG:
                        ref.add((id(s), v))
        for s, v in finals:
            if self.owner.get(id(s)) in self.ENG:
                ref.add((id(s), v))
        for e in self.ENG:
            for waits, fn, inc in self.ops[e]:
                if inc is not None and inc[0] == "c":
                    key = (id(inc[1]), inc[2])
                    if key in ref:
                        self.actual[key[0]] = self.actual.get(key[0], 0) + 1
                        self.amap[key] = self.actual[key[0]]

        def tr(s, v):
            if self.owner.get(id(s)) in self.ENG:
                return self.amap[(id(s), v)]
            return v

        engs = {"pe": "tensor", "dve": "vector", "act": "scalar", "pool": "gpsimd", "sp": "sync"}
        with nc.Block() as block:
            for e in self.ENG:
                ops = self.ops[e]
                if not ops and not (final and e == "sp"):
                    continue

                def body(engine, ops=ops, e=e):
                    for waits, fn, inc in ops:
                        for s, v in waits:
                            engine.wait_ge(s, tr(s, v))
                        if fn is not None:
                            ins = fn(engine)
                            if inc[0] == "d":
                                ins.then_inc(inc[1], 16)
                            elif (id(inc[1]), inc[2]) in self.amap:
                                ins.then_inc(inc[1], 1)
                    if final and e == "sp":
                        for s, v in finals:
                            engine.wait_ge(s, tr(s, v))

                getattr(block, engs[e])(body)
        self.nops = getattr(self, "nops", 0) + sum(len(v) for v in self.ops.values())
        self.ops = {e: [] for e in self.ENG}

    def finish(self):
        self.flush(final=True)
        self.stack.close()


class _Scope:
    def __init__(self, P):
        self.P = P

    def __enter__(self):
        self.saved = self.P.stack
        self.P.semstack = getattr(self.P, "semstack", self.saved)
        self.P.stack = ExitStack()
        return self

    def __exit__(self, *a):
        self.P.barrier()
        self.P.flush()
        self.P.stack.close()
        self.P.stack = self.saved
        return False


T = 4096
NT = 32
NTR = [32]
D = 1024
INC = 7896
EPS = 1e-6

O_AQ, O_AK, O_AV = 0, 512, 1024
O_AA, O_AB, O_AG = 1536, 1540, 1544
O_BU, O_BV, O_BG = 2056, 2568, 3080
O_CQ, O_CK, O_CV, O_CIQ, O_CIK, O_CIW, O_CG = 3592, 4104, 4168, 4232, 4744, 4808, 4816
O_DZ, O_DX, O_DDT = 5328, 5840, 6864
O_MQ, O_MG = 6872, 7384

PARAMS = [("norm_g", [2, 1024]), ("w_in", [2, 1024, INC]), ("gdn_conv_w", [2, 4, 1536]), ("gdn_a_log", [2, 4]),
          ("gdn_dt_bias", [2, 4]), ("gdn_norm_g", [2, 128]), ("sg_ln_g", [2, 512]), ("sg_ln_b", [2, 512]),
          ("sg_w", [2, 4, 128, 128]), ("sg_b", [2, 4, 128]), ("dsa_q_norm_g", [2, 64]), ("dsa_k_norm_g", [2, 64]),
          ("ssd_conv_w", [2, 4, 1024]), ("ssd_conv_b", [2, 1024]), ("ssd_a_log", [2, 8]), ("ssd_dt_bias", [2, 8]),
          ("ssd_d", [2, 8]), ("ssd_norm_g", [2, 512]), ("mem_norm_g", [2, 1024]), ("w_mem_kv", [2, 1024, 1024]),
          ("mem_q_norm_g", [2, 128]), ("mem_k_norm_g", [2, 128]), ("w_gate", [2, 5, 1024, 1024]),
          ("w_branch", [2, 5, 512, 1024]), ("w_out", [2, 1024, 1024])]


class Ctx:
    pass


STOP = [99]


def declare(nc, dbg=False, skip=()):
    C = Ctx()
    C.nc = nc
    if "x" not in skip:
        C.x = nc.dram_tensor("x", [T, D], F32, kind="ExternalInput").ap()
    C.mem = nc.dram_tensor("mem", [256, D], F32, kind="ExternalInput").ap()
    C.prm = {}
    for name, shp in PARAMS:
        if name in skip:
            continue
        C.prm[name] = nc.dram_tensor(name, shp, F32, kind="ExternalInput").ap()
    C.out = nc.dram_tensor("out", [T, D], F32, kind="ExternalOutput").ap()
    kind = "ExternalOutput" if dbg else "Internal"
    C.scr = {}

    def scr(name, shape, dt):
        C.scr[name] = nc.dram_tensor("scr_" + name, shape, dt, kind=kind).ap()

    for n in ("gq", "gk", "gv", "cq", "ciq", "mq"):
        scr(n, [512, T], BF16)
    scr("xbc", [1024, T], BF16)
    scr("ck", [64, T], BF16)
    scr("cik", [64, T], BF16)
    for n in ("ag", "bu", "bv", "bg", "cg", "dz", "mg"):
        scr(n, [T, 512], BF16)
    scr("misc", [T, 88], F32)
    scr("ysT", [5, 512, T], BF16)
    scr("x1", [T, D], F32)
    return C


def setup_consts(P, C):
    K = Ctx()
    K.ident = P.sbuf("ident", [128, 128], BF16)
    P.make_identity(K.ident)
    K.ones = P.sbuf("ones", [128, 128], BF16)
    P.memset("pool", K.ones[:], 1.0, [K.ones])
    K.blk2 = P.sbuf("blk2", [128, 128], BF16)
    P.memset("pool", K.blk2[:], 0.0, [K.blk2])
    P.memset("pool", K.blk2[0:64, 0:64], 1.0, [K.blk2])
    P.memset("pool", K.blk2[64:128, 64:128], 1.0, [K.blk2])
    K.eps = P.sbuf("epsc", [128, 1], F32)
    P.memset("pool", K.eps[:], EPS, [K.eps])
    P.barrier()
    return K


def alloc_hT(P, K):
    K.hT = P.sbuf("hT", [128, 8, T], BF16)
    K.hTb = [Buf("hT%d" % t, K.hT.ap) for t in range(NT)]


def alloc_merged(P, K):
    K.mT = P.sbuf("mT", [128, 8, T], BF16)
    K.mTb = [Buf("mT%d" % t, K.mT.ap) for t in range(8)]


def phase_norm(P, C, K, l, x_src):
    with P.scope():
        gb = P.sbuf("gb", [128, D], F32)
        P.dma("sp", gb[:], C.prm["norm_g"][l:l + 1, :].to_broadcast([128, D]), writes=[gb])
        xin = [P.sbuf("xin%d" % i, [128, D], F32) for i in range(2)]
        junk = P.sbuf("junk", [128, D], BF16)
        ss = [P.sbuf("ss%d" % i, [128, 1], F32) for i in range(2)]
        rt = [P.sbuf("rt%d" % i, [128, 1], F32) for i in range(2)]
        hb = [P.sbuf("hb%d" % i, [128, D], BF16) for i in range(2)]
        pt = [P.psum("pt%d" % i, [128, 4, 128], BF16) for i in range(2)]
        for t in range(NT):
            xi, s_, r_, h_ = xin[t % 2], ss[t % 2], rt[t % 2], hb[t % 2]
            P.dma("sp", xi[:], x_src[t * 128:(t + 1) * 128, :], writes=[xi])
            P.act(junk[:], xi[:], AF.Square, [xi], [junk, s_], accum_out=s_[:])
            P.act(r_[:], s_[:], AF.Sqrt, [s_, K.eps], [r_], scale=1.0 / D, bias=K.eps[:])
            P.recip(r_[:], r_[:], [r_], [r_])
            P.stt("dve", h_[:], xi[:], r_[:], gb[:], ALU.mult, ALU.mult, [xi, r_, gb], [h_])
            for half in range(2):
                p_ = pt[half]
                for j in range(4):
                    kc = half * 4 + j
                    P.transpose(p_[:, j, :], h_[:, kc * 128:(kc + 1) * 128], K.ident[:], [h_, K.ident], [p_])
                if half == 0:
                    P.copy("dve", K.hT[:, 0:4, t * 128:(t + 1) * 128], p_[:], [p_], [K.hTb[t]])
                else:
                    P.copy("act", K.hT[:, 4:8, t * 128:(t + 1) * 128], p_[:], [p_], [K.hTb[t]])


def phase_inproj(P, C, K, l):
    w_in = C.prm["w_in"][l]
    S = C.scr
    with P.scope():
        cwg = P.sbuf("cwg", [128, 4, 12], F32)
        cws = P.sbuf("cws", [128, 4, 8], F32)
        for k in range(4):
            P.dma("sp", cwg[:, k, :], C.prm["gdn_conv_w"][l][k].rearrange("(c p) -> p c", p=128), writes=[cwg],
                  allow_slow_non_contiguous=True)
            P.dma("sp", cws[:, k, :], C.prm["ssd_conv_w"][l][k].rearrange("(c p) -> p c", p=128), writes=[cws],
                  allow_slow_non_contiguous=True)
        cbs = P.sbuf("cbs", [128, 8], F32)
        P.dma("sp", cbs[:], C.prm["ssd_conv_b"][l].rearrange("(c p) -> p c", p=128), writes=[cbs],
              allow_slow_non_contiguous=True)
        gq2 = P.sbuf("gq2", [128, 1], F32)
        for i in range(2):
            P.dma("sp", gq2[i * 64:(i + 1) * 64, :], C.prm["dsa_q_norm_g"][l].rearrange("(p o) -> p o", o=1), writes=[gq2])
        gk1 = P.sbuf("gk1", [64, 1], F32)
        P.dma("sp", gk1[:], C.prm["dsa_k_norm_g"][l].rearrange("(p o) -> p o", o=1), writes=[gk1])
        gmq = P.sbuf("gmq", [128, 1], F32)
        P.dma("sp", gmq[:], C.prm["mem_q_norm_g"][l].rearrange("(p o) -> p o", o=1), writes=[gmq])
        P.ts("dve", gq2[:], gq2[:], 0.125, None, ALU.mult, None, [gq2], [gq2])
        P.ts("dve", gmq[:], gmq[:], 128 ** -0.5, None, ALU.mult, None, [gmq], [gmq])

        wb = [P.sbuf("wb%d" % i, [128, 8, 512], BF16) for i in range(2)]
        acc = [P.psum("acc%d" % i, [128, 512], F32) for i in range(3)]
        ssp = [P.psum("ssp%d" % i, [128, 512], F32) for i in range(2)]
        xpad = [P.sbuf("xpad%d" % i, [128, 515], F32) for i in range(4)]
        yb = [P.sbuf("yb%d" % i, [128, 512], F32) for i in range(4)]
        sb = [P.sbuf("sb%d" % i, [128, 512], F32) for i in range(2)]
        sq = [P.sbuf("sq%d" % i, [128, 512], BF16) for i in range(2)]
        rtb = [P.sbuf("rtb%d" % i, [128, 512], F32) for i in range(2)]
        ob = [P.sbuf("ob%d" % i, [128, 512], BF16) for i in range(3)]
        mo = [P.sbuf("mo%d" % i, [128, 88], F32) for i in range(2)]
        st = Ctx()
        st.g = 0
        st.a = 0
        st.e = 0
        st.o = 0

        st.loaded = {}

        def issue_load(gi, spec):
            w = wb[gi % 2]
            for (c0, n, d0) in spec:
                P.dma("pool", w[:, :, d0:d0 + n], w_in[:, c0:c0 + n].rearrange("(kc k) c -> k kc c", k=128), writes=[w])
            st.loaded[gi] = w

        def load_w(col0, ncols):
            w = st.loaded[st.g]
            st.g += 1
            return w

        def fm_group(col0, ncols, kind, dst, cw=None, cb=None, gain=None, scale=1.0):
            w = load_w(col0, ncols)
            nch = (ncols + 127) // 128
            for j in range(nch):
                m = min(128, ncols - j * 128)
                for tg in range(8):
                    a = acc[st.a % 3]
                    st.a += 1
                    for kc in range(8):
                        P.matmul(a[0:m, :], w[:, kc, j * 128:j * 128 + m], K.hT[:, kc, tg * 512:(tg + 1) * 512],
                                 kc == 0, kc == 7, [w] + K.hTb[tg * 4:tg * 4 + 4], [a])
                    e = st.e
                    st.e += 1
                    o = ob[st.o % 3]
                    st.o += 1
                    dsl = dst[j * 128:j * 128 + m, tg * 512:(tg + 1) * 512]
                    if kind == "raw":
                        P.copy("act", o[0:m, :], a[0:m, :], [a], [o])
                        P.dma("sp", dsl, o[0:m, :], reads=[o])
                        continue
                    if kind in ("conv", "conv_l2"):
                        xp, xn = xpad[tg % 4], xpad[(tg + 1) % 4]
                        y = yb[e % 4]
                        ce = "dve"
                        if tg == 0:
                            P.memset("dve", xp[:, 0:3], 0.0, [xp])
                        P.copy("act", xp[:, 3:515], a[:], [a], [xp])
                        cwb, cwo = cw
                        cj = cwo + j
                        P.ts(ce, y[:], xp[:, 0:512], cwb[:, 0, cj:cj + 1], None, ALU.mult, None, [xp, cwb], [y])
                        for k in range(1, 4):
                            P.stt(ce, y[:], xp[:, k:k + 512], cwb[:, k, cj:cj + 1], y[:], ALU.mult, ALU.add, [xp, cwb, y], [y])
                        if tg < 7:
                            P.copy("act", xn[:, 0:3], xp[:, 512:515], [xp], [xn])
                        if kind == "conv":
                            if cb is not None:
                                P.act(o[:], y[:], AF.Silu, [y, cb[0]], [o], bias=cb[0][:, cb[1] + j:cb[1] + j + 1])
                            else:
                                P.act(o[:], y[:], AF.Silu, [y], [o])
                            P.dma("sp", dsl, o[:], reads=[o])
                            continue
                        s_ = sb[e % 2]
                        P.act(s_[:], y[:], AF.Silu, [y], [s_])
                        src_ = s_
                        ones = K.ones
                        nrm_scale = 1.0
                    else:
                        s_ = sb[e % 2]
                        P.copy("act", s_[0:m, :], a[0:m, :], [a], [s_])
                        ones = K.blk2 if kind == "rms64" else K.ones
                        nrm_scale = (1.0 / 64) if kind == "rms64" else (1.0 / 128)
                    q_ = sq[e % 2]
                    P.act(q_[0:m, :], s_[0:m, :], AF.Square, [s_], [q_])
                    sp_ = ssp[e % 2]
                    P.matmul(sp_[0:m, :], ones[0:m, 0:m], q_[0:m, :], True, True, [ones, q_], [sp_])
                    r_ = rtb[e % 2]
                    P.act(r_[0:m, :], sp_[0:m, :], AF.Sqrt, [sp_, K.eps], [r_], scale=nrm_scale, bias=K.eps[0:m, :])
                    P.recip(r_[0:m, :], r_[0:m, :], [r_], [r_])
                    if gain is not None:
                        P.stt("dve", o[0:m, :], s_[0:m, :], gain[0:m, :], r_[0:m, :], ALU.mult, ALU.mult, [s_, gain, r_], [o])
                    else:
                        P.stt("dve", o[0:m, :], s_[0:m, :], scale, r_[0:m, :], ALU.mult, ALU.mult, [s_, r_], [o])
                    P.dma("sp", dsl, o[0:m, :], reads=[o])

        def tm_group(col0, func, dst):
            w = load_w(col0, 512)
            for t in range(NT):
                a = acc[st.a % 3]
                st.a += 1
                for kc in range(8):
                    P.matmul(a[:], K.hT[:, kc, t * 128:(t + 1) * 128], w[:, kc, :], kc == 0, kc == 7,
                             [w, K.hTb[t]], [a])
                o = ob[st.o % 3]
                st.o += 1
                if func is None:
                    P.copy("act", o[:], a[:], [a], [o])
                else:
                    P.act(o[:], a[:], func, [a], [o])
                P.dma("sp", dst[t * 128:(t + 1) * 128, :], o[:], reads=[o])

        def misc_group():
            w = load_w(0, 88)
            for t in range(NT):
                a = acc[st.a % 3]
                st.a += 1
                for kc in range(8):
                    P.matmul(a[:, 0:88], K.hT[:, kc, t * 128:(t + 1) * 128], w[:, kc, 0:88], kc == 0, kc == 7,
                             [w, K.hTb[t]], [a])
                o = mo[t % 2]
                P.copy("act", o[:], a[:, 0:88], [a], [o])
                P.dma("sp", S["misc"][t * 128:(t + 1) * 128, :], o[:], reads=[o])

        groups = [
            (((O_AA, 8, 0), (O_CIW, 8, 8), (O_DDT, 8, 16), (O_CV, 64, 24)), lambda: misc_group()),
            (((O_CIQ, 512, 0),), lambda: fm_group(O_CIQ, 512, "raw", S["ciq"])),
            (((O_CIK, 64, 0),), lambda: fm_group(O_CIK, 64, "raw", S["cik"])),
            (((O_CQ, 512, 0),), lambda: fm_group(O_CQ, 512, "rms64", S["cq"], gain=gq2)),
            (((O_CK, 64, 0),), lambda: fm_group(O_CK, 64, "rms64", S["ck"], gain=gk1)),
            (((O_MQ, 512, 0),), lambda: fm_group(O_MQ, 512, "rms128", S["mq"], gain=gmq)),
            (((O_AQ, 512, 0),), lambda: fm_group(O_AQ, 512, "conv_l2", S["gq"], cw=(cwg, 0), scale=128 ** -0.5)),
            (((O_AK, 512, 0),), lambda: fm_group(O_AK, 512, "conv_l2", S["gk"], cw=(cwg, 4), scale=1.0)),
            (((O_AV, 512, 0),), lambda: fm_group(O_AV, 512, "conv", S["gv"], cw=(cwg, 8))),
            (((O_DX, 512, 0),), lambda: fm_group(O_DX, 512, "conv", S["xbc"][0:512], cw=(cws, 0), cb=(cbs, 0))),
            (((O_DX + 512, 512, 0),), lambda: fm_group(O_DX + 512, 512, "conv", S["xbc"][512:1024], cw=(cws, 4), cb=(cbs, 4))),
            (((O_AG, 512, 0),), lambda: tm_group(O_AG, AF.Silu, S["ag"])),
            (((O_BG, 512, 0),), lambda: tm_group(O_BG, AF.Silu, S["bg"])),
            (((O_CG, 512, 0),), lambda: tm_group(O_CG, AF.Silu, S["cg"])),
            (((O_DZ, 512, 0),), lambda: tm_group(O_DZ, AF.Silu, S["dz"])),
            (((O_MG, 512, 0),), lambda: tm_group(O_MG, AF.Silu, S["mg"])),
            (((O_BU, 512, 0),), lambda: tm_group(O_BU, AF.Gelu, S["bu"])),
            (((O_BV, 512, 0),), lambda: tm_group(O_BV, AF.Gelu, S["bv"])),
        ]
        issue_load(0, groups[0][0])
        for gi, (spec, run) in enumerate(groups):
            if gi + 1 < len(groups):
                issue_load(gi + 1, groups[gi + 1][0])
            run()


def phase_merge(P, C, K, l):
    wgd = C.prm["w_gate"][l]
    wbd = C.prm["w_branch"][l]
    ysT = C.scr["ysT"]
    with P.scope():
        wbuf = [P.sbuf("mw%d" % i, [128, 7680], BF16) for i in range(2)]
        yT = [P.sbuf("yT%d" % i, [128, 4, 512], BF16) for i in range(3)]
        gps = [P.psum("gps%d" % i, [128, 512], F32) for i in range(2)]
        zps = [P.psum("zps%d" % i, [128, 512], F32) for i in range(2)]
        sg = [P.sbuf("sg%d" % i, [128, 512], F32) for i in range(2)]
        tmp = [P.sbuf("tmp%d" % i, [128, 512], F32) for i in range(2)]
        mac = [P.sbuf("mac%d" % i, [128, 512], F32) for i in range(2)]
        cnt = 0

        def views(w):
            return (w[:, 0:5120].rearrange("k (p kc n) -> k p kc n", p=5, kc=8),
                    w[:, 5120:7680].rearrange("k (p cc n) -> k p cc n", p=5, cc=4))

        def load(nch):
            w = wbuf[nch % 2]
            wg, wbr = views(w)
            for p in range(5):
                P.dma("pool", wg[:, p], wgd[p][:, nch * 128:(nch + 1) * 128].rearrange("(kc k) n -> k kc n", k=128), writes=[w])
                P.dma("pool", wbr[:, p], wbd[p][:, nch * 128:(nch + 1) * 128].rearrange("(cc k) n -> k cc n", k=128), writes=[w])

        load(0)
        for nch in range(8):
            w = wbuf[nch % 2]
            wg, wbr = views(w)
            if nch + 1 < 8:
                load(nch + 1)
            for tg in range(8):
                m_ = mac[tg % 2]
                for p in range(5):
                    y = yT[cnt % 3]
                    g_, z_ = gps[cnt % 2], zps[cnt % 2]
                    s_, t_ = sg[cnt % 2], tmp[cnt % 2]
                    cnt += 1
                    P.dma("sp", y[:], ysT[p][:, tg * 512:(tg + 1) * 512].rearrange("(cc c) t -> c cc t", c=128), writes=[y])
                    for kc in range(8):
                        P.matmul(g_[:], wg[:, p, kc, :], K.hT[:, kc, tg * 512:(tg + 1) * 512], kc == 0, kc == 7,
                                 [w] + K.hTb[tg * 4:tg * 4 + 4], [g_])
                    for cc in range(4):
                        P.matmul(z_[:], wbr[:, p, cc, :], y[:, cc, :], cc == 0, cc == 3, [w, y], [z_])
                    P.act(s_[:], g_[:], AF.Sigmoid, [g_], [s_])
                    if p == 0:
                        P.tt("dve", m_[:], z_[:], s_[:], ALU.mult, [z_, s_], [m_])
                    elif p < 4:
                        P.tt("dve", t_[:], z_[:], s_[:], ALU.mult, [z_, s_], [t_])
                        P.tt("pool", m_[:], m_[:], t_[:], ALU.add, [m_, t_], [m_])
                    else:
                        P.tt("dve", t_[:], z_[:], s_[:], ALU.mult, [z_, s_], [t_])
                        P.tt("pool", K.mT[:, nch, tg * 512:(tg + 1) * 512], m_[:], t_[:], ALU.add, [m_, t_], [K.mTb[tg]])


def phase_outproj(P, C, K, l, x_src, x_dst):
    with P.scope():
        wo = P.sbuf("wo", [128, 8, D], BF16)
        P.dma("pool", wo[:], C.prm["w_out"][l].rearrange("(kc k) n -> k kc n", k=128), writes=[wo])
        xin = [P.sbuf("oxin%d" % i, [128, 512], F32) for i in range(2)]
        xo = [P.sbuf("oxo%d" % i, [128, 512], F32) for i in range(2)]
        ops_ = [P.psum("ops%d" % i, [128, 512], F32) for i in range(2)]
        c = 0
        for t in range(NT):
            for hf in range(2):
                xi, o, ps = xin[c % 2], xo[c % 2], ops_[c % 2]
                c += 1
                P.dma("sp", xi[:], x_src[t * 128:(t + 1) * 128, hf * 512:(hf + 1) * 512], writes=[xi])
                for kc in range(8):
                    P.matmul(ps[:], K.mT[:, kc, t * 128:(t + 1) * 128], wo[:, kc, hf * 512:(hf + 1) * 512], kc == 0, kc == 7,
                             [wo, K.mTb[t // 4]], [ps])
                P.tt("dve", o[:], ps[:], xi[:], ALU.add, [ps, xi], [o])
                P.dma("sp", x_dst[t * 128:(t + 1) * 128, hf * 512:(hf + 1) * 512], o[:], reads=[o])


class YOut:
    def __init__(self, P, C, K, p, tag, nps=1, ps=None):
        self.P, self.C, self.K, self.p = P, C, K, p
        self.ps = ps if ps is not None else [P.psum("yo_ps%s%d" % (tag, i), [128, 4, 128], BF16) for i in range(nps)]
        self.sb = [P.sbuf("yo_sb%s%d" % (tag, i), [128, 4, 128], BF16) for i in range(2)]
        self.n = 0

    def emit(self, y, t, eng="act"):
        P, K = self.P, self.K
        ps, sb = self.ps[self.n % len(self.ps)], self.sb[self.n % 2]
        self.n += 1
        for cc in range(4):
            P.transpose(ps[:, cc, :], y[:, cc * 128:(cc + 1) * 128], K.ident[:], [y, K.ident], [ps])
        P.copy(eng, sb[:], ps[:], [ps], [sb])
        P.dma("sp", self.C.scr["ysT"][self.p][:, t * 128:(t + 1) * 128].rearrange("(cc c) t -> c cc t", c=128), sb[:], reads=[sb])


def phase_mem(P, C, K, l):
    S = C.scr
    with P.scope():
        gb = P.sbuf("m_gb", [128, D], F32)
        P.dma("sp", gb[:], C.prm["mem_norm_g"][l:l + 1, :].to_broadcast([128, D]), writes=[gb])
        gk = P.sbuf("m_gk", [128, 1], F32)
        P.dma("sp", gk[:], C.prm["mem_k_norm_g"][l].rearrange("(p o) -> p o", o=1), writes=[gk])
        wkv = P.sbuf("m_wkv", [128, 8, D], BF16)
        P.dma("pool", wkv[:], C.prm["w_mem_kv"][l].rearrange("(kc k) n -> k kc n", k=128), writes=[wkv])
        memT = P.sbuf("memT", [128, 8, 256], BF16)
        kT = P.sbuf("m_kT", [128, 4, 256], BF16)
        vaug = [P.sbuf("m_va%d" % i, [128, 4, 129], BF16) for i in range(2)]
        xin = P.sbuf("m_x", [128, D], F32)
        junk = P.sbuf("m_junk", [128, D], BF16)
        ss = P.sbuf("m_ss", [128, 1], F32)
        hb = P.sbuf("m_hb", [128, D], BF16)
        pt = P.psum("m_pt", [128, 4, 128], BF16)
        pa = P.psum("m_pa", [128, 512], F32)
        pb = P.psum("m_pb", [128, 512], F32)
        for mt in range(2):
            P.dma("sp", xin[:], C.mem[mt * 128:(mt + 1) * 128, :], writes=[xin])
            P.act(junk[:], xin[:], AF.Square, [xin], [junk, ss], accum_out=ss[:])
            P.act(ss[:], ss[:], AF.Sqrt, [ss, K.eps], [ss], scale=1.0 / D, bias=K.eps[:])
            P.recip(ss[:], ss[:], [ss], [ss])
            P.stt("dve", hb[:], xin[:], ss[:], gb[:], ALU.mult, ALU.mult, [xin, ss, gb], [hb])
            for half in range(2):
                for j in range(4):
                    kc = half * 4 + j
                    P.transpose(pt[:, j, :], hb[:, kc * 128:(kc + 1) * 128], K.ident[:], [hb, K.ident], [pt])
                P.copy("dve", memT[:, half * 4:half * 4 + 4, mt * 128:(mt + 1) * 128], pt[:], [pt], [memT])
        sq = P.sbuf("m_sq", [128, 256], BF16)
        kf = P.sbuf("m_kf", [128, 256], F32)
        rr = P.sbuf("m_rr", [128, 256], F32)
        for h in range(4):
            for kc in range(8):
                P.matmul(pa[:, 0:256], wkv[:, kc, h * 128:(h + 1) * 128], memT[:, kc, :], kc == 0, kc == 7, [wkv, memT], [pa])
            P.copy("act", kf[:], pa[:, 0:256], [pa], [kf])
            P.act(sq[:], kf[:], AF.Square, [kf], [sq])
            P.matmul(pb[:, 0:256], K.ones[:], sq[:], True, True, [K.ones, sq], [pb])
            P.act(rr[:], pb[:, 0:256], AF.Sqrt, [pb, K.eps], [rr], scale=1.0 / 128, bias=K.eps[:])
            P.recip(rr[:], rr[:], [rr], [rr])
            P.stt("dve", kT[:, h, :], kf[:], gk[:], rr[:], ALU.mult, ALU.mult, [kf, gk, rr], [kT])
        for mt in range(2):
            for kc in range(8):
                P.matmul(pa[:], memT[:, kc, mt * 128:(mt + 1) * 128], wkv[:, kc, 512:1024], kc == 0, kc == 7, [wkv, memT], [pa])
            P.memset("pool", vaug[mt][:, :, 128:129], 1.0, [vaug[mt]])
            P.copy("act", vaug[mt][:, :, 0:128], pa[:].rearrange("m (h d) -> m h d", h=4), [pa], [vaug[mt]])
        qT = [P.sbuf("m_qT%d" % i, [128, 4, 128], BF16) for i in range(2)]
        gt = [P.sbuf("m_gt%d" % i, [128, 512], BF16) for i in range(2)]
        lg = [P.psum("m_lg%d" % i, [128, 4, 128], F32) for i in range(2)]
        pT = [P.sbuf("m_pT%d" % i, [128, 4, 128], BF16) for i in range(2)]
        po = P.psum("m_po", [128, 4, 256], F32)
        rd = [P.sbuf("m_rd%d" % i, [128, 4], F32) for i in range(2)]
        yb = [P.sbuf("m_y%d" % i, [128, 512], BF16) for i in range(2)]
        yo = YOut(P, C, K, 4, "m")
        for t in range(NTR[0]):
            q, g = qT[t % 2], gt[t % 2]
            P.dma("sp", q[:], S["mq"][:, t * 128:(t + 1) * 128].rearrange("(h d) t -> d h t", d=128), writes=[q])
            P.dma("sp", g[:], S["mg"][t * 128:(t + 1) * 128, :], writes=[g])
            for mt in range(2):
                for h in range(4):
                    P.matmul(lg[mt][:, h, :], kT[:, h, mt * 128:(mt + 1) * 128], q[:, h, :], True, True, [kT, q], [lg[mt]])
                P.act(pT[mt][:], lg[mt][:], AF.Exp, [lg[mt]], [pT[mt]])
            for h in range(4):
                for mt in range(2):
                    P.matmul(po[:, h, 0:129], pT[mt][:, h, :], vaug[mt][:, h, :], mt == 0, mt == 1, [pT[mt], vaug[mt]], [po])
            r_ = rd[t % 2]
            y = yb[t % 2]
            P.recip(r_[:], po[:, :, 128], [po], [r_])
            for h in range(4):
                P.stt("dve", y[:, h * 128:(h + 1) * 128], po[:, h, 0:128], r_[:, h:h + 1], g[:, h * 128:(h + 1) * 128],
                      ALU.mult, ALU.mult, [po, r_, g], [y])
            yo.emit(y, t)


def phase_sg(P, C, K, l):
    S = C.scr
    with P.scope():
        lng = P.sbuf("b_lng", [128, 512], F32)
        lnb = P.sbuf("b_lnb", [128, 512], F32)
        P.dma("sp", lng[:], C.prm["sg_ln_g"][l:l + 1, :].to_broadcast([128, 512]), writes=[lng])
        P.dma("sp", lnb[:], C.prm["sg_ln_b"][l:l + 1, :].to_broadcast([128, 512]), writes=[lnb])
        bsT = P.sbuf("b_bsT", [128, 4], F32)
        P.dma("sp", bsT[:], C.prm["sg_b"][l].rearrange("g t -> t g"), writes=[bsT], allow_slow_non_contiguous=True)
        eps5 = P.sbuf("b_eps5", [128, 1], F32)
        P.memset("pool", eps5[:], 1e-5, [eps5])
        wf = P.sbuf("b_wf", [128, 4, 128], F32)
        wbf = P.sbuf("b_wbf", [128, 4, 128], BF16)
        WcT = P.sbuf("b_WcT", [128, 4, 128], BF16)
        pt = P.psum("b_pt", [128, 4, 128], BF16)
        P.dma("sp", wf[:], C.prm["sg_w"][l].rearrange("g t s -> t g s"), writes=[wf])
        for g in range(4):
            P.op("pool", lambda e, g=g: e.affine_select(out=wf[:, g, :], in_=wf[:, g, :], pattern=[[-1, 128]], compare_op=ALU.is_ge,
                                                        fill=0.0, base=0, channel_multiplier=1), [wf], [wf])
        P.barrier()
        P.copy("dve", wbf[:], wf[:], [wf], [wbf])
        for g in range(4):
            P.transpose(pt[:, g, :], wbf[:, g, :], K.ident[:], [wbf, K.ident], [pt])
        P.copy("dve", WcT[:], pt[:], [pt], [WcT])

        vin = [P.sbuf("b_v%d" % i, [128, 512], BF16) for i in range(2)]
        uin = [P.sbuf("b_u%d" % i, [128, 512], BF16) for i in range(2)]
        gin = [P.sbuf("b_g%d" % i, [128, 512], BF16) for i in range(2)]
        st6 = [P.sbuf("b_st%d" % i, [128, 6], F32) for i in range(2)]
        mv = [P.sbuf("b_mv%d" % i, [128, 2], F32) for i in range(2)]
        rs = [P.sbuf("b_rs%d" % i, [128, 1], F32) for i in range(2)]
        vn = [P.sbuf("b_vn%d" % i, [128, 512], F32) for i in range(2)]
        vnb = [P.sbuf("b_vnb%d" % i, [128, 512], BF16) for i in range(2)]
        mx = [P.psum("b_mx%d" % i, [128, 512], F32) for i in range(2)]
        tm = [P.sbuf("b_tm%d" % i, [128, 512], F32) for i in range(2)]
        yb = [P.sbuf("b_y%d" % i, [128, 512], BF16) for i in range(2)]
        yo = YOut(P, C, K, 1, "b")
        for t in range(NTR[0]):
            i = t % 2
            v, u, g = vin[i], uin[i], gin[i]
            sl = slice(t * 128, (t + 1) * 128)
            P.dma("sp", v[:], S["bv"][sl, :], writes=[v])
            P.dma("sp", u[:], S["bu"][sl, :], writes=[u])
            P.dma("sp", g[:], S["bg"][sl, :], writes=[g])
            P.op("dve", lambda e, i=i: e.bn_stats(st6[i][:], vin[i][:]), [v], [st6[i]])
            P.op("dve", lambda e, i=i: e.bn_aggr(mv[i][:], st6[i][:]), [st6[i]], [mv[i]])
            P.act(rs[i][:], mv[i][:, 1:2], AF.Sqrt, [mv[i], eps5], [rs[i]], bias=eps5[:])
            P.recip(rs[i][:], rs[i][:], [rs[i]], [rs[i]])
            P.ts("dve", vn[i][:], v[:], mv[i][:, 0:1], rs[i][:], ALU.subtract, ALU.mult, [v, mv[i], rs[i]], [vn[i]])
            P.tt("pool", vn[i][:], vn[i][:], lng[:], ALU.mult, [vn[i], lng], [vn[i]])
            P.tt("pool", vnb[i][:], vn[i][:], lnb[:], ALU.add, [vn[i], lnb], [vnb[i]])
            for gg in range(4):
                P.matmul(mx[i][:, gg * 128:(gg + 1) * 128], WcT[:, gg, :], vnb[i][:, gg * 128:(gg + 1) * 128], True, True,
                         [WcT, vnb[i]], [mx[i]])
            for gg in range(4):
                c = slice(gg * 128, (gg + 1) * 128)
                P.stt("dve", tm[i][:, c], mx[i][:, c], bsT[:, gg:gg + 1], u[:, c], ALU.add, ALU.mult, [mx[i], bsT, u], [tm[i]])
            P.tt("pool", yb[i][:], tm[i][:], g[:], ALU.mult, [tm[i], g], [yb[i]])
            yo.emit(yb[i], t)


def bc(ap, shape):
    return ap.to_broadcast(list(shape))


def phase_ssd(P, C, K, l):
    S = C.scr
    with P.scope():
        uinc = P.sbuf("d_uinc", [128, 128], F32)
        P.memset("pool", uinc[:], 1.0, [uinc])
        P.op("pool", lambda e: e.affine_select(out=uinc[:], in_=uinc[:], pattern=[[1, 128]], compare_op=ALU.is_ge,
                                               fill=0.0, base=0, channel_multiplier=-1), [uinc], [uinc])
        onesf = P.sbuf("d_onesf", [128, 128], F32)
        P.memset("pool", onesf[:], 1.0, [onesf])
        P.barrier()
        aB = P.sbuf("d_aB", [128, 8], F32)
        P.dma("sp", aB[:], C.prm["ssd_a_log"][l:l + 1, :].to_broadcast([128, 8]), writes=[aB])
        P.act(aB[:], aB[:], AF.Exp, [aB], [aB])
        P.ts("dve", aB[:], aB[:], -1.0, None, ALU.mult, None, [aB], [aB])
        dtb = P.sbuf("d_dtb", [128, 8], F32)
        P.dma("sp", dtb[:], C.prm["ssd_dt_bias"][l:l + 1, :].to_broadcast([128, 8]), writes=[dtb])
        dsk = P.sbuf("d_dsk", [128, 8], F32)
        P.dma("sp", dsk[:], C.prm["ssd_d"][l:l + 1, :].to_broadcast([128, 8]), writes=[dsk])
        ngB = P.sbuf("d_ngB", [128, 512], F32)
        P.dma("sp", ngB[:], C.prm["ssd_norm_g"][l:l + 1, :].to_broadcast([128, 512]), writes=[ngB])
        S32 = P.sbuf("d_S32", [128, 512], F32)
        Sbf = P.sbuf("d_Sbf", [128, 512], BF16)
        P.memset("pool", S32[:], 0.0, [S32])
        P.memset("pool", Sbf[:], 0.0, [Sbf])

        xf = [P.sbuf("d_xf%d" % i, [128, 8, 128], BF16) for i in range(2)]
        dtin = [P.sbuf("d_dtin%d" % i, [128, 8], F32) for i in range(2)]
        zg = [P.sbuf("d_zg%d" % i, [128, 512], BF16) for i in range(2)]
        dt = P.sbuf("d_dt", [128, 8], F32)
        la = P.sbuf("d_la", [128, 8], F32)
        lab = P.sbuf("d_lab", [128, 8, 128], F32)
        cs = P.sbuf("d_cs", [128, 16], F32)
        pre = P.sbuf("d_pre", [128, 24], F32)
        E = P.sbuf("d_E", [128, 24], F32)
        seg = P.sbuf("d_seg", [128, 8, 128], F32)
        Lm = P.sbuf("d_L", [128, 8, 128], F32)
        MT = P.sbuf("d_MT", [128, 8, 128], BF16)
        cbm = P.sbuf("d_cbm", [128, 2, 128], F32)
        xs = P.sbuf("d_xs", [128, 512], BF16)
        btm = P.sbuf("d_btm", [128, 2, 128], BF16)
        xdt = P.sbuf("d_xdt", [128, 512], BF16)
        xdtd = P.sbuf("d_xdtd", [128, 512], BF16)
        t1 = P.sbuf("d_t1", [128, 512], F32)
        y1 = P.sbuf("d_y1", [128, 512], F32)
        y2 = P.sbuf("d_y2", [128, 512], F32)
        junk = P.sbuf("d_junk", [128, 512], BF16)
        ssq = P.sbuf("d_ssq", [128, 1], F32)
        yb = [P.sbuf("d_yb%d" % i, [128, 512], BF16) for i in range(2)]

        small = P.psum("d_small", [128, 512], F32)
        csps = View(small, small[:, 0:16])
        cbps = View(small, small[:, 128:384])
        csrow = P.psum("d_csrow", [128, 8, 128], F32)
        tp = P.psum("d_tp", [128, 768], BF16)
        yps = P.psum("d_yps", [128, 512], F32)
        yoff = P.psum("d_yoff", [128, 512], F32)
        stp = P.psum("d_stp", [128, 512], F32)
        yo = YOut(P, C, K, 3, "d")

        for t in range(NTR[0]):
            i = t % 2
            sl = slice(t * 128, (t + 1) * 128)
            x_ = xf[i]
            P.dma("sp", x_[:], S["xbc"][:, sl].rearrange("(c p) t -> p c t", p=128), writes=[x_])
            P.dma("sp", dtin[i][:], S["misc"][sl, 16:24], writes=[dtin[i]])
            P.dma("sp", zg[i][:], S["dz"][sl, :], writes=[zg[i]])
            P.tt("dve", dt[:], dtin[i][:], dtb[:], ALU.add, [dtin[i], dtb], [dt])
            P.act(dt[:], dt[:], AF.Exp, [dt], [dt])
            P.act(dt[:], dt[:], AF.Ln, [dt], [dt], bias=1.0)
            P.tt("dve", la[:], dt[:], aB[:], ALU.mult, [dt, aB], [la])
            if STOP[0] <= 1:
                continue
            P.matmul(csps[:, 0:8], uinc[:], la[:], True, True, [uinc, la], [csps])
            P.matmul(csps[:, 8:16], onesf[:], la[:], True, True, [onesf, la], [csps])
            P.copy("dve", cs[:], csps[:], [csps], [cs])
            if STOP[0] <= 2:
                continue
            P.copy("dve", lab[:], bc(la[:].unsqueeze(2), [128, 8, 128]), [la], [lab])
            for h in range(8):
                P.matmul(csrow[:, h, :], lab[:, h, :], uinc[:], True, True, [lab, uinc], [csrow])
            if STOP[0] <= 3:
                continue
            P.tt("dve", pre[:, 0:8], cs[:, 8:16], cs[:, 0:8], ALU.subtract, [cs], [pre])
            P.copy("dve", pre[:, 8:16], cs[:, 8:16], [cs], [pre])
            P.copy("dve", pre[:, 16:24], cs[:, 0:8], [cs], [pre])
            P.act(E[:], pre[:], AF.Exp, [pre], [E])
            if STOP[0] <= 4:
                continue
            P.tt("dve", seg[:], csrow[:], bc(cs[:, 0:8].unsqueeze(2), [128, 8, 128]), ALU.subtract, [csrow, cs], [seg])
            P.ts("dve", seg[:], seg[:], 0.0, None, ALU.min, None, [seg], [seg])
            P.act(Lm[:], seg[:], AF.Exp, [seg], [Lm])
            if STOP[0] <= 5:
                continue
            for g in range(2):
                P.matmul(cbps[:, g * 128:(g + 1) * 128], x_[:, 4 + g, :], x_[:, 6 + g, :], True, True, [x_], [cbps])
            P.tt("dve", cbm[:], cbps[:].rearrange("s (g c) -> s g c", g=2), bc(uinc[:].unsqueeze(1), [128, 2, 128]), ALU.mult,
                 [cbps, uinc], [cbm])
            for g in range(2):
                P.tt("dve", MT[:, g * 4:(g + 1) * 4, :], Lm[:, g * 4:(g + 1) * 4, :], bc(cbm[:, g:g + 1, :], [128, 4, 128]), ALU.mult,
                     [Lm, cbm], [MT])
            if STOP[0] <= 6:
                continue
            for c in range(4):
                P.transpose(tp[:, c * 128:(c + 1) * 128], x_[:, c, :], K.ident[:], [x_, K.ident], [tp])
            for g in range(2):
                P.transpose(tp[:, 512 + g * 128:512 + (g + 1) * 128], x_[:, 4 + g, :], K.ident[:], [x_, K.ident], [tp])
            P.copy("act", xs[:], tp[:, 0:512], [tp], [xs])
            P.copy("act", btm[:], tp[:, 512:768].rearrange("s (g d) -> s g d", g=2), [tp], [btm])
            xs3 = xs[:].rearrange("s (h p) -> s h p", h=8)
            P.tt("dve", xdt[:].rearrange("s (h p) -> s h p", h=8), xs3, bc(dt[:].unsqueeze(2), [128, 8, 64]), ALU.mult, [xs, dt], [xdt])
            P.tt("dve", xdtd[:].rearrange("s (h p) -> s h p", h=8), xdt[:].rearrange("s (h p) -> s h p", h=8),
                 bc(E[:, 0:8].unsqueeze(2), [128, 8, 64]), ALU.mult, [xdt, E], [xdtd])
            if STOP[0] <= 7:
                continue
            for h in range(8):
                P.matmul(yps[:, h * 64:(h + 1) * 64], MT[:, h, :], xdt[:, h * 64:(h + 1) * 64], True, True, [MT, xdt], [yps])
            for g in range(2):
                P.matmul(yoff[:, g * 256:(g + 1) * 256], x_[:, 6 + g, :], Sbf[:, g * 256:(g + 1) * 256], True, True, [x_, Sbf], [yoff])
            P.tt("dve", t1[:].rearrange("s (h p) -> s h p", h=8), yoff[:].rearrange("s (h p) -> s h p", h=8),
                 bc(E[:, 16:24].unsqueeze(2), [128, 8, 64]), ALU.mult, [yoff, E], [t1])
            P.tt("dve", y1[:], yps[:], t1[:], ALU.add, [yps, t1], [y1])
            P.tt("pool", t1[:].rearrange("s (h p) -> s h p", h=8), xs3, bc(dsk[:].unsqueeze(2), [128, 8, 64]), ALU.mult, [xs, dsk], [t1])
            P.tt("pool", y1[:], y1[:], t1[:], ALU.add, [y1, t1], [y1])
            P.tt("pool", y2[:], y1[:], zg[i][:], ALU.mult, [y1, zg[i]], [y2])
            P.act(junk[:], y2[:], AF.Square, [y2], [junk, ssq], accum_out=ssq[:])
            P.act(ssq[:], ssq[:], AF.Sqrt, [ssq, K.eps], [ssq], scale=1.0 / 512, bias=K.eps[:])
            P.recip(ssq[:], ssq[:], [ssq], [ssq])
            P.stt("dve", yb[i][:], y2[:], ssq[:], ngB[:], ALU.mult, ALU.mult, [y2, ssq, ngB], [yb[i]])
            yo.emit(yb[i], t)
            if STOP[0] <= 8:
                continue
            for g in range(2):
                P.matmul(stp[:, g * 256:(g + 1) * 256], btm[:, g, :], xdtd[:, g * 256:(g + 1) * 256], True, True, [btm, xdtd], [stp])
            P.tt("dve", S32[:].rearrange("d (h p) -> d h p", h=8), S32[:].rearrange("d (h p) -> d h p", h=8),
                 bc(E[:, 8:16].unsqueeze(2), [128, 8, 64]), ALU.mult, [S32, E], [S32])
            P.tt("dve", S32[:], S32[:], stp[:], ALU.add, [S32, stp], [S32])
            P.copy("act", Sbf[:], S32[:], [S32], [Sbf])


NBIS = 18


def phase_dsa(P, C, K, l):
    S = C.scr
    I32 = mybir.dt.int32
    with P.scope():
        rb = [P.sbuf("c_rb%d" % i, [1, T], BF16) for i in range(3)]
        P.op("pool", lambda e: e.iota(rb[0][:], pattern=[[0, NT], [1, 128]], base=0, channel_multiplier=0,
                                      allow_small_or_imprecise_dtypes=True), (), [rb[0]])
        P.op("pool", lambda e: e.iota(rb[1][:], pattern=[[1, NT], [0, 128]], base=0, channel_multiplier=0,
                                      allow_small_or_imprecise_dtypes=True), (), [rb[1]])
        P.memset("pool", rb[2][:], 1.0, [rb[2]])
        posrow = P.sbuf("c_posrow", [128, T], F32)
        P.op("pool", lambda e: e.iota(posrow[:], pattern=[[1, T]], base=0, channel_multiplier=0,
                                      allow_small_or_imprecise_dtypes=True), (), [posrow])
        sl1 = P.sbuf("c_sl1", [1, 8, 128], BF16)
        sl128 = P.sbuf("c_sl128", [1, 8, 128], BF16)
        nsl64 = P.sbuf("c_nsl64", [1, 8, 128], F32)
        nsl1 = P.sbuf("c_nsl1", [1, 8, 128], F32)
        for h in range(8):
            sl = 2.0 ** -(h + 1)
            P.memset("pool", sl1[:, h, :], sl, [sl1])
            P.memset("pool", sl128[:, h, :], 128 * sl, [sl128])
            P.memset("pool", nsl64[:, h, :], -64 * sl, [nsl64])
            P.memset("pool", nsl1[:, h, :], -sl, [nsl1])
        idf = P.sbuf("c_idf", [128, 128], F32)
        P.make_identity(idf)
        clow = P.sbuf("c_clow", [128, 128], BF16)
        P.memset("pool", clow[:], 1.0, [clow])
        P.op("pool", lambda e: e.affine_select(out=clow[:], in_=clow[:], pattern=[[-1, 128]], compare_op=ALU.is_ge,
                                               fill=0.0, base=0, channel_multiplier=1), [clow], [clow])
        cneg = P.sbuf("c_cneg", [128, 128], F32)
        P.memset("pool", cneg[:], 0.0, [cneg])
        P.op("pool", lambda e: e.affine_select(out=cneg[:], in_=cneg[:], pattern=[[-1, 128]], compare_op=ALU.is_ge,
                                               fill=-1e30, base=0, channel_multiplier=1), [cneg], [cneg])
        pw = P.sbuf("c_pw", [128, NBIS], F32)
        for k in range(NBIS):
            P.memset("pool", pw[:, k:k + 1], 2.0 ** -(k + 1), [pw])
        vaug = P.sbuf("c_vaug", [128, NT, 65], BF16)
        P.memset("pool", vaug[:, :, 64:65], 1.0, [vaug])
        P.barrier()
        kTa = P.sbuf("c_kTa", [68, T], BF16)
        ikT = P.sbuf("c_ikT", [64, T], BF16)
        P.dma("sp", kTa[0:64, :], S["ck"], writes=[kTa])
        P.dma("sp", ikT[:], S["cik"], writes=[ikT])
        P.dma("sp", kTa[64:65, :], rb[0][:], reads=[rb[0]], writes=[kTa])
        P.dma("sp", kTa[65:66, :], rb[1][:], reads=[rb[1]], writes=[kTa])
        P.dma("sp", kTa[66:67, :], rb[2][:], reads=[rb[2]], writes=[kTa])
        P.dma("sp", kTa[67:68, :], rb[2][:], reads=[rb[2]], writes=[kTa])
        for s4 in range(0, NT, 4):
            P.dma("pool", vaug[:, s4:s4 + 4, 0:64], S["misc"][s4 * 128:(s4 + 4) * 128, 24:88].rearrange("(st s) d -> s st d", s=128),
                  writes=[vaug])

        iqT = [P.sbuf("c_iqT%d" % i, [64, 8, 128], BF16) for i in range(2)]
        qTa = [P.sbuf("c_qTa%d" % i, [68, 8, 128], BF16) for i in range(2)]
        for i in range(2):
            P.dma("sp", qTa[i][64:65], sl1[:], reads=[sl1], writes=[qTa[i]])
            P.dma("sp", qTa[i][65:66], sl128[:], reads=[sl128], writes=[qTa[i]])
        iw = [P.sbuf("c_iw%d" % i, [128, 8], F32) for i in range(2)]
        gt = [P.sbuf("c_gt%d" % i, [128, 512], BF16) for i in range(2)]
        iwa = P.sbuf("c_iwa", [128, 8], F32)
        iws = P.sbuf("c_iws", [128, 8], F32)
        sidx = P.sbuf("c_sidx", [128, T], F32)
        junk = P.sbuf("c_junk", [128, T], F32)
        M01 = P.sbuf("c_M01", [128, T], BF16)
        M01T = P.sbuf("c_M01T", [128, NT, 128], BF16)
        rl = [P.sbuf("c_rl%d" % i, [128, 512], F32) for i in range(4)]
        ex = [P.sbuf("c_ex%d" % i, [128, 8, 128], BF16) for i in range(3)]
        pT = [P.sbuf("c_pT%d" % i, [128, 8, 128], BF16) for i in range(3)]
        mn = P.sbuf("c_mn", [128, 1], F32)
        mxv = P.sbuf("c_mx", [128, 1], F32)
        W = P.sbuf("c_W", [128, NBIS], F32)
        lo = P.sbuf("c_lo", [128, 1], F32)
        mid = P.sbuf("c_mid", [128, 1], F32)
        cnt = P.sbuf("c_cnt", [128, 1], F32)
        dd = P.sbuf("c_dd", [128, 1], F32)
        smax = P.sbuf("c_smax", [128, 1], F32)
        smaxb = P.sbuf("c_smaxb", [128, 32], F32)
        srow_i = P.sbuf("c_srow_i", [1, 128], I32)
        nhi_i = P.sbuf("c_nhi_i", [1, 128], I32)
        nlo_i = P.sbuf("c_nlo_i", [1, 128], I32)
        nhi_f = P.sbuf("c_nhi_f", [1, 128], F32)
        nlo_f = P.sbuf("c_nlo_f", [1, 128], F32)
        r2 = [P.sbuf("c_r2%d" % i, [1, 8, 128], BF16) for i in range(2)]
        r3 = [P.sbuf("c_r3%d" % i, [1, 8, 128], BF16) for i in range(2)]
        rden = P.sbuf("c_rden", [128, 8], F32)
        ot = P.sbuf("c_ot", [128, 512], F32)
        yb = [P.sbuf("c_yb%d" % i, [128, 512], BF16) for i in range(2)]

        sps = [P.psum("c_sps%d" % i, [128, 512], F32) for i in range(2)]
        lgh = [P.psum("c_lg%d" % i, [128, 4, 128], F32) for i in range(3)]
        ops_ = P.psum("c_ops", [128, 8, 128], F32)
        mty = P.psum("c_mty", [128, 8, 128], BF16)
        mtp = View(mty, mty[:, 0:4, :])
        yo = YOut(P, C, K, 2, "c", ps=[View(mty, mty[:, 4:8, :])])
        IWS = (8 ** -0.5) * (64 ** -0.5)
        nsp = 0
        for t in range(NTR[0]):
            i = t % 2
            sl = slice(t * 128, (t + 1) * 128)
            nk = t + 1
            Lk = nk * 128
            P.dma("sp", iqT[i][:], S["ciq"][:, sl].rearrange("(h d) t -> d h t", d=64), writes=[iqT[i]])
            P.dma("sp", qTa[i][0:64], S["cq"][:, sl].rearrange("(h d) t -> d h t", d=64), writes=[qTa[i]])
            P.dma("sp", iw[i][:], S["misc"][sl, 8:16], writes=[iw[i]])
            P.dma("sp", gt[i][:], S["cg"][sl, :], writes=[gt[i]])
            if STOP[0] <= 1:
                continue
            if t >= 2:
                P.act(iwa[:], iw[i][:], AF.Abs, [iw[i]], [iwa], scale=IWS)
                P.act(iws[:], iw[i][:], AF.Sign, [iw[i]], [iws])
                for c0 in range(0, Lk, 512):
                    w_ = min(512, Lk - c0)
                    for h in range(8):
                        sp_ = sps[nsp % 2]
                        r_ = rl[nsp % 4]
                        nsp += 1
                        P.matmul(sp_[:, 0:w_], iqT[i][:, h, :], ikT[:, c0:c0 + w_], True, True, [iqT[i], ikT], [sp_])
                        P.act(r_[:, 0:w_], sp_[:, 0:w_], AF.Relu, [sp_, iwa], [r_], scale=iwa[:, h:h + 1])
                        if h == 0:
                            P.ts("dve", sidx[:, c0:c0 + w_], r_[:, 0:w_], iws[:, 0:1], None, ALU.mult, None, [r_, iws], [sidx])
                        else:
                            P.stt("dve", sidx[:, c0:c0 + w_], r_[:, 0:w_], iws[:, h:h + 1], sidx[:, c0:c0 + w_], ALU.mult, ALU.add,
                                  [r_, iws, sidx], [sidx])
                P.op("dve", lambda e, Lk=Lk: e.tensor_reduce(mn[:], sidx[:, 0:Lk], AX.X, ALU.min), [sidx], [mn])
                P.op("dve", lambda e, Lk=Lk: e.tensor_reduce(mxv[:], sidx[:, 0:Lk], AX.X, ALU.max), [sidx], [mxv])
                P.tt("dve", sidx[:, t * 128:Lk], sidx[:, t * 128:Lk], cneg[:], ALU.add, [sidx, cneg], [sidx])
                P.tt("dve", mxv[:], mxv[:], mn[:], ALU.subtract, [mxv, mn], [mxv])
                P.ts("dve", W[:], pw[:], mxv[:], None, ALU.mult, None, [pw, mxv], [W])
                P.tt("dve", mid[:], mn[:], W[:, 0:1], ALU.add, [mn, W], [mid])
                for k in range(NBIS):
                    P.ts("dve", junk[:, 0:Lk], sidx[:, 0:Lk], mid[:], None, ALU.is_ge, ALU.add, [sidx, mid], [junk, cnt],
                         accum_out=cnt[:])
                    P.ts("dve", dd[:], cnt[:], 255.5, 0.5, ALU.is_ge, ALU.subtract, [cnt], [dd])
                    if k < NBIS - 1:
                        P.stt("dve", mid[:], dd[:], W[:, k:k + 1], mid[:], ALU.mult, ALU.add, [dd, W, mid], [mid])
                    else:
                        P.ts("dve", dd[:], dd[:], 0.5, None, ALU.subtract, None, [dd], [dd])
                        P.stt("dve", lo[:], dd[:], W[:, k:k + 1], mid[:], ALU.mult, ALU.add, [dd, W, mid], [lo])
                P.ts("dve", M01[:, 0:Lk], sidx[:, 0:Lk], lo[:], None, ALU.is_ge, None, [sidx, lo], [M01])
            else:
                if t > 0:
                    P.memset("pool", M01[:, 0:t * 128], 1.0, [M01])
                P.copy("pool", M01[:, t * 128:Lk], clow[:], [clow], [M01])
            if STOP[0] <= 2:
                continue
            P.tt("dve", junk[:, 0:Lk], M01[:, 0:Lk], posrow[:, 0:Lk], ALU.mult, [M01, posrow], [junk])
            P.op("dve", lambda e, Lk=Lk: e.tensor_reduce(smax[:], junk[:, 0:Lk], AX.X, ALU.max), [junk], [smax])
            srp = sps[nsp % 2]
            nsp += 1
            P.copy("dve", smaxb[:], bc(smax[:], [128, 32]), [smax], [smaxb])
            P.matmul(srp[0:32, 0:128], smaxb[:], idf[:], True, True, [smaxb, idf], [srp])
            if STOP[0] <= 2.5:
                continue
            P.copy("dve", srow_i[:], srp[0:1, 0:128], [srp], [srow_i])
            P.ts("dve", nhi_i[:], srow_i[:], 6, None, ALU.arith_shift_right, None, [srow_i], [nhi_i])
            P.ts("dve", nlo_i[:], srow_i[:], 63, None, ALU.bitwise_and, None, [srow_i], [nlo_i])
            P.copy("dve", nhi_f[:], nhi_i[:], [nhi_i], [nhi_f])
            P.copy("dve", nlo_f[:], nlo_i[:], [nlo_i], [nlo_f])
            P.tt("dve", r2[i][:], nsl64[:], bc(nhi_f[:].unsqueeze(1), [1, 8, 128]), ALU.mult, [nsl64, nhi_f], [r2[i]])
            P.tt("dve", r3[i][:], nsl1[:], bc(nlo_f[:].unsqueeze(1), [1, 8, 128]), ALU.mult, [nsl1, nlo_f], [r3[i]])
            if STOP[0] <= 2.7:
                continue
            P.dma("sp", qTa[i][66:67], r2[i][:], reads=[r2[i]], writes=[qTa[i]])
            P.dma("sp", qTa[i][67:68], r3[i][:], reads=[r3[i]], writes=[qTa[i]])
            if STOP[0] <= 3:
                continue
            for k0 in range(0, nk, 4):
                n4 = min(4, nk - k0)
                for j in range(n4):
                    P.transpose(mtp[:, j, :], M01[:, (k0 + j) * 128:(k0 + j + 1) * 128], K.ident[:], [M01, K.ident], [mtp])
                P.copy("act", M01T[:, k0:k0 + n4, :], mtp[:, 0:n4, :], [mtp], [M01T])
            if STOP[0] <= 4:
                continue
            def qk(kt_, half):
                lb = lgh[(2 * kt_ + half) % 3]
                P.matmul(lb[:], kTa[:, kt_ * 128:(kt_ + 1) * 128],
                         qTa[i][:, half * 4:(half + 1) * 4, :], True, True, [kTa, qTa[i]], [lb])

            qk(0, 0)
            qk(0, 1)
            for kt in range(nk):
                e_ = ex[kt % 3]
                p_ = pT[kt % 3]
                if kt + 1 < nk:
                    qk(kt + 1, 0)
                for half in range(2):
                    lb = lgh[(2 * kt + half) % 3]
                    P.act(e_[:, half * 4:(half + 1) * 4, :], lb[:], AF.Exp, [lb], [e_])
                if kt + 1 < nk:
                    qk(kt + 1, 1)
                P.stt("dve", p_[:], e_[:], 3.0e38, bc(M01T[:, kt:kt + 1, :], [128, 8, 128]), ALU.min, ALU.mult,
                      [e_, M01T], [p_])
                for h in range(8):
                    P.matmul(ops_[:, h, 0:65], p_[:, h, :], vaug[:, kt, :], kt == 0 and h % 4 == 0, kt == nk - 1 and h % 4 == 3,
                             [p_, vaug], [ops_])
            if STOP[0] <= 5:
                continue
            P.recip(rden[:], ops_[:, :, 64], [ops_], [rden])
            P.tt("dve", ot[:].rearrange("t (h d) -> t h d", h=8), ops_[:, :, 0:64], bc(rden[:].unsqueeze(2), [128, 8, 64]), ALU.mult,
                 [ops_, rden], [ot])
            P.tt("pool", yb[i][:], ot[:], gt[i][:], ALU.mult, [ot, gt[i]], [yb[i]])
            yo.emit(yb[i], t)


def phase_gdn(P, C, K, l):
    S = C.scr
    NCH = NTR[0] * 2
    with P.scope():
        uinc = P.sbuf("a_uinc", [128, 128], F32)
        P.memset("pool", uinc[:], 1.0, [uinc])
        P.op("pool", lambda e: e.affine_select(out=uinc[:], in_=uinc[:], pattern=[[1, 128]], compare_op=ALU.is_ge,
                                               fill=0.0, base=0, channel_multiplier=-1), [uinc], [uinc])
        lstr = P.sbuf("a_lstr", [128, 128], F32)
        P.memset("pool", lstr[:], 1.0, [lstr])
        P.op("pool", lambda e: e.affine_select(out=lstr[:], in_=lstr[:], pattern=[[-1, 128]], compare_op=ALU.is_gt,
                                               fill=0.0, base=0, channel_multiplier=1), [lstr], [lstr])
        idf = P.sbuf("a_idf", [128, 128], F32)
        P.make_identity(idf)
        onesf = P.sbuf("a_onesf", [128, 128], F32)
        P.memset("pool", onesf[:], 1.0, [onesf])
        P.barrier()
        aB = P.sbuf("a_aB", [64, 4], F32)
        P.dma("sp", aB[:], C.prm["gdn_a_log"][l:l + 1, :].to_broadcast([64, 4]), writes=[aB])
        P.act(aB[:], aB[:], AF.Exp, [aB], [aB])
        P.ts("dve", aB[:], aB[:], -1.0, None, ALU.mult, None, [aB], [aB])
        dtb = P.sbuf("a_dtb", [64, 4], F32)
        P.dma("sp", dtb[:], C.prm["gdn_dt_bias"][l:l + 1, :].to_broadcast([64, 4]), writes=[dtb])
        ng = P.sbuf("a_ng", [64, 128], F32)
        P.dma("sp", ng[:], C.prm["gdn_norm_g"][l:l + 1, :].to_broadcast([64, 128]), writes=[ng])
        ST32 = P.sbuf("a_ST32", [128, 4, 128], F32)
        STb = P.sbuf("a_STb", [128, 4, 128], BF16)
        P.memset("pool", ST32[:], 0.0, [ST32])
        P.memset("pool", STb[:], 0.0, [STb])

        def dbl(name, shape, dt):
            return [P.sbuf("%s%d" % (name, i), shape, dt) for i in range(2)]

        qT, kT, vT = dbl("a_qT", [128, 4, 64], BF16), dbl("a_kT", [128, 4, 64], BF16), dbl("a_vT", [128, 4, 64], BF16)
        ab, gate = dbl("a_ab", [64, 8], F32), dbl("a_gate", [64, 512], BF16)
        sp_, gl, be = dbl("a_sp", [64, 4], F32), dbl("a_gl", [64, 4], F32), dbl("a_be", [64, 4], F32)
        gc, gt128, cd128 = dbl("a_gc", [64, 4], F32), dbl("a_gt", [128, 4], F32), dbl("a_cd", [128, 4], F32)
        lab = dbl("a_lab", [64, 4, 128], F32)
        pre, E, bE = dbl("a_pre", [64, 8], F32), dbl("a_E", [64, 8], F32), dbl("a_bE", [64, 4], F32)
        seg, nseg = dbl("a_seg", [64, 4, 64], F32), dbl("a_nseg", [64, 4, 64], F32)
        dI, dL = dbl("a_dI", [64, 4, 64], F32), dbl("a_dL", [64, 4, 64], F32)
        egr = dbl("a_egr", [128, 4, 64], F32)
        qdT = dbl("a_qdT", [128, 4, 64], BF16)
        qkT = dbl("a_qkT", [64, 4, 64], BF16)
        Nm = [dbl("a_N", [64, 4, 64], F32), dbl("a_N2", [64, 4, 64], F32)]
        Mm = [dbl("a_M", [64, 4, 64], F32), dbl("a_M2", [64, 4, 64], F32)]
        X, Xb = dbl("a_X", [64, 4, 64], F32), dbl("a_Xb", [64, 4, 64], BF16)
        kb, kdec, vb = dbl("a_kb", [64, 4, 128], BF16), dbl("a_kdec", [64, 4, 128], BF16), dbl("a_vb", [64, 4, 128], BF16)
        wkT, u0 = dbl("a_wkT", [128, 4, 64], BF16), dbl("a_u0", [64, 4, 128], F32)
        u = dbl("a_u", [64, 4, 128], BF16)
        o, sq = dbl("a_o", [64, 4, 128], F32), dbl("a_sq", [64, 4, 128], F32)
        ssq = dbl("a_ssq", [64, 4], F32)
        y = dbl("a_y", [64, 512], F32)
        ysb = dbl("a_ysb", [128, 4, 64], BF16)

        b0 = P.psum("a_b0", [128, 512], F32)
        gcrow, wk_ps = View(b0, b0[:, 0:256].rearrange("p (h c) -> p h c", h=4)), View(b0, b0[:, 256:512].rearrange("p (h c) -> p h c", h=4))
        b1 = P.psum("a_b1", [128, 512], F32)
        sm_ps, ytp = View(b1, b1[:, 0:16]), View(b1, b1[:, 256:512].rearrange("p (h c) -> p h c", h=4))
        b2 = P.psum("a_b2", [64, 512], F32)
        kk_ps, qk_ps = View(b2, b2[:, 0:256].rearrange("p (h c) -> p h c", h=4)), View(b2, b2[:, 256:512].rearrange("p (h c) -> p h c", h=4))
        b3 = P.psum("a_b3", [64, 512], F32)
        P_ps, Q_ps = View(b3, b3[:, 0:256].rearrange("p (h c) -> p h c", h=4)), View(b3, b3[:, 256:512].rearrange("p (h c) -> p h c", h=4))
        b4 = P.psum("a_b4", [64, 512], F32)
        XP_ps, M_ps = View(b4, b4[:, 0:256].rearrange("p (h c) -> p h c", h=4)), View(b4, b4[:, 256:512].rearrange("p (h c) -> p h c", h=4))
        kv_ps = P.psum("a_kvps", [64, 2, 512], BF16)
        ktm, vtm = View(kv_ps, kv_ps[:, 0, :].rearrange("p (h d) -> p h d", h=4)), View(kv_ps, kv_ps[:, 1, :].rearrange("p (h d) -> p h d", h=4))
        uwo = P.psum("a_uwo", [64, 4, 128], F32)
        Sn_ps = P.psum("a_Sn", [128, 4, 128], F32)
        u64, i64 = uinc[0:64, 0:64], idf[0:64, 0:64]

        def b4c(ap, n):
            return bc(ap.unsqueeze(2), [ap.shape[0], 4, n])

        def pre_fn(ch):
            i = ch % 2
            cs = slice(ch * 64, (ch + 1) * 64)
            for dst, nm in ((qT[i], "gq"), (kT[i], "gk"), (vT[i], "gv")):
                P.dma("sp", dst[:], S[nm][:, cs].rearrange("(h d) t -> d h t", d=128), writes=[dst])
            P.dma("sp", ab[i][:], S["misc"][cs, 0:8], writes=[ab[i]])
            P.dma("sp", gate[i][:], S["ag"][cs, :], writes=[gate[i]])
            P.tt("dve", sp_[i][:], ab[i][:, 0:4], dtb[:], ALU.add, [ab[i], dtb], [sp_[i]])
            P.act(sp_[i][:], sp_[i][:], AF.Exp, [sp_[i]], [sp_[i]])
            P.act(sp_[i][:], sp_[i][:], AF.Ln, [sp_[i]], [sp_[i]], bias=1.0)
            P.tt("dve", gl[i][:], sp_[i][:], aB[:], ALU.mult, [sp_[i], aB], [gl[i]])
            P.act(be[i][:], ab[i][:, 4:8], AF.Exp, [ab[i]], [be[i]], scale=-1.0)
            P.ts("dve", be[i][:], be[i][:], 1.0, None, ALU.add, None, [be[i]], [be[i]])
            P.recip(be[i][:], be[i][:], [be[i]], [be[i]])
            yield
            P.matmul(sm_ps[0:64, 0:4], u64, gl[i][:], True, True, [uinc, gl[i]], [sm_ps])
            P.matmul(sm_ps[:, 4:8], onesf[0:64, :], gl[i][:], True, True, [onesf, gl[i]], [sm_ps])
            P.copy("dve", gc[i][:], sm_ps[0:64, 0:4], [sm_ps], [gc[i]])
            P.copy("dve", gt128[i][:], sm_ps[:, 4:8], [sm_ps], [gt128[i]])
            P.copy("dve", lab[i][:], b4c(gl[i][:], 128), [gl[i]], [lab[i]])
            for h in range(4):
                P.matmul(gcrow[:, h, :], lab[i][:, h, :], u64, True, True, [lab[i], uinc], [gcrow])
            P.copy("dve", pre[i][:, 0:4], gc[i][:], [gc[i]], [pre[i]])
            P.tt("dve", pre[i][:, 4:8], gt128[i][0:64, :], gc[i][:], ALU.subtract, [gt128[i], gc[i]], [pre[i]])
            P.act(E[i][:], pre[i][:], AF.Exp, [pre[i]], [E[i]])
            P.act(cd128[i][:], gt128[i][:], AF.Exp, [gt128[i]], [cd128[i]])
            P.tt("dve", bE[i][:], be[i][:], E[i][:, 0:4], ALU.mult, [be[i], E[i]], [bE[i]])
            if STOP[0] <= 1:
                return
            yield
            P.tt("dve", seg[i][:], gcrow[0:64], b4c(gc[i][:], 64), ALU.subtract, [gcrow, gc[i]], [seg[i]])
            P.ts("dve", nseg[i][:], seg[i][:], -1.0, 0.0, ALU.mult, ALU.min, [seg[i]], [nseg[i]])
            P.ts("dve", seg[i][:], seg[i][:], 0.0, None, ALU.min, None, [seg[i]], [seg[i]])
            P.act(dI[i][:], seg[i][:], AF.Exp, [seg[i]], [dI[i]])
            P.act(dL[i][:], nseg[i][:], AF.Exp, [nseg[i]], [dL[i]])
            P.tt("dve", dI[i][:], dI[i][:], bc(u64.unsqueeze(1), [64, 4, 64]), ALU.mult, [dI[i], uinc], [dI[i]])
            P.tt("dve", dL[i][:], dL[i][:], bc(lstr[0:64, 0:64].unsqueeze(1), [64, 4, 64]), ALU.mult, [dL[i], lstr], [dL[i]])
            P.act(egr[i][:], gcrow[:], AF.Exp, [gcrow], [egr[i]])
            P.tt("dve", qdT[i][:], qT[i][:], egr[i][:], ALU.mult, [qT[i], egr[i]], [qdT[i]])
            if STOP[0] <= 2:
                return
            yield
            for h in range(4):
                P.matmul(kk_ps[:, h, :], kT[i][:, h, :], kT[i][:, h, :], True, True, [kT[i]], [kk_ps])
            for h in range(4):
                P.matmul(qk_ps[:, h, :], kT[i][:, h, :], qT[i][:, h, :], True, True, [kT[i], qT[i]], [qk_ps])
            N0, M0 = Nm[0][i], Mm[0][i]
            P.tt("dve", N0[:], kk_ps[:], dL[i][:], ALU.mult, [kk_ps, dL[i]], [N0])
            P.tt("dve", N0[:], N0[:], b4c(be[i][:], 64), ALU.mult, [N0, be[i]], [N0])
            P.tt("dve", qkT[i][:], qk_ps[:], dI[i][:], ALU.mult, [qk_ps, dI[i]], [qkT[i]])
            if STOP[0] <= 3:
                return
            for h in range(4):
                P.transpose(M_ps[:, h, :], N0[:, h, :], i64, [N0, idf], [M_ps])
            P.copy("act", M0[:], M_ps[:], [M_ps], [M0])
            if STOP[0] <= 4:
                return
            yield
            P.tt("dve", X[i][:], bc(i64.unsqueeze(1), [64, 4, 64]), M0[:], ALU.subtract, [idf, M0], [X[i]])
            Pc, Qc = N0, M0
            if STOP[0] <= 4.1:
                return
            for st in range(1, 6):
                if STOP[0] <= 4.2 and st > 1:
                    break
                Pn, Qn = Nm[st % 2][i], Mm[st % 2][i]
                for h in range(4):
                    P.matmul(P_ps[:, h, :], Qc[:, h, :], Pc[:, h, :], True, True, [Qc, Pc], [P_ps])
                if st < 5:
                    for h in range(4):
                        P.matmul(Q_ps[:, h, :], Pc[:, h, :], Qc[:, h, :], True, True, [Qc, Pc], [Q_ps])
                if STOP[0] <= 4.15:
                    break
                P.copy("act", Pn[:], P_ps[:], [P_ps], [Pn])
                if st < 5:
                    P.copy("dve", Qn[:], Q_ps[:], [Q_ps], [Qn])
                if STOP[0] <= 4.17:
                    break
                for h in range(4):
                    P.matmul(XP_ps[:, h, :], Pn[:, h, :], X[i][:, h, :], True, True, [Pn, X[i]], [XP_ps])
                P.tt("dve", X[i][:], X[i][:], XP_ps[:], ALU.add, [X[i], XP_ps], [X[i]])
                Pc, Qc = Pn, Qn
                yield
            if STOP[0] <= 4.5:
                return
            P.copy("act", Xb[i][:], X[i][:], [X[i]], [Xb[i]])
            if STOP[0] <= 5:
                return
            yield
            for h in range(4):
                P.transpose(ktm[:, h, :], kT[i][:, h, :], K.ident[:], [kT[i], K.ident], [ktm])
            for h in range(4):
                P.transpose(vtm[:, h, :], vT[i][:, h, :], K.ident[:], [vT[i], K.ident], [vtm])
            P.tt("dve", kb[i][:], ktm[:], b4c(bE[i][:], 128), ALU.mult, [ktm, bE[i]], [kb[i]])
            P.tt("dve", kdec[i][:], ktm[:], b4c(E[i][:, 4:8], 128), ALU.mult, [ktm, E[i]], [kdec[i]])
            P.tt("dve", vb[i][:], vtm[:], b4c(be[i][:], 128), ALU.mult, [vtm, be[i]], [vb[i]])
            if STOP[0] <= 6:
                return
            yield
            for h in range(4):
                P.matmul(wk_ps[:, h, :], kb[i][:, h, :], Xb[i][:, h, :], True, True, [kb[i], Xb[i]], [wk_ps])
            P.copy("act", wkT[i][:], wk_ps[:], [wk_ps], [wkT[i]])
            for h in range(4):
                P.matmul(uwo[:, h, :], Xb[i][:, h, :], vb[i][:, h, :], True, True, [Xb[i], vb[i]], [uwo])
            P.copy("act", u0[i][:], uwo[:], [uwo], [u0[i]])
            if STOP[0] <= 7:
                return
        def seq_fn(ch):
            i = ch % 2
            cs = slice(ch * 64, (ch + 1) * 64)
            for h in range(4):
                P.matmul(uwo[:, h, :], wkT[i][:, h, :], STb[:, h, :], True, True, [wkT[i], STb], [uwo])
            P.tt("dve", u[i][:], u0[i][:], uwo[:], ALU.subtract, [u0[i], uwo], [u[i]])
            yield
            for h in range(4):
                P.matmul(uwo[:, h, :], qdT[i][:, h, :], STb[:, h, :], True, False, [qdT[i], STb], [uwo])
                P.matmul(uwo[:, h, :], qkT[i][:, h, :], u[i][:, h, :], False, True, [qkT[i], u[i]], [uwo])
            P.copy("act", o[i][:], uwo[:], [uwo], [o[i]])
            if STOP[0] <= 8:
                return
            yield
            for h in range(4):
                P.matmul(Sn_ps[:, h, :], kdec[i][:, h, :], u[i][:, h, :], True, True, [kdec[i], u[i]], [Sn_ps])
            P.tt("dve", ST32[:], ST32[:], b4c(cd128[i][:], 128), ALU.mult, [ST32, cd128[i]], [ST32])
            P.tt("dve", ST32[:], ST32[:], Sn_ps[:], ALU.add, [ST32, Sn_ps], [ST32])
            P.copy("act", STb[:], ST32[:], [ST32], [STb])
            if STOP[0] <= 9:
                return
            yield
            P.tt("pool", sq[i][:], o[i][:], o[i][:], ALU.mult, [o[i]], [sq[i]])
            P.op("dve", lambda e, i=i: e.tensor_reduce(ssq[i][:], sq[i][:], AX.X, ALU.add), [sq[i]], [ssq[i]])
            P.act(ssq[i][:], ssq[i][:], AF.Sqrt, [ssq[i], K.eps], [ssq[i]], scale=1.0 / 128, bias=K.eps[0:64, :])
            P.recip(ssq[i][:], ssq[i][:], [ssq[i]], [ssq[i]])
            P.tt("pool", o[i][:], o[i][:], b4c(ssq[i][:], 128), ALU.mult, [o[i], ssq[i]], [o[i]])
            P.tt("pool", o[i][:], o[i][:], bc(ng[:].unsqueeze(1), [64, 4, 128]), ALU.mult, [o[i], ng], [o[i]])
            P.tt("pool", y[i][:], o[i][:].rearrange("c h v -> c (h v)"), gate[i][:], ALU.mult, [o[i], gate[i]], [y[i]])
            if STOP[0] <= 10:
                return
            yield
            for cc in range(4):
                P.transpose(ytp[:, cc, :], y[i][:, cc * 128:(cc + 1) * 128], i64, [y[i], idf], [ytp])
            P.copy("act", ysb[i][:], ytp[:], [ytp], [ysb[i]])
            P.dma("sp", S["ysT"][0][:, cs].rearrange("(cc c) t -> c cc t", c=128), ysb[i][:], reads=[ysb[i]])


        def drain(g):
            for _ in g:
                pass

        def interleave(ga, gb):
            a_live, b_live = True, True
            while a_live or b_live:
                if b_live:
                    try:
                        next(gb)
                    except StopIteration:
                        b_live = False
                if a_live:
                    try:
                        next(ga)
                    except StopIteration:
                        a_live = False

        drain(pre_fn(0))
        for ch in range(NCH):
            if ch + 1 < NCH:
                interleave(pre_fn(ch + 1), seq_fn(ch))
            else:
                drain(seq_fn(ch))


def build_program(nc, layers=(0, 1), dbg=False, branches="abcdm"):
    C = declare(nc, dbg=dbg)
    P = Prog(nc)
    K = setup_consts(P, C)
    for l in layers:
        x_src = C.x if l == layers[0] else C.scr["x1"]
        x_dst = C.out if l == layers[-1] else C.scr["x1"]
        with P.scope():
            alloc_hT(P, K)
            phase_norm(P, C, K, l, x_src)
            phase_inproj(P, C, K, l)
        if "a" in branches:
            phase_gdn(P, C, K, l)
        if "b" in branches:
            phase_sg(P, C, K, l)
        if "c" in branches:
            phase_dsa(P, C, K, l)
        if "d" in branches:
            phase_ssd(P, C, K, l)
        if "m" in branches:
            phase_mem(P, C, K, l)
        with P.scope():
            alloc_merged(P, K)
            with P.scope():
                alloc_hT(P, K)
                phase_norm(P, C, K, l, x_src)
                phase_merge(P, C, K, l)
            phase_outproj(P, C, K, l, x_src, x_dst)
    P.finish()
    return C, P


def kernel(**inputs):
    x = np.ascontiguousarray(np.asarray(inputs["x"], dtype=np.float32))
    mem = np.ascontiguousarray(np.asarray(inputs["mem"], dtype=np.float32))
    nb = x.shape[0]
    nc = bass.Bass("TRN2", target_bir_lowering=False)
    build_program(nc)
    prm = {n: np.ascontiguousarray(np.asarray(inputs[n], dtype=np.float32)) for n, _ in PARAMS}
    in_maps = []
    for b in range(nb):
        m = {"x": x[b], "mem": mem[b]}
        m.update(prm)
        in_maps.append(m)
    res = run_bass_kernel_spmd(nc, in_maps, core_ids=list(range(nb)))
    return np.stack([np.asarray(r["out"], dtype=np.float32) for r in res.results], axis=0)
```

```python
from contextlib import ExitStack
import numpy as np
import concourse.bass as bass
import concourse.mybir as mybir
from concourse.bass_utils import run_bass_kernel_spmd

F32 = mybir.dt.float32
BF16 = mybir.dt.bfloat16
AF = mybir.ActivationFunctionType
ALU = mybir.AluOpType
AX = mybir.AxisListType


class Buf:
    __slots__ = ("name", "ap", "w", "r", "excl")

    def __init__(self, name, ap=None):
        self.name = name
        self.ap = ap
        self.excl = False
        self.w = None
        self.r = {}

    def __getitem__(self, idx):
        return self.ap[idx]


class View:
    def __init__(self, parent, ap):
        self.parent = parent
        self.ap = ap
        self.name = parent.name
        self.excl = parent.excl

    def __getitem__(self, idx):
        return self.ap[idx]

    @property
    def w(self):
        return self.parent.w

    @w.setter
    def w(self, v):
        self.parent.w = v

    @property
    def r(self):
        return self.parent.r

    @r.setter
    def r(self, v):
        self.parent.r = v


class Prog:
    ENG = ("pe", "dve", "act", "pool", "sp")
    SEM_LIMIT = 30000
    NDMA = 6

    def __init__(self, nc, same_engine_sync=True):
        self.nc = nc
        self.same = same_engine_sync
        self.stack = ExitStack()
        self.ops = {e: [] for e in self.ENG}
        self.cnt = {e: 0 for e in self.ENG}
        self.owner = {}
        self.nsem = 0
        self.cur = {e: self._newsem(e) for e in self.ENG}
        self.seen = {e: {} for e in self.ENG}
        self.dsem = {}
        self.drr = {}
        self.allsems = []
        self.nbuf = 0

    def _newsem(self, owner):
        s = getattr(self, "semstack", self.stack).enter_context(self.nc.semaphore("s%d_%s" % (self.nsem, owner)))
        self.nsem += 1
        self.owner[id(s)] = owner
        return s

    def sbuf(self, name, shape, dtype):
        self.nbuf += 1
        t = self.stack.enter_context(self.nc.sbuf_tensor("%s_%d" % (name, self.nbuf), list(shape), dtype))
        return Buf(name, t)

    def psum(self, name, shape, dtype):
        self.nbuf += 1
        t = self.stack.enter_context(self.nc.psum_tensor("%s_%d" % (name, self.nbuf), list(shape), dtype))
        b = Buf(name, t)
        b.excl = True
        return b

    def buf(self, name, ap=None):
        return Buf(name, ap)

    def _deps(self, eng, reads, writes):
        deps = {}

        def add(ev):
            if ev is None:
                return
            s, v = ev
            k = id(s)
            if k not in deps or deps[k][1] < v:
                deps[k] = (s, v)

        for b in reads:
            add(b.w)
        for b in writes:
            add(b.w)
            for ev in b.r.values():
                add(ev)
        waits = []
        seen = self.seen[eng]
        for k, (s, v) in deps.items():
            if self.owner.get(k) == eng:
                if eng == "pe" or eng == "sp" or not self.same:
                    continue
            if seen.get(k, 0) >= v:
                continue
            seen[k] = v
            waits.append((s, v))
        return waits

    def _commit(self, ev, reads, writes):
        for b in reads:
            k = id(ev[0])
            b.r[k] = ev
        for b in writes:
            b.w = ev
            b.r = {}

    def op(self, eng, fn, reads=(), writes=()):
        ex = [b for b in reads if getattr(b, "excl", False)]
        if ex:
            writes = list(writes) + ex
        waits = self._deps(eng, reads, writes)
        if self.cnt[eng] >= self.SEM_LIMIT:
            self.cur[eng] = self._newsem(eng)
            self.cnt[eng] = 0
        self.cnt[eng] += 1
        ev = (self.cur[eng], self.cnt[eng])
        self.ops[eng].append((waits, fn, ("c", self.cur[eng], self.cnt[eng])))
        self._commit(ev, reads, writes)
        return ev

    def dma(self, q, out, in_, reads=(), writes=(), **kw):
        waits = self._deps(q, reads, writes)
        if q not in self.dsem:
            self.dsem[q] = [[self._newsem("dma_" + q), 0] for _ in range(self.NDMA)]
            self.drr[q] = 0
        slot = self.dsem[q][self.drr[q] % self.NDMA]
        self.drr[q] += 1
        s, c = slot
        if c > 0:
            seen = self.seen[q]
            if seen.get(id(s), 0) < 16 * c:
                seen[id(s)] = 16 * c
                waits.append((s, 16 * c))
        if 16 * (c + 1) > self.SEM_LIMIT:
            s = self._newsem("dma_" + q)
            slot[0] = s
            c = 0
        slot[1] = c + 1
        ev = (s, 16 * (c + 1))
        self.ops[q].append((waits, lambda e, out=out, in_=in_, kw=kw: e.dma_start(out=out, in_=in_, **kw), ("d", s, 16)))
        self._commit(ev, reads, writes)
        return ev

    def make_identity(self, b, n=128):
        self.op("pool", lambda e: e.memset(b.ap[:], 1.0), writes=[b])
        self.op("pool", lambda e: e.affine_select(out=b.ap[:], in_=b.ap[:], pattern=[[-1, n]], compare_op=ALU.is_ge,
                                                  fill=0.0, base=0, channel_multiplier=1), reads=[b], writes=[b])
        self.op("pool", lambda e: e.affine_select(out=b.ap[:], in_=b.ap[:], pattern=[[1, n]], compare_op=ALU.is_ge,
                                                  fill=0.0, base=0, channel_multiplier=-1), reads=[b], writes=[b])


    def matmul(self, out, lhsT, rhs, start, stop, reads, writes):
        return self.op("pe", lambda e: e.matmul(out, lhsT, rhs, start=start, stop=stop), reads, writes)

    def transpose(self, out, in_, ident, reads, writes):
        return self.op("pe", lambda e: e.transpose(out, in_, ident), reads, writes)

    def act(self, out, in_, func, reads, writes, **kw):
        return self.op("act", lambda e: e.activation(out, in_, func, **kw), reads, writes)

    def tt(self, eng, out, a, b, op, reads, writes):
        return self.op(eng, lambda e: e.tensor_tensor(out, a, b, op), reads, writes)

    def ts(self, eng, out, a, s1, s2, op0, op1, reads, writes, **kw):
        if op1 is None:
            return self.op(eng, lambda e: e.tensor_scalar(out, a, s1, None, op0, **kw), reads, writes)
        return self.op(eng, lambda e: e.tensor_scalar(out, a, s1, s2, op0, op1, **kw), reads, writes)

    def stt(self, eng, out, in0, scalar, in1, op0, op1, reads, writes):
        return self.op(eng, lambda e: e.scalar_tensor_tensor(out, in0, scalar, in1, op0, op1), reads, writes)

    def copy(self, eng, out, in_, reads, writes):
        if eng == "act":
            return self.op(eng, lambda e: e.copy(out, in_), reads, writes)
        return self.op(eng, lambda e: e.tensor_copy(out, in_), reads, writes)

    def memset(self, eng, ap, val, writes):
        return self.op(eng, lambda e: e.memset(ap, val), (), writes)

    def recip(self, out, in_, reads, writes):
        return self.op("dve", lambda e: e.reciprocal(out, in_), reads, writes)

    def barrier(self):
        finals = []
        for e in self.ENG:
            if self.cnt[e] > 0:
                finals.append((self.cur[e], self.cnt[e]))
        for q, slots in self.dsem.items():
            for s, c in slots:
                if c > 0:
                    finals.append((s, 16 * c))
        for e in self.ENG:
            waits = []
            for s, v in finals:
                if self.seen[e].get(id(s), 0) < v:
                    self.seen[e][id(s)] = v
                    waits.append((s, v))
            if waits:
                self.ops[e].append((waits, None, None))

    def scope(self):
        return _Scope(self)

    def flush(self, final=False):
        nc = self.nc
        finals = []
        if final:
            for e in self.ENG:
                if self.cnt[e] > 0:
                    finals.append((self.cur[e], self.cnt[e]))
            for q, slots in self.dsem.items():
                for s, c in slots:
                    if c > 0:
                        finals.append((s, 16 * c))
        if not hasattr(self, "actual"):
            self.actual = {}
            self.amap = {}
        ref = set()
        for e in self.ENG:
            for waits, fn, inc in self.ops[e]:
                for s, v in waits:
                    if self.owner.get(id(s)) in self.ENG:
                        ref.add((id(s), v))
        for s, v in finals:
            if self.owner.get(id(s)) in self.ENG:
                ref.add((id(s), v))
        for e in self.ENG:
            for waits, fn, inc in self.ops[e]:
                if inc is not None and inc[0] == "c":
                    key = (id(inc[1]), inc[2])
                    if key in ref:
                        self.actual[key[0]] = self.actual.get(key[0], 0) + 1
                        self.amap[key] = self.actual[key[0]]

        def tr(s, v):
            if self.owner.get(id(s)) in self.ENG:
                return self.amap[(id(s), v)]
            return v

        engs = {"pe": "tensor", "dve": "vector", "act": "scalar", "pool": "gpsimd", "sp": "sync"}
        with nc.Block() as block:
            for e in self.ENG:
                ops = self.ops[e]
                if not ops and not (final and e == "sp"):
                    continue

                def body(engine, ops=ops, e=e):
                    for waits, fn, inc in ops:
                        for s, v in waits:
                            engine.wait_ge(s, tr(s, v))
                        if fn is not None:
                            ins = fn(engine)
                            if inc[0] == "d":
                                ins.then_inc(inc[1], 16)
                            elif (id(inc[1]), inc[2]) in self.amap:
                                ins.then_inc(inc[1], 1)
                    if final and e == "sp":
                        for s, v in finals:
                            engine.wait_ge(s, tr(s, v))

                getattr(block, engs[e])(body)
        self.nops = getattr(self, "nops", 0) + sum(len(v) for v in self.ops.values())
        self.ops = {e: [] for e in self.ENG}

    def finish(self):
        self.flush(final=True)
        self.stack.close()


class _Scope:
    def __init__(self, P):
        self.P = P

    def __enter__(self):
        self.saved = self.P.stack
        self.P.semstack = getattr(self.P, "semstack", self.saved)
        self.P.stack = ExitStack()
        return self

    def __exit__(self, *a):
        self.P.barrier()
        self.P.flush()
        self.P.stack.close()
        self.P.stack = self.saved
        return False


T = 4096
NT = 32
NTR = [32]
D = 1024
INC = 7896
EPS = 1e-6

O_AQ, O_AK, O_AV = 0, 512, 1024
O_AA, O_AB, O_AG = 1536, 1540, 1544
O_BU, O_BV, O_BG = 2056, 2568, 3080
O_CQ, O_CK, O_CV, O_CIQ, O_CIK, O_CIW, O_CG = 3592, 4104, 4168, 4232, 4744, 4808, 4816
O_DZ, O_DX, O_DDT = 5328, 5840, 6864
O_MQ, O_MG = 6872, 7384

PARAMS = [("norm_g", [2, 1024]), ("w_in", [2, 1024, INC]), ("gdn_conv_w", [2, 4, 1536]), ("gdn_a_log", [2, 4]),
          ("gdn_dt_bias", [2, 4]), ("gdn_norm_g", [2, 128]), ("sg_ln_g", [2, 512]), ("sg_ln_b", [2, 512]),
          ("sg_w", [2, 4, 128, 128]), ("sg_b", [2, 4, 128]), ("dsa_q_norm_g", [2, 64]), ("dsa_k_norm_g", [2, 64]),
          ("ssd_conv_w", [2, 4, 1024]), ("ssd_conv_b", [2, 1024]), ("ssd_a_log", [2, 8]), ("ssd_dt_bias", [2, 8]),
          ("ssd_d", [2, 8]), ("ssd_norm_g", [2, 512]), ("mem_norm_g", [2, 1024]), ("w_mem_kv", [2, 1024, 1024]),
          ("mem_q_norm_g", [2, 128]), ("mem_k_norm_g", [2, 128]), ("w_gate", [2, 5, 1024, 1024]),
          ("w_branch", [2, 5, 512, 1024]), ("w_out", [2, 1024, 1024])]


class Ctx:
    pass


STOP = [99]


def declare(nc, dbg=False, skip=()):
    C = Ctx()
    C.nc = nc
    if "x" not in skip:
        C.x = nc.dram_tensor("x", [T, D], F32, kind="ExternalInput").ap()
    C.mem = nc.dram_tensor("mem", [256, D], F32, kind="ExternalInput").ap()
    C.prm = {}
    for name, shp in PARAMS:
        if name in skip:
            continue
        C.prm[name] = nc.dram_tensor(name, shp, F32, kind="ExternalInput").ap()
    C.out = nc.dram_tensor("out", [T, D], F32, kind="ExternalOutput").ap()
    kind = "ExternalOutput" if dbg else "Internal"
    C.scr = {}

    def scr(name, shape, dt):
        C.scr[name] = nc.dram_tensor("scr_" + name, shape, dt, kind=kind).ap()

    for n in ("gq", "gk", "gv", "cq", "ciq", "mq"):
        scr(n, [512, T], BF16)
    scr("xbc", [1024, T], BF16)
    scr("ck", [64, T], BF16)
    scr("cik", [64, T], BF16)
    for n in ("ag", "bu", "bv", "bg", "cg", "dz", "mg"):
        scr(n, [T, 512], BF16)
    scr("misc", [T, 88], F32)
    scr("ysT", [5, 512, T], BF16)
    scr("x1", [T, D], F32)
    return C


def setup_consts(P, C):
    K = Ctx()
    K.ident = P.sbuf("ident", [128, 128], BF16)
    P.make_identity(K.ident)
    K.ones = P.sbuf("ones", [128, 128], BF16)
    P.memset("pool", K.ones[:], 1.0, [K.ones])
    K.blk2 = P.sbuf("blk2", [128, 128], BF16)
    P.memset("pool", K.blk2[:], 0.0, [K.blk2])
    P.memset("pool", K.blk2[0:64, 0:64], 1.0, [K.blk2])
    P.memset("pool", K.blk2[64:128, 64:128], 1.0, [K.blk2])
    K.eps = P.sbuf("epsc", [128, 1], F32)
    P.memset("pool", K.eps[:], EPS, [K.eps])
    P.barrier()
    return K


def alloc_hT(P, K):
    K.hT = P.sbuf("hT", [128, 8, T], BF16)
    K.hTb = [Buf("hT%d" % t, K.hT.ap) for t in range(NT)]


def alloc_merged(P, K):
    K.mT = P.sbuf("mT", [128, 8, T], BF16)
    K.mTb = [Buf("mT%d" % t, K.mT.ap) for t in range(8)]


def phase_norm(P, C, K, l, x_src):
    with P.scope():
        gb = P.sbuf("gb", [128, D], F32)
        P.dma("sp", gb[:], C.prm["norm_g"][l:l + 1, :].to_broadcast([128, D]), writes=[gb])
        xin = [P.sbuf("xin%d" % i, [128, D], F32) for i in range(2)]
        junk = P.sbuf("junk", [128, D], BF16)
        ss = [P.sbuf("ss%d" % i, [128, 1], F32) for i in range(2)]
        rt = [P.sbuf("rt%d" % i, [128, 1], F32) for i in range(2)]
        hb = [P.sbuf("hb%d" % i, [128, D], BF16) for i in range(2)]
        pt = [P.psum("pt%d" % i, [128, 4, 128], BF16) for i in range(2)]
        for t in range(NT):
            xi, s_, r_, h_ = xin[t % 2], ss[t % 2], rt[t % 2], hb[t % 2]
            P.dma("sp", xi[:], x_src[t * 128:(t + 1) * 128, :], writes=[xi])
            P.act(junk[:], xi[:], AF.Square, [xi], [junk, s_], accum_out=s_[:])
            P.act(r_[:], s_[:], AF.Sqrt, [s_, K.eps], [r_], scale=1.0 / D, bias=K.eps[:])
            P.recip(r_[:], r_[:], [r_], [r_])
            P.stt("dve", h_[:], xi[:], r_[:], gb[:], ALU.mult, ALU.mult, [xi, r_, gb], [h_])
            for half in range(2):
                p_ = pt[half]
                for j in range(4):
                    kc = half * 4 + j
                    P.transpose(p_[:, j, :], h_[:, kc * 128:(kc + 1) * 128], K.ident[:], [h_, K.ident], [p_])
                if half == 0:
                    P.copy("dve", K.hT[:, 0:4, t * 128:(t + 1) * 128], p_[:], [p_], [K.hTb[t]])
                else:
                    P.copy("act", K.hT[:, 4:8, t * 128:(t + 1) * 128], p_[:], [p_], [K.hTb[t]])


def phase_inproj(P, C, K, l):
    w_in = C.prm["w_in"][l]
    S = C.scr
    with P.scope():
        cwg = P.sbuf("cwg", [128, 4, 12], F32)
        cws = P.sbuf("cws", [128, 4, 8], F32)
        for k in range(4):
            P.dma("sp", cwg[:, k, :], C.prm["gdn_conv_w"][l][k].rearrange("(c p) -> p c", p=128), writes=[cwg],
                  allow_slow_non_contiguous=True)
            P.dma("sp", cws[:, k, :], C.prm["ssd_conv_w"][l][k].rearrange("(c p) -> p c", p=128), writes=[cws],
                  allow_slow_non_contiguous=True)
        cbs = P.sbuf("cbs", [128, 8], F32)
        P.dma("sp", cbs[:], C.prm["ssd_conv_b"][l].rearrange("(c p) -> p c", p=128), writes=[cbs],
              allow_slow_non_contiguous=True)
        gq2 = P.sbuf("gq2", [128, 1], F32)
        for i in range(2):
            P.dma("sp", gq2[i * 64:(i + 1) * 64, :], C.prm["dsa_q_norm_g"][l].rearrange("(p o) -> p o", o=1), writes=[gq2])
        gk1 = P.sbuf("gk1", [64, 1], F32)
        P.dma("sp", gk1[:], C.prm["dsa_k_norm_g"][l].rearrange("(p o) -> p o", o=1), writes=[gk1])
        gmq = P.sbuf("gmq", [128, 1], F32)
        P.dma("sp", gmq[:], C.prm["mem_q_norm_g"][l].rearrange("(p o) -> p o", o=1), writes=[gmq])
        P.ts("dve", gq2[:], gq2[:], 0.125, None, ALU.mult, None, [gq2], [gq2])
        P.ts("dve", gmq[:], gmq[:], 128 ** -0.5, None, ALU.mult, None, [gmq], [gmq])

        wb = [P.sbuf("wb%d" % i, [128, 8, 512], BF16) for i in range(2)]
        acc = [P.psum("acc%d" % i, [128, 512], F32) for i in range(3)]
        ssp = [P.psum("ssp%d" % i, [128, 512], F32) for i in range(2)]
        xpad = [P.sbuf("xpad%d" % i, [128, 515], F32) for i in range(4)]
        yb = [P.sbuf("yb%d" % i, [128, 512], F32) for i in range(4)]
        sb = [P.sbuf("sb%d" % i, [128, 512], F32) for i in range(2)]
        sq = [P.sbuf("sq%d" % i, [128, 512], BF16) for i in range(2)]
        rtb = [P.sbuf("rtb%d" % i, [128, 512], F32) for i in range(2)]
        ob = [P.sbuf("ob%d" % i, [128, 512], BF16) for i in range(3)]
        mo = [P.sbuf("mo%d" % i, [128, 88], F32) for i in range(2)]
        st = Ctx()
        st.g = 0
        st.a = 0
        st.e = 0
        st.o = 0

        st.loaded = {}

        def issue_load(gi, spec):
            w = wb[gi % 2]
            for (c0, n, d0) in spec:
                P.dma("pool", w[:, :, d0:d0 + n], w_in[:, c0:c0 + n].rearrange("(kc k) c -> k kc c", k=128), writes=[w])
            st.loaded[gi] = w

        def load_w(col0, ncols):
            w = st.loaded[st.g]
            st.g += 1
            return w

        def fm_group(col0, ncols, kind, dst, cw=None, cb=None, gain=None, scale=1.0):
            w = load_w(col0, ncols)
            nch = (ncols + 127) // 128
            for j in range(nch):
                m = min(128, ncols - j * 128)
                for tg in range(8):
                    a = acc[st.a % 3]
                    st.a += 1
                    for kc in range(8):
                        P.matmul(a[0:m, :], w[:, kc, j * 128:j * 128 + m], K.hT[:, kc, tg * 512:(tg + 1) * 512],
                                 kc == 0, kc == 7, [w] + K.hTb[tg * 4:tg * 4 + 4], [a])
                    e = st.e
                    st.e += 1
                    o = ob[st.o % 3]
                    st.o += 1
                    dsl = dst[j * 128:j * 128 + m, tg * 512:(tg + 1) * 512]
                    if kind == "raw":
                        P.copy("act", o[0:m, :], a[0:m, :], [a], [o])
                        P.dma("sp", dsl, o[0:m, :], reads=[o])
                        continue
                    if kind in ("conv", "conv_l2"):
                        xp, xn = xpad[tg % 4], xpad[(tg + 1) % 4]
                        y = yb[e % 4]
                        ce = "dve"
                        if tg == 0:
                            P.memset("dve", xp[:, 0:3], 0.0, [xp])
                        P.copy("act", xp[:, 3:515], a[:], [a], [xp])
                        cwb, cwo = cw
                        cj = cwo + j
                        P.ts(ce, y[:], xp[:, 0:512], cwb[:, 0, cj:cj + 1], None, ALU.mult, None, [xp, cwb], [y])
                        for k in range(1, 4):
                            P.stt(ce, y[:], xp[:, k:k + 512], cwb[:, k, cj:cj + 1], y[:], ALU.mult, ALU.add, [xp, cwb, y], [y])
                        if tg < 7:
                            P.copy("act", xn[:, 0:3], xp[:, 512:515], [xp], [xn])
                        if kind == "conv":
                            if cb is not None:
                                P.act(o[:], y[:], AF.Silu, [y, cb[0]], [o], bias=cb[0][:, cb[1] + j:cb[1] + j + 1])
                            else:
                                P.act(o[:], y[:], AF.Silu, [y], [o])
                            P.dma("sp", dsl, o[:], reads=[o])
                            continue
                        s_ = sb[e % 2]
                        P.act(s_[:], y[:], AF.Silu, [y], [s_])
                        src_ = s_
                        ones = K.ones
                        nrm_scale = 1.0
                    else:
                        s_ = sb[e % 2]
                        P.copy("act", s_[0:m, :], a[0:m, :], [a], [s_])
                        ones = K.blk2 if kind == "rms64" else K.ones
                        nrm_scale = (1.0 / 64) if kind == "rms64" else (1.0 / 128)
                    q_ = sq[e % 2]
                    P.act(q_[0:m, :], s_[0:m, :], AF.Square, [s_], [q_])
                    sp_ = ssp[e % 2]
                    P.matmul(sp_[0:m, :], ones[0:m, 0:m], q_[0:m, :], True, True, [ones, q_], [sp_])
                    r_ = rtb[e % 2]
                    P.act(r_[0:m, :], sp_[0:m, :], AF.Sqrt, [sp_, K.eps], [r_], scale=nrm_scale, bias=K.eps[0:m, :])
                    P.recip(r_[0:m, :], r_[0:m, :], [r_], [r_])
                    if gain is not None:
                        P.stt("dve", o[0:m, :], s_[0:m, :], gain[0:m, :], r_[0:m, :], ALU.mult, ALU.mult, [s_, gain, r_], [o])
                    else:
                        P.stt("dve", o[0:m, :], s_[0:m, :], scale, r_[0:m, :], ALU.mult, ALU.mult, [s_, r_], [o])
                    P.dma("sp", dsl, o[0:m, :], reads=[o])

        def tm_group(col0, func, dst):
            w = load_w(col0, 512)
            for t in range(NT):
                a = acc[st.a % 3]
                st.a += 1
                for kc in range(8):
                    P.matmul(a[:], K.hT[:, kc, t * 128:(t + 1) * 128], w[:, kc, :], kc == 0, kc == 7,
                             [w, K.hTb[t]], [a])
                o = ob[st.o % 3]
                st.o += 1
                if func is None:
                    P.copy("act", o[:], a[:], [a], [o])
                else:
                    P.act(o[:], a[:], func, [a], [o])
                P.dma("sp", dst[t * 128:(t + 1) * 128, :], o[:], reads=[o])

        def misc_group():
            w = load_w(0, 88)
            for t in range(NT):
                a = acc[st.a % 3]
                st.a += 1
                for kc in range(8):
                    P.matmul(a[:, 0:88], K.hT[:, kc, t * 128:(t + 1) * 128], w[:, kc, 0:88], kc == 0, kc == 7,
                             [w, K.hTb[t]], [a])
                o = mo[t % 2]
                P.copy("act", o[:], a[:, 0:88], [a], [o])
                P.dma("sp", S["misc"][t * 128:(t + 1) * 128, :], o[:], reads=[o])

        groups = [
            (((O_AA, 8, 0), (O_CIW, 8, 8), (O_DDT, 8, 16), (O_CV, 64, 24)), lambda: misc_group()),
            (((O_CIQ, 512, 0),), lambda: fm_group(O_CIQ, 512, "raw", S["ciq"])),
            (((O_CIK, 64, 0),), lambda: fm_group(O_CIK, 64, "raw", S["cik"])),
            (((O_CQ, 512, 0),), lambda: fm_group(O_CQ, 512, "rms64", S["cq"], gain=gq2)),
            (((O_CK, 64, 0),), lambda: fm_group(O_CK, 64, "rms64", S["ck"], gain=gk1)),
            (((O_MQ, 512, 0),), lambda: fm_group(O_MQ, 512, "rms128", S["mq"], gain=gmq)),
            (((O_AQ, 512, 0),), lambda: fm_group(O_AQ, 512, "conv_l2", S["gq"], cw=(cwg, 0), scale=128 ** -0.5)),
            (((O_AK, 512, 0),), lambda: fm_group(O_AK, 512, "conv_l2", S["gk"], cw=(cwg, 4), scale=1.0)),
            (((O_AV, 512, 0),), lambda: fm_group(O_AV, 512, "conv", S["gv"], cw=(cwg, 8))),
            (((O_DX, 512, 0),), lambda: fm_group(O_DX, 512, "conv", S["xbc"][0:512], cw=(cws, 0), cb=(cbs, 0))),
            (((O_DX + 512, 512, 0),), lambda: fm_group(O_DX + 512, 512, "conv", S["xbc"][512:1024], cw=(cws, 4), cb=(cbs, 4))),
            (((O_AG, 512, 0),), lambda: tm_group(O_AG, AF.Silu, S["ag"])),
            (((O_BG, 512, 0),), lambda: tm_group(O_BG, AF.Silu, S["bg"])),
            (((O_CG, 512, 0),), lambda: tm_group(O_CG, AF.Silu, S["cg"])),
            (((O_DZ, 512, 0),), lambda: tm_group(O_DZ, AF.Silu, S["dz"])),
            (((O_MG, 512, 0),), lambda: tm_group(O_MG, AF.Silu, S["mg"])),
            (((O_BU, 512, 0),), lambda: tm_group(O_BU, AF.Gelu, S["bu"])),
            (((O_BV, 512, 0),), lambda: tm_group(O_BV, AF.Gelu, S["bv"])),
        ]
        issue_load(0, groups[0][0])
        for gi, (spec, run) in enumerate(groups):
            if gi + 1 < len(groups):
                issue_load(gi + 1, groups[gi + 1][0])
            run()


def phase_merge(P, C, K, l):
    wgd = C.prm["w_gate"][l]
    wbd = C.prm["w_branch"][l]
    ysT = C.scr["ysT"]
    with P.scope():
        wbuf = [P.sbuf("mw%d" % i, [128, 7680], BF16) for i in range(2)]
        yT = [P.sbuf("yT%d" % i, [128, 4, 512], BF16) for i in range(3)]
        gps = [P.psum("gps%d" % i, [128, 512], F32) for i in range(2)]
        zps = [P.psum("zps%d" % i, [128, 512], F32) for i in range(2)]
        sg = [P.sbuf("sg%d" % i, [128, 512], F32) for i in range(2)]
        tmp = [P.sbuf("tmp%d" % i, [128, 512], F32) for i in range(2)]
        mac = [P.sbuf("mac%d" % i, [128, 512], F32) for i in range(2)]
        cnt = 0

        def views(w):
            return (w[:, 0:5120].rearrange("k (p kc n) -> k p kc n", p=5, kc=8),
                    w[:, 5120:7680].rearrange("k (p cc n) -> k p cc n", p=5, cc=4))

        def load(nch):
            w = wbuf[nch % 2]
            wg, wbr = views(w)
            for p in range(5):
                P.dma("pool", wg[:, p], wgd[p][:, nch * 128:(nch + 1) * 128].rearrange("(kc k) n -> k kc n", k=128), writes=[w])
                P.dma("pool", wbr[:, p], wbd[p][:, nch * 128:(nch + 1) * 128].rearrange("(cc k) n -> k cc n", k=128), writes=[w])

        load(0)
        for nch in range(8):
            w = wbuf[nch % 2]
            wg, wbr = views(w)
            if nch + 1 < 8:
                load(nch + 1)
            for tg in range(8):
                m_ = mac[tg % 2]
                for p in range(5):
                    y = yT[cnt % 3]
                    g_, z_ = gps[cnt % 2], zps[cnt % 2]
                    s_, t_ = sg[cnt % 2], tmp[cnt % 2]
                    cnt += 1
                    P.dma("sp", y[:], ysT[p][:, tg * 512:(tg + 1) * 512].rearrange("(cc c) t -> c cc t", c=128), writes=[y])
                    for kc in range(8):
                        P.matmul(g_[:], wg[:, p, kc, :], K.hT[:, kc, tg * 512:(tg + 1) * 512], kc == 0, kc == 7,
                                 [w] + K.hTb[tg * 4:tg * 4 + 4], [g_])
                    for cc in range(4):
                        P.matmul(z_[:], wbr[:, p, cc, :], y[:, cc, :], cc == 0, cc == 3, [w, y], [z_])
                    P.act(s_[:], g_[:], AF.Sigmoid, [g_], [s_])
                    if p == 0:
                        P.tt("dve", m_[:], z_[:], s_[:], ALU.mult, [z_, s_], [m_])
                    elif p < 4:
                        P.tt("dve", t_[:], z_[:], s_[:], ALU.mult, [z_, s_], [t_])
                        P.tt("pool", m_[:], m_[:], t_[:], ALU.add, [m_, t_], [m_])
                    else:
                        P.tt("dve", t_[:], z_[:], s_[:], ALU.mult, [z_, s_], [t_])
                        P.tt("pool", K.mT[:, nch, tg * 512:(tg + 1) * 512], m_[:], t_[:], ALU.add, [m_, t_], [K.mTb[tg]])


def phase_outproj(P, C, K, l, x_src, x_dst):
    with P.scope():
        wo = P.sbuf("wo", [128, 8, D], BF16)
        P.dma("pool", wo[:], C.prm["w_out"][l].rearrange("(kc k) n -> k kc n", k=128), writes=[wo])
        xin = [P.sbuf("oxin%d" % i, [128, 512], F32) for i in range(2)]
        xo = [P.sbuf("oxo%d" % i, [128, 512], F32) for i in range(2)]
        ops_ = [P.psum("ops%d" % i, [128, 512], F32) for i in range(2)]
        c = 0
        for t in range(NT):
            for hf in range(2):
                xi, o, ps = xin[c % 2], xo[c % 2], ops_[c % 2]
                c += 1
                P.dma("sp", xi[:], x_src[t * 128:(t + 1) * 128, hf * 512:(hf + 1) * 512], writes=[xi])
                for kc in range(8):
                    P.matmul(ps[:], K.mT[:, kc, t * 128:(t + 1) * 128], wo[:, kc, hf * 512:(hf + 1) * 512], kc == 0, kc == 7,
                             [wo, K.mTb[t // 4]], [ps])
                P.tt("dve", o[:], ps[:], xi[:], ALU.add, [ps, xi], [o])
                P.dma("sp", x_dst[t * 128:(t + 1) * 128, hf * 512:(hf + 1) * 512], o[:], reads=[o])


class YOut:
    def __init__(self, P, C, K, p, tag, nps=1, ps=None):
        self.P, self.C, self.K, self.p = P, C, K, p
        self.ps = ps if ps is not None else [P.psum("yo_ps%s%d" % (tag, i), [128, 4, 128], BF16) for i in range(nps)]
        self.sb = [P.sbuf("yo_sb%s%d" % (tag, i), [128, 4, 128], BF16) for i in range(2)]
        self.n = 0

    def emit(self, y, t, eng="act"):
        P, K = self.P, self.K
        ps, sb = self.ps[self.n % len(self.ps)], self.sb[self.n % 2]
        self.n += 1
        for cc in range(4):
            P.transpose(ps[:, cc, :], y[:, cc * 128:(cc + 1) * 128], K.ident[:], [y, K.ident], [ps])
        P.copy(eng, sb[:], ps[:], [ps], [sb])
        P.dma("sp", self.C.scr["ysT"][self.p][:, t * 128:(t + 1) * 128].rearrange("(cc c) t -> c cc t", c=128), sb[:], reads=[sb])


def phase_mem(P, C, K, l):
    S = C.scr
    with P.scope():
        gb = P.sbuf("m_gb", [128, D], F32)
        P.dma("sp", gb[:], C.prm["mem_norm_g"][l:l + 1, :].to_broadcast([128, D]), writes=[gb])
        gk = P.sbuf("m_gk", [128, 1], F32)
        P.dma("sp", gk[:], C.prm["mem_k_norm_g"][l].rearrange("(p o) -> p o", o=1), writes=[gk])
        wkv = P.sbuf("m_wkv", [128, 8, D], BF16)
        P.dma("pool", wkv[:], C.prm["w_mem_kv"][l].rearrange("(kc k) n -> k kc n", k=128), writes=[wkv])
        memT = P.sbuf("memT", [128, 8, 256], BF16)
        kT = P.sbuf("m_kT", [128, 4, 256], BF16)
        vaug = [P.sbuf("m_va%d" % i, [128, 4, 129], BF16) for i in range(2)]
        xin = P.sbuf("m_x", [128, D], F32)
        junk = P.sbuf("m_junk", [128, D], BF16)
        ss = P.sbuf("m_ss", [128, 1], F32)
        hb = P.sbuf("m_hb", [128, D], BF16)
        pt = P.psum("m_pt", [128, 4, 128], BF16)
        pa = P.psum("m_pa", [128, 512], F32)
        pb = P.psum("m_pb", [128, 512], F32)
        for mt in range(2):
            P.dma("sp", xin[:], C.mem[mt * 128:(mt + 1) * 128, :], writes=[xin])
            P.act(junk[:], xin[:], AF.Square, [xin], [junk, ss], accum_out=ss[:])
            P.act(ss[:], ss[:], AF.Sqrt, [ss, K.eps], [ss], scale=1.0 / D, bias=K.eps[:])
            P.recip(ss[:], ss[:], [ss], [ss])
            P.stt("dve", hb[:], xin[:], ss[:], gb[:], ALU.mult, ALU.mult, [xin, ss, gb], [hb])
            for half in range(2):
                for j in range(4):
                    kc = half * 4 + j
                    P.transpose(pt[:, j, :], hb[:, kc * 128:(kc + 1) * 128], K.ident[:], [hb, K.ident], [pt])
                P.copy("dve", memT[:, half * 4:half * 4 + 4, mt * 128:(mt + 1) * 128], pt[:], [pt], [memT])
        sq = P.sbuf("m_sq", [128, 256], BF16)
        kf = P.sbuf("m_kf", [128, 256], F32)
        rr = P.sbuf("m_rr", [128, 256], F32)
        for h in range(4):
            for kc in range(8):
                P.matmul(pa[:, 0:256], wkv[:, kc, h * 128:(h + 1) * 128], memT[:, kc, :], kc == 0, kc == 7, [wkv, memT], [pa])
            P.copy("act", kf[:], pa[:, 0:256], [pa], [kf])
            P.act(sq[:], kf[:], AF.Square, [kf], [sq])
            P.matmul(pb[:, 0:256], K.ones[:], sq[:], True, True, [K.ones, sq], [pb])
            P.act(rr[:], pb[:, 0:256], AF.Sqrt, [pb, K.eps], [rr], scale=1.0 / 128, bias=K.eps[:])
            P.recip(rr[:], rr[:], [rr], [rr])
            P.stt("dve", kT[:, h, :], kf[:], gk[:], rr[:], ALU.mult, ALU.mult, [kf, gk, rr], [kT])
        for mt in range(2):
            for kc in range(8):
                P.matmul(pa[:], memT[:, kc, mt * 128:(mt + 1) * 128], wkv[:, kc, 512:1024], kc == 0, kc == 7, [wkv, memT], [pa])
            P.memset("pool", vaug[mt][:, :, 128:129], 1.0, [vaug[mt]])
            P.copy("act", vaug[mt][:, :, 0:128], pa[:].rearrange("m (h d) -> m h d", h=4), [pa], [vaug[mt]])
        qT = [P.sbuf("m_qT%d" % i, [128, 4, 128], BF16) for i in range(2)]
        gt = [P.sbuf("m_gt%d" % i, [128, 512], BF16) for i in range(2)]
        lg = [P.psum("m_lg%d" % i, [128, 4, 128], F32) for i in range(2)]
        pT = [P.sbuf("m_pT%d" % i, [128, 4, 128], BF16) for i in range(2)]
        po = P.psum("m_po", [128, 4, 256], F32)
        rd = [P.sbuf("m_rd%d" % i, [128, 4], F32) for i in range(2)]
        yb = [P.sbuf("m_y%d" % i, [128, 512], BF16) for i in range(2)]
        yo = YOut(P, C, K, 4, "m")
        for t in range(NTR[0]):
            q, g = qT[t % 2], gt[t % 2]
            P.dma("sp", q[:], S["mq"][:, t * 128:(t + 1) * 128].rearrange("(h d) t -> d h t", d=128), writes=[q])
            P.dma("sp", g[:], S["mg"][t * 128:(t + 1) * 128, :], writes=[g])
            for mt in range(2):
                for h in range(4):
                    P.matmul(lg[mt][:, h, :], kT[:, h, mt * 128:(mt + 1) * 128], q[:, h, :], True, True, [kT, q], [lg[mt]])
                P.act(pT[mt][:], lg[mt][:], AF.Exp, [lg[mt]], [pT[mt]])
            for h in range(4):
                for mt in range(2):
                    P.matmul(po[:, h, 0:129], pT[mt][:, h, :], vaug[mt][:, h, :], mt == 0, mt == 1, [pT[mt], vaug[mt]], [po])
            r_ = rd[t % 2]
            y = yb[t % 2]
            P.recip(r_[:], po[:, :, 128], [po], [r_])
            for h in range(4):
                P.stt("dve", y[:, h * 128:(h + 1) * 128], po[:, h, 0:128], r_[:, h:h + 1], g[:, h * 128:(h + 1) * 128],
                      ALU.mult, ALU.mult, [po, r_, g], [y])
            yo.emit(y, t)


def phase_sg(P, C, K, l):
    S = C.scr
    with P.scope():
        lng = P.sbuf("b_lng", [128, 512], F32)
        lnb = P.sbuf("b_lnb", [128, 512], F32)
        P.dma("sp", lng[:], C.prm["sg_ln_g"][l:l + 1, :].to_broadcast([128, 512]), writes=[lng])
        P.dma("sp", lnb[:], C.prm["sg_ln_b"][l:l + 1, :].to_broadcast([128, 512]), writes=[lnb])
        bsT = P.sbuf("b_bsT", [128, 4], F32)
        P.dma("sp", bsT[:], C.prm["sg_b"][l].rearrange("g t -> t g"), writes=[bsT], allow_slow_non_contiguous=True)
        eps5 = P.sbuf("b_eps5", [128, 1], F32)
        P.memset("pool", eps5[:], 1e-5, [eps5])
        wf = P.sbuf("b_wf", [128, 4, 128], F32)
        wbf = P.sbuf("b_wbf", [128, 4, 128], BF16)
        WcT = P.sbuf("b_WcT", [128, 4, 128], BF16)
        pt = P.psum("b_pt", [128, 4, 128], BF16)
        P.dma("sp", wf[:], C.prm["sg_w"][l].rearrange("g t s -> t g s"), writes=[wf])
        for g in range(4):
            P.op("pool", lambda e, g=g: e.affine_select(out=wf[:, g, :], in_=wf[:, g, :], pattern=[[-1, 128]], compare_op=ALU.is_ge,
                                                        fill=0.0, base=0, channel_multiplier=1), [wf], [wf])
        P.barrier()
        P.copy("dve", wbf[:], wf[:], [wf], [wbf])
        for g in range(4):
            P.transpose(pt[:, g, :], wbf[:, g, :], K.ident[:], [wbf, K.ident], [pt])
        P.copy("dve", WcT[:], pt[:], [pt], [WcT])

        vin = [P.sbuf("b_v%d" % i, [128, 512], BF16) for i in range(2)]
        uin = [P.sbuf("b_u%d" % i, [128, 512], BF16) for i in range(2)]
        gin = [P.sbuf("b_g%d" % i, [128, 512], BF16) for i in range(2)]
        st6 = [P.sbuf("b_st%d" % i, [128, 6], F32) for i in range(2)]
        mv = [P.sbuf("b_mv%d" % i, [128, 2], F32) for i in range(2)]
        rs = [P.sbuf("b_rs%d" % i, [128, 1], F32) for i in range(2)]
        vn = [P.sbuf("b_vn%d" % i, [128, 512], F32) for i in range(2)]
        vnb = [P.sbuf("b_vnb%d" % i, [128, 512], BF16) for i in range(2)]
        mx = [P.psum("b_mx%d" % i, [128, 512], F32) for i in range(2)]
        tm = [P.sbuf("b_tm%d" % i, [128, 512], F32) for i in range(2)]
        yb = [P.sbuf("b_y%d" % i, [128, 512], BF16) for i in range(2)]
        yo = YOut(P, C, K, 1, "b")
        for t in range(NTR[0]):
            i = t % 2
            v, u, g = vin[i], uin[i], gin[i]
            sl = slice(t * 128, (t + 1) * 128)
            P.dma("sp", v[:], S["bv"][sl, :], writes=[v])
            P.dma("sp", u[:], S["bu"][sl, :], writes=[u])
            P.dma("sp", g[:], S["bg"][sl, :], writes=[g])
            P.op("dve", lambda e, i=i: e.bn_stats(st6[i][:], vin[i][:]), [v], [st6[i]])
            P.op("dve", lambda e, i=i: e.bn_aggr(mv[i][:], st6[i][:]), [st6[i]], [mv[i]])
            P.act(rs[i][:], mv[i][:, 1:2], AF.Sqrt, [mv[i], eps5], [rs[i]], bias=eps5[:])
            P.recip(rs[i][:], rs[i][:], [rs[i]], [rs[i]])
            P.ts("dve", vn[i][:], v[:], mv[i][:, 0:1], rs[i][:], ALU.subtract, ALU.mult, [v, mv[i], rs[i]], [vn[i]])
            P.tt("pool", vn[i][:], vn[i][:], lng[:], ALU.mult, [vn[i], lng], [vn[i]])
            P.tt("pool", vnb[i][:], vn[i][:], lnb[:], ALU.add, [vn[i], lnb], [vnb[i]])
            for gg in range(4):
                P.matmul(mx[i][:, gg * 128:(gg + 1) * 128], WcT[:, gg, :], vnb[i][:, gg * 128:(gg + 1) * 128], True, True,
                         [WcT, vnb[i]], [mx[i]])
            for gg in range(4):
                c = slice(gg * 128, (gg + 1) * 128)
                P.stt("dve", tm[i][:, c], mx[i][:, c], bsT[:, gg:gg + 1], u[:, c], ALU.add, ALU.mult, [mx[i], bsT, u], [tm[i]])
            P.tt("pool", yb[i][:], tm[i][:], g[:], ALU.mult, [tm[i], g], [yb[i]])
            yo.emit(yb[i], t)


def bc(ap, shape):
    return ap.to_broadcast(list(shape))


def phase_ssd(P, C, K, l):
    S = C.scr
    with P.scope():
        uinc = P.sbuf("d_uinc", [128, 128], F32)
        P.memset("pool", uinc[:], 1.0, [uinc])
        P.op("pool", lambda e: e.affine_select(out=uinc[:], in_=uinc[:], pattern=[[1, 128]], compare_op=ALU.is_ge,
                                               fill=0.0, base=0, channel_multiplier=-1), [uinc], [uinc])
        onesf = P.sbuf("d_onesf", [128, 128], F32)
        P.memset("pool", onesf[:], 1.0, [onesf])
        P.barrier()
        aB = P.sbuf("d_aB", [128, 8], F32)
        P.dma("sp", aB[:], C.prm["ssd_a_log"][l:l + 1, :].to_broadcast([128, 8]), writes=[aB])
        P.act(aB[:], aB[:], AF.Exp, [aB], [aB])
        P.ts("dve", aB[:], aB[:], -1.0, None, ALU.mult, None, [aB], [aB])
        dtb = P.sbuf("d_dtb", [128, 8], F32)
        P.dma("sp", dtb[:], C.prm["ssd_dt_bias"][l:l + 1, :].to_broadcast([128, 8]), writes=[dtb])
        dsk = P.sbuf("d_dsk", [128, 8], F32)
        P.dma("sp", dsk[:], C.prm["ssd_d"][l:l + 1, :].to_broadcast([128, 8]), writes=[dsk])
        ngB = P.sbuf("d_ngB", [128, 512], F32)
        P.dma("sp", ngB[:], C.prm["ssd_norm_g"][l:l + 1, :].to_broadcast([128, 512]), writes=[ngB])
        S32 = P.sbuf("d_S32", [128, 512], F32)
        Sbf = P.sbuf("d_Sbf", [128, 512], BF16)
        P.memset("pool", S32[:], 0.0, [S32])
        P.memset("pool", Sbf[:], 0.0, [Sbf])

        xf = [P.sbuf("d_xf%d" % i, [128, 8, 128], BF16) for i in range(2)]
        dtin = [P.sbuf("d_dtin%d" % i, [128, 8], F32) for i in range(2)]
        zg = [P.sbuf("d_zg%d" % i, [128, 512], BF16) for i in range(2)]
        dt = P.sbuf("d_dt", [128, 8], F32)
        la = P.sbuf("d_la", [128, 8], F32)
        lab = P.sbuf("d_lab", [128, 8, 128], F32)
        cs = P.sbuf("d_cs", [128, 16], F32)
        pre = P.sbuf("d_pre", [128, 24], F32)
        E = P.sbuf("d_E", [128, 24], F32)
        seg = P.sbuf("d_seg", [128, 8, 128], F32)
        Lm = P.sbuf("d_L", [128, 8, 128], F32)
        MT = P.sbuf("d_MT", [128, 8, 128], BF16)
        cbm = P.sbuf("d_cbm", [128, 2, 128], F32)
        xs = P.sbuf("d_xs", [128, 512], BF16)
        btm = P.sbuf("d_btm", [128, 2, 128], BF16)
        xdt = P.sbuf("d_xdt", [128, 512], BF16)
        xdtd = P.sbuf("d_xdtd", [128, 512], BF16)
        t1 = P.sbuf("d_t1", [128, 512], F32)
        y1 = P.sbuf("d_y1", [128, 512], F32)
        y2 = P.sbuf("d_y2", [128, 512], F32)
        junk = P.sbuf("d_junk", [128, 512], BF16)
        ssq = P.sbuf("d_ssq", [128, 1], F32)
        yb = [P.sbuf("d_yb%d" % i, [128, 512], BF16) for i in range(2)]

        small = P.psum("d_small", [128, 512], F32)
        csps = View(small, small[:, 0:16])
        cbps = View(small, small[:, 128:384])
        csrow = P.psum("d_csrow", [128, 8, 128], F32)
        tp = P.psum("d_tp", [128, 768], BF16)
        yps = P.psum("d_yps", [128, 512], F32)
        yoff = P.psum("d_yoff", [128, 512], F32)
        stp = P.psum("d_stp", [128, 512], F32)
        yo = YOut(P, C, K, 3, "d")

        for t in range(NTR[0]):
            i = t % 2
            sl = slice(t * 128, (t + 1) * 128)
            x_ = xf[i]
            P.dma("sp", x_[:], S["xbc"][:, sl].rearrange("(c p) t -> p c t", p=128), writes=[x_])
            P.dma("sp", dtin[i][:], S["misc"][sl, 16:24], writes=[dtin[i]])
            P.dma("sp", zg[i][:], S["dz"][sl, :], writes=[zg[i]])
            P.tt("dve", dt[:], dtin[i][:], dtb[:], ALU.add, [dtin[i], dtb], [dt])
            P.act(dt[:], dt[:], AF.Exp, [dt], [dt])
            P.act(dt[:], dt[:], AF.Ln, [dt], [dt], bias=1.0)
            P.tt("dve", la[:], dt[:], aB[:], ALU.mult, [dt, aB], [la])
            if STOP[0] <= 1:
                continue
            P.matmul(csps[:, 0:8], uinc[:], la[:], True, True, [uinc, la], [csps])
            P.matmul(csps[:, 8:16], onesf[:], la[:], True, True, [onesf, la], [csps])
            P.copy("dve", cs[:], csps[:], [csps], [cs])
            if STOP[0] <= 2:
                continue
            P.copy("dve", lab[:], bc(la[:].unsqueeze(2), [128, 8, 128]), [la], [lab])
            for h in range(8):
                P.matmul(csrow[:, h, :], lab[:, h, :], uinc[:], True, True, [lab, uinc], [csrow])
            if STOP[0] <= 3:
                continue
            P.tt("dve", pre[:, 0:8], cs[:, 8:16], cs[:, 0:8], ALU.subtract, [cs], [pre])
            P.copy("dve", pre[:, 8:16], cs[:, 8:16], [cs], [pre])
            P.copy("dve", pre[:, 16:24], cs[:, 0:8], [cs], [pre])
            P.act(E[:], pre[:], AF.Exp, [pre], [E])
            if STOP[0] <= 4:
                continue
            P.tt("dve", seg[:], csrow[:], bc(cs[:, 0:8].unsqueeze(2), [128, 8, 128]), ALU.subtract, [csrow, cs], [seg])
            P.ts("dve", seg[:], seg[:], 0.0, None, ALU.min, None, [seg], [seg])
            P.act(Lm[:], seg[:], AF.Exp, [seg], [Lm])
            if STOP[0] <= 5:
                continue
            for g in range(2):
                P.matmul(cbps[:, g * 128:(g + 1) * 128], x_[:, 4 + g, :], x_[:, 6 + g, :], True, True, [x_], [cbps])
            P.tt("dve", cbm[:], cbps[:].rearrange("s (g c) -> s g c", g=2), bc(uinc[:].unsqueeze(1), [128, 2, 128]), ALU.mult,
                 [cbps, uinc], [cbm])
            for g in range(2):
                P.tt("dve", MT[:, g * 4:(g + 1) * 4, :], Lm[:, g * 4:(g + 1) * 4, :], bc(cbm[:, g:g + 1, :], [128, 4, 128]), ALU.mult,
                     [Lm, cbm], [MT])
            if STOP[0] <= 6:
                continue
            for c in range(4):
                P.transpose(tp[:, c * 128:(c + 1) * 128], x_[:, c, :], K.ident[:], [x_, K.ident], [tp])
            for g in range(2):
                P.transpose(tp[:, 512 + g * 128:512 + (g + 1) * 128], x_[:, 4 + g, :], K.ident[:], [x_, K.ident], [tp])
            P.copy("act", xs[:], tp[:, 0:512], [tp], [xs])
            P.copy("act", btm[:], tp[:, 512:768].rearrange("s (g d) -> s g d", g=2), [tp], [btm])
            xs3 = xs[:].rearrange("s (h p) -> s h p", h=8)
            P.tt("dve", xdt[:].rearrange("s (h p) -> s h p", h=8), xs3, bc(dt[:].unsqueeze(2), [128, 8, 64]), ALU.mult, [xs, dt], [xdt])
            P.tt("dve", xdtd[:].rearrange("s (h p) -> s h p", h=8), xdt[:].rearrange("s (h p) -> s h p", h=8),
                 bc(E[:, 0:8].unsqueeze(2), [128, 8, 64]), ALU.mult, [xdt, E], [xdtd])
            if STOP[0] <= 7:
                continue
            for h in range(8):
                P.matmul(yps[:, h * 64:(h + 1) * 64], MT[:, h, :], xdt[:, h * 64:(h + 1) * 64], True, True, [MT, xdt], [yps])
            for g in range(2):
                P.matmul(yoff[:, g * 256:(g + 1) * 256], x_[:, 6 + g, :], Sbf[:, g * 256:(g + 1) * 256], True, True, [x_, Sbf], [yoff])
            P.tt("dve", t1[:].rearrange("s (h p) -> s h p", h=8), yoff[:].rearrange("s (h p) -> s h p", h=8),
                 bc(E[:, 16:24].unsqueeze(2), [128, 8, 64]), ALU.mult, [yoff, E], [t1])
            P.tt("dve", y1[:], yps[:], t1[:], ALU.add, [yps, t1], [y1])
            P.tt("pool", t1[:].rearrange("s (h p) -> s h p", h=8), xs3, bc(dsk[:].unsqueeze(2), [128, 8, 64]), ALU.mult, [xs, dsk], [t1])
            P.tt("pool", y1[:], y1[:], t1[:], ALU.add, [y1, t1], [y1])
            P.tt("pool", y2[:], y1[:], zg[i][:], ALU.mult, [y1, zg[i]], [y2])
            P.act(junk[:], y2[:], AF.Square, [y2], [junk, ssq], accum_out=ssq[:])
            P.act(ssq[:], ssq[:], AF.Sqrt, [ssq, K.eps], [ssq], scale=1.0 / 512, bias=K.eps[:])
            P.recip(ssq[:], ssq[:], [ssq], [ssq])
            P.stt("dve", yb[i][:], y2[:], ssq[:], ngB[:], ALU.mult, ALU.mult, [y2, ssq, ngB], [yb[i]])
            yo.emit(yb[i], t)
            if STOP[0] <= 8:
                continue
            for g in range(2):
                P.matmul(stp[:, g * 256:(g + 1) * 256], btm[:, g, :], xdtd[:, g * 256:(g + 1) * 256], True, True, [btm, xdtd], [stp])
            P.tt("dve", S32[:].rearrange("d (h p) -> d h p", h=8), S32[:].rearrange("d (h p) -> d h p", h=8),
                 bc(E[:, 8:16].unsqueeze(2), [128, 8, 64]), ALU.mult, [S32, E], [S32])
            P.tt("dve", S32[:], S32[:], stp[:], ALU.add, [S32, stp], [S32])
            P.copy("act", Sbf[:], S32[:], [S32], [Sbf])


NBIS = 18


def phase_dsa(P, C, K, l):
    S = C.scr
    I32 = mybir.dt.int32
    with P.scope():
        rb = [P.sbuf("c_rb%d" % i, [1, T], BF16) for i in range(3)]
        P.op("pool", lambda e: e.iota(rb[0][:], pattern=[[0, NT], [1, 128]], base=0, channel_multiplier=0,
                                      allow_small_or_imprecise_dtypes=True), (), [rb[0]])
        P.op("pool", lambda e: e.iota(rb[1][:], pattern=[[1, NT], [0, 128]], base=0, channel_multiplier=0,
                                      allow_small_or_imprecise_dtypes=True), (), [rb[1]])
        P.memset("pool", rb[2][:], 1.0, [rb[2]])
        posrow = P.sbuf("c_posrow", [128, T], F32)
        P.op("pool", lambda e: e.iota(posrow[:], pattern=[[1, T]], base=0, channel_multiplier=0,
                                      allow_small_or_imprecise_dtypes=True), (), [posrow])
        sl1 = P.sbuf("c_sl1", [1, 8, 128], BF16)
        sl128 = P.sbuf("c_sl128", [1, 8, 128], BF16)
        nsl64 = P.sbuf("c_nsl64", [1, 8, 128], F32)
        nsl1 = P.sbuf("c_nsl1", [1, 8, 128], F32)
        for h in range(8):
            sl = 2.0 ** -(h + 1)
            P.memset("pool", sl1[:, h, :], sl, [sl1])
            P.memset("pool", sl128[:, h, :], 128 * sl, [sl128])
            P.memset("pool", nsl64[:, h, :], -64 * sl, [nsl64])
            P.memset("pool", nsl1[:, h, :], -sl, [nsl1])
        idf = P.sbuf("c_idf", [128, 128], F32)
        P.make_identity(idf)
        clow = P.sbuf("c_clow", [128, 128], BF16)
        P.memset("pool", clow[:], 1.0, [clow])
        P.op("pool", lambda e: e.affine_select(out=clow[:], in_=clow[:], pattern=[[-1, 128]], compare_op=ALU.is_ge,
                                               fill=0.0, base=0, channel_multiplier=1), [clow], [clow])
        cneg = P.sbuf("c_cneg", [128, 128], F32)
        P.memset("pool", cneg[:], 0.0, [cneg])
        P.op("pool", lambda e: e.affine_select(out=cneg[:], in_=cneg[:], pattern=[[-1, 128]], compare_op=ALU.is_ge,
                                               fill=-1e30, base=0, channel_multiplier=1), [cneg], [cneg])
        pw = P.sbuf("c_pw", [128, NBIS], F32)
        for k in range(NBIS):
            P.memset("pool", pw[:, k:k + 1], 2.0 ** -(k + 1), [pw])
        vaug = P.sbuf("c_vaug", [128, NT, 65], BF16)
        P.memset("pool", vaug[:, :, 64:65], 1.0, [vaug])
        P.barrier()
        kTa = P.sbuf("c_kTa", [68, T], BF16)
        ikT = P.sbuf("c_ikT", [64, T], BF16)
        P.dma("sp", kTa[0:64, :], S["ck"], writes=[kTa])
        P.dma("sp", ikT[:], S["cik"], writes=[ikT])
        P.dma("sp", kTa[64:65, :], rb[0][:], reads=[rb[0]], writes=[kTa])
        P.dma("sp", kTa[65:66, :], rb[1][:], reads=[rb[1]], writes=[kTa])
        P.dma("sp", kTa[66:67, :], rb[2][:], reads=[rb[2]], writes=[kTa])
        P.dma("sp", kTa[67:68, :], rb[2][:], reads=[rb[2]], writes=[kTa])
        for s4 in range(0, NT, 4):
            P.dma("pool", vaug[:, s4:s4 + 4, 0:64], S["misc"][s4 * 128:(s4 + 4) * 128, 24:88].rearrange("(st s) d -> s st d", s=128),
                  writes=[vaug])

        iqT = [P.sbuf("c_iqT%d" % i, [64, 8, 128], BF16) for i in range(2)]
        qTa = [P.sbuf("c_qTa%d" % i, [68, 8, 128], BF16) for i in range(2)]
        for i in range(2):
            P.dma("sp", qTa[i][64:65], sl1[:], reads=[sl1], writes=[qTa[i]])
            P.dma("sp", qTa[i][65:66], sl128[:], reads=[sl128], writes=[qTa[i]])
        iw = [P.sbuf("c_iw%d" % i, [128, 8], F32) for i in range(2)]
        gt = [P.sbuf("c_gt%d" % i, [128, 512], BF16) for i in range(2)]
        iwa = P.sbuf("c_iwa", [128, 8], F32)
        iws = P.sbuf("c_iws", [128, 8], F32)
        sidx = P.sbuf("c_sidx", [128, T], F32)
        junk = P.sbuf("c_junk", [128, T], F32)
        M01 = P.sbuf("c_M01", [128, T], BF16)
        M01T = P.sbuf("c_M01T", [128, NT, 128], BF16)
        rl = [P.sbuf("c_rl%d" % i, [128, 512], F32) for i in range(4)]
        ex = [P.sbuf("c_ex%d" % i, [128, 8, 128], BF16) for i in range(3)]
        pT = [P.sbuf("c_pT%d" % i, [128, 8, 128], BF16) for i in range(3)]
        mn = P.sbuf("c_mn", [128, 1], F32)
        mxv = P.sbuf("c_mx", [128, 1], F32)
        W = P.sbuf("c_W", [128, NBIS], F32)
        lo = P.sbuf("c_lo", [128, 1], F32)
        mid = P.sbuf("c_mid", [128, 1], F32)
        cnt = P.sbuf("c_cnt", [128, 1], F32)
        dd = P.sbuf("c_dd", [128, 1], F32)
        smax = P.sbuf("c_smax", [128, 1], F32)
        smaxb = P.sbuf("c_smaxb", [128, 32], F32)
        srow_i = P.sbuf("c_srow_i", [1, 128], I32)
        nhi_i = P.sbuf("c_nhi_i", [1, 128], I32)
        nlo_i = P.sbuf("c_nlo_i", [1, 128], I32)
        nhi_f = P.sbuf("c_nhi_f", [1, 128], F32)
        nlo_f = P.sbuf("c_nlo_f", [1, 128], F32)
        r2 = [P.sbuf("c_r2%d" % i, [1, 8, 128], BF16) for i in range(2)]
        r3 = [P.sbuf("c_r3%d" % i, [1, 8, 128], BF16) for i in range(2)]
        rden = P.sbuf("c_rden", [128, 8], F32)
        ot = P.sbuf("c_ot", [128, 512], F32)
        yb = [P.sbuf("c_yb%d" % i, [128, 512], BF16) for i in range(2)]

        sps = [P.psum("c_sps%d" % i, [128, 512], F32) for i in range(2)]
        lgh = [P.psum("c_lg%d" % i, [128, 4, 128], F32) for i in range(3)]
        ops_ = P.psum("c_ops", [128, 8, 128], F32)
        mty = P.psum("c_mty", [128, 8, 128], BF16)
        mtp = View(mty, mty[:, 0:4, :])
        yo = YOut(P, C, K, 2, "c", ps=[View(mty, mty[:, 4:8, :])])
        IWS = (8 ** -0.5) * (64 ** -0.5)
        nsp = 0
        def front_fn(t):
            nonlocal nsp
            i = t % 2
            sl = slice(t * 128, (t + 1) * 128)
            nk = t + 1
            Lk = nk * 128
            P.dma("sp", iqT[i][:], S["ciq"][:, sl].rearrange("(h d) t -> d h t", d=64), writes=[iqT[i]])
            P.dma("sp", qTa[i][0:64], S["cq"][:, sl].rearrange("(h d) t -> d h t", d=64), writes=[qTa[i]])
            P.dma("sp", iw[i][:], S["misc"][sl, 8:16], writes=[iw[i]])
            P.dma("sp", gt[i][:], S["cg"][sl, :], writes=[gt[i]])
            if STOP[0] <= 1:
                return
            if t >= 2:
                P.act(iwa[:], iw[i][:], AF.Abs, [iw[i]], [iwa], scale=IWS)
                P.act(iws[:], iw[i][:], AF.Sign, [iw[i]], [iws])
                for c0 in range(0, Lk, 512):
                    yield
                    w_ = min(512, Lk - c0)
                    for h in range(8):
                        sp_ = sps[nsp % 2]
                        r_ = rl[nsp % 4]
                        nsp += 1
                        P.matmul(sp_[:, 0:w_], iqT[i][:, h, :], ikT[:, c0:c0 + w_], True, True, [iqT[i], ikT], [sp_])
                        P.act(r_[:, 0:w_], sp_[:, 0:w_], AF.Relu, [sp_, iwa], [r_], scale=iwa[:, h:h + 1])
                        if h == 0:
                            P.ts("dve", sidx[:, c0:c0 + w_], r_[:, 0:w_], iws[:, 0:1], None, ALU.mult, None, [r_, iws], [sidx])
                        else:
                            P.stt("dve", sidx[:, c0:c0 + w_], r_[:, 0:w_], iws[:, h:h + 1], sidx[:, c0:c0 + w_], ALU.mult, ALU.add,
                                  [r_, iws, sidx], [sidx])
                P.op("dve", lambda e, Lk=Lk: e.tensor_reduce(mn[:], sidx[:, 0:Lk], AX.X, ALU.min), [sidx], [mn])
                P.op("dve", lambda e, Lk=Lk: e.tensor_reduce(mxv[:], sidx[:, 0:Lk], AX.X, ALU.max), [sidx], [mxv])
                P.tt("dve", sidx[:, t * 128:Lk], sidx[:, t * 128:Lk], cneg[:], ALU.add, [sidx, cneg], [sidx])
                P.tt("dve", mxv[:], mxv[:], mn[:], ALU.subtract, [mxv, mn], [mxv])
                P.ts("dve", W[:], pw[:], mxv[:], None, ALU.mult, None, [pw, mxv], [W])
                P.tt("dve", mid[:], mn[:], W[:, 0:1], ALU.add, [mn, W], [mid])
                for k in range(NBIS):
                    yield
                    P.ts("dve", junk[:, 0:Lk], sidx[:, 0:Lk], mid[:], None, ALU.is_ge, ALU.add, [sidx, mid], [junk, cnt],
                         accum_out=cnt[:])
                    P.ts("dve", dd[:], cnt[:], 255.5, 0.5, ALU.is_ge, ALU.subtract, [cnt], [dd])
                    if k < NBIS - 1:
                        P.stt("dve", mid[:], dd[:], W[:, k:k + 1], mid[:], ALU.mult, ALU.add, [dd, W, mid], [mid])
                    else:
                        P.ts("dve", dd[:], dd[:], 0.5, None, ALU.subtract, None, [dd], [dd])
                        P.stt("dve", lo[:], dd[:], W[:, k:k + 1], mid[:], ALU.mult, ALU.add, [dd, W, mid], [lo])
                P.ts("dve", M01[:, 0:Lk], sidx[:, 0:Lk], lo[:], None, ALU.is_ge, None, [sidx, lo], [M01])
            else:
                if t > 0:
                    P.memset("pool", M01[:, 0:t * 128], 1.0, [M01])
                P.copy("pool", M01[:, t * 128:Lk], clow[:], [clow], [M01])
            if STOP[0] <= 2:
                return
            P.tt("dve", junk[:, 0:Lk], M01[:, 0:Lk], posrow[:, 0:Lk], ALU.mult, [M01, posrow], [junk])
            P.op("dve", lambda e, Lk=Lk: e.tensor_reduce(smax[:], junk[:, 0:Lk], AX.X, ALU.max), [junk], [smax])
            srp = sps[nsp % 2]
            nsp += 1
            P.copy("dve", smaxb[:], bc(smax[:], [128, 32]), [smax], [smaxb])
            P.matmul(srp[0:32, 0:128], smaxb[:], idf[:], True, True, [smaxb, idf], [srp])
            if STOP[0] <= 2.5:
                return
            P.copy("dve", srow_i[:], srp[0:1, 0:128], [srp], [srow_i])
            P.ts("dve", nhi_i[:], srow_i[:], 6, None, ALU.arith_shift_right, None, [srow_i], [nhi_i])
            P.ts("dve", nlo_i[:], srow_i[:], 63, None, ALU.bitwise_and, None, [srow_i], [nlo_i])
            P.copy("dve", nhi_f[:], nhi_i[:], [nhi_i], [nhi_f])
            P.copy("dve", nlo_f[:], nlo_i[:], [nlo_i], [nlo_f])
            P.tt("dve", r2[i][:], nsl64[:], bc(nhi_f[:].unsqueeze(1), [1, 8, 128]), ALU.mult, [nsl64, nhi_f], [r2[i]])
            P.tt("dve", r3[i][:], nsl1[:], bc(nlo_f[:].unsqueeze(1), [1, 8, 128]), ALU.mult, [nsl1, nlo_f], [r3[i]])
            if STOP[0] <= 2.7:
                return
            P.dma("sp", qTa[i][66:67], r2[i][:], reads=[r2[i]], writes=[qTa[i]])
            P.dma("sp", qTa[i][67:68], r3[i][:], reads=[r3[i]], writes=[qTa[i]])
            if STOP[0] <= 3:
                return
        def back_fn(t):
            i = t % 2
            sl = slice(t * 128, (t + 1) * 128)
            nk = t + 1
            Lk = nk * 128
            for k0 in range(0, nk, 4):
                n4 = min(4, nk - k0)
                for j in range(n4):
                    P.transpose(mtp[:, j, :], M01[:, (k0 + j) * 128:(k0 + j + 1) * 128], K.ident[:], [M01, K.ident], [mtp])
                P.copy("act", M01T[:, k0:k0 + n4, :], mtp[:, 0:n4, :], [mtp], [M01T])
            if STOP[0] <= 4:
                return
            def qk(kt_, half):
                lb = lgh[(2 * kt_ + half) % 3]
                P.matmul(lb[:], kTa[:, kt_ * 128:(kt_ + 1) * 128],
                         qTa[i][:, half * 4:(half + 1) * 4, :], True, True, [kTa, qTa[i]], [lb])

            qk(0, 0)
            qk(0, 1)
            for kt in range(nk):
                yield
                e_ = ex[kt % 3]
                p_ = pT[kt % 3]
                if kt + 1 < nk:
                    qk(kt + 1, 0)
                for half in range(2):
                    lb = lgh[(2 * kt + half) % 3]
                    P.act(e_[:, half * 4:(half + 1) * 4, :], lb[:], AF.Exp, [lb], [e_])
                if kt + 1 < nk:
                    qk(kt + 1, 1)
                P.stt("dve", p_[:], e_[:], 3.0e38, bc(M01T[:, kt:kt + 1, :], [128, 8, 128]), ALU.min, ALU.mult,
                      [e_, M01T], [p_])
                for h in range(8):
                    P.matmul(ops_[:, h, 0:65], p_[:, h, :], vaug[:, kt, :], kt == 0 and h % 4 == 0, kt == nk - 1 and h % 4 == 3,
                             [p_, vaug], [ops_])
            if STOP[0] <= 5:
                return
            P.recip(rden[:], ops_[:, :, 64], [ops_], [rden])
            P.tt("dve", ot[:].rearrange("t (h d) -> t h d", h=8), ops_[:, :, 0:64], bc(rden[:].unsqueeze(2), [128, 8, 64]), ALU.mult,
                 [ops_, rden], [ot])
            P.tt("pool", yb[i][:], ot[:], gt[i][:], ALU.mult, [ot, gt[i]], [yb[i]])
            yo.emit(yb[i], t)

        def drain(g):
            for _ in g:
                pass

        def interleave(ga, gb):
            a_live, b_live = True, True
            while a_live or b_live:
                if b_live:
                    try:
                        next(gb)
                    except StopIteration:
                        b_live = False
                if a_live:
                    try:
                        next(ga)
                    except StopIteration:
                        a_live = False

        NTT = NTR[0]
        drain(front_fn(0))
        for t in range(NTT):
            if t + 1 < NTT:
                interleave(front_fn(t + 1), back_fn(t))
            else:
                drain(back_fn(t))


def phase_gdn(P, C, K, l):
    S = C.scr
    NCH = NTR[0] * 2
    with P.scope():
        uinc = P.sbuf("a_uinc", [128, 128], F32)
        P.memset("pool", uinc[:], 1.0, [uinc])
        P.op("pool", lambda e: e.affine_select(out=uinc[:], in_=uinc[:], pattern=[[1, 128]], compare_op=ALU.is_ge,
                                               fill=0.0, base=0, channel_multiplier=-1), [uinc], [uinc])
        lstr = P.sbuf("a_lstr", [128, 128], F32)
        P.memset("pool", lstr[:], 1.0, [lstr])
        P.op("pool", lambda e: e.affine_select(out=lstr[:], in_=lstr[:], pattern=[[-1, 128]], compare_op=ALU.is_gt,
                                               fill=0.0, base=0, channel_multiplier=1), [lstr], [lstr])
        idf = P.sbuf("a_idf", [128, 128], F32)
        P.make_identity(idf)
        onesf = P.sbuf("a_onesf", [128, 128], F32)
        P.memset("pool", onesf[:], 1.0, [onesf])
        P.barrier()
        aB = P.sbuf("a_aB", [64, 4], F32)
        P.dma("sp", aB[:], C.prm["gdn_a_log"][l:l + 1, :].to_broadcast([64, 4]), writes=[aB])
        P.act(aB[:], aB[:], AF.Exp, [aB], [aB])
        P.ts("dve", aB[:], aB[:], -1.0, None, ALU.mult, None, [aB], [aB])
        dtb = P.sbuf("a_dtb", [64, 4], F32)
        P.dma("sp", dtb[:], C.prm["gdn_dt_bias"][l:l + 1, :].to_broadcast([64, 4]), writes=[dtb])
        ng = P.sbuf("a_ng", [64, 128], F32)
        P.dma("sp", ng[:], C.prm["gdn_norm_g"][l:l + 1, :].to_broadcast([64, 128]), writes=[ng])
        ST32 = P.sbuf("a_ST32", [128, 4, 128], F32)
        STb = P.sbuf("a_STb", [128, 4, 128], BF16)
        P.memset("pool", ST32[:], 0.0, [ST32])
        P.memset("pool", STb[:], 0.0, [STb])

        def dbl(name, shape, dt):
            return [P.sbuf("%s%d" % (name, i), shape, dt) for i in range(2)]

        qT, kT, vT = dbl("a_qT", [128, 4, 64], BF16), dbl("a_kT", [128, 4, 64], BF16), dbl("a_vT", [128, 4, 64], BF16)
        ab, gate = dbl("a_ab", [64, 8], F32), dbl("a_gate", [64, 512], BF16)
        sp_, gl, be = dbl("a_sp", [64, 4], F32), dbl("a_gl", [64, 4], F32), dbl("a_be", [64, 4], F32)
        gc, gt128, cd128 = dbl("a_gc", [64, 4], F32), dbl("a_gt", [128, 4], F32), dbl("a_cd", [128, 4], F32)
        lab = dbl("a_lab", [64, 4, 128], F32)
        pre, E, bE = dbl("a_pre", [64, 8], F32), dbl("a_E", [64, 8], F32), dbl("a_bE", [64, 4], F32)
        seg, nseg = dbl("a_seg", [64, 4, 64], F32), dbl("a_nseg", [64, 4, 64], F32)
        dI, dL = dbl("a_dI", [64, 4, 64], F32), dbl("a_dL", [64, 4, 64], F32)
        egr = dbl("a_egr", [128, 4, 64], F32)
        qdT = dbl("a_qdT", [128, 4, 64], BF16)
        qkT = dbl("a_qkT", [64, 4, 64], BF16)
        Nm = [dbl("a_N", [64, 4, 64], F32), dbl("a_N2", [64, 4, 64], F32)]
        Mm = [dbl("a_M", [64, 4, 64], F32), dbl("a_M2", [64, 4, 64], F32)]
        X, Xb = dbl("a_X", [64, 4, 64], F32), dbl("a_Xb", [64, 4, 64], BF16)
        kb, kdec, vb = dbl("a_kb", [64, 4, 128], BF16), dbl("a_kdec", [64, 4, 128], BF16), dbl("a_vb", [64, 4, 128], BF16)
        wkT, u0 = dbl("a_wkT", [128, 4, 64], BF16), dbl("a_u0", [64, 4, 128], F32)
        u = dbl("a_u", [64, 4, 128], BF16)
        o, sq = dbl("a_o", [64, 4, 128], F32), dbl("a_sq", [64, 4, 128], F32)
        ssq = dbl("a_ssq", [64, 4], F32)
        y = dbl("a_y", [64, 512], F32)
        ysb = dbl("a_ysb", [128, 4, 64], BF16)

        b0 = P.psum("a_b0", [128, 512], F32)
        gcrow, wk_ps = View(b0, b0[:, 0:256].rearrange("p (h c) -> p h c", h=4)), View(b0, b0[:, 256:512].rearrange("p (h c) -> p h c", h=4))
        b1 = P.psum("a_b1", [128, 512], F32)
        sm_ps, ytp = View(b1, b1[:, 0:16]), View(b1, b1[:, 256:512].rearrange("p (h c) -> p h c", h=4))
        b2 = P.psum("a_b2", [64, 512], F32)
        kk_ps, qk_ps = View(b2, b2[:, 0:256].rearrange("p (h c) -> p h c", h=4)), View(b2, b2[:, 256:512].rearrange("p (h c) -> p h c", h=4))
        b3 = P.psum("a_b3", [64, 512], F32)
        P_ps, Q_ps = View(b3, b3[:, 0:256].rearrange("p (h c) -> p h c", h=4)), View(b3, b3[:, 256:512].rearrange("p (h c) -> p h c", h=4))
        b4 = P.psum("a_b4", [64, 512], F32)
        XP_ps, M_ps = View(b4, b4[:, 0:256].rearrange("p (h c) -> p h c", h=4)), View(b4, b4[:, 256:512].rearrange("p (h c) -> p h c", h=4))
        kv_ps = P.psum("a_kvps", [64, 2, 512], BF16)
        ktm, vtm = View(kv_ps, kv_ps[:, 0, :].rearrange("p (h d) -> p h d", h=4)), View(kv_ps, kv_ps[:, 1, :].rearrange("p (h d) -> p h d", h=4))
        uwo = P.psum("a_uwo", [64, 4, 128], F32)
        Sn_ps = P.psum("a_Sn", [128, 4, 128], F32)
        u64, i64 = uinc[0:64, 0:64], idf[0:64, 0:64]

        def b4c(ap, n):
            return bc(ap.unsqueeze(2), [ap.shape[0], 4, n])

        def pre_fn(ch):
            i = ch % 2
            cs = slice(ch * 64, (ch + 1) * 64)
            for dst, nm in ((qT[i], "gq"), (kT[i], "gk"), (vT[i], "gv")):
                P.dma("sp", dst[:], S[nm][:, cs].rearrange("(h d) t -> d h t", d=128), writes=[dst])
            P.dma("sp", ab[i][:], S["misc"][cs, 0:8], writes=[ab[i]])
            P.dma("sp", gate[i][:], S["ag"][cs, :], writes=[gate[i]])
            P.tt("dve", sp_[i][:], ab[i][:, 0:4], dtb[:], ALU.add, [ab[i], dtb], [sp_[i]])
            P.act(sp_[i][:], sp_[i][:], AF.Exp, [sp_[i]], [sp_[i]])
            P.act(sp_[i][:], sp_[i][:], AF.Ln, [sp_[i]], [sp_[i]], bias=1.0)
            P.tt("dve", gl[i][:], sp_[i][:], aB[:], ALU.mult, [sp_[i], aB], [gl[i]])
            P.act(be[i][:], ab[i][:, 4:8], AF.Exp, [ab[i]], [be[i]], scale=-1.0)
            P.ts("dve", be[i][:], be[i][:], 1.0, None, ALU.add, None, [be[i]], [be[i]])
            P.recip(be[i][:], be[i][:], [be[i]], [be[i]])
            yield
            P.matmul(sm_ps[0:64, 0:4], u64, gl[i][:], True, True, [uinc, gl[i]], [sm_ps])
            P.matmul(sm_ps[:, 4:8], onesf[0:64, :], gl[i][:], True, True, [onesf, gl[i]], [sm_ps])
            P.copy("dve", gc[i][:], sm_ps[0:64, 0:4], [sm_ps], [gc[i]])
            P.copy("dve", gt128[i][:], sm_ps[:, 4:8], [sm_ps], [gt128[i]])
            P.copy("dve", lab[i][:], b4c(gl[i][:], 128), [gl[i]], [lab[i]])
            for h in range(4):
                P.matmul(gcrow[:, h, :], lab[i][:, h, :], u64, True, True, [lab[i], uinc], [gcrow])
            P.copy("dve", pre[i][:, 0:4], gc[i][:], [gc[i]], [pre[i]])
            P.tt("dve", pre[i][:, 4:8], gt128[i][0:64, :], gc[i][:], ALU.subtract, [gt128[i], gc[i]], [pre[i]])
            P.act(E[i][:], pre[i][:], AF.Exp, [pre[i]], [E[i]])
            P.act(cd128[i][:], gt128[i][:], AF.Exp, [gt128[i]], [cd128[i]])
            P.tt("dve", bE[i][:], be[i][:], E[i][:, 0:4], ALU.mult, [be[i], E[i]], [bE[i]])
            if STOP[0] <= 1:
                return
            yield
            P.tt("dve", seg[i][:], gcrow[0:64], b4c(gc[i][:], 64), ALU.subtract, [gcrow, gc[i]], [seg[i]])
            P.ts("dve", nseg[i][:], seg[i][:], -1.0, 0.0, ALU.mult, ALU.min, [seg[i]], [nseg[i]])
            P.ts("dve", seg[i][:], seg[i][:], 0.0, None, ALU.min, None, [seg[i]], [seg[i]])
            P.act(dI[i][:], seg[i][:], AF.Exp, [seg[i]], [dI[i]])
            P.act(dL[i][:], nseg[i][:], AF.Exp, [nseg[i]], [dL[i]])
            P.tt("dve", dI[i][:], dI[i][:], bc(u64.unsqueeze(1), [64, 4, 64]), ALU.mult, [dI[i], uinc], [dI[i]])
            P.tt("dve", dL[i][:], dL[i][:], bc(lstr[0:64, 0:64].unsqueeze(1), [64, 4, 64]), ALU.mult, [dL[i], lstr], [dL[i]])
            P.act(egr[i][:], gcrow[:], AF.Exp, [gcrow], [egr[i]])
            P.tt("dve", qdT[i][:], qT[i][:], egr[i][:], ALU.mult, [qT[i], egr[i]], [qdT[i]])
            if STOP[0] <= 2:
                return
            yield
            for h in range(4):
                P.matmul(kk_ps[:, h, :], kT[i][:, h, :], kT[i][:, h, :], True, True, [kT[i]], [kk_ps])
            for h in range(4):
                P.matmul(qk_ps[:, h, :], kT[i][:, h, :], qT[i][:, h, :], True, True, [kT[i], qT[i]], [qk_ps])
            N0, M0 = Nm[0][i], Mm[0][i]
            P.tt("dve", N0[:], kk_ps[:], dL[i][:], ALU.mult, [kk_ps, dL[i]], [N0])
            P.tt("dve", N0[:], N0[:], b4c(be[i][:], 64), ALU.mult, [N0, be[i]], [N0])
            P.tt("dve", qkT[i][:], qk_ps[:], dI[i][:], ALU.mult, [qk_ps, dI[i]], [qkT[i]])
            if STOP[0] <= 3:
                return
            for h in range(4):
                P.transpose(M_ps[:, h, :], N0[:, h, :], i64, [N0, idf], [M_ps])
            P.copy("act", M0[:], M_ps[:], [M_ps], [M0])
            if STOP[0] <= 4:
                return
            yield
            P.tt("dve", X[i][:], bc(i64.unsqueeze(1), [64, 4, 64]), M0[:], ALU.subtract, [idf, M0], [X[i]])
            Pc, Qc = N0, M0
            if STOP[0] <= 4.1:
                return
            for st in range(1, 6):
                if STOP[0] <= 4.2 and st > 1:
                    break
                Pn, Qn = Nm[st % 2][i], Mm[st % 2][i]
                for h in range(4):
                    P.matmul(P_ps[:, h, :], Qc[:, h, :], Pc[:, h, :], True, True, [Qc, Pc], [P_ps])
                if st < 5:
                    for h in range(4):
                        P.matmul(Q_ps[:, h, :], Pc[:, h, :], Qc[:, h, :], True, True, [Qc, Pc], [Q_ps])
                if STOP[0] <= 4.15:
                    break
                P.copy("act", Pn[:], P_ps[:], [P_ps], [Pn])
                if st < 5:
                    P.copy("dve", Qn[:], Q_ps[:], [Q_ps], [Qn])
                if STOP[0] <= 4.17:
                    break
                for h in range(4):
                    P.matmul(XP_ps[:, h, :], Pn[:, h, :], X[i][:, h, :], True, True, [Pn, X[i]], [XP_ps])
                P.tt("dve", X[i][:], X[i][:], XP_ps[:], ALU.add, [X[i], XP_ps], [X[i]])
                Pc, Qc = Pn, Qn
                yield
            if STOP[0] <= 4.5:
                return
            P.copy("act", Xb[i][:], X[i][:], [X[i]], [Xb[i]])
            if STOP[0] <= 5:
                return
            yield
            for h in range(4):
                P.transpose(ktm[:, h, :], kT[i][:, h, :], K.ident[:], [kT[i], K.ident], [ktm])
            for h in range(4):
                P.transpose(vtm[:, h, :], vT[i][:, h, :], K.ident[:], [vT[i], K.ident], [vtm])
            P.tt("dve", kb[i][:], ktm[:], b4c(bE[i][:], 128), ALU.mult, [ktm, bE[i]], [kb[i]])
            P.tt("dve", kdec[i][:], ktm[:], b4c(E[i][:, 4:8], 128), ALU.mult, [ktm, E[i]], [kdec[i]])
            P.tt("dve", vb[i][:], vtm[:], b4c(be[i][:], 128), ALU.mult, [vtm, be[i]], [vb[i]])
            if STOP[0] <= 6:
                return
            yield
            for h in range(4):
                P.matmul(wk_ps[:, h, :], kb[i][:, h, :], Xb[i][:, h, :], True, True, [kb[i], Xb[i]], [wk_ps])
            P.copy("act", wkT[i][:], wk_ps[:], [wk_ps], [wkT[i]])
            for h in range(4):
                P.matmul(uwo[:, h, :], Xb[i][:, h, :], vb[i][:, h, :], True, True, [Xb[i], vb[i]], [uwo])
            P.copy("act", u0[i][:], uwo[:], [uwo], [u0[i]])
            if STOP[0] <= 7:
                return
        def seq_fn(ch):
            i = ch % 2
            cs = slice(ch * 64, (ch + 1) * 64)
            for h in range(4):
                P.matmul(uwo[:, h, :], wkT[i][:, h, :], STb[:, h, :], True, True, [wkT[i], STb], [uwo])
            P.tt("dve", u[i][:], u0[i][:], uwo[:], ALU.subtract, [u0[i], uwo], [u[i]])
            yield
            for h in range(4):
                P.matmul(uwo[:, h, :], qdT[i][:, h, :], STb[:, h, :], True, False, [qdT[i], STb], [uwo])
                P.matmul(uwo[:, h, :], qkT[i][:, h, :], u[i][:, h, :], False, True, [qkT[i], u[i]], [uwo])
            P.copy("act", o[i][:], uwo[:], [uwo], [o[i]])
            if STOP[0] <= 8:
                return
            yield
            for h in range(4):
                P.matmul(Sn_ps[:, h, :], kdec[i][:, h, :], u[i][:, h, :], True, True, [kdec[i], u[i]], [Sn_ps])
            P.tt("dve", ST32[:], ST32[:], b4c(cd128[i][:], 128), ALU.mult, [ST32, cd128[i]], [ST32])
            P.tt("dve", ST32[:], ST32[:], Sn_ps[:], ALU.add, [ST32, Sn_ps], [ST32])
            P.copy("act", STb[:], ST32[:], [ST32], [STb])
            if STOP[0] <= 9:
                return
            yield
            P.tt("pool", sq[i][:], o[i][:], o[i][:], ALU.mult, [o[i]], [sq[i]])
            P.op("dve", lambda e, i=i: e.tensor_reduce(ssq[i][:], sq[i][:], AX.X, ALU.add), [sq[i]], [ssq[i]])
            P.act(ssq[i][:], ssq[i][:], AF.Sqrt, [ssq[i], K.eps], [ssq[i]], scale=1.0 / 128, bias=K.eps[0:64, :])
            P.recip(ssq[i][:], ssq[i][:], [ssq[i]], [ssq[i]])
            P.tt("pool", o[i][:], o[i][:], b4c(ssq[i][:], 128), ALU.mult, [o[i], ssq[i]], [o[i]])
            P.tt("pool", o[i][:], o[i][:], bc(ng[:].unsqueeze(1), [64, 4, 128]), ALU.mult, [o[i], ng], [o[i]])
            P.tt("pool", y[i][:], o[i][:].rearrange("c h v -> c (h v)"), gate[i][:], ALU.mult, [o[i], gate[i]], [y[i]])
            if STOP[0] <= 10:
                return
            yield
            for cc in range(4):
                P.transpose(ytp[:, cc, :], y[i][:, cc * 128:(cc + 1) * 128], i64, [y[i], idf], [ytp])
            P.copy("act", ysb[i][:], ytp[:], [ytp], [ysb[i]])
            P.dma("sp", S["ysT"][0][:, cs].rearrange("(cc c) t -> c cc t", c=128), ysb[i][:], reads=[ysb[i]])


        def drain(g):
            for _ in g:
                pass

        def interleave(ga, gb):
            a_live, b_live = True, True
            while a_live or b_live:
                if b_live:
                    try:
                        next(gb)
                    except StopIteration:
                        b_live = False
                if a_live:
                    try:
                        next(ga)
                    except StopIteration:
                        a_live = False

        drain(pre_fn(0))
        for ch in range(NCH):
            if ch + 1 < NCH:
                interleave(pre_fn(ch + 1), seq_fn(ch))
            else:
                drain(seq_fn(ch))


def build_program(nc, layers=(0, 1), dbg=False, branches="abcdm"):
    C = declare(nc, dbg=dbg)
    P = Prog(nc)
    K = setup_consts(P, C)
    for l in layers:
        x_src = C.x if l == layers[0] else C.scr["x1"]
        x_dst = C.out if l == layers[-1] else C.scr["x1"]
        with P.scope():
            alloc_hT(P, K)
            phase_norm(P, C, K, l, x_src)
            phase_inproj(P, C, K, l)
        if "a" in branches:
            phase_gdn(P, C, K, l)
        if "b" in branches:
            phase_sg(P, C, K, l)
        if "c" in branches:
            phase_dsa(P, C, K, l)
        if "d" in branches:
            phase_ssd(P, C, K, l)
        if "m" in branches:
            phase_mem(P, C, K, l)
        with P.scope():
            alloc_merged(P, K)
            with P.scope():
                alloc_hT(P, K)
                phase_norm(P, C, K, l, x_src)
                phase_merge(P, C, K, l)
            phase_outproj(P, C, K, l, x_src, x_dst)
    P.finish()
    return C, P


def kernel(**inputs):
    x = np.ascontiguousarray(np.asarray(inputs["x"], dtype=np.float32))
    mem = np.ascontiguousarray(np.asarray(inputs["mem"], dtype=np.float32))
    nb = x.shape[0]
    nc = bass.Bass("TRN2", target_bir_lowering=False)
    build_program(nc)
    prm = {n: np.ascontiguousarray(np.asarray(inputs[n], dtype=np.float32)) for n, _ in PARAMS}
    in_maps = []
    for b in range(nb):
        m = {"x": x[b], "mem": mem[b]}
        m.update(prm)
        in_maps.append(m)
    res = run_bass_kernel_spmd(nc, in_maps, core_ids=list(range(nb)))
    return np.stack([np.asarray(r["out"], dtype=np.float32) for r in res.results], axis=0)
```

```python
from contextlib import ExitStack
import numpy as np
import concourse.bass as bass
import concourse.mybir as mybir
from concourse.bass_utils import run_bass_kernel_spmd

F32 = mybir.dt.float32
BF16 = mybir.dt.bfloat16
AF = mybir.ActivationFunctionType
ALU = mybir.AluOpType
AX = mybir.AxisListType


class Buf:
    __slots__ = ("name", "ap", "w", "r", "excl")

    def __init__(self, name, ap=None):
        self.name = name
        self.ap = ap
        self.excl = False
        self.w = None
        self.r = {}

    def __getitem__(self, idx):
        return self.ap[idx]


class View:
    def __init__(self, parent, ap):
        self.parent = parent
        self.ap = ap
        self.name = parent.name
        self.excl = parent.excl

    def __getitem__(self, idx):
        return self.ap[idx]

    @property
    def w(self):
        return self.parent.w

    @w.setter
    def w(self, v):
        self.parent.w = v

    @property
    def r(self):
        return self.parent.r

    @r.setter
    def r(self, v):
        self.parent.r = v


class Prog:
    ENG = ("pe", "dve", "act", "pool", "sp")
    SEM_LIMIT = 30000
    NDMA = 6

    def __init__(self, nc, same_engine_sync=True):
        self.nc = nc
        self.same = same_engine_sync
        self.stack = ExitStack()
        self.ops = {e: [] for e in self.ENG}
        self.cnt = {e: 0 for e in self.ENG}
        self.owner = {}
        self.nsem = 0
        self.cur = {e: self._newsem(e) for e in self.ENG}
        self.seen = {e: {} for e in self.ENG}
        self.dsem = {}
        self.drr = {}
        self.allsems = []
        self.nbuf = 0

    def _newsem(self, owner):
        s = getattr(self, "semstack", self.stack).enter_context(self.nc.semaphore("s%d_%s" % (self.nsem, owner)))
        self.nsem += 1
        self.owner[id(s)] = owner
        return s

    def sbuf(self, name, shape, dtype):
        self.nbuf += 1
        t = self.stack.enter_context(self.nc.sbuf_tensor("%s_%d" % (name, self.nbuf), list(shape), dtype))
        return Buf(name, t)

    def psum(self, name, shape, dtype):
        self.nbuf += 1
        t = self.stack.enter_context(self.nc.psum_tensor("%s_%d" % (name, self.nbuf), list(shape), dtype))
        b = Buf(name, t)
        b.excl = True
        return b

    def buf(self, name, ap=None):
        return Buf(name, ap)

    def _deps(self, eng, reads, writes):
        deps = {}

        def add(ev):
            if ev is None:
                return
            s, v = ev
            k = id(s)
            if k not in deps or deps[k][1] < v:
                deps[k] = (s, v)

        for b in reads:
            add(b.w)
        for b in writes:
            add(b.w)
            for ev in b.r.values():
                add(ev)
        waits = []
        seen = self.seen[eng]
        for k, (s, v) in deps.items():
            if self.owner.get(k) == eng:
                if eng == "pe" or eng == "sp" or not self.same:
                    continue
            if seen.get(k, 0) >= v:
                continue
            seen[k] = v
            waits.append((s, v))
        return waits

    def _commit(self, ev, reads, writes):
        for b in reads:
            k = id(ev[0])
            b.r[k] = ev
        for b in writes:
            b.w = ev
            b.r = {}

    def op(self, eng, fn, reads=(), writes=()):
        ex = [b for b in reads if getattr(b, "excl", False)]
        if ex:
            writes = list(writes) + ex
        waits = self._deps(eng, reads, writes)
        if self.cnt[eng] >= self.SEM_LIMIT:
            self.cur[eng] = self._newsem(eng)
            self.cnt[eng] = 0
        self.cnt[eng] += 1
        ev = (self.cur[eng], self.cnt[eng])
        self.ops[eng].append((waits, fn, ("c", self.cur[eng], self.cnt[eng])))
        self._commit(ev, reads, writes)
        return ev

    def dma(self, q, out, in_, reads=(), writes=(), **kw):
        waits = self._deps(q, reads, writes)
        if q not in self.dsem:
            self.dsem[q] = [[self._newsem("dma_" + q), 0] for _ in range(self.NDMA)]
            self.drr[q] = 0
        slot = self.dsem[q][self.drr[q] % self.NDMA]
        self.drr[q] += 1
        s, c = slot
        if c > 0:
            seen = self.seen[q]
            if seen.get(id(s), 0) < 16 * c:
                seen[id(s)] = 16 * c
                waits.append((s, 16 * c))
        if 16 * (c + 1) > self.SEM_LIMIT:
            s = self._newsem("dma_" + q)
            slot[0] = s
            c = 0
        slot[1] = c + 1
        ev = (s, 16 * (c + 1))
        self.ops[q].append((waits, lambda e, out=out, in_=in_, kw=kw: e.dma_start(out=out, in_=in_, **kw), ("d", s, 16)))
        self._commit(ev, reads, writes)
        return ev

    def make_identity(self, b, n=128):
        self.op("pool", lambda e: e.memset(b.ap[:], 1.0), writes=[b])
        self.op("pool", lambda e: e.affine_select(out=b.ap[:], in_=b.ap[:], pattern=[[-1, n]], compare_op=ALU.is_ge,
                                                  fill=0.0, base=0, channel_multiplier=1), reads=[b], writes=[b])
        self.op("pool", lambda e: e.affine_select(out=b.ap[:], in_=b.ap[:], pattern=[[1, n]], compare_op=ALU.is_ge,
                                                  fill=0.0, base=0, channel_multiplier=-1), reads=[b], writes=[b])


    def matmul(self, out, lhsT, rhs, start, stop, reads, writes):
        return self.op("pe", lambda e: e.matmul(out, lhsT, rhs, start=start, stop=stop), reads, writes)

    def transpose(self, out, in_, ident, reads, writes):
        return self.op("pe", lambda e: e.transpose(out, in_, ident), reads, writes)

    def act(self, out, in_, func, reads, writes, **kw):
        return self.op("act", lambda e: e.activation(out, in_, func, **kw), reads, writes)

    def tt(self, eng, out, a, b, op, reads, writes):
        return self.op(eng, lambda e: e.tensor_tensor(out, a, b, op), reads, writes)

    def ts(self, eng, out, a, s1, s2, op0, op1, reads, writes, **kw):
        if op1 is None:
            return self.op(eng, lambda e: e.tensor_scalar(out, a, s1, None, op0, **kw), reads, writes)
        return self.op(eng, lambda e: e.tensor_scalar(out, a, s1, s2, op0, op1, **kw), reads, writes)

    def stt(self, eng, out, in0, scalar, in1, op0, op1, reads, writes):
        return self.op(eng, lambda e: e.scalar_tensor_tensor(out, in0, scalar, in1, op0, op1), reads, writes)

    def copy(self, eng, out, in_, reads, writes):
        if eng == "act":
            return self.op(eng, lambda e: e.copy(out, in_), reads, writes)
        return self.op(eng, lambda e: e.tensor_copy(out, in_), reads, writes)

    def memset(self, eng, ap, val, writes):
        return self.op(eng, lambda e: e.memset(ap, val), (), writes)

    def recip(self, out, in_, reads, writes):
        return self.op("dve", lambda e: e.reciprocal(out, in_), reads, writes)

    def barrier(self):
        finals = []
        for e in self.ENG:
            if self.cnt[e] > 0:
                finals.append((self.cur[e], self.cnt[e]))
        for q, slots in self.dsem.items():
            for s, c in slots:
                if c > 0:
                    finals.append((s, 16 * c))
        for e in self.ENG:
            waits = []
            for s, v in finals:
                if self.seen[e].get(id(s), 0) < v:
                    self.seen[e][id(s)] = v
                    waits.append((s, v))
            if waits:
                self.ops[e].append((waits, None, None))

    def scope(self):
        return _Scope(self)

    def flush(self, final=False):
        nc = self.nc
        finals = []
        if final:
            for e in self.ENG:
                if self.cnt[e] > 0:
                    finals.append((self.cur[e], self.cnt[e]))
            for q, slots in self.dsem.items():
                for s, c in slots:
                    if c > 0:
                        finals.append((s, 16 * c))
        if not hasattr(self, "actual"):
            self.actual = {}
            self.amap = {}
        ref = set()
        for e in self.ENG:
            for waits, fn, inc in self.ops[e]:
                for s, v in waits:
                    if self.owner.get(id(s)) in self.ENG:
                        ref.add((id(s), v))
        for s, v in finals:
            if self.owner.get(id(s)) in self.ENG:
                ref.add((id(s), v))
        for e in self.ENG:
            for waits, fn, inc in self.ops[e]:
                if inc is not None and inc[0] == "c":
                    key = (id(inc[1]), inc[2])
                    if key in ref:
                        self.actual[key[0]] = self.actual.get(key[0], 0) + 1
                        self.amap[key] = self.actual[key[0]]

        def tr(s, v):
            if self.owner.get(id(s)) in self.ENG:
                return self.amap[(id(s), v)]
            return v

        engs = {"pe": "tensor", "dve": "vector", "act": "scalar", "pool": "gpsimd", "sp": "sync"}
        with nc.Block() as block:
            for e in self.ENG:
                ops = self.ops[e]
                if not ops and not (final and e == "sp"):
                    continue

                def body(engine, ops=ops, e=e):
                    for waits, fn, inc in ops:
                        for s, v in waits:
                            engine.wait_ge(s, tr(s, v))
                        if fn is not None:
                            ins = fn(engine)
                            if inc[0] == "d":
                                ins.then_inc(inc[1], 16)
                            elif (id(inc[1]), inc[2]) in self.amap:
                                ins.then_inc(inc[1], 1)
                    if final and e == "sp":
                        for s, v in finals:
                            engine.wait_ge(s, tr(s, v))

                getattr(block, engs[e])(body)
        self.nops = getattr(self, "nops", 0) + sum(len(v) for v in self.ops.values())
        self.ops = {e: [] for e in self.ENG}

    def finish(self):
        self.flush(final=True)
        self.stack.close()


class _Scope:
    def __init__(self, P):
        self.P = P

    def __enter__(self):
        self.saved = self.P.stack
        self.P.semstack = getattr(self.P, "semstack", self.saved)
        self.P.stack = ExitStack()
        return self

    def __exit__(self, *a):
        self.P.barrier()
        self.P.flush()
        self.P.stack.close()
        self.P.stack = self.saved
        return False


T = 4096
NT = 32
NTR = [32]
D = 1024
INC = 7896
EPS = 1e-6

O_AQ, O_AK, O_AV = 0, 512, 1024
O_AA, O_AB, O_AG = 1536, 1540, 1544
O_BU, O_BV, O_BG = 2056, 2568, 3080
O_CQ, O_CK, O_CV, O_CIQ, O_CIK, O_CIW, O_CG = 3592, 4104, 4168, 4232, 4744, 4808, 4816
O_DZ, O_DX, O_DDT = 5328, 5840, 6864
O_MQ, O_MG = 6872, 7384

PARAMS = [("norm_g", [2, 1024]), ("w_in", [2, 1024, INC]), ("gdn_conv_w", [2, 4, 1536]), ("gdn_a_log", [2, 4]),
          ("gdn_dt_bias", [2, 4]), ("gdn_norm_g", [2, 128]), ("sg_ln_g", [2, 512]), ("sg_ln_b", [2, 512]),
          ("sg_w", [2, 4, 128, 128]), ("sg_b", [2, 4, 128]), ("dsa_q_norm_g", [2, 64]), ("dsa_k_norm_g", [2, 64]),
          ("ssd_conv_w", [2, 4, 1024]), ("ssd_conv_b", [2, 1024]), ("ssd_a_log", [2, 8]), ("ssd_dt_bias", [2, 8]),
          ("ssd_d", [2, 8]), ("ssd_norm_g", [2, 512]), ("mem_norm_g", [2, 1024]), ("w_mem_kv", [2, 1024, 1024]),
          ("mem_q_norm_g", [2, 128]), ("mem_k_norm_g", [2, 128]), ("w_gate", [2, 5, 1024, 1024]),
          ("w_branch", [2, 5, 512, 1024]), ("w_out", [2, 1024, 1024])]


class Ctx:
    pass


STOP = [99]


def declare(nc, dbg=False, skip=()):
    C = Ctx()
    C.nc = nc
    if "x" not in skip:
        C.x = nc.dram_tensor("x", [T, D], F32, kind="ExternalInput").ap()
    C.mem = nc.dram_tensor("mem", [256, D], F32, kind="ExternalInput").ap()
    C.prm = {}
    for name, shp in PARAMS:
        if name in skip:
            continue
        C.prm[name] = nc.dram_tensor(name, shp, F32, kind="ExternalInput").ap()
    C.out = nc.dram_tensor("out", [T, D], F32, kind="ExternalOutput").ap()
    kind = "ExternalOutput" if dbg else "Internal"
    C.scr = {}

    def scr(name, shape, dt):
        C.scr[name] = nc.dram_tensor("scr_" + name, shape, dt, kind=kind).ap()

    for n in ("gq", "gk", "gv", "cq", "ciq", "mq"):
        scr(n, [512, T], BF16)
    scr("xbc", [1024, T], BF16)
    scr("ck", [64, T], BF16)
    scr("cik", [64, T], BF16)
    for n in ("ag", "bu", "bv", "bg", "cg", "dz", "mg"):
        scr(n, [T, 512], BF16)
    scr("misc", [T, 88], F32)
    scr("ysT", [5, 512, T], BF16)
    scr("x1", [T, D], F32)
    return C


def setup_consts(P, C):
    K = Ctx()
    K.ident = P.sbuf("ident", [128, 128], BF16)
    P.make_identity(K.ident)
    K.ones = P.sbuf("ones", [128, 128], BF16)
    P.memset("pool", K.ones[:], 1.0, [K.ones])
    K.blk2 = P.sbuf("blk2", [128, 128], BF16)
    P.memset("pool", K.blk2[:], 0.0, [K.blk2])
    P.memset("pool", K.blk2[0:64, 0:64], 1.0, [K.blk2])
    P.memset("pool", K.blk2[64:128, 64:128], 1.0, [K.blk2])
    K.eps = P.sbuf("epsc", [128, 1], F32)
    P.memset("pool", K.eps[:], EPS, [K.eps])
    P.barrier()
    return K


def alloc_hT(P, K):
    K.hT = P.sbuf("hT", [128, 8, T], BF16)
    K.hTb = [Buf("hT%d" % t, K.hT.ap) for t in range(NT)]


def alloc_merged(P, K):
    K.mT = P.sbuf("mT", [128, 8, T], BF16)
    K.mTb = [Buf("mT%d" % t, K.mT.ap) for t in range(8)]


def phase_norm(P, C, K, l, x_src):
    with P.scope():
        gb = P.sbuf("gb", [128, D], F32)
        P.dma("sp", gb[:], C.prm["norm_g"][l:l + 1, :].to_broadcast([128, D]), writes=[gb])
        xin = [P.sbuf("xin%d" % i, [128, D], F32) for i in range(2)]
        junk = P.sbuf("junk", [128, D], BF16)
        ss = [P.sbuf("ss%d" % i, [128, 1], F32) for i in range(2)]
        rt = [P.sbuf("rt%d" % i, [128, 1], F32) for i in range(2)]
        hb = [P.sbuf("hb%d" % i, [128, D], BF16) for i in range(2)]
        pt = [P.psum("pt%d" % i, [128, 4, 128], BF16) for i in range(2)]
        for t in range(NT):
            xi, s_, r_, h_ = xin[t % 2], ss[t % 2], rt[t % 2], hb[t % 2]
            P.dma("sp", xi[:], x_src[t * 128:(t + 1) * 128, :], writes=[xi])
            P.act(junk[:], xi[:], AF.Square, [xi], [junk, s_], accum_out=s_[:])
            P.act(r_[:], s_[:], AF.Sqrt, [s_, K.eps], [r_], scale=1.0 / D, bias=K.eps[:])
            P.recip(r_[:], r_[:], [r_], [r_])
            P.stt("dve", h_[:], xi[:], r_[:], gb[:], ALU.mult, ALU.mult, [xi, r_, gb], [h_])
            for half in range(2):
                p_ = pt[half]
                for j in range(4):
                    kc = half * 4 + j
                    P.transpose(p_[:, j, :], h_[:, kc * 128:(kc + 1) * 128], K.ident[:], [h_, K.ident], [p_])
                if half == 0:
                    P.copy("dve", K.hT[:, 0:4, t * 128:(t + 1) * 128], p_[:], [p_], [K.hTb[t]])
                else:
                    P.copy("act", K.hT[:, 4:8, t * 128:(t + 1) * 128], p_[:], [p_], [K.hTb[t]])


def phase_inproj(P, C, K, l):
    w_in = C.prm["w_in"][l]
    S = C.scr
    with P.scope():
        cwg = P.sbuf("cwg", [128, 4, 12], F32)
        cws = P.sbuf("cws", [128, 4, 8], F32)
        for k in range(4):
            P.dma("sp", cwg[:, k, :], C.prm["gdn_conv_w"][l][k].rearrange("(c p) -> p c", p=128), writes=[cwg],
                  allow_slow_non_contiguous=True)
            P.dma("sp", cws[:, k, :], C.prm["ssd_conv_w"][l][k].rearrange("(c p) -> p c", p=128), writes=[cws],
                  allow_slow_non_contiguous=True)
        cbs = P.sbuf("cbs", [128, 8], F32)
        P.dma("sp", cbs[:], C.prm["ssd_conv_b"][l].rearrange("(c p) -> p c", p=128), writes=[cbs],
              allow_slow_non_contiguous=True)
        gq2 = P.sbuf("gq2", [128, 1], F32)
        for i in range(2):
            P.dma("sp", gq2[i * 64:(i + 1) * 64, :], C.prm["dsa_q_norm_g"][l].rearrange("(p o) -> p o", o=1), writes=[gq2])
        gk1 = P.sbuf("gk1", [64, 1], F32)
        P.dma("sp", gk1[:], C.prm["dsa_k_norm_g"][l].rearrange("(p o) -> p o", o=1), writes=[gk1])
        gmq = P.sbuf("gmq", [128, 1], F32)
        P.dma("sp", gmq[:], C.prm["mem_q_norm_g"][l].rearrange("(p o) -> p o", o=1), writes=[gmq])
        P.ts("dve", gq2[:], gq2[:], 0.125, None, ALU.mult, None, [gq2], [gq2])
        P.ts("dve", gmq[:], gmq[:], 128 ** -0.5, None, ALU.mult, None, [gmq], [gmq])

        wb = [P.sbuf("wb%d" % i, [128, 8, 512], BF16) for i in range(2)]
        acc = [P.psum("acc%d" % i, [128, 512], F32) for i in range(3)]
        ssp = [P.psum("ssp%d" % i, [128, 512], F32) for i in range(2)]
        xpad = [P.sbuf("xpad%d" % i, [128, 515], F32) for i in range(4)]
        yb = [P.sbuf("yb%d" % i, [128, 512], F32) for i in range(4)]
        sb = [P.sbuf("sb%d" % i, [128, 512], F32) for i in range(2)]
        sq = [P.sbuf("sq%d" % i, [128, 512], BF16) for i in range(2)]
        rtb = [P.sbuf("rtb%d" % i, [128, 512], F32) for i in range(2)]
        ob = [P.sbuf("ob%d" % i, [128, 512], BF16) for i in range(3)]
        mo = [P.sbuf("mo%d" % i, [128, 88], F32) for i in range(2)]
        st = Ctx()
        st.g = 0
        st.a = 0
        st.e = 0
        st.o = 0

        st.loaded = {}

        def issue_load(gi, spec):
            w = wb[gi % 2]
            for (c0, n, d0) in spec:
                P.dma("pool", w[:, :, d0:d0 + n], w_in[:, c0:c0 + n].rearrange("(kc k) c -> k kc c", k=128), writes=[w])
            st.loaded[gi] = w

        def load_w(col0, ncols):
            w = st.loaded[st.g]
            st.g += 1
            return w

        def fm_group(col0, ncols, kind, dst, cw=None, cb=None, gain=None, scale=1.0):
            w = load_w(col0, ncols)
            nch = (ncols + 127) // 128
            for j in range(nch):
                m = min(128, ncols - j * 128)
                for tg in range(8):
                    a = acc[st.a % 3]
                    st.a += 1
                    for kc in range(8):
                        P.matmul(a[0:m, :], w[:, kc, j * 128:j * 128 + m], K.hT[:, kc, tg * 512:(tg + 1) * 512],
                                 kc == 0, kc == 7, [w] + K.hTb[tg * 4:tg * 4 + 4], [a])
                    e = st.e
                    st.e += 1
                    o = ob[st.o % 3]
                    st.o += 1
                    dsl = dst[j * 128:j * 128 + m, tg * 512:(tg + 1) * 512]
                    if kind == "raw":
                        P.copy("act", o[0:m, :], a[0:m, :], [a], [o])
                        P.dma("sp", dsl, o[0:m, :], reads=[o])
                        continue
                    if kind in ("conv", "conv_l2"):
                        xp, xn = xpad[tg % 4], xpad[(tg + 1) % 4]
                        y = yb[e % 4]
                        ce = "dve"
                        if tg == 0:
                            P.memset("dve", xp[:, 0:3], 0.0, [xp])
                        P.copy("act", xp[:, 3:515], a[:], [a], [xp])
                        cwb, cwo = cw
                        cj = cwo + j
                        P.ts(ce, y[:], xp[:, 0:512], cwb[:, 0, cj:cj + 1], None, ALU.mult, None, [xp, cwb], [y])
                        for k in range(1, 4):
                            P.stt(ce, y[:], xp[:, k:k + 512], cwb[:, k, cj:cj + 1], y[:], ALU.mult, ALU.add, [xp, cwb, y], [y])
                        if tg < 7:
                            P.copy("act", xn[:, 0:3], xp[:, 512:515], [xp], [xn])
                        if kind == "conv":
                            if cb is not None:
                                P.act(o[:], y[:], AF.Silu, [y, cb[0]], [o], bias=cb[0][:, cb[1] + j:cb[1] + j + 1])
                            else:
                                P.act(o[:], y[:], AF.Silu, [y], [o])
                            P.dma("sp", dsl, o[:], reads=[o])
                            continue
                        s_ = sb[e % 2]
                        P.act(s_[:], y[:], AF.Silu, [y], [s_])
                        src_ = s_
                        ones = K.ones
                        nrm_scale = 1.0
                    else:
                        s_ = sb[e % 2]
                        P.copy("act", s_[0:m, :], a[0:m, :], [a], [s_])
                        ones = K.blk2 if kind == "rms64" else K.ones
                        nrm_scale = (1.0 / 64) if kind == "rms64" else (1.0 / 128)
                    q_ = sq[e % 2]
                    P.act(q_[0:m, :], s_[0:m, :], AF.Square, [s_], [q_])
                    sp_ = ssp[e % 2]
                    P.matmul(sp_[0:m, :], ones[0:m, 0:m], q_[0:m, :], True, True, [ones, q_], [sp_])
                    r_ = rtb[e % 2]
                    P.act(r_[0:m, :], sp_[0:m, :], AF.Sqrt, [sp_, K.eps], [r_], scale=nrm_scale, bias=K.eps[0:m, :])
                    P.recip(r_[0:m, :], r_[0:m, :], [r_], [r_])
                    if gain is not None:
                        P.stt("dve", o[0:m, :], s_[0:m, :], gain[0:m, :], r_[0:m, :], ALU.mult, ALU.mult, [s_, gain, r_], [o])
                    else:
                        P.stt("dve", o[0:m, :], s_[0:m, :], scale, r_[0:m, :], ALU.mult, ALU.mult, [s_, r_], [o])
                    P.dma("sp", dsl, o[0:m, :], reads=[o])

        def tm_group(col0, func, dst):
            w = load_w(col0, 512)
            for t in range(NT):
                a = acc[st.a % 3]
                st.a += 1
                for kc in range(8):
                    P.matmul(a[:], K.hT[:, kc, t * 128:(t + 1) * 128], w[:, kc, :], kc == 0, kc == 7,
                             [w, K.hTb[t]], [a])
                o = ob[st.o % 3]
                st.o += 1
                if func is None:
                    P.copy("act", o[:], a[:], [a], [o])
                else:
                    P.act(o[:], a[:], func, [a], [o])
                P.dma("sp", dst[t * 128:(t + 1) * 128, :], o[:], reads=[o])

        def misc_group():
            w = load_w(0, 88)
            for t in range(NT):
                a = acc[st.a % 3]
                st.a += 1
                for kc in range(8):
                    P.matmul(a[:, 0:88], K.hT[:, kc, t * 128:(t + 1) * 128], w[:, kc, 0:88], kc == 0, kc == 7,
                             [w, K.hTb[t]], [a])
                o = mo[t % 2]
                P.copy("act", o[:], a[:, 0:88], [a], [o])
                P.dma("sp", S["misc"][t * 128:(t + 1) * 128, :], o[:], reads=[o])

        groups = [
            (((O_AA, 8, 0), (O_CIW, 8, 8), (O_DDT, 8, 16), (O_CV, 64, 24)), lambda: misc_group()),
            (((O_CIQ, 512, 0),), lambda: fm_group(O_CIQ, 512, "raw", S["ciq"])),
            (((O_CIK, 64, 0),), lambda: fm_group(O_CIK, 64, "raw", S["cik"])),
            (((O_CQ, 512, 0),), lambda: fm_group(O_CQ, 512, "rms64", S["cq"], gain=gq2)),
            (((O_CK, 64, 0),), lambda: fm_group(O_CK, 64, "rms64", S["ck"], gain=gk1)),
            (((O_MQ, 512, 0),), lambda: fm_group(O_MQ, 512, "rms128", S["mq"], gain=gmq)),
            (((O_AQ, 512, 0),), lambda: fm_group(O_AQ, 512, "conv_l2", S["gq"], cw=(cwg, 0), scale=128 ** -0.5)),
            (((O_AK, 512, 0),), lambda: fm_group(O_AK, 512, "conv_l2", S["gk"], cw=(cwg, 4), scale=1.0)),
            (((O_AV, 512, 0),), lambda: fm_group(O_AV, 512, "conv", S["gv"], cw=(cwg, 8))),
            (((O_DX, 512, 0),), lambda: fm_group(O_DX, 512, "conv", S["xbc"][0:512], cw=(cws, 0), cb=(cbs, 0))),
            (((O_DX + 512, 512, 0),), lambda: fm_group(O_DX + 512, 512, "conv", S["xbc"][512:1024], cw=(cws, 4), cb=(cbs, 4))),
            (((O_AG, 512, 0),), lambda: tm_group(O_AG, AF.Silu, S["ag"])),
            (((O_BG, 512, 0),), lambda: tm_group(O_BG, AF.Silu, S["bg"])),
            (((O_CG, 512, 0),), lambda: tm_group(O_CG, AF.Silu, S["cg"])),
            (((O_DZ, 512, 0),), lambda: tm_group(O_DZ, AF.Silu, S["dz"])),
            (((O_MG, 512, 0),), lambda: tm_group(O_MG, AF.Silu, S["mg"])),
            (((O_BU, 512, 0),), lambda: tm_group(O_BU, AF.Gelu, S["bu"])),
            (((O_BV, 512, 0),), lambda: tm_group(O_BV, AF.Gelu, S["bv"])),
        ]
        issue_load(0, groups[0][0])
        for gi, (spec, run) in enumerate(groups):
            if gi + 1 < len(groups):
                issue_load(gi + 1, groups[gi + 1][0])
            run()


def phase_merge(P, C, K, l):
    wgd = C.prm["w_gate"][l]
    wbd = C.prm["w_branch"][l]
    ysT = C.scr["ysT"]
    with P.scope():
        wbuf = [P.sbuf("mw%d" % i, [128, 7680], BF16) for i in range(2)]
        yT = [P.sbuf("yT%d" % i, [128, 4, 512], BF16) for i in range(3)]
        gps = [P.psum("gps%d" % i, [128, 512], F32) for i in range(2)]
        zps = [P.psum("zps%d" % i, [128, 512], F32) for i in range(2)]
        sg = [P.sbuf("sg%d" % i, [128, 512], F32) for i in range(2)]
        tmp = [P.sbuf("tmp%d" % i, [128, 512], F32) for i in range(2)]
        mac = [P.sbuf("mac%d" % i, [128, 512], F32) for i in range(2)]
        cnt = 0

        def views(w):
            return (w[:, 0:5120].rearrange("k (p kc n) -> k p kc n", p=5, kc=8),
                    w[:, 5120:7680].rearrange("k (p cc n) -> k p cc n", p=5, cc=4))

        def load(nch):
            w = wbuf[nch % 2]
            wg, wbr = views(w)
            for p in range(5):
                P.dma("pool", wg[:, p], wgd[p][:, nch * 128:(nch + 1) * 128].rearrange("(kc k) n -> k kc n", k=128), writes=[w])
                P.dma("pool", wbr[:, p], wbd[p][:, nch * 128:(nch + 1) * 128].rearrange("(cc k) n -> k cc n", k=128), writes=[w])

        load(0)
        for nch in range(8):
            w = wbuf[nch % 2]
            wg, wbr = views(w)
            if nch + 1 < 8:
                load(nch + 1)
            for tg in range(8):
                m_ = mac[tg % 2]
                for p in range(5):
                    y = yT[cnt % 3]
                    g_, z_ = gps[cnt % 2], zps[cnt % 2]
                    s_, t_ = sg[cnt % 2], tmp[cnt % 2]
                    cnt += 1
                    P.dma("sp", y[:], ysT[p][:, tg * 512:(tg + 1) * 512].rearrange("(cc c) t -> c cc t", c=128), writes=[y])
                    for kc in range(8):
                        P.matmul(g_[:], wg[:, p, kc, :], K.hT[:, kc, tg * 512:(tg + 1) * 512], kc == 0, kc == 7,
                                 [w] + K.hTb[tg * 4:tg * 4 + 4], [g_])
                    for cc in range(4):
                        P.matmul(z_[:], wbr[:, p, cc, :], y[:, cc, :], cc == 0, cc == 3, [w, y], [z_])
                    P.act(s_[:], g_[:], AF.Sigmoid, [g_], [s_])
                    if p == 0:
                        P.tt("dve", m_[:], z_[:], s_[:], ALU.mult, [z_, s_], [m_])
                    elif p < 4:
                        P.tt("dve", t_[:], z_[:], s_[:], ALU.mult, [z_, s_], [t_])
                        P.tt("pool", m_[:], m_[:], t_[:], ALU.add, [m_, t_], [m_])
                    else:
                        P.tt("dve", t_[:], z_[:], s_[:], ALU.mult, [z_, s_], [t_])
                        P.tt("pool", K.mT[:, nch, tg * 512:(tg + 1) * 512], m_[:], t_[:], ALU.add, [m_, t_], [K.mTb[tg]])


def phase_outproj(P, C, K, l, x_src, x_dst):
    with P.scope():
        wo = P.sbuf("wo", [128, 8, D], BF16)
        P.dma("pool", wo[:], C.prm["w_out"][l].rearrange("(kc k) n -> k kc n", k=128), writes=[wo])
        xin = [P.sbuf("oxin%d" % i, [128, 512], F32) for i in range(2)]
        xo = [P.sbuf("oxo%d" % i, [128, 512], F32) for i in range(2)]
        ops_ = [P.psum("ops%d" % i, [128, 512], F32) for i in range(2)]
        c = 0
        for t in range(NT):
            for hf in range(2):
                xi, o, ps = xin[c % 2], xo[c % 2], ops_[c % 2]
                c += 1
                P.dma("sp", xi[:], x_src[t * 128:(t + 1) * 128, hf * 512:(hf + 1) * 512], writes=[xi])
                for kc in range(8):
                    P.matmul(ps[:], K.mT[:, kc, t * 128:(t + 1) * 128], wo[:, kc, hf * 512:(hf + 1) * 512], kc == 0, kc == 7,
                             [wo, K.mTb[t // 4]], [ps])
                P.tt("dve", o[:], ps[:], xi[:], ALU.add, [ps, xi], [o])
                P.dma("sp", x_dst[t * 128:(t + 1) * 128, hf * 512:(hf + 1) * 512], o[:], reads=[o])


class YOut:
    def __init__(self, P, C, K, p, tag, nps=1, ps=None):
        self.P, self.C, self.K, self.p = P, C, K, p
        self.ps = ps if ps is not None else [P.psum("yo_ps%s%d" % (tag, i), [128, 4, 128], BF16) for i in range(nps)]
        self.sb = [P.sbuf("yo_sb%s%d" % (tag, i), [128, 4, 128], BF16) for i in range(2)]
        self.n = 0

    def emit(self, y, t, eng="act"):
        P, K = self.P, self.K
        ps, sb = self.ps[self.n % len(self.ps)], self.sb[self.n % 2]
        self.n += 1
        for cc in range(4):
            P.transpose(ps[:, cc, :], y[:, cc * 128:(cc + 1) * 128], K.ident[:], [y, K.ident], [ps])
        P.copy(eng, sb[:], ps[:], [ps], [sb])
        P.dma("sp", self.C.scr["ysT"][self.p][:, t * 128:(t + 1) * 128].rearrange("(cc c) t -> c cc t", c=128), sb[:], reads=[sb])


def phase_mem(P, C, K, l):
    S = C.scr
    with P.scope():
        gb = P.sbuf("m_gb", [128, D], F32)
        P.dma("sp", gb[:], C.prm["mem_norm_g"][l:l + 1, :].to_broadcast([128, D]), writes=[gb])
        gk = P.sbuf("m_gk", [128, 1], F32)
        P.dma("sp", gk[:], C.prm["mem_k_norm_g"][l].rearrange("(p o) -> p o", o=1), writes=[gk])
        wkv = P.sbuf("m_wkv", [128, 8, D], BF16)
        P.dma("pool", wkv[:], C.prm["w_mem_kv"][l].rearrange("(kc k) n -> k kc n", k=128), writes=[wkv])
        memT = P.sbuf("memT", [128, 8, 256], BF16)
        kT = P.sbuf("m_kT", [128, 4, 256], BF16)
        vaug = [P.sbuf("m_va%d" % i, [128, 4, 129], BF16) for i in range(2)]
        xin = P.sbuf("m_x", [128, D], F32)
        junk = P.sbuf("m_junk", [128, D], BF16)
        ss = P.sbuf("m_ss", [128, 1], F32)
        hb = P.sbuf("m_hb", [128, D], BF16)
        pt = P.psum("m_pt", [128, 4, 128], BF16)
        pa = P.psum("m_pa", [128, 512], F32)
        pb = P.psum("m_pb", [128, 512], F32)
        for mt in range(2):
            P.dma("sp", xin[:], C.mem[mt * 128:(mt + 1) * 128, :], writes=[xin])
            P.act(junk[:], xin[:], AF.Square, [xin], [junk, ss], accum_out=ss[:])
            P.act(ss[:], ss[:], AF.Sqrt, [ss, K.eps], [ss], scale=1.0 / D, bias=K.eps[:])
            P.recip(ss[:], ss[:], [ss], [ss])
            P.stt("dve", hb[:], xin[:], ss[:], gb[:], ALU.mult, ALU.mult, [xin, ss, gb], [hb])
            for half in range(2):
                for j in range(4):
                    kc = half * 4 + j
                    P.transpose(pt[:, j, :], hb[:, kc * 128:(kc + 1) * 128], K.ident[:], [hb, K.ident], [pt])
                P.copy("dve", memT[:, half * 4:half * 4 + 4, mt * 128:(mt + 1) * 128], pt[:], [pt], [memT])
        sq = P.sbuf("m_sq", [128, 256], BF16)
        kf = P.sbuf("m_kf", [128, 256], F32)
        rr = P.sbuf("m_rr", [128, 256], F32)
        for h in range(4):
            for kc in range(8):
                P.matmul(pa[:, 0:256], wkv[:, kc, h * 128:(h + 1) * 128], memT[:, kc, :], kc == 0, kc == 7, [wkv, memT], [pa])
            P.copy("act", kf[:], pa[:, 0:256], [pa], [kf])
            P.act(sq[:], kf[:], AF.Square, [kf], [sq])
            P.matmul(pb[:, 0:256], K.ones[:], sq[:], True, True, [K.ones, sq], [pb])
            P.act(rr[:], pb[:, 0:256], AF.Sqrt, [pb, K.eps], [rr], scale=1.0 / 128, bias=K.eps[:])
            P.recip(rr[:], rr[:], [rr], [rr])
            P.stt("dve", kT[:, h, :], kf[:], gk[:], rr[:], ALU.mult, ALU.mult, [kf, gk, rr], [kT])
        for mt in range(2):
            for kc in range(8):
                P.matmul(pa[:], memT[:, kc, mt * 128:(mt + 1) * 128], wkv[:, kc, 512:1024], kc == 0, kc == 7, [wkv, memT], [pa])
            P.memset("pool", vaug[mt][:, :, 128:129], 1.0, [vaug[mt]])
            P.copy("act", vaug[mt][:, :, 0:128], pa[:].rearrange("m (h d) -> m h d", h=4), [pa], [vaug[mt]])
        qT = [P.sbuf("m_qT%d" % i, [128, 4, 128], BF16) for i in range(2)]
        gt = [P.sbuf("m_gt%d" % i, [128, 512], BF16) for i in range(2)]
        lg = [P.psum("m_lg%d" % i, [128, 4, 128], F32) for i in range(2)]
        pT = [P.sbuf("m_pT%d" % i, [128, 4, 128], BF16) for i in range(2)]
        po = P.psum("m_po", [128, 4, 256], F32)
        rd = [P.sbuf("m_rd%d" % i, [128, 4], F32) for i in range(2)]
        yb = [P.sbuf("m_y%d" % i, [128, 512], BF16) for i in range(2)]
        yo = YOut(P, C, K, 4, "m")
        def m_load(t):
            q, g = qT[t % 2], gt[t % 2]
            P.dma("sp", q[:], S["mq"][:, t * 128:(t + 1) * 128].rearrange("(h d) t -> d h t", d=128), writes=[q])
            P.dma("sp", g[:], S["mg"][t * 128:(t + 1) * 128, :], writes=[g])

        m_load(0)
        for t in range(NTR[0]):
            q, g = qT[t % 2], gt[t % 2]
            if t + 1 < NTR[0]:
                m_load(t + 1)
            for mt in range(2):
                for h in range(4):
                    P.matmul(lg[mt][:, h, :], kT[:, h, mt * 128:(mt + 1) * 128], q[:, h, :], True, True, [kT, q], [lg[mt]])
                P.act(pT[mt][:], lg[mt][:], AF.Exp, [lg[mt]], [pT[mt]])
            for h in range(4):
                for mt in range(2):
                    P.matmul(po[:, h, 0:129], pT[mt][:, h, :], vaug[mt][:, h, :], mt == 0, mt == 1, [pT[mt], vaug[mt]], [po])
            r_ = rd[t % 2]
            y = yb[t % 2]
            P.recip(r_[:], po[:, :, 128], [po], [r_])
            for h in range(4):
                P.stt("dve", y[:, h * 128:(h + 1) * 128], po[:, h, 0:128], r_[:, h:h + 1], g[:, h * 128:(h + 1) * 128],
                      ALU.mult, ALU.mult, [po, r_, g], [y])
            yo.emit(y, t)


def phase_sg(P, C, K, l):
    S = C.scr
    with P.scope():
        lng = P.sbuf("b_lng", [128, 512], F32)
        lnb = P.sbuf("b_lnb", [128, 512], F32)
        P.dma("sp", lng[:], C.prm["sg_ln_g"][l:l + 1, :].to_broadcast([128, 512]), writes=[lng])
        P.dma("sp", lnb[:], C.prm["sg_ln_b"][l:l + 1, :].to_broadcast([128, 512]), writes=[lnb])
        bsT = P.sbuf("b_bsT", [128, 4], F32)
        P.dma("sp", bsT[:], C.prm["sg_b"][l].rearrange("g t -> t g"), writes=[bsT], allow_slow_non_contiguous=True)
        eps5 = P.sbuf("b_eps5", [128, 1], F32)
        P.memset("pool", eps5[:], 1e-5, [eps5])
        wf = P.sbuf("b_wf", [128, 4, 128], F32)
        wbf = P.sbuf("b_wbf", [128, 4, 128], BF16)
        WcT = P.sbuf("b_WcT", [128, 4, 128], BF16)
        pt = P.psum("b_pt", [128, 4, 128], BF16)
        P.dma("sp", wf[:], C.prm["sg_w"][l].rearrange("g t s -> t g s"), writes=[wf])
        for g in range(4):
            P.op("pool", lambda e, g=g: e.affine_select(out=wf[:, g, :], in_=wf[:, g, :], pattern=[[-1, 128]], compare_op=ALU.is_ge,
                                                        fill=0.0, base=0, channel_multiplier=1), [wf], [wf])
        P.barrier()
        P.copy("dve", wbf[:], wf[:], [wf], [wbf])
        for g in range(4):
            P.transpose(pt[:, g, :], wbf[:, g, :], K.ident[:], [wbf, K.ident], [pt])
        P.copy("dve", WcT[:], pt[:], [pt], [WcT])

        vin = [P.sbuf("b_v%d" % i, [128, 512], BF16) for i in range(2)]
        uin = [P.sbuf("b_u%d" % i, [128, 512], BF16) for i in range(2)]
        gin = [P.sbuf("b_g%d" % i, [128, 512], BF16) for i in range(2)]
        st6 = [P.sbuf("b_st%d" % i, [128, 6], F32) for i in range(2)]
        mv = [P.sbuf("b_mv%d" % i, [128, 2], F32) for i in range(2)]
        rs = [P.sbuf("b_rs%d" % i, [128, 1], F32) for i in range(2)]
        vn = [P.sbuf("b_vn%d" % i, [128, 512], F32) for i in range(2)]
        vnb = [P.sbuf("b_vnb%d" % i, [128, 512], BF16) for i in range(2)]
        mx = [P.psum("b_mx%d" % i, [128, 512], F32) for i in range(2)]
        tm = [P.sbuf("b_tm%d" % i, [128, 512], F32) for i in range(2)]
        yb = [P.sbuf("b_y%d" % i, [128, 512], BF16) for i in range(2)]
        yo = YOut(P, C, K, 1, "b")
        def b_load(t):
            i = t % 2
            sl = slice(t * 128, (t + 1) * 128)
            P.dma("sp", vin[i][:], S["bv"][sl, :], writes=[vin[i]])
            P.dma("sp", uin[i][:], S["bu"][sl, :], writes=[uin[i]])
            P.dma("sp", gin[i][:], S["bg"][sl, :], writes=[gin[i]])

        b_load(0)
        for t in range(NTR[0]):
            i = t % 2
            v, u, g = vin[i], uin[i], gin[i]
            sl = slice(t * 128, (t + 1) * 128)
            if t + 1 < NTR[0]:
                b_load(t + 1)
            P.op("dve", lambda e, i=i: e.bn_stats(st6[i][:], vin[i][:]), [v], [st6[i]])
            P.op("dve", lambda e, i=i: e.bn_aggr(mv[i][:], st6[i][:]), [st6[i]], [mv[i]])
            P.act(rs[i][:], mv[i][:, 1:2], AF.Sqrt, [mv[i], eps5], [rs[i]], bias=eps5[:])
            P.recip(rs[i][:], rs[i][:], [rs[i]], [rs[i]])
            P.ts("dve", vn[i][:], v[:], mv[i][:, 0:1], rs[i][:], ALU.subtract, ALU.mult, [v, mv[i], rs[i]], [vn[i]])
            P.tt("pool", vn[i][:], vn[i][:], lng[:], ALU.mult, [vn[i], lng], [vn[i]])
            P.tt("pool", vnb[i][:], vn[i][:], lnb[:], ALU.add, [vn[i], lnb], [vnb[i]])
            for gg in range(4):
                P.matmul(mx[i][:, gg * 128:(gg + 1) * 128], WcT[:, gg, :], vnb[i][:, gg * 128:(gg + 1) * 128], True, True,
                         [WcT, vnb[i]], [mx[i]])
            for gg in range(4):
                c = slice(gg * 128, (gg + 1) * 128)
                P.stt("dve", tm[i][:, c], mx[i][:, c], bsT[:, gg:gg + 1], u[:, c], ALU.add, ALU.mult, [mx[i], bsT, u], [tm[i]])
            P.tt("pool", yb[i][:], tm[i][:], g[:], ALU.mult, [tm[i], g], [yb[i]])
            yo.emit(yb[i], t)


def bc(ap, shape):
    return ap.to_broadcast(list(shape))


def phase_ssd(P, C, K, l):
    S = C.scr
    with P.scope():
        uinc = P.sbuf("d_uinc", [128, 128], F32)
        P.memset("pool", uinc[:], 1.0, [uinc])
        P.op("pool", lambda e: e.affine_select(out=uinc[:], in_=uinc[:], pattern=[[1, 128]], compare_op=ALU.is_ge,
                                               fill=0.0, base=0, channel_multiplier=-1), [uinc], [uinc])
        onesf = P.sbuf("d_onesf", [128, 128], F32)
        P.memset("pool", onesf[:], 1.0, [onesf])
        P.barrier()
        aB = P.sbuf("d_aB", [128, 8], F32)
        P.dma("sp", aB[:], C.prm["ssd_a_log"][l:l + 1, :].to_broadcast([128, 8]), writes=[aB])
        P.act(aB[:], aB[:], AF.Exp, [aB], [aB])
        P.ts("dve", aB[:], aB[:], -1.0, None, ALU.mult, None, [aB], [aB])
        dtb = P.sbuf("d_dtb", [128, 8], F32)
        P.dma("sp", dtb[:], C.prm["ssd_dt_bias"][l:l + 1, :].to_broadcast([128, 8]), writes=[dtb])
        dsk = P.sbuf("d_dsk", [128, 8], F32)
        P.dma("sp", dsk[:], C.prm["ssd_d"][l:l + 1, :].to_broadcast([128, 8]), writes=[dsk])
        ngB = P.sbuf("d_ngB", [128, 512], F32)
        P.dma("sp", ngB[:], C.prm["ssd_norm_g"][l:l + 1, :].to_broadcast([128, 512]), writes=[ngB])
        S32 = P.sbuf("d_S32", [128, 512], F32)
        Sbf = P.sbuf("d_Sbf", [128, 512], BF16)
        P.memset("pool", S32[:], 0.0, [S32])
        P.memset("pool", Sbf[:], 0.0, [Sbf])

        xf = [P.sbuf("d_xf%d" % i, [128, 8, 128], BF16) for i in range(2)]
        dtin = [P.sbuf("d_dtin%d" % i, [128, 8], F32) for i in range(2)]
        zg = [P.sbuf("d_zg%d" % i, [128, 512], BF16) for i in range(2)]
        dt = P.sbuf("d_dt", [128, 8], F32)
        la = P.sbuf("d_la", [128, 8], F32)
        lab = P.sbuf("d_lab", [128, 8, 128], F32)
        cs = P.sbuf("d_cs", [128, 16], F32)
        pre = P.sbuf("d_pre", [128, 24], F32)
        E = P.sbuf("d_E", [128, 24], F32)
        seg = P.sbuf("d_seg", [128, 8, 128], F32)
        Lm = P.sbuf("d_L", [128, 8, 128], F32)
        MT = P.sbuf("d_MT", [128, 8, 128], BF16)
        cbm = P.sbuf("d_cbm", [128, 2, 128], F32)
        xs = P.sbuf("d_xs", [128, 512], BF16)
        btm = P.sbuf("d_btm", [128, 2, 128], BF16)
        xdt = P.sbuf("d_xdt", [128, 512], BF16)
        xdtd = P.sbuf("d_xdtd", [128, 512], BF16)
        t1 = P.sbuf("d_t1", [128, 512], F32)
        y1 = P.sbuf("d_y1", [128, 512], F32)
        y2 = P.sbuf("d_y2", [128, 512], F32)
        junk = P.sbuf("d_junk", [128, 512], BF16)
        ssq = P.sbuf("d_ssq", [128, 1], F32)
        yb = [P.sbuf("d_yb%d" % i, [128, 512], BF16) for i in range(2)]

        small = P.psum("d_small", [128, 512], F32)
        csps = View(small, small[:, 0:16])
        cbps = View(small, small[:, 128:384])
        csrow = P.psum("d_csrow", [128, 8, 128], F32)
        tp = P.psum("d_tp", [128, 768], BF16)
        yps = P.psum("d_yps", [128, 512], F32)
        yoff = P.psum("d_yoff", [128, 512], F32)
        stp = P.psum("d_stp", [128, 512], F32)
        yo = YOut(P, C, K, 3, "d")

        def d_load(t):
            i = t % 2
            sl = slice(t * 128, (t + 1) * 128)
            P.dma("sp", xf[i][:], S["xbc"][:, sl].rearrange("(c p) t -> p c t", p=128), writes=[xf[i]])
            P.dma("sp", dtin[i][:], S["misc"][sl, 16:24], writes=[dtin[i]])
            P.dma("sp", zg[i][:], S["dz"][sl, :], writes=[zg[i]])

        d_load(0)
        for t in range(NTR[0]):
            i = t % 2
            sl = slice(t * 128, (t + 1) * 128)
            x_ = xf[i]
            if t + 1 < NTR[0]:
                d_load(t + 1)
            P.tt("dve", dt[:], dtin[i][:], dtb[:], ALU.add, [dtin[i], dtb], [dt])
            P.act(dt[:], dt[:], AF.Exp, [dt], [dt])
            P.act(dt[:], dt[:], AF.Ln, [dt], [dt], bias=1.0)
            P.tt("dve", la[:], dt[:], aB[:], ALU.mult, [dt, aB], [la])
            if STOP[0] <= 1:
                continue
            P.matmul(csps[:, 0:8], uinc[:], la[:], True, True, [uinc, la], [csps])
            P.matmul(csps[:, 8:16], onesf[:], la[:], True, True, [onesf, la], [csps])
            P.copy("dve", cs[:], csps[:], [csps], [cs])
            if STOP[0] <= 2:
                continue
            P.copy("dve", lab[:], bc(la[:].unsqueeze(2), [128, 8, 128]), [la], [lab])
            for h in range(8):
                P.matmul(csrow[:, h, :], lab[:, h, :], uinc[:], True, True, [lab, uinc], [csrow])
            if STOP[0] <= 3:
                continue
            P.tt("dve", pre[:, 0:8], cs[:, 8:16], cs[:, 0:8], ALU.subtract, [cs], [pre])
            P.copy("dve", pre[:, 8:16], cs[:, 8:16], [cs], [pre])
            P.copy("dve", pre[:, 16:24], cs[:, 0:8], [cs], [pre])
            P.act(E[:], pre[:], AF.Exp, [pre], [E])
            if STOP[0] <= 4:
                continue
            P.tt("dve", seg[:], csrow[:], bc(cs[:, 0:8].unsqueeze(2), [128, 8, 128]), ALU.subtract, [csrow, cs], [seg])
            P.ts("dve", seg[:], seg[:], 0.0, None, ALU.min, None, [seg], [seg])
            P.act(Lm[:], seg[:], AF.Exp, [seg], [Lm])
            if STOP[0] <= 5:
                continue
            for g in range(2):
                P.matmul(cbps[:, g * 128:(g + 1) * 128], x_[:, 4 + g, :], x_[:, 6 + g, :], True, True, [x_], [cbps])
            P.tt("dve", cbm[:], cbps[:].rearrange("s (g c) -> s g c", g=2), bc(uinc[:].unsqueeze(1), [128, 2, 128]), ALU.mult,
                 [cbps, uinc], [cbm])
            for g in range(2):
                P.tt("dve", MT[:, g * 4:(g + 1) * 4, :], Lm[:, g * 4:(g + 1) * 4, :], bc(cbm[:, g:g + 1, :], [128, 4, 128]), ALU.mult,
                     [Lm, cbm], [MT])
            if STOP[0] <= 6:
                continue
            for c in range(4):
                P.transpose(tp[:, c * 128:(c + 1) * 128], x_[:, c, :], K.ident[:], [x_, K.ident], [tp])
            for g in range(2):
                P.transpose(tp[:, 512 + g * 128:512 + (g + 1) * 128], x_[:, 4 + g, :], K.ident[:], [x_, K.ident], [tp])
            P.copy("act", xs[:], tp[:, 0:512], [tp], [xs])
            P.copy("act", btm[:], tp[:, 512:768].rearrange("s (g d) -> s g d", g=2), [tp], [btm])
            xs3 = xs[:].rearrange("s (h p) -> s h p", h=8)
            P.tt("dve", xdt[:].rearrange("s (h p) -> s h p", h=8), xs3, bc(dt[:].unsqueeze(2), [128, 8, 64]), ALU.mult, [xs, dt], [xdt])
            P.tt("dve", xdtd[:].rearrange("s (h p) -> s h p", h=8), xdt[:].rearrange("s (h p) -> s h p", h=8),
                 bc(E[:, 0:8].unsqueeze(2), [128, 8, 64]), ALU.mult, [xdt, E], [xdtd])
            if STOP[0] <= 7:
                continue
            for h in range(8):
                P.matmul(yps[:, h * 64:(h + 1) * 64], MT[:, h, :], xdt[:, h * 64:(h + 1) * 64], True, True, [MT, xdt], [yps])
            for g in range(2):
                P.matmul(yoff[:, g * 256:(g + 1) * 256], x_[:, 6 + g, :], Sbf[:, g * 256:(g + 1) * 256], True, True, [x_, Sbf], [yoff])
            P.tt("dve", t1[:].rearrange("s (h p) -> s h p", h=8), yoff[:].rearrange("s (h p) -> s h p", h=8),
                 bc(E[:, 16:24].unsqueeze(2), [128, 8, 64]), ALU.mult, [yoff, E], [t1])
            P.tt("dve", y1[:], yps[:], t1[:], ALU.add, [yps, t1], [y1])
            P.tt("pool", t1[:].rearrange("s (h p) -> s h p", h=8), xs3, bc(dsk[:].unsqueeze(2), [128, 8, 64]), ALU.mult, [xs, dsk], [t1])
            P.tt("pool", y1[:], y1[:], t1[:], ALU.add, [y1, t1], [y1])
            P.tt("pool", y2[:], y1[:], zg[i][:], ALU.mult, [y1, zg[i]], [y2])
            P.act(junk[:], y2[:], AF.Square, [y2], [junk, ssq], accum_out=ssq[:])
            P.act(ssq[:], ssq[:], AF.Sqrt, [ssq, K.eps], [ssq], scale=1.0 / 512, bias=K.eps[:])
            P.recip(ssq[:], ssq[:], [ssq], [ssq])
            P.stt("dve", yb[i][:], y2[:], ssq[:], ngB[:], ALU.mult, ALU.mult, [y2, ssq, ngB], [yb[i]])
            yo.emit(yb[i], t)
            if STOP[0] <= 8:
                continue
            for g in range(2):
                P.matmul(stp[:, g * 256:(g + 1) * 256], btm[:, g, :], xdtd[:, g * 256:(g + 1) * 256], True, True, [btm, xdtd], [stp])
            P.tt("dve", S32[:].rearrange("d (h p) -> d h p", h=8), S32[:].rearrange("d (h p) -> d h p", h=8),
                 bc(E[:, 8:16].unsqueeze(2), [128, 8, 64]), ALU.mult, [S32, E], [S32])
            P.tt("dve", S32[:], S32[:], stp[:], ALU.add, [S32, stp], [S32])
            P.copy("act", Sbf[:], S32[:], [S32], [Sbf])


NBIS = 18


def phase_dsa(P, C, K, l):
    S = C.scr
    I32 = mybir.dt.int32
    with P.scope():
        rb = [P.sbuf("c_rb%d" % i, [1, T], BF16) for i in range(3)]
        P.op("pool", lambda e: e.iota(rb[0][:], pattern=[[0, NT], [1, 128]], base=0, channel_multiplier=0,
                                      allow_small_or_imprecise_dtypes=True), (), [rb[0]])
        P.op("pool", lambda e: e.iota(rb[1][:], pattern=[[1, NT], [0, 128]], base=0, channel_multiplier=0,
                                      allow_small_or_imprecise_dtypes=True), (), [rb[1]])
        P.memset("pool", rb[2][:], 1.0, [rb[2]])
        posrow = P.sbuf("c_posrow", [128, T], F32)
        P.op("pool", lambda e: e.iota(posrow[:], pattern=[[1, T]], base=0, channel_multiplier=0,
                                      allow_small_or_imprecise_dtypes=True), (), [posrow])
        sl1 = P.sbuf("c_sl1", [1, 8, 128], BF16)
        sl128 = P.sbuf("c_sl128", [1, 8, 128], BF16)
        nsl64 = P.sbuf("c_nsl64", [1, 8, 128], F32)
        nsl1 = P.sbuf("c_nsl1", [1, 8, 128], F32)
        for h in range(8):
            sl = 2.0 ** -(h + 1)
            P.memset("pool", sl1[:, h, :], sl, [sl1])
            P.memset("pool", sl128[:, h, :], 128 * sl, [sl128])
            P.memset("pool", nsl64[:, h, :], -64 * sl, [nsl64])
            P.memset("pool", nsl1[:, h, :], -sl, [nsl1])
        idf = P.sbuf("c_idf", [128, 128], F32)
        P.make_identity(idf)
        clow = P.sbuf("c_clow", [128, 128], BF16)
        P.memset("pool", clow[:], 1.0, [clow])
        P.op("pool", lambda e: e.affine_select(out=clow[:], in_=clow[:], pattern=[[-1, 128]], compare_op=ALU.is_ge,
                                               fill=0.0, base=0, channel_multiplier=1), [clow], [clow])
        cneg = P.sbuf("c_cneg", [128, 128], F32)
        P.memset("pool", cneg[:], 0.0, [cneg])
        P.op("pool", lambda e: e.affine_select(out=cneg[:], in_=cneg[:], pattern=[[-1, 128]], compare_op=ALU.is_ge,
                                               fill=-1e30, base=0, channel_multiplier=1), [cneg], [cneg])
        pw = P.sbuf("c_pw", [128, NBIS], F32)
        for k in range(NBIS):
            P.memset("pool", pw[:, k:k + 1], 2.0 ** -(k + 1), [pw])
        vaug = P.sbuf("c_vaug", [128, NT, 65], BF16)
        P.memset("pool", vaug[:, :, 64:65], 1.0, [vaug])
        P.barrier()
        kTa = P.sbuf("c_kTa", [68, T], BF16)
        ikT = P.sbuf("c_ikT", [64, T], BF16)
        P.dma("sp", kTa[0:64, :], S["ck"], writes=[kTa])
        P.dma("sp", ikT[:], S["cik"], writes=[ikT])
        P.dma("sp", kTa[64:65, :], rb[0][:], reads=[rb[0]], writes=[kTa])
        P.dma("sp", kTa[65:66, :], rb[1][:], reads=[rb[1]], writes=[kTa])
        P.dma("sp", kTa[66:67, :], rb[2][:], reads=[rb[2]], writes=[kTa])
        P.dma("sp", kTa[67:68, :], rb[2][:], reads=[rb[2]], writes=[kTa])
        for s4 in range(0, NT, 4):
            P.dma("pool", vaug[:, s4:s4 + 4, 0:64], S["misc"][s4 * 128:(s4 + 4) * 128, 24:88].rearrange("(st s) d -> s st d", s=128),
                  writes=[vaug])

        iqT = [P.sbuf("c_iqT%d" % i, [64, 8, 128], BF16) for i in range(2)]
        qTa = [P.sbuf("c_qTa%d" % i, [68, 8, 128], BF16) for i in range(2)]
        for i in range(2):
            P.dma("sp", qTa[i][64:65], sl1[:], reads=[sl1], writes=[qTa[i]])
            P.dma("sp", qTa[i][65:66], sl128[:], reads=[sl128], writes=[qTa[i]])
        iw = [P.sbuf("c_iw%d" % i, [128, 8], F32) for i in range(2)]
        gt = [P.sbuf("c_gt%d" % i, [128, 512], BF16) for i in range(2)]
        iwa = P.sbuf("c_iwa", [128, 8], F32)
        iws = P.sbuf("c_iws", [128, 8], F32)
        sidx = P.sbuf("c_sidx", [128, T], F32)
        junk = P.sbuf("c_junk", [128, T], F32)
        M01 = P.sbuf("c_M01", [128, T], BF16)
        M01T = P.sbuf("c_M01T", [128, NT, 128], BF16)
        rl = [P.sbuf("c_rl%d" % i, [128, 512], F32) for i in range(4)]
        ex = [P.sbuf("c_ex%d" % i, [128, 8, 128], BF16) for i in range(3)]
        pT = [P.sbuf("c_pT%d" % i, [128, 8, 128], BF16) for i in range(3)]
        mn = P.sbuf("c_mn", [128, 1], F32)
        mxv = P.sbuf("c_mx", [128, 1], F32)
        W = P.sbuf("c_W", [128, NBIS], F32)
        lo = P.sbuf("c_lo", [128, 1], F32)
        mid = P.sbuf("c_mid", [128, 1], F32)
        cnt = P.sbuf("c_cnt", [128, 1], F32)
        dd = P.sbuf("c_dd", [128, 1], F32)
        smax = P.sbuf("c_smax", [128, 1], F32)
        smaxb = P.sbuf("c_smaxb", [128, 32], F32)
        srow_i = P.sbuf("c_srow_i", [1, 128], I32)
        nhi_i = P.sbuf("c_nhi_i", [1, 128], I32)
        nlo_i = P.sbuf("c_nlo_i", [1, 128], I32)
        nhi_f = P.sbuf("c_nhi_f", [1, 128], F32)
        nlo_f = P.sbuf("c_nlo_f", [1, 128], F32)
        r2 = [P.sbuf("c_r2%d" % i, [1, 8, 128], BF16) for i in range(2)]
        r3 = [P.sbuf("c_r3%d" % i, [1, 8, 128], BF16) for i in range(2)]
        rden = P.sbuf("c_rden", [128, 8], F32)
        ot = P.sbuf("c_ot", [128, 512], F32)
        yb = [P.sbuf("c_yb%d" % i, [128, 512], BF16) for i in range(2)]

        sps = [P.psum("c_sps%d" % i, [128, 512], F32) for i in range(2)]
        lgh = [P.psum("c_lg%d" % i, [128, 4, 128], F32) for i in range(3)]
        ops_ = P.psum("c_ops", [128, 8, 128], F32)
        mty = P.psum("c_mty", [128, 8, 128], BF16)
        mtp = View(mty, mty[:, 0:4, :])
        yo = YOut(P, C, K, 2, "c", ps=[View(mty, mty[:, 4:8, :])])
        IWS = (8 ** -0.5) * (64 ** -0.5)
        nsp = 0
        def front_fn(t):
            nonlocal nsp
            i = t % 2
            sl = slice(t * 128, (t + 1) * 128)
            nk = t + 1
            Lk = nk * 128
            P.dma("sp", iqT[i][:], S["ciq"][:, sl].rearrange("(h d) t -> d h t", d=64), writes=[iqT[i]])
            P.dma("sp", qTa[i][0:64], S["cq"][:, sl].rearrange("(h d) t -> d h t", d=64), writes=[qTa[i]])
            P.dma("sp", iw[i][:], S["misc"][sl, 8:16], writes=[iw[i]])
            P.dma("sp", gt[i][:], S["cg"][sl, :], writes=[gt[i]])
            if STOP[0] <= 1:
                return
            if t >= 2:
                P.act(iwa[:], iw[i][:], AF.Abs, [iw[i]], [iwa], scale=IWS)
                P.act(iws[:], iw[i][:], AF.Sign, [iw[i]], [iws])
                for c0 in range(0, Lk, 512):
                    yield
                    w_ = min(512, Lk - c0)
                    for h in range(8):
                        sp_ = sps[nsp % 2]
                        r_ = rl[nsp % 4]
                        nsp += 1
                        P.matmul(sp_[:, 0:w_], iqT[i][:, h, :], ikT[:, c0:c0 + w_], True, True, [iqT[i], ikT], [sp_])
                        P.act(r_[:, 0:w_], sp_[:, 0:w_], AF.Relu, [sp_, iwa], [r_], scale=iwa[:, h:h + 1])
                        if h == 0:
                            P.ts("dve", sidx[:, c0:c0 + w_], r_[:, 0:w_], iws[:, 0:1], None, ALU.mult, None, [r_, iws], [sidx])
                        else:
                            P.stt("dve", sidx[:, c0:c0 + w_], r_[:, 0:w_], iws[:, h:h + 1], sidx[:, c0:c0 + w_], ALU.mult, ALU.add,
                                  [r_, iws, sidx], [sidx])
                P.op("dve", lambda e, Lk=Lk: e.tensor_reduce(mn[:], sidx[:, 0:Lk], AX.X, ALU.min), [sidx], [mn])
                P.op("dve", lambda e, Lk=Lk: e.tensor_reduce(mxv[:], sidx[:, 0:Lk], AX.X, ALU.max), [sidx], [mxv])
                P.tt("dve", sidx[:, t * 128:Lk], sidx[:, t * 128:Lk], cneg[:], ALU.add, [sidx, cneg], [sidx])
                P.tt("dve", mxv[:], mxv[:], mn[:], ALU.subtract, [mxv, mn], [mxv])
                P.ts("dve", W[:], pw[:], mxv[:], None, ALU.mult, None, [pw, mxv], [W])
                P.tt("dve", mid[:], mn[:], W[:, 0:1], ALU.add, [mn, W], [mid])
                for k in range(NBIS):
                    yield
                    P.ts("dve", junk[:, 0:Lk], sidx[:, 0:Lk], mid[:], None, ALU.is_ge, ALU.add, [sidx, mid], [junk, cnt],
                         accum_out=cnt[:])
                    P.ts("dve", dd[:], cnt[:], 255.5, 0.5, ALU.is_ge, ALU.subtract, [cnt], [dd])
                    if k < NBIS - 1:
                        P.stt("dve", mid[:], dd[:], W[:, k:k + 1], mid[:], ALU.mult, ALU.add, [dd, W, mid], [mid])
                    else:
                        P.ts("dve", dd[:], dd[:], 0.5, None, ALU.subtract, None, [dd], [dd])
                        P.stt("dve", lo[:], dd[:], W[:, k:k + 1], mid[:], ALU.mult, ALU.add, [dd, W, mid], [lo])
                P.ts("dve", M01[:, 0:Lk], sidx[:, 0:Lk], lo[:], None, ALU.is_ge, None, [sidx, lo], [M01])
            else:
                if t > 0:
                    P.memset("pool", M01[:, 0:t * 128], 1.0, [M01])
                P.copy("pool", M01[:, t * 128:Lk], clow[:], [clow], [M01])
            if STOP[0] <= 2:
                return
            P.tt("dve", junk[:, 0:Lk], M01[:, 0:Lk], posrow[:, 0:Lk], ALU.mult, [M01, posrow], [junk])
            P.op("dve", lambda e, Lk=Lk: e.tensor_reduce(smax[:], junk[:, 0:Lk], AX.X, ALU.max), [junk], [smax])
            srp = sps[nsp % 2]
            nsp += 1
            P.copy("dve", smaxb[:], bc(smax[:], [128, 32]), [smax], [smaxb])
            P.matmul(srp[0:32, 0:128], smaxb[:], idf[:], True, True, [smaxb, idf], [srp])
            if STOP[0] <= 2.5:
                return
            P.copy("dve", srow_i[:], srp[0:1, 0:128], [srp], [srow_i])
            P.ts("dve", nhi_i[:], srow_i[:], 6, None, ALU.arith_shift_right, None, [srow_i], [nhi_i])
            P.ts("dve", nlo_i[:], srow_i[:], 63, None, ALU.bitwise_and, None, [srow_i], [nlo_i])
            P.copy("dve", nhi_f[:], nhi_i[:], [nhi_i], [nhi_f])
            P.copy("dve", nlo_f[:], nlo_i[:], [nlo_i], [nlo_f])
            P.tt("dve", r2[i][:], nsl64[:], bc(nhi_f[:].unsqueeze(1), [1, 8, 128]), ALU.mult, [nsl64, nhi_f], [r2[i]])
            P.tt("dve", r3[i][:], nsl1[:], bc(nlo_f[:].unsqueeze(1), [1, 8, 128]), ALU.mult, [nsl1, nlo_f], [r3[i]])
            if STOP[0] <= 2.7:
                return
            P.dma("sp", qTa[i][66:67], r2[i][:], reads=[r2[i]], writes=[qTa[i]])
            P.dma("sp", qTa[i][67:68], r3[i][:], reads=[r3[i]], writes=[qTa[i]])
            if STOP[0] <= 3:
                return
        def back_fn(t):
            i = t % 2
            sl = slice(t * 128, (t + 1) * 128)
            nk = t + 1
            Lk = nk * 128
            for k0 in range(0, nk, 4):
                n4 = min(4, nk - k0)
                for j in range(n4):
                    P.transpose(mtp[:, j, :], M01[:, (k0 + j) * 128:(k0 + j + 1) * 128], K.ident[:], [M01, K.ident], [mtp])
                P.copy("act", M01T[:, k0:k0 + n4, :], mtp[:, 0:n4, :], [mtp], [M01T])
            if STOP[0] <= 4:
                return
            def qk(kt_, half):
                lb = lgh[(2 * kt_ + half) % 3]
                P.matmul(lb[:], kTa[:, kt_ * 128:(kt_ + 1) * 128],
                         qTa[i][:, half * 4:(half + 1) * 4, :], True, True, [kTa, qTa[i]], [lb])

            qk(0, 0)
            qk(0, 1)
            for kt in range(nk):
                yield
                e_ = ex[kt % 3]
                p_ = pT[kt % 3]
                if kt + 1 < nk:
                    qk(kt + 1, 0)
                for half in range(2):
                    lb = lgh[(2 * kt + half) % 3]
                    P.act(e_[:, half * 4:(half + 1) * 4, :], lb[:], AF.Exp, [lb], [e_])
                if kt + 1 < nk:
                    qk(kt + 1, 1)
                P.stt("dve", p_[:], e_[:], 3.0e38, bc(M01T[:, kt:kt + 1, :], [128, 8, 128]), ALU.min, ALU.mult,
                      [e_, M01T], [p_])
                for h in range(8):
                    P.matmul(ops_[:, h, 0:65], p_[:, h, :], vaug[:, kt, :], kt == 0 and h % 4 == 0, kt == nk - 1 and h % 4 == 3,
                             [p_, vaug], [ops_])
            if STOP[0] <= 5:
                return
            P.recip(rden[:], ops_[:, :, 64], [ops_], [rden])
            P.tt("dve", ot[:].rearrange("t (h d) -> t h d", h=8), ops_[:, :, 0:64], bc(rden[:].unsqueeze(2), [128, 8, 64]), ALU.mult,
                 [ops_, rden], [ot])
            P.tt("pool", yb[i][:], ot[:], gt[i][:], ALU.mult, [ot, gt[i]], [yb[i]])
            yo.emit(yb[i], t)

        def drain(g):
            for _ in g:
                pass

        def interleave(ga, gb):
            a_live, b_live = True, True
            while a_live or b_live:
                if b_live:
                    try:
                        next(gb)
                    except StopIteration:
                        b_live = False
                if a_live:
                    try:
                        next(ga)
                    except StopIteration:
                        a_live = False

        NTT = NTR[0]
        drain(front_fn(0))
        for t in range(NTT):
            if t + 1 < NTT:
                interleave(front_fn(t + 1), back_fn(t))
            else:
                drain(back_fn(t))


def phase_gdn(P, C, K, l):
    S = C.scr
    NCH = NTR[0] * 2
    with P.scope():
        uinc = P.sbuf("a_uinc", [128, 128], F32)
        P.memset("pool", uinc[:], 1.0, [uinc])
        P.op("pool", lambda e: e.affine_select(out=uinc[:], in_=uinc[:], pattern=[[1, 128]], compare_op=ALU.is_ge,
                                               fill=0.0, base=0, channel_multiplier=-1), [uinc], [uinc])
        lstr = P.sbuf("a_lstr", [128, 128], F32)
        P.memset("pool", lstr[:], 1.0, [lstr])
        P.op("pool", lambda e: e.affine_select(out=lstr[:], in_=lstr[:], pattern=[[-1, 128]], compare_op=ALU.is_gt,
                                               fill=0.0, base=0, channel_multiplier=1), [lstr], [lstr])
        idf = P.sbuf("a_idf", [128, 128], F32)
        P.make_identity(idf)
        onesf = P.sbuf("a_onesf", [128, 128], F32)
        P.memset("pool", onesf[:], 1.0, [onesf])
        P.barrier()
        aB = P.sbuf("a_aB", [64, 4], F32)
        P.dma("sp", aB[:], C.prm["gdn_a_log"][l:l + 1, :].to_broadcast([64, 4]), writes=[aB])
        P.act(aB[:], aB[:], AF.Exp, [aB], [aB])
        P.ts("dve", aB[:], aB[:], -1.0, None, ALU.mult, None, [aB], [aB])
        dtb = P.sbuf("a_dtb", [64, 4], F32)
        P.dma("sp", dtb[:], C.prm["gdn_dt_bias"][l:l + 1, :].to_broadcast([64, 4]), writes=[dtb])
        ng = P.sbuf("a_ng", [64, 128], F32)
        P.dma("sp", ng[:], C.prm["gdn_norm_g"][l:l + 1, :].to_broadcast([64, 128]), writes=[ng])
        ST32 = P.sbuf("a_ST32", [128, 4, 128], F32)
        STb = P.sbuf("a_STb", [128, 4, 128], BF16)
        P.memset("pool", ST32[:], 0.0, [ST32])
        P.memset("pool", STb[:], 0.0, [STb])

        def dbl(name, shape, dt):
            return [P.sbuf("%s%d" % (name, i), shape, dt) for i in range(2)]

        qT, kT, vT = dbl("a_qT", [128, 4, 64], BF16), dbl("a_kT", [128, 4, 64], BF16), dbl("a_vT", [128, 4, 64], BF16)
        ab, gate = dbl("a_ab", [64, 8], F32), dbl("a_gate", [64, 512], BF16)
        sp_, gl, be = dbl("a_sp", [64, 4], F32), dbl("a_gl", [64, 4], F32), dbl("a_be", [64, 4], F32)
        gc, gt128, cd128 = dbl("a_gc", [64, 4], F32), dbl("a_gt", [128, 4], F32), dbl("a_cd", [128, 4], F32)
        lab = dbl("a_lab", [64, 4, 128], F32)
        pre, E, bE = dbl("a_pre", [64, 8], F32), dbl("a_E", [64, 8], F32), dbl("a_bE", [64, 4], F32)
        seg, nseg = dbl("a_seg", [64, 4, 64], F32), dbl("a_nseg", [64, 4, 64], F32)
        dI, dL = dbl("a_dI", [64, 4, 64], F32), dbl("a_dL", [64, 4, 64], F32)
        egr = dbl("a_egr", [128, 4, 64], F32)
        qdT = dbl("a_qdT", [128, 4, 64], BF16)
        qkT = dbl("a_qkT", [64, 4, 64], BF16)
        Nm = [dbl("a_N", [64, 4, 64], F32), dbl("a_N2", [64, 4, 64], F32)]
        Mm = [dbl("a_M", [64, 4, 64], F32), dbl("a_M2", [64, 4, 64], F32)]
        X, Xb = dbl("a_X", [64, 4, 64], F32), dbl("a_Xb", [64, 4, 64], BF16)
        kb, kdec, vb = dbl("a_kb", [64, 4, 128], BF16), dbl("a_kdec", [64, 4, 128], BF16), dbl("a_vb", [64, 4, 128], BF16)
        wkT, u0 = dbl("a_wkT", [128, 4, 64], BF16), dbl("a_u0", [64, 4, 128], F32)
        u = dbl("a_u", [64, 4, 128], BF16)
        o, sq = dbl("a_o", [64, 4, 128], F32), dbl("a_sq", [64, 4, 128], F32)
        ssq = dbl("a_ssq", [64, 4], F32)
        y = dbl("a_y", [64, 512], F32)
        ysb = dbl("a_ysb", [128, 4, 64], BF16)

        b0 = P.psum("a_b0", [128, 512], F32)
        gcrow, wk_ps = View(b0, b0[:, 0:256].rearrange("p (h c) -> p h c", h=4)), View(b0, b0[:, 256:512].rearrange("p (h c) -> p h c", h=4))
        b1 = P.psum("a_b1", [128, 512], F32)
        sm_ps, ytp = View(b1, b1[:, 0:16]), View(b1, b1[:, 256:512].rearrange("p (h c) -> p h c", h=4))
        b2 = P.psum("a_b2", [64, 512], F32)
        kk_ps, qk_ps = View(b2, b2[:, 0:256].rearrange("p (h c) -> p h c", h=4)), View(b2, b2[:, 256:512].rearrange("p (h c) -> p h c", h=4))
        b3 = P.psum("a_b3", [64, 512], F32)
        P_ps, Q_ps = View(b3, b3[:, 0:256].rearrange("p (h c) -> p h c", h=4)), View(b3, b3[:, 256:512].rearrange("p (h c) -> p h c", h=4))
        b4 = P.psum("a_b4", [64, 512], F32)
        XP_ps, M_ps = View(b4, b4[:, 0:256].rearrange("p (h c) -> p h c", h=4)), View(b4, b4[:, 256:512].rearrange("p (h c) -> p h c", h=4))
        kv_ps = P.psum("a_kvps", [64, 2, 512], BF16)
        ktm, vtm = View(kv_ps, kv_ps[:, 0, :].rearrange("p (h d) -> p h d", h=4)), View(kv_ps, kv_ps[:, 1, :].rearrange("p (h d) -> p h d", h=4))
        uwo = P.psum("a_uwo", [64, 4, 128], F32)
        Sn_ps = P.psum("a_Sn", [128, 4, 128], F32)
        u64, i64 = uinc[0:64, 0:64], idf[0:64, 0:64]

        def b4c(ap, n):
            return bc(ap.unsqueeze(2), [ap.shape[0], 4, n])

        def pre_fn(ch):
            i = ch % 2
            cs = slice(ch * 64, (ch + 1) * 64)
            for dst, nm in ((qT[i], "gq"), (kT[i], "gk"), (vT[i], "gv")):
                P.dma("sp", dst[:], S[nm][:, cs].rearrange("(h d) t -> d h t", d=128), writes=[dst])
            P.dma("sp", ab[i][:], S["misc"][cs, 0:8], writes=[ab[i]])
            P.dma("sp", gate[i][:], S["ag"][cs, :], writes=[gate[i]])
            P.tt("dve", sp_[i][:], ab[i][:, 0:4], dtb[:], ALU.add, [ab[i], dtb], [sp_[i]])
            P.act(sp_[i][:], sp_[i][:], AF.Exp, [sp_[i]], [sp_[i]])
            P.act(sp_[i][:], sp_[i][:], AF.Ln, [sp_[i]], [sp_[i]], bias=1.0)
            P.tt("dve", gl[i][:], sp_[i][:], aB[:], ALU.mult, [sp_[i], aB], [gl[i]])
            P.act(be[i][:], ab[i][:, 4:8], AF.Exp, [ab[i]], [be[i]], scale=-1.0)
            P.ts("dve", be[i][:], be[i][:], 1.0, None, ALU.add, None, [be[i]], [be[i]])
            P.recip(be[i][:], be[i][:], [be[i]], [be[i]])
            yield
            P.matmul(sm_ps[0:64, 0:4], u64, gl[i][:], True, True, [uinc, gl[i]], [sm_ps])
            P.matmul(sm_ps[:, 4:8], onesf[0:64, :], gl[i][:], True, True, [onesf, gl[i]], [sm_ps])
            P.copy("dve", gc[i][:], sm_ps[0:64, 0:4], [sm_ps], [gc[i]])
            P.copy("dve", gt128[i][:], sm_ps[:, 4:8], [sm_ps], [gt128[i]])
            P.copy("dve", lab[i][:], b4c(gl[i][:], 128), [gl[i]], [lab[i]])
            for h in range(4):
                P.matmul(gcrow[:, h, :], lab[i][:, h, :], u64, True, True, [lab[i], uinc], [gcrow])
            P.copy("dve", pre[i][:, 0:4], gc[i][:], [gc[i]], [pre[i]])
            P.tt("dve", pre[i][:, 4:8], gt128[i][0:64, :], gc[i][:], ALU.subtract, [gt128[i], gc[i]], [pre[i]])
            P.act(E[i][:], pre[i][:], AF.Exp, [pre[i]], [E[i]])
            P.act(cd128[i][:], gt128[i][:], AF.Exp, [gt128[i]], [cd128[i]])
            P.tt("dve", bE[i][:], be[i][:], E[i][:, 0:4], ALU.mult, [be[i], E[i]], [bE[i]])
            if STOP[0] <= 1:
                return
            yield
            P.tt("dve", seg[i][:], gcrow[0:64], b4c(gc[i][:], 64), ALU.subtract, [gcrow, gc[i]], [seg[i]])
            P.ts("dve", nseg[i][:], seg[i][:], -1.0, 0.0, ALU.mult, ALU.min, [seg[i]], [nseg[i]])
            P.ts("dve", seg[i][:], seg[i][:], 0.0, None, ALU.min, None, [seg[i]], [seg[i]])
            P.act(dI[i][:], seg[i][:], AF.Exp, [seg[i]], [dI[i]])
            P.act(dL[i][:], nseg[i][:], AF.Exp, [nseg[i]], [dL[i]])
            P.tt("dve", dI[i][:], dI[i][:], bc(u64.unsqueeze(1), [64, 4, 64]), ALU.mult, [dI[i], uinc], [dI[i]])
            P.tt("dve", dL[i][:], dL[i][:], bc(lstr[0:64, 0:64].unsqueeze(1), [64, 4, 64]), ALU.mult, [dL[i], lstr], [dL[i]])
            P.act(egr[i][:], gcrow[:], AF.Exp, [gcrow], [egr[i]])
            P.tt("dve", qdT[i][:], qT[i][:], egr[i][:], ALU.mult, [qT[i], egr[i]], [qdT[i]])
            if STOP[0] <= 2:
                return
            yield
            for h in range(4):
                P.matmul(kk_ps[:, h, :], kT[i][:, h, :], kT[i][:, h, :], True, True, [kT[i]], [kk_ps])
            for h in range(4):
                P.matmul(qk_ps[:, h, :], kT[i][:, h, :], qT[i][:, h, :], True, True, [kT[i], qT[i]], [qk_ps])
            N0, M0 = Nm[0][i], Mm[0][i]
            P.tt("dve", N0[:], kk_ps[:], dL[i][:], ALU.mult, [kk_ps, dL[i]], [N0])
            P.tt("dve", N0[:], N0[:], b4c(be[i][:], 64), ALU.mult, [N0, be[i]], [N0])
            P.tt("dve", qkT[i][:], qk_ps[:], dI[i][:], ALU.mult, [qk_ps, dI[i]], [qkT[i]])
            if STOP[0] <= 3:
                return
            for h in range(4):
                P.transpose(M_ps[:, h, :], N0[:, h, :], i64, [N0, idf], [M_ps])
            P.copy("act", M0[:], M_ps[:], [M_ps], [M0])
            if STOP[0] <= 4:
                return
            yield
            P.tt("dve", X[i][:], bc(i64.unsqueeze(1), [64, 4, 64]), M0[:], ALU.subtract, [idf, M0], [X[i]])
            Pc, Qc = N0, M0
            if STOP[0] <= 4.1:
                return
            for st in range(1, 6):
                if STOP[0] <= 4.2 and st > 1:
                    break
                Pn, Qn = Nm[st % 2][i], Mm[st % 2][i]
                for h in range(4):
                    P.matmul(P_ps[:, h, :], Qc[:, h, :], Pc[:, h, :], True, True, [Qc, Pc], [P_ps])
                if st < 5:
                    for h in range(4):
                        P.matmul(Q_ps[:, h, :], Pc[:, h, :], Qc[:, h, :], True, True, [Qc, Pc], [Q_ps])
                if STOP[0] <= 4.15:
                    break
                P.copy("act", Pn[:], P_ps[:], [P_ps], [Pn])
                if st < 5:
                    P.copy("dve", Qn[:], Q_ps[:], [Q_ps], [Qn])
                if STOP[0] <= 4.17:
                    break
                for h in range(4):
                    P.matmul(XP_ps[:, h, :], Pn[:, h, :], X[i][:, h, :], True, True, [Pn, X[i]], [XP_ps])
                P.tt("dve", X[i][:], X[i][:], XP_ps[:], ALU.add, [X[i], XP_ps], [X[i]])
                Pc, Qc = Pn, Qn
                yield
            if STOP[0] <= 4.5:
                return
            P.copy("act", Xb[i][:], X[i][:], [X[i]], [Xb[i]])
            if STOP[0] <= 5:
                return
            yield
            for h in range(4):
                P.transpose(ktm[:, h, :], kT[i][:, h, :], K.ident[:], [kT[i], K.ident], [ktm])
            for h in range(4):
                P.transpose(vtm[:, h, :], vT[i][:, h, :], K.ident[:], [vT[i], K.ident], [vtm])
            P.tt("dve", kb[i][:], ktm[:], b4c(bE[i][:], 128), ALU.mult, [ktm, bE[i]], [kb[i]])
            P.tt("dve", kdec[i][:], ktm[:], b4c(E[i][:, 4:8], 128), ALU.mult, [ktm, E[i]], [kdec[i]])
            P.tt("dve", vb[i][:], vtm[:], b4c(be[i][:], 128), ALU.mult, [vtm, be[i]], [vb[i]])
            if STOP[0] <= 6:
                return
            yield
            for h in range(4):
                P.matmul(wk_ps[:, h, :], kb[i][:, h, :], Xb[i][:, h, :], True, True, [kb[i], Xb[i]], [wk_ps])
            P.copy("act", wkT[i][:], wk_ps[:], [wk_ps], [wkT[i]])
            for h in range(4):
                P.matmul(uwo[:, h, :], Xb[i][:, h, :], vb[i][:, h, :], True, True, [Xb[i], vb[i]], [uwo])
            P.copy("act", u0[i][:], uwo[:], [uwo], [u0[i]])
            if STOP[0] <= 7:
                return
        def seq_fn(ch):
            i = ch % 2
            cs = slice(ch * 64, (ch + 1) * 64)
            for h in range(4):
                P.matmul(uwo[:, h, :], wkT[i][:, h, :], STb[:, h, :], True, True, [wkT[i], STb], [uwo])
            P.tt("dve", u[i][:], u0[i][:], uwo[:], ALU.subtract, [u0[i], uwo], [u[i]])
            yield
            for h in range(4):
                P.matmul(uwo[:, h, :], qdT[i][:, h, :], STb[:, h, :], True, False, [qdT[i], STb], [uwo])
                P.matmul(uwo[:, h, :], qkT[i][:, h, :], u[i][:, h, :], False, True, [qkT[i], u[i]], [uwo])
            P.copy("act", o[i][:], uwo[:], [uwo], [o[i]])
            if STOP[0] <= 8:
                return
            yield
            for h in range(4):
                P.matmul(Sn_ps[:, h, :], kdec[i][:, h, :], u[i][:, h, :], True, True, [kdec[i], u[i]], [Sn_ps])
            P.tt("dve", ST32[:], ST32[:], b4c(cd128[i][:], 128), ALU.mult, [ST32, cd128[i]], [ST32])
            P.tt("dve", ST32[:], ST32[:], Sn_ps[:], ALU.add, [ST32, Sn_ps], [ST32])
            P.copy("act", STb[:], ST32[:], [ST32], [STb])
            if STOP[0] <= 9:
                return
            yield
            P.tt("pool", sq[i][:], o[i][:], o[i][:], ALU.mult, [o[i]], [sq[i]])
            P.op("dve", lambda e, i=i: e.tensor_reduce(ssq[i][:], sq[i][:], AX.X, ALU.add), [sq[i]], [ssq[i]])
            P.act(ssq[i][:], ssq[i][:], AF.Sqrt, [ssq[i], K.eps], [ssq[i]], scale=1.0 / 128, bias=K.eps[0:64, :])
            P.recip(ssq[i][:], ssq[i][:], [ssq[i]], [ssq[i]])
            P.tt("pool", o[i][:], o[i][:], b4c(ssq[i][:], 128), ALU.mult, [o[i], ssq[i]], [o[i]])
            P.tt("pool", o[i][:], o[i][:], bc(ng[:].unsqueeze(1), [64, 4, 128]), ALU.mult, [o[i], ng], [o[i]])
            P.tt("pool", y[i][:], o[i][:].rearrange("c h v -> c (h v)"), gate[i][:], ALU.mult, [o[i], gate[i]], [y[i]])
            if STOP[0] <= 10:
                return
            yield
            for cc in range(4):
                P.transpose(ytp[:, cc, :], y[i][:, cc * 128:(cc + 1) * 128], i64, [y[i], idf], [ytp])
            P.copy("act", ysb[i][:], ytp[:], [ytp], [ysb[i]])
            P.dma("sp", S["ysT"][0][:, cs].rearrange("(cc c) t -> c cc t", c=128), ysb[i][:], reads=[ysb[i]])


        def drain(g):
            for _ in g:
                pass

        def interleave(ga, gb):
            a_live, b_live = True, True
            while a_live or b_live:
                if b_live:
                    try:
                        next(gb)
                    except StopIteration:
                        b_live = False
                if a_live:
                    try:
                        next(ga)
                    except StopIteration:
                        a_live = False

        drain(pre_fn(0))
        for ch in range(NCH):
            if ch + 1 < NCH:
                interleave(pre_fn(ch + 1), seq_fn(ch))
            else:
                drain(seq_fn(ch))


def build_program(nc, layers=(0, 1), dbg=False, branches="abcdm"):
    C = declare(nc, dbg=dbg)
    P = Prog(nc)
    K = setup_consts(P, C)
    for l in layers:
        x_src = C.x if l == layers[0] else C.scr["x1"]
        x_dst = C.out if l == layers[-1] else C.scr["x1"]
        with P.scope():
            alloc_hT(P, K)
            phase_norm(P, C, K, l, x_src)
            phase_inproj(P, C, K, l)
        if "a" in branches:
            phase_gdn(P, C, K, l)
        if "b" in branches:
            phase_sg(P, C, K, l)
        if "c" in branches:
            phase_dsa(P, C, K, l)
        if "d" in branches:
            phase_ssd(P, C, K, l)
        if "m" in branches:
            phase_mem(P, C, K, l)
        with P.scope():
            alloc_merged(P, K)
            with P.scope():
                alloc_hT(P, K)
                phase_norm(P, C, K, l, x_src)
                phase_merge(P, C, K, l)
            phase_outproj(P, C, K, l, x_src, x_dst)
    P.finish()
    return C, P


def kernel(**inputs):
    x = np.ascontiguousarray(np.asarray(inputs["x"], dtype=np.float32))
    mem = np.ascontiguousarray(np.asarray(inputs["mem"], dtype=np.float32))
    nb = x.shape[0]
    nc = bass.Bass("TRN2", target_bir_lowering=False)
    build_program(nc)
    prm = {n: np.ascontiguousarray(np.asarray(inputs[n], dtype=np.float32)) for n, _ in PARAMS}
    in_maps = []
    for b in range(nb):
        m = {"x": x[b], "mem": mem[b]}
        m.update(prm)
        in_maps.append(m)
    res = run_bass_kernel_spmd(nc, in_maps, core_ids=list(range(nb)))
    return np.stack([np.asarray(r["out"], dtype=np.float32) for r in res.results], axis=0)
```
